# Optimizing a Trainium2 kernel written in Bass

```python
import jax, jax.numpy as jnp
from jax import lax
import numpy as np

D_MODEL = 1024
BATCH = 8
SEQ = 4096
DEPTH = 1

D_MIX = D_MODEL
D_MLSTM = D_MIX // 2
D_POOL = D_MIX - D_MLSTM
N_MLSTM_HEADS = 4
HEAD_DIM = D_MLSTM // N_MLSTM_HEADS
POOL_WINDOWS = (2, 4, 8, 16)
N_POOL_GROUPS = len(POOL_WINDOWS)
POOL_GROUP_DIM = D_POOL // N_POOL_GROUPS
N_DIRS = 2
N_GATE_COLS = N_DIRS * 2 * N_MLSTM_HEADS
D_IN_PROJ = 4 * D_MLSTM + N_GATE_COLS + D_POOL
D_FF = 4 * D_MODEL
CHUNK = 128
EPS = 1e-6

kernel_name = "hybrid_mlstm_pool_adaln_encoder"


def _rmsnorm(x, g):
    xf = x.astype(jnp.float32)
    y = xf * lax.rsqrt(jnp.mean(xf * xf, axis=-1, keepdims=True) + EPS)
    return (y * g.astype(jnp.float32)).astype(x.dtype)


def _modulate(h, shift, scale):
    return h * (1 + scale[:, None, :]) + shift[:, None, :]


def _mlstm_scan(q, k, v, log_i, log_f):
    B, H, S, Dh = q.shape
    nc = S // CHUNK

    def to_chunks(a):
        a = a.reshape((B, H, nc, CHUNK) + a.shape[3:])
        return jnp.moveaxis(a, 2, 0)

    xs = (to_chunks(q), to_chunks(k), to_chunks(v), to_chunks(log_f), to_chunks(log_i))
    tril = jnp.tril(jnp.ones((CHUNK, CHUNK), dtype=bool))

    def step(carry, inp):
        C, n, m = carry
        q_c, k_c, v_c, lf, li = inp
        b = jnp.cumsum(lf, axis=-1)
        d = b[..., :, None] - b[..., None, :] + li[..., None, :]
        d = jnp.where(tril, d, -jnp.inf)
        inter = b + m[..., None]
        m_t = jnp.maximum(inter, jnp.max(d, axis=-1))
        w = jnp.exp(d - m_t[..., None])
        a = jnp.exp(inter - m_t)
        s = jnp.einsum('bhtd,bhsd->bhts', q_c, k_c) * w
        num = (jnp.einsum('bhts,bhse->bhte', s, v_c)
               + a[..., None] * jnp.einsum('bhtd,bhde->bhte', q_c, C))
        den = jnp.sum(s, axis=-1) + a * jnp.einsum('bhtd,bhd->bht', q_c, n)
        h = num / jnp.maximum(jnp.abs(den), jnp.exp(-m_t))[..., None]
        b_last = b[..., -1]
        g = b_last[..., None] - b + li
        m_new = jnp.maximum(b_last + m, jnp.max(g, axis=-1))
        decay = jnp.exp(b_last + m - m_new)
        wk = jnp.exp(g - m_new[..., None])
        C_new = decay[..., None, None] * C + jnp.einsum('bhs,bhsd,bhse->bhde', wk, k_c, v_c)
        n_new = decay[..., None] * n + jnp.einsum('bhs,bhsd->bhd', wk, k_c)
        return (C_new, n_new, m_new), h

    init = (jnp.zeros((B, H, Dh, Dh), jnp.float32),
            jnp.zeros((B, H, Dh), jnp.float32),
            jnp.zeros((B, H), jnp.float32))
    _, hs = lax.scan(step, init, xs)
    return jnp.moveaxis(hs, 0, 2).reshape(B, H, S, Dh)


def _multiscale_pool(u, w_pool, pool_scale):
    B, S, _ = u.shape
    uf = u.astype(jnp.float32).reshape(B, S, N_POOL_GROUPS, POOL_GROUP_DIM)
    prefix = jnp.concatenate([jnp.zeros_like(uf[:, :1]), jnp.cumsum(uf, axis=1)], axis=1)
    t = jnp.arange(S)
    means = []
    for gi, win in enumerate(POOL_WINDOWS):
        lo = jnp.clip(t - win // 2, 0, S)
        hi = jnp.clip(t + win // 2, 0, S)
        p = prefix[:, :, gi]
        cnt = (hi - lo).astype(jnp.float32)
        means.append((p[:, hi] - p[:, lo]) / cnt[None, :, None])
    pooled = jnp.stack(means, axis=2)
    mixed = (pooled - uf).astype(u.dtype)
    y = jnp.einsum('bsgc,gce->bsge', mixed, w_pool).reshape(B, S, D_POOL)
    return y * pool_scale


def _mixer(h, w_in, b_igate, b_fgate, g_head, w_pool, pool_scale, w_out):
    B, S, _ = h.shape
    H = N_MLSTM_HEADS
    proj = jnp.einsum('bsd,dp->bsp', h, w_in)
    cuts = [D_MLSTM, 2 * D_MLSTM, 3 * D_MLSTM, 4 * D_MLSTM, 4 * D_MLSTM + N_GATE_COLS]
    q, k, v, o, gates, u = jnp.split(proj, cuts, axis=-1)

    def heads(a):
        return a.reshape(B, S, H, HEAD_DIM).transpose(0, 2, 1, 3).astype(jnp.float32)

    qh = heads(q) * (HEAD_DIM ** -0.5)
    kh = heads(k)
    vh = heads(v)
    gates = gates.astype(jnp.float32).reshape(B, S, N_DIRS, 2, H).transpose(2, 3, 0, 4, 1)
    log_i = gates[:, 0] + b_igate.astype(jnp.float32)[:, None, :, None]
    log_f = jax.nn.log_sigmoid(gates[:, 1] + b_fgate.astype(jnp.float32)[:, None, :, None])

    def flip(a):
        return jnp.flip(a, axis=2)

    h_fwd = _mlstm_scan(qh, kh, vh, log_i[0], log_f[0])
    h_bwd = flip(_mlstm_scan(flip(qh), flip(kh), flip(vh), flip(log_i[1]), flip(log_f[1])))
    hm = h_fwd + h_bwd
    hm = hm * lax.rsqrt(jnp.mean(hm * hm, axis=-1, keepdims=True) + EPS)
    hm = hm * g_head.astype(jnp.float32).reshape(H, 1, HEAD_DIM)
    hm = hm.transpose(0, 2, 1, 3).reshape(B, S, D_MLSTM)
    y_a = (jax.nn.sigmoid(o.astype(jnp.float32)) * hm).astype(h.dtype)

    y_b = _multiscale_pool(u, w_pool, pool_scale)
    y = jnp.concatenate([y_a, y_b], axis=-1)
    return jnp.einsum('bsm,md->bsd', y, w_out)


def setup_inputs(seed: int = 0) -> dict:
    key = jax.random.key(seed)
    ks = jax.random.split(key, 17)
    f32 = jnp.float32
    H = N_MLSTM_HEADS

    def nrm(k, shape, scale):
        return jax.random.normal(k, shape, f32) * scale

    x = nrm(ks[0], (BATCH, SEQ, D_MODEL), 1.0)
    c = nrm(ks[1], (BATCH, D_MODEL), 1.0)
    w_ada = nrm(ks[2], (DEPTH, D_MODEL, 6 * D_MODEL), D_MODEL ** -0.5)
    b_ada = nrm(ks[3], (DEPTH, 6 * D_MODEL), 0.02)
    g_mix = 1.0 + nrm(ks[4], (DEPTH, D_MODEL), 0.05)
    w_in = nrm(ks[5], (DEPTH, D_MODEL, D_IN_PROJ), D_MODEL ** -0.5)
    b_igate = nrm(ks[6], (DEPTH, N_DIRS, H), 0.1)
    b_fgate = (jnp.broadcast_to(jnp.linspace(3.0, 6.0, H, dtype=f32), (DEPTH, N_DIRS, H))
               + nrm(ks[7], (DEPTH, N_DIRS, H), 0.1))
    g_head = 1.0 + nrm(ks[8], (DEPTH, D_MLSTM), 0.05)
    w_pool = nrm(ks[9], (DEPTH, N_POOL_GROUPS, POOL_GROUP_DIM, POOL_GROUP_DIM), POOL_GROUP_DIM ** -0.5)
    pool_scale = 1.0 + nrm(ks[10], (DEPTH, D_POOL), 0.1)
    w_out = nrm(ks[11], (DEPTH, D_MIX, D_MODEL), D_MIX ** -0.5)
    g_ffn = 1.0 + nrm(ks[12], (DEPTH, D_MODEL), 0.05)
    w_ff1 = nrm(ks[13], (DEPTH, D_MODEL, D_FF), D_MODEL ** -0.5)
    w_ff2 = nrm(ks[14], (DEPTH, D_FF, D_MODEL), D_FF ** -0.5)
    g_final = 1.0 + nrm(ks[15], (D_MODEL,), 0.05)
    return {"x": x, "c": c, "w_ada": w_ada, "b_ada": b_ada, "g_mix": g_mix, "w_in": w_in,
            "b_igate": b_igate, "b_fgate": b_fgate, "g_head": g_head, "w_pool": w_pool,
            "pool_scale": pool_scale, "w_out": w_out, "g_ffn": g_ffn, "w_ff1": w_ff1,
            "w_ff2": w_ff2, "g_final": g_final}


def reference(x, c, w_ada, b_ada, g_mix, w_in, b_igate, b_fgate, g_head, w_pool,
              pool_scale, w_out, g_ffn, w_ff1, w_ff2, g_final):
    c_act = jax.nn.silu(c)
    for l in range(DEPTH):
        mod = c_act @ w_ada[l] + b_ada[l]
        sh1, sc1, gt1, sh2, sc2, gt2 = jnp.split(mod, 6, axis=-1)
        h = _modulate(_rmsnorm(x, g_mix[l]), sh1, sc1)
        x = x + gt1[:, None, :] * _mixer(h, w_in[l], b_igate[l], b_fgate[l], g_head[l],
                                         w_pool[l], pool_scale[l], w_out[l])
        h = _modulate(_rmsnorm(x, g_ffn[l]), sh2, sc2)
        a = jnp.square(jax.nn.relu(jnp.einsum('bsd,df->bsf', h, w_ff1[l])))
        x = x + gt2[:, None, :] * jnp.einsum('bsf,fd->bsd', a, w_ff2[l])
    return _rmsnorm(x, g_final)
```

```python
import numpy as np
import ml_dtypes
from contextlib import ExitStack

import concourse.bass as bass
import concourse.mybir as mybir
from concourse.bass_utils import run_bass_kernel_spmd

F32 = mybir.dt.float32
BF16 = mybir.dt.bfloat16
AF = mybir.ActivationFunctionType
ALU = mybir.AluOpType
AX = mybir.AxisListType

P = 128
S = 4096
D = 1024
KD = 8
NCH = 32
NG = 8
DFF = 4096
EPS = 1e-6
POOL_W = (2, 4, 8, 16)
N_CORES = 8


class T:
    __slots__ = ("ap", "w", "r", "dsem", "dcount", "name", "excl")

    def __init__(self, ap, name="", excl=False):
        self.ap = ap
        self.excl = excl
        self.w = []
        self.r = []
        self.dsem = None
        self.dcount = 0
        self.name = name

    def __getitem__(self, k):
        return self.ap[k]


class _Op:
    __slots__ = ("eng", "idx", "method", "kwargs", "waits", "marked", "dma_inc", "rank")


class Sched:
    ENG = ("pe", "act", "dve", "pool", "sp")

    def __init__(self, nc, es):
        self.nc = nc
        self.es = es
        self.h = {"pe": nc.tensor, "act": nc.scalar, "dve": nc.vector, "pool": nc.gpsimd, "sp": nc.sync}
        self.sem = {e: es.enter_context(nc.semaphore("s_" + e)) for e in ("pe", "act", "dve", "pool")}
        self.ops = []
        self.eng_ops = {e: [] for e in self.ENG}
        self.waited_c = {e: {x: -1 for x in self.ENG} for e in self.ENG}
        self.waited_d = {e: {} for e in self.ENG}
        self.dsems = []
        self.nsem = 0

    def _collect(self, eng, reads, writes, joins):
        toks = []
        for t in reads:
            toks.extend(t.w)
            if t.excl:
                toks.extend(x for x in t.r if x[0] == "c" and x[1].eng != eng)
        for t in writes:
            toks.extend(t.w)
            toks.extend(t.r)
        for t in joins:
            toks.extend(t.r)
        waits = []
        wc = self.waited_c[eng]
        wd = self.waited_d[eng]
        for tk in toks:
            if tk[0] == "c":
                d = tk[1]
                if d.eng == eng and eng == "pe":
                    continue
                if wc[d.eng] >= d.idx:
                    continue
                wc[d.eng] = d.idx
                d.marked = True
                waits.append(tk)
            else:
                _, sem, val = tk
                k = id(sem)
                if wd.get(k, 0) >= val:
                    continue
                wd[k] = val
                waits.append(tk)
        return waits

    def _post(self, tok, reads, writes, joins):
        for t in writes:
            t.w = [tok]
            t.r = []
        def same(x):
            if x[0] != tok[0]:
                return False
            if tok[0] == "c":
                return x[1].eng == tok[1].eng
            return x[1] is tok[1]
        for t in joins:
            t.w = [x for x in t.w if not same(x)] + [tok]
        for t in reads:
            t.r = [x for x in t.r if not same(x)] + [tok]

    def op(self, eng, method, kwargs, reads=(), writes=(), joins=()):
        o = _Op()
        o.eng = eng
        o.idx = len(self.eng_ops[eng])
        o.method = method
        o.kwargs = kwargs
        o.marked = False
        o.dma_inc = None
        o.waits = self._collect(eng, reads, writes, joins)
        self.eng_ops[eng].append(o)
        self.ops.append(o)
        self._post(("c", o), reads, writes, joins)
        return o

    def _dsem(self, t):
        if t.dsem is None:
            t.dsem = self.es.enter_context(self.nc.semaphore("d%d" % self.nsem))
            self.nsem += 1
            self.dsems.append(t)
        return t.dsem

    def dma(self, q, kwargs, reads=(), writes=(), joins=()):
        o = _Op()
        o.eng = q
        o.idx = -1
        o.method = self.h[q].dma_start
        o.kwargs = kwargs
        o.marked = False
        o.waits = self._collect(q, reads, writes, joins)
        owner = (list(writes) + list(joins) + list(reads))[0]
        sem = self._dsem(owner)
        owner.dcount += 16
        o.dma_inc = (sem, owner.dcount)
        self.ops.append(o)
        self._post(("d", sem, owner.dcount), reads, writes, joins)
        return o

    def barrier(self):
        for e in self.ENG:
            o = _Op()
            o.eng = e
            o.idx = -1
            o.method = None
            o.kwargs = None
            o.marked = False
            o.dma_inc = None
            waits = []
            for x in ("pe", "act", "dve", "pool"):
                if self.eng_ops[x]:
                    d = self.eng_ops[x][-1]
                    if x == e or self.waited_c[e][x] >= d.idx:
                        continue
                    self.waited_c[e][x] = d.idx
                    d.marked = True
                    waits.append(("c", d))
            for t in self.dsems:
                k = id(t.dsem)
                if self.waited_d[e].get(k, 0) < t.dcount:
                    self.waited_d[e][k] = t.dcount
                    waits.append(("d", t.dsem, t.dcount))
            o.waits = waits
            self.ops.append(o)

    def emit(self):
        for e in ("pe", "act", "dve", "pool"):
            r = 0
            for o in self.eng_ops[e]:
                if o.marked:
                    r += 1
                o.rank = r
        nw = 0
        for o in self.ops:
            h = self.h[o.eng]
            for tk in o.waits:
                if tk[0] == "c":
                    h.wait_ge(self.sem[tk[1].eng], tk[1].rank)
                else:
                    h.wait_ge(tk[1], tk[2])
                nw += 1
            if o.method is None:
                continue
            ins = o.method(**o.kwargs)
            if o.dma_inc is not None:
                ins.then_inc(o.dma_inc[0], 16)
            elif o.marked:
                ins.then_inc(self.sem[o.eng], 1)
        self.stats = {e: len(self.eng_ops[e]) for e in self.ENG}
        self.stats["waits"] = nw
        self.stats["ops"] = len(self.ops)


class Arena:
    def __init__(self, base_ap_f32, nbytes):
        self.base = base_ap_f32
        self.n = nbytes
        self.off = 0
        self.peak = 0

    def mark(self):
        return self.off

    def release(self, m):
        self.off = m

    def alloc(self, free_shape, dtype, name=""):
        esz = 2 if dtype == BF16 else 4
        n = int(np.prod(free_shape))
        nb = (n * esz + 63) // 64 * 64
        assert self.off + nb <= self.n, "SBUF arena overflow at %s: %d + %d > %d" % (name, self.off, nb, self.n)
        ap = self.base[:, self.off // 4:(self.off + nb) // 4]
        if dtype == BF16:
            ap = ap.bitcast(BF16)
        ap = ap[:, 0:n]
        if len(free_shape) == 2:
            ap = ap.rearrange("p (a b) -> p a b", a=free_shape[0])
        elif len(free_shape) == 3:
            ap = ap.rearrange("p (a b c) -> p a b c", a=free_shape[0], b=free_shape[1])
        self.off += nb
        self.peak = max(self.peak, self.off)
        return ap


def build_program(stop_after=None, dbg=None, dbg_dt=None):
    nc = bass.Bass("TRN2", target_bir_lowering=False)

    def din(name, shape, dt=F32):
        return nc.dram_tensor(name, list(shape), dt, kind="ExternalInput").ap()

    x_d = din("x", [S, D])
    cT_d = din("cT", [P, KD])
    w_ada_d = din("w_ada", [D, 6 * D])
    b_adaP_d = din("b_adaP", [P, 48])
    b_adaR_d = din("b_adaR", [1, 6 * D])
    g_mixP_d = din("g_mixP", [P, KD])
    g_ffnP_d = din("g_ffnP", [P, KD])
    g_finR_d = din("g_finR", [1, D])
    w_in_d = din("w_in", [D, 2576])
    b_ig_d = din("b_ig", [1, 8])
    b_fg_d = din("b_fg", [1, 8])
    g_headP_d = din("g_headP", [P, 4])
    psclP_d = din("pool_scaleP", [P, 4])
    w_pool_d = din("w_pool", [P, 4, P])
    w_out_d = din("w_out", [D, D])
    w_ff1_d = din("w_ff1", [D, DFF])
    w_ff2_d = din("w_ff2", [DFF, D])
    ident_f_d = din("ident_f", [P, P])
    ident_b_d = din("ident_b", [P, P], BF16)
    maskF_d = din("maskF", [P, P])
    maskB_d = din("maskB", [P, P])
    ones_d = din("ones_f", [P, P])
    edge_d = din("edge_fac", [1, 64])
    out_d = nc.dram_tensor("out", [S, D], F32, kind="ExternalOutput").ap()
    dbg_d = None
    if dbg is not None:
        dbg_d = nc.dram_tensor("dbg", list(dbg), dbg_dt or F32, kind="ExternalOutput").ap()

    w_ada_v = w_ada_d.rearrange("(k p) n -> p k n", p=P)
    w_in_v = w_in_d.rearrange("(k p) n -> p k n", p=P)
    w_out_v = w_out_d.rearrange("(k p) n -> p k n", p=P)
    w_ff1_v = w_ff1_d.rearrange("(k p) n -> p k n", p=P)
    w_ff2_v = w_ff2_d.rearrange("(k p) n -> p k n", p=P)
    x_v = x_d.rearrange("(c p) d -> c p d", p=P)
    out_v = out_d.rearrange("(c p) d -> c p d", p=P)

    SB_BYTES = 207 * 1024
    es = ExitStack()
    with es:
        sb_all = es.enter_context(nc.sbuf_tensor("sb_all", [P, SB_BYTES // 4], F32))
        ps_all = es.enter_context(nc.psum_tensor("ps_all", [P, 4096], F32))
        es.enter_context(nc.Block())
        Sd = Sched(nc, es)
        A = Arena(sb_all[:], SB_BYTES)
        V, ACT, PE, POOL = nc.vector, nc.scalar, nc.tensor, nc.gpsimd

        def bank(b, n=1):
            return ps_all[:, b * 512:(b + n) * 512]

        PS = [T(bank(b), "bank%d" % b, excl=True) for b in range(8)]

        hT_ap = A.alloc([KD, S], BF16, "hT")
        hT = [[T(hT_ap[:, k, g * 512:(g + 1) * 512], "hT%d_%d" % (k, g)) for g in range(NG)] for k in range(KD)]
        ident_b = T(A.alloc([P], BF16, "ident_b"))
        ident_f = T(A.alloc([P], F32, "ident_f"))
        maskF = T(A.alloc([P], F32, "maskF"))
        maskB = T(A.alloc([P], F32, "maskB"))
        ones_f = T(A.alloc([P], F32, "ones"))
        cst = T(A.alloc([64], F32, "cst_misc"))
        modP = T(A.alloc([48], F32, "modP"))
        sc1 = T(A.alloc([KD], F32, "sc1"))
        sc2 = T(A.alloc([KD], F32, "sc2"))
        gt1B = T(A.alloc([D], F32, "gt1B"))
        gt2B = T(A.alloc([D], F32, "gt2B"))
        WGT = T(A.alloc([8, NCH], F32, "WGT"))
        STAB = T(A.alloc([8, NCH], F32, "STAB"))
        DEC = T(A.alloc([8, NCH], F32, "DEC"))

        Sd.op("dve", V.memset, dict(ap=cst.ap[:, 0:1], constant=EPS), writes=[cst])
        for t, d in ((ident_b, ident_b_d), (ident_f, ident_f_d), (maskF, maskF_d), (maskB, maskB_d), (ones_f, ones_d)):
            Sd.dma("sp", dict(out=t.ap, in_=d[:, :]), writes=[t])

        def finish():
            Sd.barrier()
            Sd.emit()
            return nc, Sd, A


        mA = A.mark()
        cT = T(A.alloc([KD], F32, "cT"))
        c_act = T(A.alloc([KD], F32, "c_act"))
        c_rep = T(A.alloc([KD, P], F32, "c_rep"))
        b_adaP = T(A.alloc([48], F32, "b_adaP"))
        g_mixP = T(A.alloc([KD], F32, "g_mixP"))
        g_ffnP = T(A.alloc([KD], F32, "g_ffnP"))
        wA = [T(A.alloc([KD, 512], F32, "wA%d" % i)) for i in range(2)]
        psM = PS[0]
        psBc = PS[1]

        Sd.dma("sp", dict(out=cT.ap, in_=cT_d[:, :]), writes=[cT])
        Sd.dma("sp", dict(out=b_adaP.ap, in_=b_adaP_d[:, :]), writes=[b_adaP])
        Sd.dma("sp", dict(out=g_mixP.ap, in_=g_mixP_d[:, :]), writes=[g_mixP])
        Sd.dma("sp", dict(out=g_ffnP.ap, in_=g_ffnP_d[:, :]), writes=[g_ffnP])
        Sd.dma("sp", dict(out=gt1B.ap, in_=b_adaR_d[:, 2 * D:3 * D].partition_broadcast(P)), writes=[gt1B])
        Sd.dma("sp", dict(out=gt2B.ap, in_=b_adaR_d[:, 5 * D:6 * D].partition_broadcast(P)), writes=[gt2B])
        Sd.op("act", ACT.activation, dict(out=c_act.ap, in_=cT.ap, func=AF.Silu), reads=[cT], writes=[c_act])
        Sd.op("dve", V.tensor_copy, dict(out=c_rep.ap, in_=c_act.ap.unsqueeze(2).to_broadcast([P, KD, P])),
              reads=[c_act], writes=[c_rep])

        piece_order = [0, 1, 2, 3, 6, 7, 8, 9, 4, 5, 10, 11]
        for n, pc in enumerate(piece_order):
            buf = wA[n % 2]
            Sd.dma("pool", dict(out=buf.ap, in_=w_ada_v[:, :, pc * 512:(pc + 1) * 512]), writes=[buf])
            if pc in (4, 5, 10, 11):
                gt = gt1B if pc in (4, 5) else gt2B
                half = pc % 2
                for k in range(KD):
                    Sd.op("pe", PE.matmul, dict(out=psBc.ap, lhsT=c_rep.ap[:, k, :], rhs=buf.ap[:, k, :],
                                                start=(k == 0), stop=(k == KD - 1)),
                          reads=[c_rep, buf], writes=[psBc] if k == 0 else [], joins=[] if k == 0 else [psBc])
                sl = gt.ap[:, half * 512:(half + 1) * 512]
                Sd.op("dve", V.tensor_tensor, dict(out=sl, in0=psBc.ap, in1=sl, op=ALU.add),
                      reads=[psBc, gt], joins=[gt])
            else:
                for j in range(4):
                    blk = pc * 4 + j
                    for k in range(KD):
                        Sd.op("pe", PE.matmul, dict(out=psM.ap[:, blk:blk + 1], lhsT=buf.ap[:, k, j * P:(j + 1) * P],
                                                    rhs=c_act.ap[:, k:k + 1], start=(k == 0), stop=(k == KD - 1)),
                              reads=[c_act, buf], joins=[psM])
                if pc in (3, 9):
                    lo = 0 if pc == 3 else 24
                    Sd.op("dve", V.tensor_tensor, dict(out=modP.ap[:, lo:lo + 16], in0=psM.ap[:, lo:lo + 16],
                                                       in1=b_adaP.ap[:, lo:lo + 16], op=ALU.add),
                          reads=[psM, b_adaP], joins=[modP])
                    sc = sc1 if pc == 3 else sc2
                    gP = g_mixP if pc == 3 else g_ffnP
                    Sd.op("dve", V.scalar_tensor_tensor, dict(out=sc.ap, in0=modP.ap[:, lo + 8:lo + 16], scalar=1.0,
                                                              in1=gP.ap, op0=ALU.add, op1=ALU.mult),
                          reads=[modP, gP], writes=[sc])
        if stop_after == "0":
            Sd.barrier()
            Sd.dma("sp", dict(out=dbg_d[:, 0:48], in_=modP.ap), reads=[modP])
            Sd.dma("sp", dict(out=dbg_d[:, 48:56], in_=sc1.ap), reads=[sc1])
            Sd.dma("sp", dict(out=dbg_d[:, 56:64], in_=sc2.ap), reads=[sc2])
            Sd.dma("sp", dict(out=dbg_d[:, 64:64 + D], in_=gt1B.ap), reads=[gt1B])
            Sd.dma("sp", dict(out=dbg_d[:, 64 + D:64 + 2 * D], in_=gt2B.ap), reads=[gt2B])
            return finish()

        xin = [T(A.alloc([D], F32, "xin%d" % i)) for i in range(4)]
        xn = [T(A.alloc([D], BF16, "xn%d" % i)) for i in range(2)]
        junk = A.alloc([D], BF16, "junk")
        ssq = [T(A.alloc([4], F32, "ssq%d" % i)) for i in range(4)]
        psT_ap = [bank(2 + 2 * i, 2).bitcast(BF16).rearrange("p (k t) -> p k t", k=KD) for i in range(2)]
        psT_t = [[PS[2], PS[3]], [PS[4], PS[5]]]

        import os as _os
        _nch = int(_os.environ.get('K_NCH', NCH)); _var = _os.environ.get('K_VAR', '')
        for c in range(_nch):
            xt = xin[c % 4]
            sq = ssq[c % 4]
            xb = xn[c % 2]
            pT = psT_ap[(c // 2) % 2]
            pTt = psT_t[(c // 2) % 2]
            Sd.dma("sp", dict(out=xt.ap, in_=x_v[c]), writes=[xt])
            Sd.op("act", ACT.activation, dict(out=junk, in_=xt.ap, func=AF.Square, accum_out=sq.ap[:, 0:1]),
                  reads=[xt], writes=[sq])
            Sd.op("act", ACT.activation, dict(out=sq.ap[:, 1:2], in_=sq.ap[:, 0:1], func=AF.Ln, scale=1.0 / D,
                                              bias=cst.ap[:, 0:1]), reads=[sq, cst], joins=[sq])
            Sd.op("act", ACT.activation, dict(out=sq.ap[:, 2:3], in_=sq.ap[:, 1:2], func=AF.Exp, scale=-0.5),
                  reads=[sq], joins=[sq])
            Sd.op("dve", V.tensor_scalar, dict(out=xb.ap, in0=xt.ap, scalar1=sq.ap[:, 2:3], scalar2=None, op0=ALU.mult),
                  reads=[xt, sq], writes=[xb])
            for k in range(KD):
                first = (c % 2 == 0 and k == 0)
                Sd.op("pe", PE.transpose, dict(out=pT[:, k, (c % 2) * P:(c % 2 + 1) * P], in_=xb.ap[:, k * P:(k + 1) * P],
                                               identity=ident_b.ap),
                      reads=[xb, ident_b], writes=pTt if first else [], joins=[] if first else pTt)
            if c % 2 == 1:
                g = c // 4
                off = ((c // 2) % 2) * 256
                for k in range(KD):
                    dst = hT[k][g].ap[:, off:off + 256]
                    Sd.op("act", ACT.activation, dict(out=dst, in_=pT[:, k, :], func=AF.Identity,
                                                      scale=sc1.ap[:, k:k + 1], bias=modP.ap[:, k:k + 1]),
                          reads=[pTt[k // 4], sc1, modP], joins=[hT[k][g]])

        if stop_after == "A2":
            Sd.barrier()
            Sd.dma("sp", dict(out=dbg_d[:, 0:8], in_=sc1.ap), reads=[sc1])
            return finish()
        if stop_after == "A":
            Sd.barrier()
            for k in range(KD):
                for g in range(NG):
                    Sd.dma("sp", dict(out=dbg_d[:, k * S + g * 512:k * S + (g + 1) * 512], in_=hT[k][g].ap), reads=[hT[k][g]])
            return finish()

        w_g = T(A.alloc([KD, 16], BF16, "w_g"))
        Sd.dma("pool", dict(out=w_g.ap, in_=w_in_v[:, :, 2048:2064]), writes=[w_g])
        bI = T(A.alloc([8], F32, "bI"))
        bF = T(A.alloc([8], F32, "bF"))
        Sd.dma("sp", dict(out=bI.ap, in_=b_ig_d.partition_broadcast(P)), writes=[bI])
        Sd.dma("sp", dict(out=bF.ap, in_=b_fg_d.partition_broadcast(P)), writes=[bF])
        Sd.op("dve", V.memset, dict(ap=cst.ap[:, 1:2], constant=1.0), joins=[cst])
        psG = PS[6]
        psG_ap = bank(6).rearrange("p (d j g) -> p d j g", d=2, j=NCH)
        for c in range(NCH):
            for dr in range(2):
                j = c if dr == 0 else NCH - 1 - c
                for k in range(KD):
                    Sd.op("pe", PE.matmul, dict(out=psG_ap[:, dr, j, :], lhsT=hT[k][c // 4].ap[:, (c % 4) * P:(c % 4 + 1) * P],
                                                rhs=w_g.ap[:, k, dr * 8:(dr + 1) * 8], start=(k == 0), stop=(k == KD - 1)),
                          reads=[hT[k][c // 4], w_g], joins=[psG])

        def tab(name, n=NCH):
            return T(A.alloc([8, n], F32, name))

        def v4(ap):
            return ap.rearrange("p (d h) j -> p d h j", d=2)

        def fl(ap):
            return ap.rearrange("p a j -> p (a j)")

        LI, ZZ, CS, GP, GB, BT, RR, TMP = (tab(n) for n in ("LI", "ZZ", "CS", "GP", "GB", "BT", "RR", "TMP"))
        MN = tab("MN", NCH + 1)
        Gcol = T(A.alloc([2], F32, "Gcol"))
        Grow = T(A.alloc([256], F32, "Grow"))
        g_i = psG_ap[:, :, :, 0:4].rearrange("p d j h -> p d h j")
        g_f = psG_ap[:, :, :, 4:8].rearrange("p d j h -> p d h j")
        bIb = bI.ap.rearrange("p (d h) -> p d h", d=2).unsqueeze(3).to_broadcast([P, 2, 4, NCH])
        bFb = bF.ap.rearrange("p (d h) -> p d h", d=2).unsqueeze(3).to_broadcast([P, 2, 4, NCH])
        Sd.op("dve", V.tensor_tensor, dict(out=v4(LI.ap), in0=g_i, in1=bIb, op=ALU.add), reads=[psG, bI], writes=[LI])
        Sd.op("dve", V.tensor_tensor, dict(out=v4(ZZ.ap), in0=g_f, in1=bFb, op=ALU.add), reads=[psG, bF], writes=[ZZ])
        Sd.op("act", ACT.activation, dict(out=fl(ZZ.ap), in_=fl(ZZ.ap), func=AF.Exp, scale=-1.0), reads=[ZZ], writes=[ZZ])
        Sd.op("act", ACT.activation, dict(out=fl(ZZ.ap), in_=fl(ZZ.ap), func=AF.Ln, bias=cst.ap[:, 1:2]), reads=[ZZ, cst], writes=[ZZ])
        if stop_after == "G1":
            Sd.barrier()
            for i, t in enumerate((LI, ZZ)):
                Sd.dma("sp", dict(out=dbg_d[:, i * 256:(i + 1) * 256].rearrange("p (a j) -> p a j", a=8), in_=t.ap[:, :, 0:NCH]), reads=[t])
            return finish()
        psC, psC_ap = PS[7], bank(7)[:, 0:256]
        psBt, psBt_ap = PS[0], bank(0)[:, 0:256]
        Sd.op("pe", PE.matmul, dict(out=psC_ap[:, 0:128], lhsT=maskF.ap, rhs=fl(ZZ.ap)[:, 0:128], start=True, stop=True),
              reads=[maskF, ZZ], writes=[psC])
        Sd.op("pe", PE.matmul, dict(out=psC_ap[:, 128:256], lhsT=maskB.ap, rhs=fl(ZZ.ap)[:, 128:256], start=True, stop=True),
              reads=[maskB, ZZ], joins=[psC])
        Sd.op("pe", PE.matmul, dict(out=psBt_ap, lhsT=ones_f.ap, rhs=fl(ZZ.ap), start=True, stop=True),
              reads=[ones_f, ZZ], writes=[psBt])
        Sd.op("act", ACT.activation, dict(out=fl(CS.ap), in_=psC_ap, func=AF.Copy), reads=[psC], writes=[CS])
        if stop_after == "G2a":
            Sd.barrier()
            Sd.dma("sp", dict(out=dbg_d[:, 0:256].rearrange("p (a j) -> p a j", a=8), in_=CS.ap), reads=[CS])
            return finish()
        Sd.op("dve", V.tensor_tensor, dict(out=fl(GP.ap), in0=psC_ap, in1=fl(LI.ap), op=ALU.add), reads=[psC, LI], writes=[GP])
        Sd.op("act", ACT.activation, dict(out=fl(BT.ap), in_=psBt_ap, func=AF.Copy, scale=-1.0), reads=[psBt], writes=[BT])
        if stop_after == "G2":
            Sd.barrier()
            for i, t in enumerate((CS, GP, BT)):
                Sd.dma("sp", dict(out=dbg_d[:, i * 256:(i + 1) * 256].rearrange("p (a j) -> p a j", a=8), in_=t.ap[:, :, 0:NCH]), reads=[t])
            return finish()
        psX, psX_ap = PS[1], bank(1)[:, 0:256]
        psR, psR_ap = PS[2], bank(2)[0:1, 0:256]
        psGB, psGB_ap = PS[3], bank(3)[:, 0:256]
        for hf in range(2):
            Sd.op("pe", PE.transpose, dict(out=psX_ap[:, hf * P:(hf + 1) * P], in_=fl(GP.ap)[:, hf * P:(hf + 1) * P], identity=ident_f.ap),
                  reads=[GP, ident_f], writes=[psX] if hf == 0 else [], joins=[] if hf == 0 else [psX])
        Sd.op("dve", V.tensor_reduce, dict(out=Gcol.ap, in_=psX_ap.rearrange("p (a s) -> p a s", a=2), axis=AX.X, op=ALU.max),
              reads=[psX], writes=[Gcol])
        for hf in range(2):
            Sd.op("pe", PE.transpose, dict(out=psR_ap[:, hf * P:(hf + 1) * P], in_=Gcol.ap[:, hf:hf + 1], identity=ident_f.ap),
                  reads=[Gcol, ident_f], writes=[psR] if hf == 0 else [], joins=[] if hf == 0 else [psR])
        Sd.op("act", ACT.activation, dict(out=Grow.ap[0:1, :], in_=psR_ap, func=AF.Copy), reads=[psR], writes=[Grow])
        Sd.op("pe", PE.matmul, dict(out=psGB_ap, lhsT=ones_f.ap[0:1, :], rhs=Grow.ap[0:1, :], start=True, stop=True),
              reads=[ones_f, Grow], writes=[psGB])
        Sd.op("act", ACT.activation, dict(out=fl(GB.ap), in_=psGB_ap, func=AF.Copy), reads=[psGB], writes=[GB])
        if stop_after == "G3":
            Sd.barrier()
            for i, t in enumerate((GB, GP)):
                Sd.dma("sp", dict(out=dbg_d[:, i * 256:(i + 1) * 256].rearrange("p (a j) -> p a j", a=8), in_=t.ap[:, :, 0:NCH]), reads=[t])
            return finish()
        Sd.op("dve", V.memset, dict(ap=MN.ap[:, :, 0:1], constant=0.0), writes=[MN])
        for dh in range(8):
            Sd.op("dve", V.tensor_tensor_scan, dict(out=MN.ap[:, dh, 1:NCH + 1], data0=GB.ap[:, dh, :], data1=BT.ap[:, dh, :],
                                                    initial=0.0, op0=ALU.max, op1=ALU.add), reads=[GB, BT], joins=[MN])
        Sd.op("dve", V.tensor_tensor, dict(out=RR.ap, in0=MN.ap[:, :, 0:NCH], in1=GB.ap, op=ALU.max), reads=[MN, GB], writes=[RR])
        for src, dst in ((MN, DEC), (GP, WGT), (CS, STAB)):
            sap = src.ap[:, :, 0:NCH]
            Sd.op("dve", V.tensor_tensor, dict(out=TMP.ap, in0=sap, in1=RR.ap, op=ALU.subtract), reads=[src, RR], writes=[TMP])
            Sd.op("act", ACT.activation, dict(out=fl(dst.ap), in_=fl(TMP.ap), func=AF.Exp), reads=[TMP], writes=[dst])

        if stop_after == "G":
            Sd.barrier()
            for i, t in enumerate((WGT, STAB, DEC, MN)):
                Sd.dma("sp", dict(out=dbg_d[:, i * 256:(i + 1) * 256].rearrange("p (a j) -> p a j", a=8), in_=t.ap[:, :, 0:NCH]), reads=[t])
            return finish()

        Sd.barrier()
        A.release(mA)
        hm_ap = A.alloc([NCH, 512], F32, "hm")
        hm = [[T(hm_ap[:, c, h * P:(h + 1) * P]) for h in range(4)] for c in range(NCH)]
        mH = A.mark()
        wqkv = T(A.alloc([KD, 3, P], BF16, "wqkv"))
        qT_ap = A.alloc([S], BF16, "qT")
        kT_ap = A.alloc([S], BF16, "kT")
        qT = [T(qT_ap[:, g * 512:(g + 1) * 512]) for g in range(NG)]
        kT = [T(kT_ap[:, g * 512:(g + 1) * 512]) for g in range(NG)]
        va_ap = A.alloc([NCH, 130], BF16, "va")
        va = [T(va_ap[:, c, :]) for c in range(NCH)]
        kwF_ap = A.alloc([NCH, P], BF16, "kwF")
        kwB_ap = A.alloc([NCH, P], BF16, "kwB")
        kwF = [T(kwF_ap[:, c, :]) for c in range(NCH)]
        kwB = [T(kwB_ap[:, c, :]) for c in range(NCH)]
        sTw = [T(A.alloc([P], BF16, "sTw%d" % i)) for i in range(4)]
        Cst = [T(A.alloc([130], F32, "Cst%d" % i)) for i in range(2)]
        Csb = [[T(A.alloc([130], BF16, "Csb%d%d" % (i, n))) for n in range(2)] for i in range(2)]
        dnr = [T(A.alloc([2], F32, "dnr%d" % i)) for i in range(4)]
        Sd.op("dve", V.memset, dict(ap=va_ap[:, :, 128:130], constant=1.0), writes=va)
        QSCALE = float(P) ** -0.5
        _nheads = int(_os.environ.get('K_NH', 4))

        for h in range(_nheads):
            for i, off in enumerate((0, 512, 1024)):
                Sd.dma("pool", dict(out=wqkv.ap[:, :, i, :], in_=w_in_v[:, :, off + h * P:off + (h + 1) * P]),
                       writes=[wqkv] if i == 0 else [], joins=[] if i == 0 else [wqkv])
            for g in range(NG):
                for which, dstT, bb in ((0, qT, g % 2), (1, kT, 2 + g % 2)):
                    for k in range(KD):
                        Sd.op("pe", PE.matmul, dict(out=bank(bb), lhsT=wqkv.ap[:, k, which, :], rhs=hT[k][g].ap,
                                                    start=(k == 0), stop=(k == KD - 1)),
                              reads=[wqkv, hT[k][g]], writes=[PS[bb]] if k == 0 else [], joins=[] if k == 0 else [PS[bb]])
                    if which == 0:
                        Sd.op("act", ACT.activation, dict(out=dstT[g].ap, in_=bank(bb), func=AF.Copy, scale=QSCALE),
                              reads=[PS[bb]], writes=[dstT[g]])
                    else:
                        Sd.op("dve", V.tensor_copy, dict(out=dstT[g].ap, in_=bank(bb)), reads=[PS[bb]], writes=[dstT[g]])
                for ci in range(4):
                    c = g * 4 + ci
                    bb = 4 + ci
                    for k in range(KD):
                        Sd.op("pe", PE.matmul, dict(out=bank(bb)[:, 0:256], lhsT=hT[k][g].ap[:, ci * P:(ci + 1) * P],
                                                    rhs=wqkv.ap[:, k, 1:3, :].rearrange("p a b -> p (a b)"),
                                                    start=(k == 0), stop=(k == KD - 1)),
                              reads=[wqkv, hT[k][g]], writes=[PS[bb]] if k == 0 else [], joins=[] if k == 0 else [PS[bb]])
                    Sd.op("act", ACT.activation, dict(out=va[c].ap[:, 0:P], in_=bank(bb)[:, P:2 * P], func=AF.Copy),
                          reads=[PS[bb]], joins=[va[c]])
                    Sd.op("dve", V.tensor_scalar, dict(out=kwF[c].ap, in0=bank(bb)[:, 0:P], scalar1=WGT.ap[:, h, c:c + 1],
                                                       scalar2=None, op0=ALU.mult), reads=[PS[bb], WGT], writes=[kwF[c]])
                    jb = NCH - 1 - c
                    Sd.op("dve", V.tensor_scalar, dict(out=kwB[c].ap, in0=bank(bb)[:, 0:P], scalar1=WGT.ap[:, 4 + h, jb:jb + 1],
                                                       scalar2=None, op0=ALU.mult), reads=[PS[bb], WGT], writes=[kwB[c]])
            for j in range(NCH):
                for dr in range(2):
                    c = j if dr == 0 else NCH - 1 - j
                    dh = dr * 4 + h
                    g = c // 4
                    o = (c % 4) * P
                    mask = maskF if dr == 0 else maskB
                    kw = kwF if dr == 0 else kwB
                    st = sTw[(2 * j + dr) % 4]
                    d_ = dnr[(2 * j + dr) % 4]
                    Sd.op("pe", PE.matmul, dict(out=bank(dr)[:, 0:P], lhsT=kT[g].ap[:, o:o + P], rhs=qT[g].ap[:, o:o + P],
                                                start=True, stop=True), reads=[kT[g], qT[g]], writes=[PS[dr]])
                    Sd.op("dve", V.scalar_tensor_tensor, dict(out=st.ap, in0=bank(dr)[:, 0:P], scalar=WGT.ap[:, dh, j:j + 1],
                                                              in1=mask.ap, op0=ALU.mult, op1=ALU.mult),
                          reads=[PS[dr], WGT, mask], writes=[st])
                    Sd.op("pe", PE.matmul, dict(out=bank(2 + dr)[:, 0:129], lhsT=st.ap, rhs=va[c].ap[:, 0:129],
                                                start=True, stop=(j == 0)), reads=[st, va[c]], writes=[PS[2 + dr]])
                    if j > 0:
                        Sd.op("pe", PE.matmul, dict(out=bank(2 + dr)[:, 0:129], lhsT=qT[g].ap[:, o:o + P],
                                                    rhs=Csb[dr][j % 2].ap[:, 0:129], start=False, stop=True),
                              reads=[qT[g], Csb[dr][j % 2]], joins=[PS[2 + dr]])
                    Sd.op("act", ACT.activation, dict(out=d_.ap[:, 0:1], in_=bank(2 + dr)[:, 128:129], func=AF.Abs),
                          reads=[PS[2 + dr]], writes=[d_])
                    Sd.op("dve", V.tensor_scalar, dict(out=d_.ap[:, 0:1], in0=d_.ap[:, 0:1],
                                                       scalar1=STAB.ap[:, dh, j:j + 1], scalar2=None, op0=ALU.max),
                          reads=[d_, STAB], joins=[d_])
                    Sd.op("dve", V.reciprocal, dict(out=d_.ap[:, 1:2], in_=d_.ap[:, 0:1]), reads=[d_], joins=[d_])
                    if j < NCH // 2:
                        Sd.op("act", ACT.activation, dict(out=hm[c][h].ap, in_=bank(2 + dr)[:, 0:P], func=AF.Copy,
                                                          scale=d_.ap[:, 1:2]), reads=[PS[2 + dr], d_], writes=[hm[c][h]])
                    else:
                        Sd.op("dve", V.scalar_tensor_tensor, dict(out=hm[c][h].ap, in0=bank(2 + dr)[:, 0:P], scalar=d_.ap[:, 1:2],
                                                                  in1=hm[c][h].ap, op0=ALU.mult, op1=ALU.add),
                              reads=[PS[2 + dr], d_, hm[c][h]], writes=[hm[c][h]])
                    if j < NCH - 1:
                        Sd.op("pe", PE.matmul, dict(out=bank(4 + dr)[:, 0:129], lhsT=kw[c].ap, rhs=va[c].ap[:, 0:129],
                                                    start=True, stop=True), reads=[kw[c], va[c]], writes=[PS[4 + dr]])
                        if j == 0:
                            Sd.op("dve", V.tensor_copy, dict(out=Cst[dr].ap[:, 0:129], in_=bank(4 + dr)[:, 0:129]),
                                  reads=[PS[4 + dr]], writes=[Cst[dr]])
                        else:
                            Sd.op("dve", V.scalar_tensor_tensor, dict(out=Cst[dr].ap[:, 0:129], in0=Cst[dr].ap[:, 0:129],
                                                                      scalar=DEC.ap[:, dh, j:j + 1], in1=bank(4 + dr)[:, 0:129],
                                                                      op0=ALU.mult, op1=ALU.add),
                                  reads=[Cst[dr], DEC, PS[4 + dr]], writes=[Cst[dr]])
                        Sd.op("act", ACT.activation, dict(out=Csb[dr][(j + 1) % 2].ap[:, 0:129], in_=Cst[dr].ap[:, 0:129],
                                                          func=AF.Copy, scale=DEC.ap[:, dh, j + 1:j + 2]),
                              reads=[Cst[dr], DEC], writes=[Csb[dr][(j + 1) % 2]])

        if stop_after == "H":
            Sd.barrier()
            for c in range(NCH):
                Sd.dma("sp", dict(out=dbg_d[c * P:(c + 1) * P, 0:P * _nheads], in_=hm_ap[:, c, 0:P * _nheads]), reads=hm[c])
            return finish()

        Sd.barrier()
        A.release(mH)
        mY = A.mark()
        w_o = T(A.alloc([KD, 512], BF16, "w_o"))
        w_u = T(A.alloc([KD, 512], BF16, "w_u"))
        w_pl = T(A.alloc([4, P], BF16, "w_pl"))
        psw = T(A.alloc([4], F32, "psw"))
        pscl = T(A.alloc([4], F32, "pscl"))
        edge = T(A.alloc([4, 16], F32, "edge"))
        ubuf = T(A.alloc([4, 528], F32, "ubuf"))
        tP = T(A.alloc([4, 528], F32, "tP"))
        tQ = T(A.alloc([2, 528], F32, "tQ"))
        tA = T(A.alloc([4, 512], F32, "tA"))
        mixed = T(A.alloc([4, 512], BF16, "mixed"))
        ucarry = T(A.alloc([4, 8], F32, "ucarry"))
        sig = [T(A.alloc([512], F32, "sig%d" % i)) for i in range(4)]
        sqb = [T(A.alloc([512], F32, "sqb%d" % i)) for i in range(2)]
        ssh = T(A.alloc([16], F32, "ssh"))
        rsh = T(A.alloc([16], F32, "rsh"))
        yab = [T(A.alloc([512], BF16, "ya%d" % i)) for i in range(2)]
        Sd.dma("pool", dict(out=w_o.ap, in_=w_in_v[:, :, 1536:2048]), writes=[w_o])
        Sd.dma("pool", dict(out=w_u.ap, in_=w_in_v[:, :, 2064:2576]), writes=[w_u])
        Sd.dma("pool", dict(out=w_pl.ap, in_=w_pool_d[:, :, :]), writes=[w_pl])
        Sd.dma("sp", dict(out=pscl.ap, in_=psclP_d[:, :]), writes=[pscl])
        Sd.dma("sp", dict(out=edge.ap.rearrange("p a b -> p (a b)"), in_=edge_d.partition_broadcast(P)), writes=[edge])
        for pg, w in enumerate(POOL_W):
            Sd.op("dve", V.tensor_scalar, dict(out=psw.ap[:, pg:pg + 1], in0=pscl.ap[:, pg:pg + 1], scalar1=1.0 / w, scalar2=None,
                                               op0=ALU.mult), reads=[pscl], joins=[psw])
        Sd.op("dve", V.memset, dict(ap=cst.ap[:, 2:3], constant=EPS), joins=[cst])
        ptr_ap = bank(6, 2).bitcast(BF16).rearrange("p (m t) -> p m t", m=4)

        for G in range(NG):
            hTg = [hT[k][G] for k in range(KD)]
            for pg in range(4):
                bb = pg % 2
                for k in range(KD):
                    Sd.op("pe", PE.matmul, dict(out=bank(bb), lhsT=w_u.ap[:, k, pg * P:(pg + 1) * P], rhs=hT[k][G].ap,
                                                start=(k == 0), stop=(k == KD - 1)),
                          reads=[w_u, hT[k][G]], writes=[PS[bb]] if k == 0 else [], joins=[] if k == 0 else [PS[bb]])
                Sd.op("act", ACT.activation, dict(out=ubuf.ap[:, pg, 8:520], in_=bank(bb), func=AF.Copy),
                      reads=[PS[bb]], writes=[ubuf] if pg == 0 else [], joins=[] if pg == 0 else [ubuf])
            if G < NG - 1:
                for pg in range(4):
                    for k in range(KD):
                        Sd.op("pe", PE.matmul, dict(out=bank(2)[:, pg * 8:(pg + 1) * 8], lhsT=w_u.ap[:, k, pg * P:(pg + 1) * P],
                                                    rhs=hT[k][G + 1].ap[:, 0:8], start=(k == 0), stop=(k == KD - 1)),
                              reads=[w_u, hT[k][G + 1]], writes=[PS[2]] if (k == 0 and pg == 0) else [],
                              joins=[] if (k == 0 and pg == 0) else [PS[2]])
                Sd.op("act", ACT.activation, dict(out=ubuf.ap[:, :, 520:528], in_=bank(2)[:, 0:32].rearrange("p (a b) -> p a b", a=4),
                                                  func=AF.Copy), reads=[PS[2]], joins=[ubuf])
            else:
                Sd.op("pool", POOL.memset, dict(ap=ubuf.ap[:, :, 520:528], constant=0.0), joins=[ubuf])
            if G == 0:
                Sd.op("pool", POOL.memset, dict(ap=ubuf.ap[:, :, 0:8], constant=0.0), joins=[ubuf])
            else:
                Sd.op("pool", POOL.tensor_copy, dict(out=ubuf.ap[:, :, 0:8], in_=ucarry.ap), reads=[ucarry], joins=[ubuf])
            Sd.op("pool", POOL.tensor_copy, dict(out=ucarry.ap, in_=ubuf.ap[:, :, 512:520]), reads=[ubuf], writes=[ucarry])
            U = ubuf.ap
            Sd.op("pool", POOL.tensor_tensor, dict(out=tP.ap[:, :, 0:527], in0=U[:, :, 0:527], in1=U[:, :, 1:528], op=ALU.add),
                  reads=[ubuf], writes=[tP])
            Sd.op("pool", POOL.tensor_tensor, dict(out=tQ.ap[:, 0:2, 0:525], in0=tP.ap[:, 2:4, 0:525], in1=tP.ap[:, 2:4, 2:527], op=ALU.add),
                  reads=[tP], writes=[tQ])
            Sd.op("pool", POOL.tensor_tensor, dict(out=tP.ap[:, 3, 0:521], in0=tQ.ap[:, 1, 0:521], in1=tQ.ap[:, 1, 4:525], op=ALU.add),
                  reads=[tQ, tP], joins=[tP])
            Sd.op("pool", POOL.tensor_copy, dict(out=tA.ap[:, 0, :], in_=tP.ap[:, 0, 7:519]), reads=[tP], writes=[tA])
            Sd.op("pool", POOL.tensor_tensor, dict(out=tA.ap[:, 1, :], in0=tP.ap[:, 1, 6:518], in1=tP.ap[:, 1, 8:520], op=ALU.add),
                  reads=[tP], joins=[tA])
            Sd.op("pool", POOL.tensor_tensor, dict(out=tA.ap[:, 2, :], in0=tQ.ap[:, 0, 4:516], in1=tQ.ap[:, 0, 8:520], op=ALU.add),
                  reads=[tQ], joins=[tA])
            Sd.op("pool", POOL.tensor_tensor, dict(out=tA.ap[:, 3, :], in0=tP.ap[:, 3, 0:512], in1=tP.ap[:, 3, 8:520], op=ALU.add),
                  reads=[tP], joins=[tA])
            if G == 0:
                Sd.op("pool", POOL.tensor_tensor, dict(out=tA.ap[:, :, 0:8], in0=tA.ap[:, :, 0:8], in1=edge.ap[:, :, 0:8], op=ALU.mult),
                      reads=[tA, edge], joins=[tA])
            if G == NG - 1:
                Sd.op("pool", POOL.tensor_tensor, dict(out=tA.ap[:, :, 504:512], in0=tA.ap[:, :, 504:512], in1=edge.ap[:, :, 8:16], op=ALU.mult),
                      reads=[tA, edge], joins=[tA])
            for pg, w in enumerate(POOL_W):
                Sd.op("dve", V.scalar_tensor_tensor, dict(out=mixed.ap[:, pg, :], in0=U[:, pg, 8:520], scalar=-float(w), in1=tA.ap[:, pg, :],
                                                          op0=ALU.mult, op1=ALU.add),
                      reads=[ubuf, tA], writes=[mixed] if pg == 0 else [], joins=[] if pg == 0 else [mixed])
            for i in range(4):
                c = G * 4 + i
                bb = 3 + i % 2
                for k in range(KD):
                    Sd.op("pe", PE.matmul, dict(out=bank(bb), lhsT=hT[k][G].ap[:, i * P:(i + 1) * P], rhs=w_o.ap[:, k, :],
                                                start=(k == 0), stop=(k == KD - 1)),
                          reads=[w_o, hT[k][G]], writes=[PS[bb]] if k == 0 else [], joins=[] if k == 0 else [PS[bb]])
                Sd.op("act", ACT.activation, dict(out=sig[i].ap, in_=bank(bb), func=AF.Sigmoid), reads=[PS[bb]], writes=[sig[i]])
                sq = sqb[i % 2]
                Sd.op("dve", V.tensor_tensor, dict(out=sq.ap, in0=hm_ap[:, c, :], in1=hm_ap[:, c, :], op=ALU.mult),
                      reads=hm[c], writes=[sq])
                Sd.op("dve", V.tensor_reduce, dict(out=ssh.ap[:, i * 4:(i + 1) * 4], in_=sq.ap.rearrange("p (a b) -> p a b", a=4),
                                                   axis=AX.X, op=ALU.add), reads=[sq], writes=[ssh] if i == 0 else [],
                      joins=[] if i == 0 else [ssh])
            Sd.op("act", ACT.activation, dict(out=rsh.ap, in_=ssh.ap, func=AF.Ln, scale=1.0 / P, bias=cst.ap[:, 2:3]),
                  reads=[ssh, cst], writes=[rsh])
            Sd.op("act", ACT.activation, dict(out=rsh.ap, in_=rsh.ap, func=AF.Exp, scale=-0.5), reads=[rsh], writes=[rsh])
            for pg in range(4):
                Sd.op("pe", PE.matmul, dict(out=bank(5), lhsT=w_pl.ap[:, pg, :], rhs=mixed.ap[:, pg, :], start=True, stop=True),
                      reads=[w_pl, mixed], writes=[PS[5]])
                Sd.op("act", ACT.activation, dict(out=hT[4 + pg][G].ap, in_=bank(5), func=AF.Copy, scale=psw.ap[:, pg:pg + 1]),
                      reads=[PS[5], psw], writes=[hT[4 + pg][G]])
            for i in range(4):
                c = G * 4 + i
                ya = yab[i % 2]
                for h in range(4):
                    Sd.op("dve", V.scalar_tensor_tensor, dict(out=ya.ap[:, h * P:(h + 1) * P], in0=hm[c][h].ap,
                                                              scalar=rsh.ap[:, i * 4 + h:i * 4 + h + 1], in1=sig[i].ap[:, h * P:(h + 1) * P],
                                                              op0=ALU.mult, op1=ALU.mult),
                          reads=[hm[c][h], rsh, sig[i]], writes=[ya] if h == 0 else [], joins=[] if h == 0 else [ya])
                for h in range(4):
                    first = (i == 0 and h == 0)
                    Sd.op("pe", PE.transpose, dict(out=ptr_ap[:, h, i * P:(i + 1) * P], in_=ya.ap[:, h * P:(h + 1) * P], identity=ident_b.ap),
                          reads=[ya, ident_b], writes=[PS[6], PS[7]] if first else [], joins=[] if first else [PS[6], PS[7]])
            for m in range(4):
                if m < 2:
                    Sd.op("act", ACT.activation, dict(out=hT[m][G].ap, in_=ptr_ap[:, m, :], func=AF.Copy), reads=[PS[6]], writes=[hT[m][G]])
                else:
                    Sd.op("dve", V.tensor_copy, dict(out=hT[m][G].ap, in_=ptr_ap[:, m, :]), reads=[PS[7]], writes=[hT[m][G]])

        if stop_after == "Y":
            Sd.barrier()
            for k in range(KD):
                for g in range(NG):
                    Sd.dma("sp", dict(out=dbg_d[:, k * S + g * 512:k * S + (g + 1) * 512], in_=hT[k][g].ap), reads=[hT[k][g]])
            return finish()

        Sd.barrier()
        A.release(mA)
        w_outp = T(A.alloc([KD, D], BF16, "w_outp"))
        ghP = T(A.alloc([4], F32, "ghP"))
        gfinB = T(A.alloc([D], F32, "gfinB"))
        x1_ap = A.alloc([8, D], F32, "x1")
        x1 = [T(x1_ap[:, i, :]) for i in range(8)]
        aT_ap = A.alloc([8, 2, 512], BF16, "aT")
        aT = [[T(aT_ap[:, fc, tg, :]) for tg in range(2)] for fc in range(8)]
        w1s = [T(A.alloc([KD, 512], BF16, "w1s%d" % i)) for i in range(3)]
        w2s = [T(A.alloc([4, D], BF16, "w2s%d" % i)) for i in range(3)]
        xn2 = [T(A.alloc([D], BF16, "xn2%d" % i)) for i in range(2)]
        rr = [T(A.alloc([512], F32, "rr%d" % i)) for i in range(2)]
        st2 = [T(A.alloc([4], F32, "st2%d" % i)) for i in range(4)]
        junk2 = A.alloc([D], BF16, "junk2")
        stg = [x1[0], x1[1]]
        Sd.dma("sp", dict(out=ghP.ap, in_=g_headP_d[:, :]), writes=[ghP])
        Sd.dma("sp", dict(out=gfinB.ap, in_=g_finR_d.partition_broadcast(P)), writes=[gfinB])
        for k in range(KD):
            sg = stg[k % 2]
            Sd.dma("sp", dict(out=sg.ap, in_=w_out_v[:, k, :]), writes=[sg])
            if k < 4:
                Sd.op("dve", V.scalar_tensor_tensor, dict(out=w_outp.ap[:, k, :], in0=sg.ap, scalar=ghP.ap[:, k:k + 1], in1=gt1B.ap,
                                                          op0=ALU.mult, op1=ALU.mult), reads=[sg, ghP, gt1B], joins=[w_outp])
            else:
                Sd.op("dve", V.tensor_tensor, dict(out=w_outp.ap[:, k, :], in0=sg.ap, in1=gt1B.ap, op=ALU.mult),
                      reads=[sg, gt1B], joins=[w_outp])
        NT4 = 4
        w1_blocks = [(Tt, q, bl) for Tt in range(NT4) for q in range(4) for bl in range(2)]
        gt2b4 = gt2B.ap.unsqueeze(1).to_broadcast([P, 4, D])

        def load_w1(n):
            if n >= len(w1_blocks):
                return
            Tt, q, bl = w1_blocks[n]
            c0 = q * 1024 + bl * 512
            Sd.dma("pool", dict(out=w1s[n % 3].ap, in_=w_ff1_v[:, :, c0:c0 + 512]), writes=[w1s[n % 3]])

        def load_w2(n):
            if n >= len(w1_blocks):
                return
            Tt, q, bl = w1_blocks[n]
            f0 = q * 8 + bl * 4
            t = w2s[n % 3]
            Sd.dma("pool", dict(out=t.ap, in_=w_ff2_v[:, f0:f0 + 4, :]), writes=[t])
            Sd.op("pool", POOL.tensor_tensor, dict(out=t.ap, in0=t.ap, in1=gt2b4, op=ALU.mult), reads=[t, gt2B], writes=[t])

        load_w1(0)
        load_w1(1)
        load_w2(0)
        load_w2(1)
        ps01 = ps_all[:, 0:1024]
        ps67 = ps_all[:, 3072:4096]
        psT2_ap = bank(2, 2).bitcast(BF16).rearrange("p (k t) -> p k t", k=KD)
        blk = 0
        for Tt in range(NT4):
            for st_ in range(8):
                c = Tt * 8 + st_
                g, o = c // 4, (c % 4) * P
                Sd.dma("sp", dict(out=x1[st_].ap, in_=x_v[c]), writes=[x1[st_]])
                for half in range(2):
                    for k in range(KD):
                        Sd.op("pe", PE.matmul, dict(out=bank(half), lhsT=hT[k][g].ap[:, o:o + P], rhs=w_outp.ap[:, k, half * 512:(half + 1) * 512],
                                                    start=(k == 0), stop=(k == KD - 1)),
                              reads=[hT[k][g], w_outp], writes=[PS[half]] if k == 0 else [], joins=[] if k == 0 else [PS[half]])
                Sd.op("dve", V.tensor_tensor, dict(out=x1[st_].ap, in0=ps01, in1=x1[st_].ap, op=ALU.add),
                      reads=[PS[0], PS[1], x1[st_]], writes=[x1[st_]])
            for st_ in range(8):
                c = Tt * 8 + st_
                g = c // 4
                s2 = st2[st_ % 4]
                xb = xn2[st_ % 2]
                Sd.op("act", ACT.activation, dict(out=junk2, in_=x1[st_].ap, func=AF.Square, accum_out=s2.ap[:, 0:1]),
                      reads=[x1[st_]], writes=[s2])
                Sd.op("act", ACT.activation, dict(out=s2.ap[:, 1:2], in_=s2.ap[:, 0:1], func=AF.Ln, scale=1.0 / D, bias=cst.ap[:, 0:1]),
                      reads=[s2, cst], joins=[s2])
                Sd.op("act", ACT.activation, dict(out=s2.ap[:, 2:3], in_=s2.ap[:, 1:2], func=AF.Exp, scale=-0.5), reads=[s2], joins=[s2])
                Sd.op("dve", V.tensor_scalar, dict(out=xb.ap, in0=x1[st_].ap, scalar1=s2.ap[:, 2:3], scalar2=None, op0=ALU.mult),
                      reads=[x1[st_], s2], writes=[xb])
                for k in range(KD):
                    first = (st_ % 2 == 0 and k == 0)
                    Sd.op("pe", PE.transpose, dict(out=psT2_ap[:, k, (st_ % 2) * P:(st_ % 2 + 1) * P], in_=xb.ap[:, k * P:(k + 1) * P],
                                                   identity=ident_b.ap),
                          reads=[xb, ident_b], writes=[PS[2], PS[3]] if first else [], joins=[] if first else [PS[2], PS[3]])
                if st_ % 2 == 1:
                    off = ((c // 2) % 2) * 256
                    for k in range(KD):
                        Sd.op("act", ACT.activation, dict(out=hT[k][g].ap[:, off:off + 256], in_=psT2_ap[:, k, :], func=AF.Identity,
                                                          scale=sc2.ap[:, k:k + 1], bias=modP.ap[:, 24 + k:25 + k]),
                              reads=[PS[2 + k // 4], sc2, modP], joins=[hT[k][g]])
            for q in range(4):
                for bl in range(2):
                    n = blk + bl
                    load_w1(n + 2)
                    wt = w1s[n % 3]
                    for f4 in range(4):
                        fc = bl * 4 + f4
                        for tg in range(2):
                            bb = 4 + (fc * 2 + tg) % 2
                            for k in range(KD):
                                Sd.op("pe", PE.matmul, dict(out=bank(bb), lhsT=wt.ap[:, k, f4 * P:(f4 + 1) * P], rhs=hT[k][Tt * 2 + tg].ap,
                                                            start=(k == 0), stop=(k == KD - 1)),
                                      reads=[wt, hT[k][Tt * 2 + tg]], writes=[PS[bb]] if k == 0 else [], joins=[] if k == 0 else [PS[bb]])
                            r_ = rr[(fc * 2 + tg) % 2]
                            Sd.op("act", ACT.activation, dict(out=r_.ap, in_=bank(bb), func=AF.Relu), reads=[PS[bb]], writes=[r_])
                            Sd.op("dve", V.tensor_tensor, dict(out=aT[fc][tg].ap, in0=r_.ap, in1=r_.ap, op=ALU.mult),
                                  reads=[r_], writes=[aT[fc][tg]])
                load_w2(blk + 2)
                for st_ in range(8):
                    tg, o = st_ // 4, (st_ % 4) * P
                    pb, pt = ((0, 1), ps01) if st_ % 2 == 0 else ((6, 7), ps67)
                    for half in range(2):
                        bb = pb[half]
                        for fc in range(8):
                            wt = w2s[(blk + fc // 4) % 3]
                            Sd.op("pe", PE.matmul, dict(out=bank(bb), lhsT=aT[fc][tg].ap[:, o:o + P], rhs=wt.ap[:, fc % 4, half * 512:(half + 1) * 512],
                                                        start=(fc == 0), stop=(fc == 7)),
                                  reads=[aT[fc][tg], wt], writes=[PS[bb]] if fc == 0 else [], joins=[] if fc == 0 else [PS[bb]])
                    Sd.op("dve", V.tensor_tensor, dict(out=x1[st_].ap, in0=pt, in1=x1[st_].ap, op=ALU.add),
                          reads=[PS[pb[0]], PS[pb[1]], x1[st_]], writes=[x1[st_]])
                load_w2(blk + 3)
                blk += 2
            for st_ in range(8):
                c = Tt * 8 + st_
                s2 = st2[st_ % 4]
                Sd.op("act", ACT.activation, dict(out=junk2, in_=x1[st_].ap, func=AF.Square, accum_out=s2.ap[:, 0:1]),
                      reads=[x1[st_]], writes=[s2])
                Sd.op("act", ACT.activation, dict(out=s2.ap[:, 1:2], in_=s2.ap[:, 0:1], func=AF.Ln, scale=1.0 / D, bias=cst.ap[:, 0:1]),
                      reads=[s2, cst], joins=[s2])
                Sd.op("act", ACT.activation, dict(out=s2.ap[:, 2:3], in_=s2.ap[:, 1:2], func=AF.Exp, scale=-0.5), reads=[s2], joins=[s2])
                Sd.op("dve", V.scalar_tensor_tensor, dict(out=x1[st_].ap, in0=x1[st_].ap, scalar=s2.ap[:, 2:3], in1=gfinB.ap,
                                                          op0=ALU.mult, op1=ALU.mult), reads=[x1[st_], s2, gfinB], writes=[x1[st_]])
                Sd.dma("sp", dict(out=out_v[c], in_=x1[st_].ap), reads=[x1[st_]])
        return finish()


def _consts():
    s = np.arange(P)
    maskF = (s[:, None] <= s[None, :]).astype(np.float32)
    maskB = (s[:, None] >= s[None, :]).astype(np.float32)
    edge = np.ones((4, 16), np.float32)
    for g, w in enumerate(POOL_W):
        for t in range(8):
            lo = max(t - w // 2, 0)
            hi = min(t + w // 2, S)
            edge[g, t] = w / float(hi - lo)
            tt = S - 8 + t
            lo = max(tt - w // 2, 0)
            hi = min(tt + w // 2, S)
            edge[g, 8 + t] = w / float(hi - lo)
    return dict(
        ident_f=np.eye(P, dtype=np.float32),
        ident_b=np.eye(P).astype(ml_dtypes.bfloat16),
        maskF=maskF, maskB=maskB,
        ones_f=np.ones((P, P), np.float32),
        edge_fac=edge.reshape(1, 64),
    )


def make_in_maps(x, c, w_ada, b_ada, g_mix, w_in, b_igate, b_fgate, g_head, w_pool, pool_scale,
                 w_out, g_ffn, w_ff1, w_ff2, g_final):
    f = lambda a: np.ascontiguousarray(np.asarray(a, dtype=np.float32))
    cs = _consts()
    shared = dict(
        w_ada=f(w_ada[0]),
        b_adaP=f(np.asarray(b_ada[0]).reshape(48, P).T),
        b_adaR=f(np.asarray(b_ada[0]).reshape(1, 6 * D)),
        g_mixP=f(np.asarray(g_mix[0]).reshape(KD, P).T),
        g_ffnP=f(np.asarray(g_ffn[0]).reshape(KD, P).T),
        g_finR=f(np.asarray(g_final).reshape(1, D)),
        w_in=f(w_in[0]),
        b_ig=f(np.asarray(b_igate[0]).reshape(1, 8)),
        b_fg=f(np.asarray(b_fgate[0]).reshape(1, 8)),
        g_headP=f(np.asarray(g_head[0]).reshape(4, P).T),
        pool_scaleP=f(np.asarray(pool_scale[0]).reshape(4, P).T),
        w_pool=f(np.asarray(w_pool[0]).transpose(1, 0, 2)),
        w_out=f(w_out[0]), w_ff1=f(w_ff1[0]), w_ff2=f(w_ff2[0]),
        **cs,
    )
    maps = []
    for b in range(N_CORES):
        m = dict(shared)
        m["x"] = f(x[b])
        m["cT"] = f(np.asarray(c[b]).reshape(KD, P).T)
        maps.append(m)
    return maps


_CACHE = {}


def kernel(**inputs):
    if "nc" not in _CACHE:
        _CACHE["nc"] = build_program()[0]
    nc = _CACHE["nc"]
    in_maps = make_in_maps(**inputs)
    res = run_bass_kernel_spmd(nc, in_maps, core_ids=list(range(N_CORES)))
    out = np.stack([np.asarray(r["out"]) for r in res.results], axis=0)
    return out.astype(np.float32)
```

```python
import numpy as np
import ml_dtypes
from contextlib import ExitStack

import concourse.bass as bass
import concourse.mybir as mybir
from concourse.bass_utils import run_bass_kernel_spmd

F32 = mybir.dt.float32
BF16 = mybir.dt.bfloat16
AF = mybir.ActivationFunctionType
ALU = mybir.AluOpType
AX = mybir.AxisListType

P = 128
S = 4096
D = 1024
KD = 8
NCH = 32
NG = 8
DFF = 4096
EPS = 1e-6
POOL_W = (2, 4, 8, 16)
N_CORES = 8


class T:
    __slots__ = ("ap", "w", "r", "dsem", "dcount", "name", "excl")

    def __init__(self, ap, name="", excl=False):
        self.ap = ap
        self.excl = excl
        self.w = []
        self.r = []
        self.dsem = None
        self.dcount = 0
        self.name = name

    def __getitem__(self, k):
        return self.ap[k]


class _Op:
    __slots__ = ("eng", "idx", "method", "kwargs", "waits", "marked", "dma_inc", "rank")


class Sched:
    ENG = ("pe", "act", "dve", "pool", "sp")

    def __init__(self, nc, es):
        self.nc = nc
        self.es = es
        self.h = {"pe": nc.tensor, "act": nc.scalar, "dve": nc.vector, "pool": nc.gpsimd, "sp": nc.sync}
        self.sem = {e: es.enter_context(nc.semaphore("s_" + e)) for e in ("pe", "act", "dve", "pool")}
        self.ops = []
        self.eng_ops = {e: [] for e in self.ENG}
        self.waited_c = {e: {x: -1 for x in self.ENG} for e in self.ENG}
        self.waited_d = {e: {} for e in self.ENG}
        self.dsems = []
        self.nsem = 0

    def _collect(self, eng, reads, writes, joins):
        toks = []
        for t in reads:
            toks.extend(t.w)
            if t.excl:
                toks.extend(x for x in t.r if x[0] == "c" and x[1].eng != eng)
        for t in writes:
            toks.extend(t.w)
            toks.extend(t.r)
        for t in joins:
            toks.extend(t.r)
        waits = []
        wc = self.waited_c[eng]
        wd = self.waited_d[eng]
        for tk in toks:
            if tk[0] == "c":
                d = tk[1]
                if d.eng == eng and eng == "pe":
                    continue
                if wc[d.eng] >= d.idx:
                    continue
                wc[d.eng] = d.idx
                d.marked = True
                waits.append(tk)
            else:
                _, sem, val = tk
                k = id(sem)
                if wd.get(k, 0) >= val:
                    continue
                wd[k] = val
                waits.append(tk)
        return waits

    def _post(self, tok, reads, writes, joins):
        for t in writes:
            t.w = [tok]
            t.r = []
        def same(x):
            if x[0] != tok[0]:
                return False
            if tok[0] == "c":
                return x[1].eng == tok[1].eng
            return x[1] is tok[1]
        for t in joins:
            t.w = [x for x in t.w if not same(x)] + [tok]
        for t in reads:
            t.r = [x for x in t.r if not same(x)] + [tok]

    def op(self, eng, method, kwargs, reads=(), writes=(), joins=()):
        o = _Op()
        o.eng = eng
        o.idx = len(self.eng_ops[eng])
        o.method = method
        o.kwargs = kwargs
        o.marked = False
        o.dma_inc = None
        o.waits = self._collect(eng, reads, writes, joins)
        self.eng_ops[eng].append(o)
        self.ops.append(o)
        self._post(("c", o), reads, writes, joins)
        return o

    def _dsem(self, t):
        if t.dsem is None:
            t.dsem = self.es.enter_context(self.nc.semaphore("d%d" % self.nsem))
            self.nsem += 1
            self.dsems.append(t)
        return t.dsem

    def dma(self, q, kwargs, reads=(), writes=(), joins=()):
        o = _Op()
        o.eng = q
        o.idx = -1
        o.method = self.h[q].dma_start
        o.kwargs = kwargs
        o.marked = False
        o.waits = self._collect(q, reads, writes, joins)
        owner = (list(writes) + list(joins) + list(reads))[0]
        sem = self._dsem(owner)
        owner.dcount += 16
        o.dma_inc = (sem, owner.dcount)
        self.ops.append(o)
        self._post(("d", sem, owner.dcount), reads, writes, joins)
        return o

    def barrier(self):
        for e in self.ENG:
            o = _Op()
            o.eng = e
            o.idx = -1
            o.method = None
            o.kwargs = None
            o.marked = False
            o.dma_inc = None
            waits = []
            for x in ("pe", "act", "dve", "pool"):
                if self.eng_ops[x]:
                    d = self.eng_ops[x][-1]
                    if x == e or self.waited_c[e][x] >= d.idx:
                        continue
                    self.waited_c[e][x] = d.idx
                    d.marked = True
                    waits.append(("c", d))
            for t in self.dsems:
                k = id(t.dsem)
                if self.waited_d[e].get(k, 0) < t.dcount:
                    self.waited_d[e][k] = t.dcount
                    waits.append(("d", t.dsem, t.dcount))
            o.waits = waits
            self.ops.append(o)

    def emit(self):
        for e in ("pe", "act", "dve", "pool"):
            r = 0
            for o in self.eng_ops[e]:
                if o.marked:
                    r += 1
                o.rank = r
        nw = 0
        for o in self.ops:
            h = self.h[o.eng]
            for tk in o.waits:
                if tk[0] == "c":
                    h.wait_ge(self.sem[tk[1].eng], tk[1].rank)
                else:
                    h.wait_ge(tk[1], tk[2])
                nw += 1
            if o.method is None:
                continue
            ins = o.method(**o.kwargs)
            if o.dma_inc is not None:
                ins.then_inc(o.dma_inc[0], 16)
            elif o.marked:
                ins.then_inc(self.sem[o.eng], 1)
        self.stats = {e: len(self.eng_ops[e]) for e in self.ENG}
        self.stats["waits"] = nw
        self.stats["ops"] = len(self.ops)


class Arena:
    def __init__(self, base_ap_f32, nbytes):
        self.base = base_ap_f32
        self.n = nbytes
        self.off = 0
        self.peak = 0

    def mark(self):
        return self.off

    def release(self, m):
        self.off = m

    def alloc(self, free_shape, dtype, name=""):
        esz = 2 if dtype == BF16 else 4
        n = int(np.prod(free_shape))
        nb = (n * esz + 63) // 64 * 64
        assert self.off + nb <= self.n, "SBUF arena overflow at %s: %d + %d > %d" % (name, self.off, nb, self.n)
        ap = self.base[:, self.off // 4:(self.off + nb) // 4]
        if dtype == BF16:
            ap = ap.bitcast(BF16)
        ap = ap[:, 0:n]
        if len(free_shape) == 2:
            ap = ap.rearrange("p (a b) -> p a b", a=free_shape[0])
        elif len(free_shape) == 3:
            ap = ap.rearrange("p (a b c) -> p a b c", a=free_shape[0], b=free_shape[1])
        self.off += nb
        self.peak = max(self.peak, self.off)
        return ap


def build_program(stop_after=None, dbg=None, dbg_dt=None):
    nc = bass.Bass("TRN2", target_bir_lowering=False)

    def din(name, shape, dt=F32):
        return nc.dram_tensor(name, list(shape), dt, kind="ExternalInput").ap()

    x_d = din("x", [S, D])
    cT_d = din("cT", [P, KD])
    w_ada_d = din("w_ada", [D, 6 * D])
    b_adaP_d = din("b_adaP", [P, 48])
    b_adaR_d = din("b_adaR", [1, 6 * D])
    g_mixP_d = din("g_mixP", [P, KD])
    g_ffnP_d = din("g_ffnP", [P, KD])
    g_finR_d = din("g_finR", [1, D])
    w_in_d = din("w_in", [D, 2576])
    b_ig_d = din("b_ig", [1, 8])
    b_fg_d = din("b_fg", [1, 8])
    g_headP_d = din("g_headP", [P, 4])
    psclP_d = din("pool_scaleP", [P, 4])
    w_pool_d = din("w_pool", [P, 4, P])
    w_out_d = din("w_out", [D, D])
    w_ff1_d = din("w_ff1", [D, DFF])
    w_ff2_d = din("w_ff2", [DFF, D])
    ident_f_d = din("ident_f", [P, P])
    ident_b_d = din("ident_b", [P, P], BF16)
    maskF_d = din("maskF", [P, P])
    maskB_d = din("maskB", [P, P])
    ones_d = din("ones_f", [P, P])
    edge_d = din("edge_fac", [1, 64])
    out_d = nc.dram_tensor("out", [S, D], F32, kind="ExternalOutput").ap()
    dbg_d = None
    if dbg is not None:
        dbg_d = nc.dram_tensor("dbg", list(dbg), dbg_dt or F32, kind="ExternalOutput").ap()

    w_ada_v = w_ada_d.rearrange("(k p) n -> p k n", p=P)
    w_in_v = w_in_d.rearrange("(k p) n -> p k n", p=P)
    w_out_v = w_out_d.rearrange("(k p) n -> p k n", p=P)
    w_ff1_v = w_ff1_d.rearrange("(k p) n -> p k n", p=P)
    w_ff2_v = w_ff2_d.rearrange("(k p) n -> p k n", p=P)
    x_v = x_d.rearrange("(c p) d -> c p d", p=P)
    out_v = out_d.rearrange("(c p) d -> c p d", p=P)

    SB_BYTES = 207 * 1024
    es = ExitStack()
    with es:
        sb_all = es.enter_context(nc.sbuf_tensor("sb_all", [P, SB_BYTES // 4], F32))
        ps_all = es.enter_context(nc.psum_tensor("ps_all", [P, 4096], F32))
        es.enter_context(nc.Block())
        Sd = Sched(nc, es)
        A = Arena(sb_all[:], SB_BYTES)
        V, ACT, PE, POOL = nc.vector, nc.scalar, nc.tensor, nc.gpsimd

        def bank(b, n=1):
            return ps_all[:, b * 512:(b + n) * 512]

        PS = [T(bank(b), "bank%d" % b, excl=True) for b in range(8)]

        hT_ap = A.alloc([KD, S], BF16, "hT")
        hT = [[T(hT_ap[:, k, g * 512:(g + 1) * 512], "hT%d_%d" % (k, g)) for g in range(NG)] for k in range(KD)]
        ident_b = T(A.alloc([P], BF16, "ident_b"))
        ident_f = T(A.alloc([P], F32, "ident_f"))
        maskF = T(A.alloc([P], F32, "maskF"))
        maskB = T(A.alloc([P], F32, "maskB"))
        ones_f = T(A.alloc([P], F32, "ones"))
        cst = T(A.alloc([64], F32, "cst_misc"))
        modP = T(A.alloc([48], F32, "modP"))
        sc1 = T(A.alloc([KD], F32, "sc1"))
        sc2 = T(A.alloc([KD], F32, "sc2"))
        gt1B = T(A.alloc([D], F32, "gt1B"))
        gt2B = T(A.alloc([D], F32, "gt2B"))
        WGT = T(A.alloc([8, NCH], F32, "WGT"))
        STAB = T(A.alloc([8, NCH], F32, "STAB"))
        DEC = T(A.alloc([8, NCH], F32, "DEC"))

        Sd.op("dve", V.memset, dict(ap=cst.ap[:, 0:1], constant=EPS), writes=[cst])
        for t, d in ((ident_b, ident_b_d), (ident_f, ident_f_d), (maskF, maskF_d), (maskB, maskB_d), (ones_f, ones_d)):
            Sd.dma("sp", dict(out=t.ap, in_=d[:, :]), writes=[t])

        def finish():
            Sd.barrier()
            Sd.emit()
            return nc, Sd, A


        mA = A.mark()
        cT = T(A.alloc([KD], F32, "cT"))
        c_act = T(A.alloc([KD], F32, "c_act"))
        c_rep = T(A.alloc([KD, P], F32, "c_rep"))
        b_adaP = T(A.alloc([48], F32, "b_adaP"))
        g_mixP = T(A.alloc([KD], F32, "g_mixP"))
        g_ffnP = T(A.alloc([KD], F32, "g_ffnP"))
        wA = [T(A.alloc([KD, 512], F32, "wA%d" % i)) for i in range(2)]
        psM = PS[0]
        psBc = PS[1]

        Sd.dma("sp", dict(out=cT.ap, in_=cT_d[:, :]), writes=[cT])
        Sd.dma("sp", dict(out=b_adaP.ap, in_=b_adaP_d[:, :]), writes=[b_adaP])
        Sd.dma("sp", dict(out=g_mixP.ap, in_=g_mixP_d[:, :]), writes=[g_mixP])
        Sd.dma("sp", dict(out=g_ffnP.ap, in_=g_ffnP_d[:, :]), writes=[g_ffnP])
        Sd.dma("sp", dict(out=gt1B.ap, in_=b_adaR_d[:, 2 * D:3 * D].partition_broadcast(P)), writes=[gt1B])
        Sd.dma("sp", dict(out=gt2B.ap, in_=b_adaR_d[:, 5 * D:6 * D].partition_broadcast(P)), writes=[gt2B])
        Sd.op("act", ACT.activation, dict(out=c_act.ap, in_=cT.ap, func=AF.Silu), reads=[cT], writes=[c_act])
        Sd.op("dve", V.tensor_copy, dict(out=c_rep.ap, in_=c_act.ap.unsqueeze(2).to_broadcast([P, KD, P])),
              reads=[c_act], writes=[c_rep])

        piece_order = [0, 1, 2, 3, 6, 7, 8, 9, 4, 5, 10, 11]

        def ada_piece(n, pc):
            buf = wA[n % 2]
            Sd.dma("pool", dict(out=buf.ap, in_=w_ada_v[:, :, pc * 512:(pc + 1) * 512]), writes=[buf])
            if pc in (4, 5, 10, 11):
                gt = gt1B if pc in (4, 5) else gt2B
                half = pc % 2
                for k in range(KD):
                    Sd.op("pe", PE.matmul, dict(out=psBc.ap, lhsT=c_rep.ap[:, k, :], rhs=buf.ap[:, k, :],
                                                start=(k == 0), stop=(k == KD - 1)),
                          reads=[c_rep, buf], writes=[psBc] if k == 0 else [], joins=[] if k == 0 else [psBc])
                sl = gt.ap[:, half * 512:(half + 1) * 512]
                Sd.op("dve", V.tensor_tensor, dict(out=sl, in0=psBc.ap, in1=sl, op=ALU.add),
                      reads=[psBc, gt], joins=[gt])
            else:
                for j in range(4):
                    blk = pc * 4 + j
                    for k in range(KD):
                        Sd.op("pe", PE.matmul, dict(out=psM.ap[:, blk:blk + 1], lhsT=buf.ap[:, k, j * P:(j + 1) * P],
                                                    rhs=c_act.ap[:, k:k + 1], start=(k == 0), stop=(k == KD - 1)),
                              reads=[c_act, buf], joins=[psM])
                if pc in (3, 9):
                    lo = 0 if pc == 3 else 24
                    Sd.op("dve", V.tensor_tensor, dict(out=modP.ap[:, lo:lo + 16], in0=psM.ap[:, lo:lo + 16],
                                                       in1=b_adaP.ap[:, lo:lo + 16], op=ALU.add),
                          reads=[psM, b_adaP], joins=[modP])
                    sc = sc1 if pc == 3 else sc2
                    gP = g_mixP if pc == 3 else g_ffnP
                    Sd.op("dve", V.scalar_tensor_tensor, dict(out=sc.ap, in0=modP.ap[:, lo + 8:lo + 16], scalar=1.0,
                                                              in1=gP.ap, op0=ALU.add, op1=ALU.mult),
                          reads=[modP, gP], writes=[sc])
        for n in range(4):
            ada_piece(n, piece_order[n])
        if stop_after == "0":
            Sd.barrier()
            Sd.dma("sp", dict(out=dbg_d[:, 0:48], in_=modP.ap), reads=[modP])
            Sd.dma("sp", dict(out=dbg_d[:, 48:56], in_=sc1.ap), reads=[sc1])
            Sd.dma("sp", dict(out=dbg_d[:, 56:64], in_=sc2.ap), reads=[sc2])
            Sd.dma("sp", dict(out=dbg_d[:, 64:64 + D], in_=gt1B.ap), reads=[gt1B])
            Sd.dma("sp", dict(out=dbg_d[:, 64 + D:64 + 2 * D], in_=gt2B.ap), reads=[gt2B])
            return finish()

        xin = [T(A.alloc([D], F32, "xin%d" % i)) for i in range(4)]
        xn = [T(A.alloc([D], BF16, "xn%d" % i)) for i in range(2)]
        junk = A.alloc([D], BF16, "junk")
        ssq = [T(A.alloc([4], F32, "ssq%d" % i)) for i in range(4)]
        psT_ap = [bank(2 + 2 * i, 2).bitcast(BF16).rearrange("p (k t) -> p k t", k=KD) for i in range(2)]
        psT_t = [[PS[2], PS[3]], [PS[4], PS[5]]]

        import os as _os
        _nch = int(_os.environ.get('K_NCH', NCH)); _var = _os.environ.get('K_VAR', '')
        for c in range(_nch):
            xt = xin[c % 4]
            sq = ssq[c % 4]
            xb = xn[c % 2]
            pT = psT_ap[(c // 2) % 2]
            pTt = psT_t[(c // 2) % 2]
            Sd.dma("sp", dict(out=xt.ap, in_=x_v[c]), writes=[xt])
            Sd.op("act", ACT.activation, dict(out=junk, in_=xt.ap, func=AF.Square, accum_out=sq.ap[:, 0:1]),
                  reads=[xt], writes=[sq])
            Sd.op("act", ACT.activation, dict(out=sq.ap[:, 1:2], in_=sq.ap[:, 0:1], func=AF.Ln, scale=1.0 / D,
                                              bias=cst.ap[:, 0:1]), reads=[sq, cst], joins=[sq])
            Sd.op("act", ACT.activation, dict(out=sq.ap[:, 2:3], in_=sq.ap[:, 1:2], func=AF.Exp, scale=-0.5),
                  reads=[sq], joins=[sq])
            Sd.op("dve", V.tensor_scalar, dict(out=xb.ap, in0=xt.ap, scalar1=sq.ap[:, 2:3], scalar2=None, op0=ALU.mult),
                  reads=[xt, sq], writes=[xb])
            for k in range(KD):
                first = (c % 2 == 0 and k == 0)
                Sd.op("pe", PE.transpose, dict(out=pT[:, k, (c % 2) * P:(c % 2 + 1) * P], in_=xb.ap[:, k * P:(k + 1) * P],
                                               identity=ident_b.ap),
                      reads=[xb, ident_b], writes=pTt if first else [], joins=[] if first else pTt)
            if c % 2 == 1:
                g = c // 4
                off = ((c // 2) % 2) * 256
                for k in range(KD):
                    dst = hT[k][g].ap[:, off:off + 256]
                    Sd.op("act", ACT.activation, dict(out=dst, in_=pT[:, k, :], func=AF.Identity,
                                                      scale=sc1.ap[:, k:k + 1], bias=modP.ap[:, k:k + 1]),
                          reads=[pTt[k // 4], sc1, modP], joins=[hT[k][g]])

        if stop_after == "A2":
            Sd.barrier()
            Sd.dma("sp", dict(out=dbg_d[:, 0:8], in_=sc1.ap), reads=[sc1])
            return finish()
        if stop_after == "A":
            Sd.barrier()
            for k in range(KD):
                for g in range(NG):
                    Sd.dma("sp", dict(out=dbg_d[:, k * S + g * 512:k * S + (g + 1) * 512], in_=hT[k][g].ap), reads=[hT[k][g]])
            return finish()

        w_g = T(A.alloc([KD, 16], BF16, "w_g"))
        Sd.dma("pool", dict(out=w_g.ap, in_=w_in_v[:, :, 2048:2064]), writes=[w_g])
        bI = T(A.alloc([8], F32, "bI"))
        bF = T(A.alloc([8], F32, "bF"))
        Sd.dma("sp", dict(out=bI.ap, in_=b_ig_d.partition_broadcast(P)), writes=[bI])
        Sd.dma("sp", dict(out=bF.ap, in_=b_fg_d.partition_broadcast(P)), writes=[bF])
        Sd.op("dve", V.memset, dict(ap=cst.ap[:, 1:2], constant=1.0), joins=[cst])
        psG = PS[6]
        psG_ap = bank(6).rearrange("p (d j g) -> p d j g", d=2, j=NCH)
        for c in range(NCH):
            for dr in range(2):
                j = c if dr == 0 else NCH - 1 - c
                for k in range(KD):
                    Sd.op("pe", PE.matmul, dict(out=psG_ap[:, dr, j, :], lhsT=hT[k][c // 4].ap[:, (c % 4) * P:(c % 4 + 1) * P],
                                                rhs=w_g.ap[:, k, dr * 8:(dr + 1) * 8], start=(k == 0), stop=(k == KD - 1)),
                          reads=[hT[k][c // 4], w_g], joins=[psG])

        for n in range(4, 12):
            ada_piece(n, piece_order[n])

        def tab(name, n=NCH):
            return T(A.alloc([8, n], F32, name))

        def v4(ap):
            return ap.rearrange("p (d h) j -> p d h j", d=2)

        def fl(ap):
            return ap.rearrange("p a j -> p (a j)")

        LI, ZZ, CS, GP, GB, BT, RR, TMP = (tab(n) for n in ("LI", "ZZ", "CS", "GP", "GB", "BT", "RR", "TMP"))
        MN = tab("MN", NCH + 1)
        Gcol = T(A.alloc([2], F32, "Gcol"))
        Grow = T(A.alloc([256], F32, "Grow"))
        g_i = psG_ap[:, :, :, 0:4].rearrange("p d j h -> p d h j")
        g_f = psG_ap[:, :, :, 4:8].rearrange("p d j h -> p d h j")
        bIb = bI.ap.rearrange("p (d h) -> p d h", d=2).unsqueeze(3).to_broadcast([P, 2, 4, NCH])
        bFb = bF.ap.rearrange("p (d h) -> p d h", d=2).unsqueeze(3).to_broadcast([P, 2, 4, NCH])
        Sd.op("dve", V.tensor_tensor, dict(out=v4(LI.ap), in0=g_i, in1=bIb, op=ALU.add), reads=[psG, bI], writes=[LI])
        Sd.op("dve", V.tensor_tensor, dict(out=v4(ZZ.ap), in0=g_f, in1=bFb, op=ALU.add), reads=[psG, bF], writes=[ZZ])
        Sd.op("act", ACT.activation, dict(out=fl(ZZ.ap), in_=fl(ZZ.ap), func=AF.Exp, scale=-1.0), reads=[ZZ], writes=[ZZ])
        Sd.op("act", ACT.activation, dict(out=fl(ZZ.ap), in_=fl(ZZ.ap), func=AF.Ln, bias=cst.ap[:, 1:2]), reads=[ZZ, cst], writes=[ZZ])
        if stop_after == "G1":
            Sd.barrier()
            for i, t in enumerate((LI, ZZ)):
                Sd.dma("sp", dict(out=dbg_d[:, i * 256:(i + 1) * 256].rearrange("p (a j) -> p a j", a=8), in_=t.ap[:, :, 0:NCH]), reads=[t])
            return finish()
        psC, psC_ap = PS[7], bank(7)[:, 0:256]
        psBt, psBt_ap = PS[4], bank(4)[:, 0:256]
        Sd.op("pe", PE.matmul, dict(out=psC_ap[:, 0:128], lhsT=maskF.ap, rhs=fl(ZZ.ap)[:, 0:128], start=True, stop=True),
              reads=[maskF, ZZ], writes=[psC])
        Sd.op("pe", PE.matmul, dict(out=psC_ap[:, 128:256], lhsT=maskB.ap, rhs=fl(ZZ.ap)[:, 128:256], start=True, stop=True),
              reads=[maskB, ZZ], joins=[psC])
        Sd.op("pe", PE.matmul, dict(out=psBt_ap, lhsT=ones_f.ap, rhs=fl(ZZ.ap), start=True, stop=True),
              reads=[ones_f, ZZ], writes=[psBt])
        Sd.op("act", ACT.activation, dict(out=fl(CS.ap), in_=psC_ap, func=AF.Copy), reads=[psC], writes=[CS])
        if stop_after == "G2a":
            Sd.barrier()
            Sd.dma("sp", dict(out=dbg_d[:, 0:256].rearrange("p (a j) -> p a j", a=8), in_=CS.ap), reads=[CS])
            return finish()
        Sd.op("dve", V.tensor_tensor, dict(out=fl(GP.ap), in0=psC_ap, in1=fl(LI.ap), op=ALU.add), reads=[psC, LI], writes=[GP])
        Sd.op("act", ACT.activation, dict(out=fl(BT.ap), in_=psBt_ap, func=AF.Copy, scale=-1.0), reads=[psBt], writes=[BT])
        if stop_after == "G2":
            Sd.barrier()
            for i, t in enumerate((CS, GP, BT)):
                Sd.dma("sp", dict(out=dbg_d[:, i * 256:(i + 1) * 256].rearrange("p (a j) -> p a j", a=8), in_=t.ap[:, :, 0:NCH]), reads=[t])
            return finish()
        psX, psX_ap = PS[5], bank(5)[:, 0:256]
        psR, psR_ap = PS[2], bank(2)[0:1, 0:256]
        psGB, psGB_ap = PS[3], bank(3)[:, 0:256]
        for hf in range(2):
            Sd.op("pe", PE.transpose, dict(out=psX_ap[:, hf * P:(hf + 1) * P], in_=fl(GP.ap)[:, hf * P:(hf + 1) * P], identity=ident_f.ap),
                  reads=[GP, ident_f], writes=[psX] if hf == 0 else [], joins=[] if hf == 0 else [psX])
        Sd.op("dve", V.tensor_reduce, dict(out=Gcol.ap, in_=psX_ap.rearrange("p (a s) -> p a s", a=2), axis=AX.X, op=ALU.max),
              reads=[psX], writes=[Gcol])
        for hf in range(2):
            Sd.op("pe", PE.transpose, dict(out=psR_ap[:, hf * P:(hf + 1) * P], in_=Gcol.ap[:, hf:hf + 1], identity=ident_f.ap),
                  reads=[Gcol, ident_f], writes=[psR] if hf == 0 else [], joins=[] if hf == 0 else [psR])
        Sd.op("act", ACT.activation, dict(out=Grow.ap[0:1, :], in_=psR_ap, func=AF.Copy), reads=[psR], writes=[Grow])
        Sd.op("pe", PE.matmul, dict(out=psGB_ap, lhsT=ones_f.ap[0:1, :], rhs=Grow.ap[0:1, :], start=True, stop=True),
              reads=[ones_f, Grow], writes=[psGB])
        Sd.op("act", ACT.activation, dict(out=fl(GB.ap), in_=psGB_ap, func=AF.Copy), reads=[psGB], writes=[GB])
        if stop_after == "G3":
            Sd.barrier()
            for i, t in enumerate((GB, GP)):
                Sd.dma("sp", dict(out=dbg_d[:, i * 256:(i + 1) * 256].rearrange("p (a j) -> p a j", a=8), in_=t.ap[:, :, 0:NCH]), reads=[t])
            return finish()
        Sd.op("dve", V.memset, dict(ap=MN.ap[:, :, 0:1], constant=0.0), writes=[MN])
        for dh in range(8):
            Sd.op("dve", V.tensor_tensor_scan, dict(out=MN.ap[:, dh, 1:NCH + 1], data0=GB.ap[:, dh, :], data1=BT.ap[:, dh, :],
                                                    initial=0.0, op0=ALU.max, op1=ALU.add), reads=[GB, BT], joins=[MN])
        Sd.op("dve", V.tensor_tensor, dict(out=RR.ap, in0=MN.ap[:, :, 0:NCH], in1=GB.ap, op=ALU.max), reads=[MN, GB], writes=[RR])
        for src, dst in ((MN, DEC), (GP, WGT), (CS, STAB)):
            sap = src.ap[:, :, 0:NCH]
            Sd.op("dve", V.tensor_tensor, dict(out=TMP.ap, in0=sap, in1=RR.ap, op=ALU.subtract), reads=[src, RR], writes=[TMP])
            Sd.op("act", ACT.activation, dict(out=fl(dst.ap), in_=fl(TMP.ap), func=AF.Exp), reads=[TMP], writes=[dst])

        if stop_after == "G":
            Sd.barrier()
            for i, t in enumerate((WGT, STAB, DEC, MN)):
                Sd.dma("sp", dict(out=dbg_d[:, i * 256:(i + 1) * 256].rearrange("p (a j) -> p a j", a=8), in_=t.ap[:, :, 0:NCH]), reads=[t])
            return finish()

        Sd.barrier()
        A.release(mA)
        hm_ap = A.alloc([NCH, 512], F32, "hm")
        hm = [[T(hm_ap[:, c, h * P:(h + 1) * P]) for h in range(4)] for c in range(NCH)]
        mH = A.mark()
        wqkv = T(A.alloc([KD, 3, P], BF16, "wqkv"))
        qT_ap = A.alloc([S], BF16, "qT")
        kT_ap = A.alloc([S], BF16, "kT")
        qT = [T(qT_ap[:, g * 512:(g + 1) * 512]) for g in range(NG)]
        kT = [T(kT_ap[:, g * 512:(g + 1) * 512]) for g in range(NG)]
        va_ap = A.alloc([NCH, 130], BF16, "va")
        va = [T(va_ap[:, c, :]) for c in range(NCH)]
        kwF_ap = A.alloc([NCH, P], BF16, "kwF")
        kwB_ap = A.alloc([NCH, P], BF16, "kwB")
        kwF = [T(kwF_ap[:, c, :]) for c in range(NCH)]
        kwB = [T(kwB_ap[:, c, :]) for c in range(NCH)]
        sTw = [T(A.alloc([P], BF16, "sTw%d" % i)) for i in range(4)]
        Cst = [T(A.alloc([130], F32, "Cst%d" % i)) for i in range(2)]
        Csb = [[T(A.alloc([130], BF16, "Csb%d%d" % (i, n))) for n in range(3)] for i in range(2)]
        dnr = [T(A.alloc([2], F32, "dnr%d" % i)) for i in range(4)]
        Sd.op("dve", V.memset, dict(ap=va_ap[:, :, 128:130], constant=1.0), writes=va)
        QSCALE = float(P) ** -0.5
        _nheads = int(_os.environ.get('K_NH', 4))

        for h in range(_nheads):
            for i, off in enumerate((0, 512, 1024)):
                Sd.dma("pool", dict(out=wqkv.ap[:, :, i, :], in_=w_in_v[:, :, off + h * P:off + (h + 1) * P]),
                       writes=[wqkv] if i == 0 else [], joins=[] if i == 0 else [wqkv])
            for g in range(NG):
                for which, dstT, bb in ((0, qT, g % 2), (1, kT, 2 + g % 2)):
                    for k in range(KD):
                        Sd.op("pe", PE.matmul, dict(out=bank(bb), lhsT=wqkv.ap[:, k, which, :], rhs=hT[k][g].ap,
                                                    start=(k == 0), stop=(k == KD - 1)),
                              reads=[wqkv, hT[k][g]], writes=[PS[bb]] if k == 0 else [], joins=[] if k == 0 else [PS[bb]])
                    if which == 0:
                        Sd.op("act", ACT.activation, dict(out=dstT[g].ap, in_=bank(bb), func=AF.Copy, scale=QSCALE),
                              reads=[PS[bb]], writes=[dstT[g]])
                    else:
                        Sd.op("dve", V.tensor_copy, dict(out=dstT[g].ap, in_=bank(bb)), reads=[PS[bb]], writes=[dstT[g]])
                for ci in range(4):
                    c = g * 4 + ci
                    bb = 4 + ci
                    for k in range(KD):
                        Sd.op("pe", PE.matmul, dict(out=bank(bb)[:, 0:256], lhsT=hT[k][g].ap[:, ci * P:(ci + 1) * P],
                                                    rhs=wqkv.ap[:, k, 1:3, :].rearrange("p a b -> p (a b)"),
                                                    start=(k == 0), stop=(k == KD - 1)),
                              reads=[wqkv, hT[k][g]], writes=[PS[bb]] if k == 0 else [], joins=[] if k == 0 else [PS[bb]])
                    Sd.op("act", ACT.activation, dict(out=va[c].ap[:, 0:P], in_=bank(bb)[:, P:2 * P], func=AF.Copy),
                          reads=[PS[bb]], joins=[va[c]])
                    Sd.op("dve", V.tensor_scalar, dict(out=kwF[c].ap, in0=bank(bb)[:, 0:P], scalar1=WGT.ap[:, h, c:c + 1],
                                                       scalar2=None, op0=ALU.mult), reads=[PS[bb], WGT], writes=[kwF[c]])
                    jb = NCH - 1 - c
                    Sd.op("dve", V.tensor_scalar, dict(out=kwB[c].ap, in0=bank(bb)[:, 0:P], scalar1=WGT.ap[:, 4 + h, jb:jb + 1],
                                                       scalar2=None, op0=ALU.mult), reads=[PS[bb], WGT], writes=[kwB[c]])
            NI = 2 * NCH
            ND = (2, 3, 6, 7)

            def item(i):
                j, dr = divmod(i, 2)
                c = j if dr == 0 else NCH - 1 - j
                return j, dr, c, dr * 4 + h, c // 4, (c % 4) * P

            def S1(i):
                j, dr, c, dh, g, o = item(i)
                st = sTw[i % 4]
                Sd.op("pe", PE.matmul, dict(out=bank(dr)[:, 0:P], lhsT=kT[g].ap[:, o:o + P], rhs=qT[g].ap[:, o:o + P],
                                            start=True, stop=True), reads=[kT[g], qT[g]], writes=[PS[dr]])
                Sd.op("dve", V.scalar_tensor_tensor, dict(out=st.ap, in0=bank(dr)[:, 0:P], scalar=WGT.ap[:, dh, j:j + 1],
                                                          in1=(maskF if dr == 0 else maskB).ap, op0=ALU.mult, op1=ALU.mult),
                      reads=[PS[dr], WGT, maskF if dr == 0 else maskB], writes=[st])

            def S2(i):
                j, dr, c, dh, g, o = item(i)
                if j >= NCH - 1:
                    return
                kw = kwF if dr == 0 else kwB
                Sd.op("pe", PE.matmul, dict(out=bank(4 + dr)[:, 0:129], lhsT=kw[c].ap, rhs=va[c].ap[:, 0:129],
                                            start=True, stop=True), reads=[kw[c], va[c]], writes=[PS[4 + dr]])
                if j == 0:
                    Sd.op("dve", V.tensor_copy, dict(out=Cst[dr].ap[:, 0:129], in_=bank(4 + dr)[:, 0:129]),
                          reads=[PS[4 + dr]], writes=[Cst[dr]])
                else:
                    Sd.op("dve", V.scalar_tensor_tensor, dict(out=Cst[dr].ap[:, 0:129], in0=Cst[dr].ap[:, 0:129],
                                                              scalar=DEC.ap[:, dh, j:j + 1], in1=bank(4 + dr)[:, 0:129],
                                                              op0=ALU.mult, op1=ALU.add),
                          reads=[Cst[dr], DEC, PS[4 + dr]], writes=[Cst[dr]])
                Sd.op("act", ACT.activation, dict(out=Csb[dr][(j + 1) % 3].ap[:, 0:129], in_=Cst[dr].ap[:, 0:129],
                                                  func=AF.Copy, scale=DEC.ap[:, dh, j + 1:j + 2]),
                      reads=[Cst[dr], DEC], writes=[Csb[dr][(j + 1) % 3]])

            def S3(i):
                j, dr, c, dh, g, o = item(i)
                bb = ND[i % 4]
                st = sTw[i % 4]
                Sd.op("pe", PE.matmul, dict(out=bank(bb)[:, 0:129], lhsT=st.ap, rhs=va[c].ap[:, 0:129],
                                            start=True, stop=(j == 0)), reads=[st, va[c]], writes=[PS[bb]])
                if j > 0:
                    Sd.op("pe", PE.matmul, dict(out=bank(bb)[:, 0:129], lhsT=qT[g].ap[:, o:o + P],
                                                rhs=Csb[dr][j % 3].ap[:, 0:129], start=False, stop=True),
                          reads=[qT[g], Csb[dr][j % 3]], joins=[PS[bb]])

            def S4(i):
                j, dr, c, dh, g, o = item(i)
                bb = ND[i % 4]
                d_ = dnr[i % 4]
                Sd.op("act", ACT.activation, dict(out=d_.ap[:, 0:1], in_=bank(bb)[:, 128:129], func=AF.Abs),
                      reads=[PS[bb]], writes=[d_])
                Sd.op("dve", V.tensor_scalar, dict(out=d_.ap[:, 0:1], in0=d_.ap[:, 0:1],
                                                   scalar1=STAB.ap[:, dh, j:j + 1], scalar2=None, op0=ALU.max),
                      reads=[d_, STAB], joins=[d_])
                Sd.op("dve", V.reciprocal, dict(out=d_.ap[:, 1:2], in_=d_.ap[:, 0:1]), reads=[d_], joins=[d_])

            def S5(i):
                j, dr, c, dh, g, o = item(i)
                bb = ND[i % 4]
                d_ = dnr[i % 4]
                if j < NCH // 2:
                    Sd.op("act", ACT.activation, dict(out=hm[c][h].ap, in_=bank(bb)[:, 0:P], func=AF.Copy,
                                                      scale=d_.ap[:, 1:2]), reads=[PS[bb], d_], writes=[hm[c][h]])
                else:
                    Sd.op("dve", V.scalar_tensor_tensor, dict(out=hm[c][h].ap, in0=bank(bb)[:, 0:P], scalar=d_.ap[:, 1:2],
                                                              in1=hm[c][h].ap, op0=ALU.mult, op1=ALU.add),
                          reads=[PS[bb], d_, hm[c][h]], writes=[hm[c][h]])

            for t in range(NI + 3):
                if t < NI:
                    S1(t)
                    S2(t)
                if 0 <= t - 1 < NI:
                    S3(t - 1)
                if 0 <= t - 2 < NI:
                    S4(t - 2)
                if 0 <= t - 3 < NI:
                    S5(t - 3)

        if stop_after == "H":
            Sd.barrier()
            for c in range(NCH):
                Sd.dma("sp", dict(out=dbg_d[c * P:(c + 1) * P, 0:P * _nheads], in_=hm_ap[:, c, 0:P * _nheads]), reads=hm[c])
            return finish()

        Sd.barrier()
        A.release(mH)
        mY = A.mark()
        w_o = T(A.alloc([KD, 512], BF16, "w_o"))
        w_u = T(A.alloc([KD, 512], BF16, "w_u"))
        w_pl = T(A.alloc([4, P], BF16, "w_pl"))
        psw = T(A.alloc([4], F32, "psw"))
        pscl = T(A.alloc([4], F32, "pscl"))
        edge = T(A.alloc([4, 16], F32, "edge"))
        ubuf = T(A.alloc([4, 528], F32, "ubuf"))
        tP = T(A.alloc([4, 528], F32, "tP"))
        tQ = T(A.alloc([2, 528], F32, "tQ"))
        tA = T(A.alloc([4, 512], F32, "tA"))
        mixed = T(A.alloc([4, 512], BF16, "mixed"))
        ucarry = T(A.alloc([4, 8], F32, "ucarry"))
        sig = [T(A.alloc([512], F32, "sig%d" % i)) for i in range(4)]
        sqb = [T(A.alloc([512], F32, "sqb%d" % i)) for i in range(2)]
        ssh = T(A.alloc([16], F32, "ssh"))
        rsh = T(A.alloc([16], F32, "rsh"))
        yab = [T(A.alloc([512], BF16, "ya%d" % i)) for i in range(2)]
        Sd.dma("pool", dict(out=w_o.ap, in_=w_in_v[:, :, 1536:2048]), writes=[w_o])
        Sd.dma("pool", dict(out=w_u.ap, in_=w_in_v[:, :, 2064:2576]), writes=[w_u])
        Sd.dma("pool", dict(out=w_pl.ap, in_=w_pool_d[:, :, :]), writes=[w_pl])
        Sd.dma("sp", dict(out=pscl.ap, in_=psclP_d[:, :]), writes=[pscl])
        Sd.dma("sp", dict(out=edge.ap.rearrange("p a b -> p (a b)"), in_=edge_d.partition_broadcast(P)), writes=[edge])
        for pg, w in enumerate(POOL_W):
            Sd.op("dve", V.tensor_scalar, dict(out=psw.ap[:, pg:pg + 1], in0=pscl.ap[:, pg:pg + 1], scalar1=1.0 / w, scalar2=None,
                                               op0=ALU.mult), reads=[pscl], joins=[psw])
        Sd.op("dve", V.memset, dict(ap=cst.ap[:, 2:3], constant=EPS), joins=[cst])
        ptr_ap = bank(6, 2).bitcast(BF16).rearrange("p (m t) -> p m t", m=4)

        for G in range(NG):
            hTg = [hT[k][G] for k in range(KD)]
            for pg in range(4):
                bb = pg % 2
                for k in range(KD):
                    Sd.op("pe", PE.matmul, dict(out=bank(bb), lhsT=w_u.ap[:, k, pg * P:(pg + 1) * P], rhs=hT[k][G].ap,
                                                start=(k == 0), stop=(k == KD - 1)),
                          reads=[w_u, hT[k][G]], writes=[PS[bb]] if k == 0 else [], joins=[] if k == 0 else [PS[bb]])
                Sd.op("act", ACT.activation, dict(out=ubuf.ap[:, pg, 8:520], in_=bank(bb), func=AF.Copy),
                      reads=[PS[bb]], writes=[ubuf] if pg == 0 else [], joins=[] if pg == 0 else [ubuf])
            if G < NG - 1:
                for pg in range(4):
                    for k in range(KD):
                        Sd.op("pe", PE.matmul, dict(out=bank(2)[:, pg * 8:(pg + 1) * 8], lhsT=w_u.ap[:, k, pg * P:(pg + 1) * P],
                                                    rhs=hT[k][G + 1].ap[:, 0:8], start=(k == 0), stop=(k == KD - 1)),
                              reads=[w_u, hT[k][G + 1]], writes=[PS[2]] if (k == 0 and pg == 0) else [],
                              joins=[] if (k == 0 and pg == 0) else [PS[2]])
                Sd.op("act", ACT.activation, dict(out=ubuf.ap[:, :, 520:528], in_=bank(2)[:, 0:32].rearrange("p (a b) -> p a b", a=4),
                                                  func=AF.Copy), reads=[PS[2]], joins=[ubuf])
            else:
                Sd.op("pool", POOL.memset, dict(ap=ubuf.ap[:, :, 520:528], constant=0.0), joins=[ubuf])
            if G == 0:
                Sd.op("pool", POOL.memset, dict(ap=ubuf.ap[:, :, 0:8], constant=0.0), joins=[ubuf])
            else:
                Sd.op("pool", POOL.tensor_copy, dict(out=ubuf.ap[:, :, 0:8], in_=ucarry.ap), reads=[ucarry], joins=[ubuf])
            Sd.op("pool", POOL.tensor_copy, dict(out=ucarry.ap, in_=ubuf.ap[:, :, 512:520]), reads=[ubuf], writes=[ucarry])
            U = ubuf.ap
            Sd.op("pool", POOL.tensor_tensor, dict(out=tP.ap[:, :, 0:527], in0=U[:, :, 0:527], in1=U[:, :, 1:528], op=ALU.add),
                  reads=[ubuf], writes=[tP])
            Sd.op("pool", POOL.tensor_tensor, dict(out=tQ.ap[:, 0:2, 0:525], in0=tP.ap[:, 2:4, 0:525], in1=tP.ap[:, 2:4, 2:527], op=ALU.add),
                  reads=[tP], writes=[tQ])
            Sd.op("pool", POOL.tensor_tensor, dict(out=tP.ap[:, 3, 0:521], in0=tQ.ap[:, 1, 0:521], in1=tQ.ap[:, 1, 4:525], op=ALU.add),
                  reads=[tQ, tP], joins=[tP])
            Sd.op("pool", POOL.tensor_copy, dict(out=tA.ap[:, 0, :], in_=tP.ap[:, 0, 7:519]), reads=[tP], writes=[tA])
            Sd.op("pool", POOL.tensor_tensor, dict(out=tA.ap[:, 1, :], in0=tP.ap[:, 1, 6:518], in1=tP.ap[:, 1, 8:520], op=ALU.add),
                  reads=[tP], joins=[tA])
            Sd.op("pool", POOL.tensor_tensor, dict(out=tA.ap[:, 2, :], in0=tQ.ap[:, 0, 4:516], in1=tQ.ap[:, 0, 8:520], op=ALU.add),
                  reads=[tQ], joins=[tA])
            Sd.op("pool", POOL.tensor_tensor, dict(out=tA.ap[:, 3, :], in0=tP.ap[:, 3, 0:512], in1=tP.ap[:, 3, 8:520], op=ALU.add),
                  reads=[tP], joins=[tA])
            if G == 0:
                Sd.op("pool", POOL.tensor_tensor, dict(out=tA.ap[:, :, 0:8], in0=tA.ap[:, :, 0:8], in1=edge.ap[:, :, 0:8], op=ALU.mult),
                      reads=[tA, edge], joins=[tA])
            if G == NG - 1:
                Sd.op("pool", POOL.tensor_tensor, dict(out=tA.ap[:, :, 504:512], in0=tA.ap[:, :, 504:512], in1=edge.ap[:, :, 8:16], op=ALU.mult),
                      reads=[tA, edge], joins=[tA])
            for i in range(4):
                c = G * 4 + i
                bb = 3 + i % 2
                for k in range(KD):
                    Sd.op("pe", PE.matmul, dict(out=bank(bb), lhsT=hT[k][G].ap[:, i * P:(i + 1) * P], rhs=w_o.ap[:, k, :],
                                                start=(k == 0), stop=(k == KD - 1)),
                          reads=[w_o, hT[k][G]], writes=[PS[bb]] if k == 0 else [], joins=[] if k == 0 else [PS[bb]])
                Sd.op("act", ACT.activation, dict(out=sig[i].ap, in_=bank(bb), func=AF.Sigmoid), reads=[PS[bb]], writes=[sig[i]])
                sq = sqb[i % 2]
                Sd.op("dve", V.tensor_tensor, dict(out=sq.ap, in0=hm_ap[:, c, :], in1=hm_ap[:, c, :], op=ALU.mult),
                      reads=hm[c], writes=[sq])
                Sd.op("dve", V.tensor_reduce, dict(out=ssh.ap[:, i * 4:(i + 1) * 4], in_=sq.ap.rearrange("p (a b) -> p a b", a=4),
                                                   axis=AX.X, op=ALU.add), reads=[sq], writes=[ssh] if i == 0 else [],
                      joins=[] if i == 0 else [ssh])
            Sd.op("act", ACT.activation, dict(out=rsh.ap, in_=ssh.ap, func=AF.Ln, scale=1.0 / P, bias=cst.ap[:, 2:3]),
                  reads=[ssh, cst], writes=[rsh])
            Sd.op("act", ACT.activation, dict(out=rsh.ap, in_=rsh.ap, func=AF.Exp, scale=-0.5), reads=[rsh], writes=[rsh])
            for i in range(4):
                c = G * 4 + i
                ya = yab[i % 2]
                for h in range(4):
                    Sd.op("dve", V.scalar_tensor_tensor, dict(out=ya.ap[:, h * P:(h + 1) * P], in0=hm[c][h].ap,
                                                              scalar=rsh.ap[:, i * 4 + h:i * 4 + h + 1], in1=sig[i].ap[:, h * P:(h + 1) * P],
                                                              op0=ALU.mult, op1=ALU.mult),
                          reads=[hm[c][h], rsh, sig[i]], writes=[ya] if h == 0 else [], joins=[] if h == 0 else [ya])
                for h in range(4):
                    first = (i == 0 and h == 0)
                    Sd.op("pe", PE.transpose, dict(out=ptr_ap[:, h, i * P:(i + 1) * P], in_=ya.ap[:, h * P:(h + 1) * P], identity=ident_b.ap),
                          reads=[ya, ident_b], writes=[PS[6], PS[7]] if first else [], joins=[] if first else [PS[6], PS[7]])
            for m in range(4):
                if m < 2:
                    Sd.op("act", ACT.activation, dict(out=hT[m][G].ap, in_=ptr_ap[:, m, :], func=AF.Copy), reads=[PS[6]], writes=[hT[m][G]])
                else:
                    Sd.op("dve", V.tensor_copy, dict(out=hT[m][G].ap, in_=ptr_ap[:, m, :]), reads=[PS[7]], writes=[hT[m][G]])

            for pg, w in enumerate(POOL_W):
                Sd.op("dve", V.scalar_tensor_tensor, dict(out=mixed.ap[:, pg, :], in0=U[:, pg, 8:520], scalar=-float(w), in1=tA.ap[:, pg, :],
                                                          op0=ALU.mult, op1=ALU.add),
                      reads=[ubuf, tA], writes=[mixed] if pg == 0 else [], joins=[] if pg == 0 else [mixed])
            for pg in range(4):
                Sd.op("pe", PE.matmul, dict(out=bank(5), lhsT=w_pl.ap[:, pg, :], rhs=mixed.ap[:, pg, :], start=True, stop=True),
                      reads=[w_pl, mixed], writes=[PS[5]])
                Sd.op("act", ACT.activation, dict(out=hT[4 + pg][G].ap, in_=bank(5), func=AF.Copy, scale=psw.ap[:, pg:pg + 1]),
                      reads=[PS[5], psw], writes=[hT[4 + pg][G]])

        if stop_after == "Y":
            Sd.barrier()
            for k in range(KD):
                for g in range(NG):
                    Sd.dma("sp", dict(out=dbg_d[:, k * S + g * 512:k * S + (g + 1) * 512], in_=hT[k][g].ap), reads=[hT[k][g]])
            return finish()

        Sd.barrier()
        A.release(mA)
        w_outp = T(A.alloc([KD, D], BF16, "w_outp"))
        ghP = T(A.alloc([4], F32, "ghP"))
        gfinB = T(A.alloc([D], F32, "gfinB"))
        x1_ap = A.alloc([8, D], F32, "x1")
        x1 = [T(x1_ap[:, i, :]) for i in range(8)]
        aT_ap = A.alloc([8, 2, 512], BF16, "aT")
        aT = [[T(aT_ap[:, fc, tg, :]) for tg in range(2)] for fc in range(8)]
        w1s = [T(A.alloc([KD, 512], BF16, "w1s%d" % i)) for i in range(3)]
        w2s = [T(A.alloc([4, D], BF16, "w2s%d" % i)) for i in range(3)]
        xn2 = [T(A.alloc([D], BF16, "xn2%d" % i)) for i in range(2)]
        rr = [T(A.alloc([512], F32, "rr%d" % i)) for i in range(2)]
        st2 = [T(A.alloc([4], F32, "st2%d" % i)) for i in range(4)]
        junk2 = A.alloc([D], BF16, "junk2")
        stg = [x1[0], x1[1]]
        Sd.dma("sp", dict(out=ghP.ap, in_=g_headP_d[:, :]), writes=[ghP])
        Sd.dma("sp", dict(out=gfinB.ap, in_=g_finR_d.partition_broadcast(P)), writes=[gfinB])
        for k in range(KD):
            sg = stg[k % 2]
            Sd.dma("sp", dict(out=sg.ap, in_=w_out_v[:, k, :]), writes=[sg])
            if k < 4:
                Sd.op("dve", V.scalar_tensor_tensor, dict(out=w_outp.ap[:, k, :], in0=sg.ap, scalar=ghP.ap[:, k:k + 1], in1=gt1B.ap,
                                                          op0=ALU.mult, op1=ALU.mult), reads=[sg, ghP, gt1B], joins=[w_outp])
            else:
                Sd.op("dve", V.tensor_tensor, dict(out=w_outp.ap[:, k, :], in0=sg.ap, in1=gt1B.ap, op=ALU.mult),
                      reads=[sg, gt1B], joins=[w_outp])
        NT4 = 4
        w1_blocks = [(Tt, q, bl) for Tt in range(NT4) for q in range(4) for bl in range(2)]
        gt2b4 = gt2B.ap.unsqueeze(1).to_broadcast([P, 4, D])

        def load_w1(n):
            if n >= len(w1_blocks):
                return
            Tt, q, bl = w1_blocks[n]
            c0 = q * 1024 + bl * 512
            Sd.dma("pool", dict(out=w1s[n % 3].ap, in_=w_ff1_v[:, :, c0:c0 + 512]), writes=[w1s[n % 3]])

        def load_w2(n):
            if n >= len(w1_blocks):
                return
            Tt, q, bl = w1_blocks[n]
            f0 = q * 8 + bl * 4
            t = w2s[n % 3]
            Sd.dma("pool", dict(out=t.ap, in_=w_ff2_v[:, f0:f0 + 4, :]), writes=[t])
            Sd.op("pool", POOL.tensor_tensor, dict(out=t.ap, in0=t.ap, in1=gt2b4, op=ALU.mult), reads=[t, gt2B], writes=[t])

        load_w1(0)
        load_w1(1)
        load_w2(0)
        load_w2(1)
        ps01 = ps_all[:, 0:1024]
        ps67 = ps_all[:, 3072:4096]
        psT2_ap = bank(2, 2).bitcast(BF16).rearrange("p (k t) -> p k t", k=KD)
        blk = 0
        for Tt in range(NT4):
            for st_ in range(8):
                c = Tt * 8 + st_
                g, o = c // 4, (c % 4) * P
                Sd.dma("sp", dict(out=x1[st_].ap, in_=x_v[c]), writes=[x1[st_]])
                for half in range(2):
                    for k in range(KD):
                        Sd.op("pe", PE.matmul, dict(out=bank(half), lhsT=hT[k][g].ap[:, o:o + P], rhs=w_outp.ap[:, k, half * 512:(half + 1) * 512],
                                                    start=(k == 0), stop=(k == KD - 1)),
                              reads=[hT[k][g], w_outp], writes=[PS[half]] if k == 0 else [], joins=[] if k == 0 else [PS[half]])
                Sd.op("dve", V.tensor_tensor, dict(out=x1[st_].ap, in0=ps01, in1=x1[st_].ap, op=ALU.add),
                      reads=[PS[0], PS[1], x1[st_]], writes=[x1[st_]])
            for st_ in range(8):
                c = Tt * 8 + st_
                g = c // 4
                s2 = st2[st_ % 4]
                xb = xn2[st_ % 2]
                Sd.op("act", ACT.activation, dict(out=junk2, in_=x1[st_].ap, func=AF.Square, accum_out=s2.ap[:, 0:1]),
                      reads=[x1[st_]], writes=[s2])
                Sd.op("act", ACT.activation, dict(out=s2.ap[:, 1:2], in_=s2.ap[:, 0:1], func=AF.Ln, scale=1.0 / D, bias=cst.ap[:, 0:1]),
                      reads=[s2, cst], joins=[s2])
                Sd.op("act", ACT.activation, dict(out=s2.ap[:, 2:3], in_=s2.ap[:, 1:2], func=AF.Exp, scale=-0.5), reads=[s2], joins=[s2])
                Sd.op("dve", V.tensor_scalar, dict(out=xb.ap, in0=x1[st_].ap, scalar1=s2.ap[:, 2:3], scalar2=None, op0=ALU.mult),
                      reads=[x1[st_], s2], writes=[xb])
                for k in range(KD):
                    first = (st_ % 2 == 0 and k == 0)
                    Sd.op("pe", PE.transpose, dict(out=psT2_ap[:, k, (st_ % 2) * P:(st_ % 2 + 1) * P], in_=xb.ap[:, k * P:(k + 1) * P],
                                                   identity=ident_b.ap),
                          reads=[xb, ident_b], writes=[PS[2], PS[3]] if first else [], joins=[] if first else [PS[2], PS[3]])
                if st_ % 2 == 1:
                    off = ((c // 2) % 2) * 256
                    for k in range(KD):
                        Sd.op("act", ACT.activation, dict(out=hT[k][g].ap[:, off:off + 256], in_=psT2_ap[:, k, :], func=AF.Identity,
                                                          scale=sc2.ap[:, k:k + 1], bias=modP.ap[:, 24 + k:25 + k]),
                              reads=[PS[2 + k // 4], sc2, modP], joins=[hT[k][g]])
            for q in range(4):
                for bl in range(2):
                    n = blk + bl
                    load_w1(n + 2)
                    wt = w1s[n % 3]
                    for f4 in range(4):
                        fc = bl * 4 + f4
                        for tg in range(2):
                            bb = 4 + (fc * 2 + tg) % 2
                            for k in range(KD):
                                Sd.op("pe", PE.matmul, dict(out=bank(bb), lhsT=wt.ap[:, k, f4 * P:(f4 + 1) * P], rhs=hT[k][Tt * 2 + tg].ap,
                                                            start=(k == 0), stop=(k == KD - 1)),
                                      reads=[wt, hT[k][Tt * 2 + tg]], writes=[PS[bb]] if k == 0 else [], joins=[] if k == 0 else [PS[bb]])
                            r_ = rr[(fc * 2 + tg) % 2]
                            Sd.op("act", ACT.activation, dict(out=r_.ap, in_=bank(bb), func=AF.Relu), reads=[PS[bb]], writes=[r_])
                            Sd.op("dve", V.tensor_tensor, dict(out=aT[fc][tg].ap, in0=r_.ap, in1=r_.ap, op=ALU.mult),
                                  reads=[r_], writes=[aT[fc][tg]])
                load_w2(blk + 2)
                for st_ in range(8):
                    tg, o = st_ // 4, (st_ % 4) * P
                    pb, pt = ((0, 1), ps01) if st_ % 2 == 0 else ((6, 7), ps67)
                    for half in range(2):
                        bb = pb[half]
                        for fc in range(8):
                            wt = w2s[(blk + fc // 4) % 3]
                            Sd.op("pe", PE.matmul, dict(out=bank(bb), lhsT=aT[fc][tg].ap[:, o:o + P], rhs=wt.ap[:, fc % 4, half * 512:(half + 1) * 512],
                                                        start=(fc == 0), stop=(fc == 7)),
                                  reads=[aT[fc][tg], wt], writes=[PS[bb]] if fc == 0 else [], joins=[] if fc == 0 else [PS[bb]])
                    Sd.op("dve", V.tensor_tensor, dict(out=x1[st_].ap, in0=pt, in1=x1[st_].ap, op=ALU.add),
                          reads=[PS[pb[0]], PS[pb[1]], x1[st_]], writes=[x1[st_]])
                load_w2(blk + 3)
                blk += 2
            for st_ in range(8):
                c = Tt * 8 + st_
                s2 = st2[st_ % 4]
                Sd.op("act", ACT.activation, dict(out=junk2, in_=x1[st_].ap, func=AF.Square, accum_out=s2.ap[:, 0:1]),
                      reads=[x1[st_]], writes=[s2])
                Sd.op("act", ACT.activation, dict(out=s2.ap[:, 1:2], in_=s2.ap[:, 0:1], func=AF.Ln, scale=1.0 / D, bias=cst.ap[:, 0:1]),
                      reads=[s2, cst], joins=[s2])
                Sd.op("act", ACT.activation, dict(out=s2.ap[:, 2:3], in_=s2.ap[:, 1:2], func=AF.Exp, scale=-0.5), reads=[s2], joins=[s2])
                Sd.op("dve", V.scalar_tensor_tensor, dict(out=x1[st_].ap, in0=x1[st_].ap, scalar=s2.ap[:, 2:3], in1=gfinB.ap,
                                                          op0=ALU.mult, op1=ALU.mult), reads=[x1[st_], s2, gfinB], writes=[x1[st_]])
                Sd.dma("sp", dict(out=out_v[c], in_=x1[st_].ap), reads=[x1[st_]])
        return finish()


def _consts():
    s = np.arange(P)
    maskF = (s[:, None] <= s[None, :]).astype(np.float32)
    maskB = (s[:, None] >= s[None, :]).astype(np.float32)
    edge = np.ones((4, 16), np.float32)
    for g, w in enumerate(POOL_W):
        for t in range(8):
            lo = max(t - w // 2, 0)
            hi = min(t + w // 2, S)
            edge[g, t] = w / float(hi - lo)
            tt = S - 8 + t
            lo = max(tt - w // 2, 0)
            hi = min(tt + w // 2, S)
            edge[g, 8 + t] = w / float(hi - lo)
    return dict(
        ident_f=np.eye(P, dtype=np.float32),
        ident_b=np.eye(P).astype(ml_dtypes.bfloat16),
        maskF=maskF, maskB=maskB,
        ones_f=np.ones((P, P), np.float32),
        edge_fac=edge.reshape(1, 64),
    )


def make_in_maps(x, c, w_ada, b_ada, g_mix, w_in, b_igate, b_fgate, g_head, w_pool, pool_scale,
                 w_out, g_ffn, w_ff1, w_ff2, g_final):
    f = lambda a: np.ascontiguousarray(np.asarray(a, dtype=np.float32))
    cs = _consts()
    shared = dict(
        w_ada=f(w_ada[0]),
        b_adaP=f(np.asarray(b_ada[0]).reshape(48, P).T),
        b_adaR=f(np.asarray(b_ada[0]).reshape(1, 6 * D)),
        g_mixP=f(np.asarray(g_mix[0]).reshape(KD, P).T),
        g_ffnP=f(np.asarray(g_ffn[0]).reshape(KD, P).T),
        g_finR=f(np.asarray(g_final).reshape(1, D)),
        w_in=f(w_in[0]),
        b_ig=f(np.asarray(b_igate[0]).reshape(1, 8)),
        b_fg=f(np.asarray(b_fgate[0]).reshape(1, 8)),
        g_headP=f(np.asarray(g_head[0]).reshape(4, P).T),
        pool_scaleP=f(np.asarray(pool_scale[0]).reshape(4, P).T),
        w_pool=f(np.asarray(w_pool[0]).transpose(1, 0, 2)),
        w_out=f(w_out[0]), w_ff1=f(w_ff1[0]), w_ff2=f(w_ff2[0]),
        **cs,
    )
    maps = []
    for b in range(N_CORES):
        m = dict(shared)
        m["x"] = f(x[b])
        m["cT"] = f(np.asarray(c[b]).reshape(KD, P).T)
        maps.append(m)
    return maps


_CACHE = {}


def kernel(**inputs):
    if "nc" not in _CACHE:
        _CACHE["nc"] = build_program()[0]
    nc = _CACHE["nc"]
    in_maps = make_in_maps(**inputs)
    res = run_bass_kernel_spmd(nc, in_maps, core_ids=list(range(N_CORES)))
    out = np.stack([np.asarray(r["out"]) for r in res.results], axis=0)
    return out.astype(np.float32)
```

```python
import numpy as np
import ml_dtypes
from contextlib import ExitStack

import concourse.bass as bass
import concourse.mybir as mybir
from concourse.bass_utils import run_bass_kernel_spmd

F32 = mybir.dt.float32
BF16 = mybir.dt.bfloat16
AF = mybir.ActivationFunctionType
ALU = mybir.AluOpType
AX = mybir.AxisListType

P = 128
S = 4096
D = 1024
KD = 8
NCH = 32
NG = 8
DFF = 4096
EPS = 1e-6
POOL_W = (2, 4, 8, 16)
N_CORES = 8


class T:
    __slots__ = ("ap", "w", "r", "dsem", "dcount", "name", "excl")

    def __init__(self, ap, name="", excl=False):
        self.ap = ap
        self.excl = excl
        self.w = []
        self.r = []
        self.dsem = None
        self.dcount = 0
        self.name = name

    def __getitem__(self, k):
        return self.ap[k]


class _Op:
    __slots__ = ("eng", "idx", "method", "kwargs", "waits", "marked", "dma_inc", "rank")


class Sched:
    ENG = ("pe", "act", "dve", "pool", "sp")

    def __init__(self, nc, es):
        self.nc = nc
        self.es = es
        self.h = {"pe": nc.tensor, "act": nc.scalar, "dve": nc.vector, "pool": nc.gpsimd, "sp": nc.sync}
        self.sem = {e: es.enter_context(nc.semaphore("s_" + e)) for e in ("pe", "act", "dve", "pool")}
        self.ops = []
        self.eng_ops = {e: [] for e in self.ENG}
        self.waited_c = {e: {x: -1 for x in self.ENG} for e in self.ENG}
        self.waited_d = {e: {} for e in self.ENG}
        self.dsems = []
        self.nsem = 0

    def _collect(self, eng, reads, writes, joins):
        toks = []
        for t in reads:
            toks.extend(t.w)
            if t.excl:
                toks.extend(x for x in t.r if x[0] == "c" and x[1].eng != eng)
        for t in writes:
            toks.extend(t.w)
            toks.extend(t.r)
        for t in joins:
            toks.extend(t.r)
        waits = []
        wc = self.waited_c[eng]
        wd = self.waited_d[eng]
        for tk in toks:
            if tk[0] == "c":
                d = tk[1]
                if d.eng == eng and eng == "pe":
                    continue
                if wc[d.eng] >= d.idx:
                    continue
                wc[d.eng] = d.idx
                d.marked = True
                waits.append(tk)
            else:
                _, sem, val = tk
                k = id(sem)
                if wd.get(k, 0) >= val:
                    continue
                wd[k] = val
                waits.append(tk)
        return waits

    def _post(self, tok, reads, writes, joins):
        for t in writes:
            t.w = [tok]
            t.r = []
        def same(x):
            if x[0] != tok[0]:
                return False
            if tok[0] == "c":
                return x[1].eng == tok[1].eng
            return x[1] is tok[1]
        for t in joins:
            t.w = [x for x in t.w if not same(x)] + [tok]
        for t in reads:
            t.r = [x for x in t.r if not same(x)] + [tok]

    def op(self, eng, method, kwargs, reads=(), writes=(), joins=()):
        o = _Op()
        o.eng = eng
        o.idx = len(self.eng_ops[eng])
        o.method = method
        o.kwargs = kwargs
        o.marked = False
        o.dma_inc = None
        o.waits = self._collect(eng, reads, writes, joins)
        self.eng_ops[eng].append(o)
        self.ops.append(o)
        self._post(("c", o), reads, writes, joins)
        return o

    def _dsem(self, t):
        if t.dsem is None:
            t.dsem = self.es.enter_context(self.nc.semaphore("d%d" % self.nsem))
            self.nsem += 1
            self.dsems.append(t)
        return t.dsem

    def dma(self, q, kwargs, reads=(), writes=(), joins=()):
        o = _Op()
        o.eng = q
        o.idx = -1
        o.method = self.h[q].dma_start
        o.kwargs = kwargs
        o.marked = False
        o.waits = self._collect(q, reads, writes, joins)
        owner = (list(writes) + list(joins) + list(reads))[0]
        sem = self._dsem(owner)
        owner.dcount += 16
        o.dma_inc = (sem, owner.dcount)
        self.ops.append(o)
        self._post(("d", sem, owner.dcount), reads, writes, joins)
        return o

    def barrier(self):
        for e in self.ENG:
            o = _Op()
            o.eng = e
            o.idx = -1
            o.method = None
            o.kwargs = None
            o.marked = False
            o.dma_inc = None
            waits = []
            for x in ("pe", "act", "dve", "pool"):
                if self.eng_ops[x]:
                    d = self.eng_ops[x][-1]
                    if x == e or self.waited_c[e][x] >= d.idx:
                        continue
                    self.waited_c[e][x] = d.idx
                    d.marked = True
                    waits.append(("c", d))
            for t in self.dsems:
                k = id(t.dsem)
                if self.waited_d[e].get(k, 0) < t.dcount:
                    self.waited_d[e][k] = t.dcount
                    waits.append(("d", t.dsem, t.dcount))
            o.waits = waits
            self.ops.append(o)

    def emit(self):
        for e in ("pe", "act", "dve", "pool"):
            r = 0
            for o in self.eng_ops[e]:
                if o.marked:
                    r += 1
                o.rank = r
        nw = 0
        for o in self.ops:
            h = self.h[o.eng]
            for tk in o.waits:
                if tk[0] == "c":
                    h.wait_ge(self.sem[tk[1].eng], tk[1].rank)
                else:
                    h.wait_ge(tk[1], tk[2])
                nw += 1
            if o.method is None:
                continue
            ins = o.method(**o.kwargs)
            if o.dma_inc is not None:
                ins.then_inc(o.dma_inc[0], 16)
            elif o.marked:
                ins.then_inc(self.sem[o.eng], 1)
        self.stats = {e: len(self.eng_ops[e]) for e in self.ENG}
        self.stats["waits"] = nw
        self.stats["ops"] = len(self.ops)


class Arena:
    def __init__(self, base_ap_f32, nbytes):
        self.base = base_ap_f32
        self.n = nbytes
        self.off = 0
        self.peak = 0

    def mark(self):
        return self.off

    def release(self, m):
        self.off = m

    def alloc(self, free_shape, dtype, name=""):
        esz = 2 if dtype == BF16 else 4
        n = int(np.prod(free_shape))
        nb = (n * esz + 63) // 64 * 64
        assert self.off + nb <= self.n, "SBUF arena overflow at %s: %d + %d > %d" % (name, self.off, nb, self.n)
        ap = self.base[:, self.off // 4:(self.off + nb) // 4]
        if dtype == BF16:
            ap = ap.bitcast(BF16)
        ap = ap[:, 0:n]
        if len(free_shape) == 2:
            ap = ap.rearrange("p (a b) -> p a b", a=free_shape[0])
        elif len(free_shape) == 3:
            ap = ap.rearrange("p (a b c) -> p a b c", a=free_shape[0], b=free_shape[1])
        self.off += nb
        self.peak = max(self.peak, self.off)
        return ap


def build_program(stop_after=None, dbg=None, dbg_dt=None):
    nc = bass.Bass("TRN2", target_bir_lowering=False)

    def din(name, shape, dt=F32):
        return nc.dram_tensor(name, list(shape), dt, kind="ExternalInput").ap()

    x_d = din("x", [S, D])
    cT_d = din("cT", [P, KD])
    w_ada_d = din("w_ada", [D, 6 * D])
    b_adaP_d = din("b_adaP", [P, 48])
    b_adaR_d = din("b_adaR", [1, 6 * D])
    g_mixP_d = din("g_mixP", [P, KD])
    g_ffnP_d = din("g_ffnP", [P, KD])
    g_finR_d = din("g_finR", [1, D])
    w_in_d = din("w_in", [D, 2576])
    b_ig_d = din("b_ig", [1, 8])
    b_fg_d = din("b_fg", [1, 8])
    g_headP_d = din("g_headP", [P, 4])
    psclP_d = din("pool_scaleP", [P, 4])
    w_pool_d = din("w_pool", [P, 4, P])
    w_out_d = din("w_out", [D, D])
    w_ff1_d = din("w_ff1", [D, DFF])
    w_ff2_d = din("w_ff2", [DFF, D])
    ident_f_d = din("ident_f", [P, P])
    ident_b_d = din("ident_b", [P, P], BF16)
    maskF_d = din("maskF", [P, P])
    maskB_d = din("maskB", [P, P])
    ones_d = din("ones_f", [P, P])
    edge_d = din("edge_fac", [1, 64])
    out_d = nc.dram_tensor("out", [S, D], F32, kind="ExternalOutput").ap()
    dbg_d = None
    if dbg is not None:
        dbg_d = nc.dram_tensor("dbg", list(dbg), dbg_dt or F32, kind="ExternalOutput").ap()

    w_ada_v = w_ada_d.rearrange("(k p) n -> p k n", p=P)
    w_in_v = w_in_d.rearrange("(k p) n -> p k n", p=P)
    w_out_v = w_out_d.rearrange("(k p) n -> p k n", p=P)
    w_ff1_v = w_ff1_d.rearrange("(k p) n -> p k n", p=P)
    w_ff2_v = w_ff2_d.rearrange("(k p) n -> p k n", p=P)
    x_v = x_d.rearrange("(c p) d -> c p d", p=P)
    out_v = out_d.rearrange("(c p) d -> c p d", p=P)

    SB_BYTES = 207 * 1024
    es = ExitStack()
    with es:
        sb_all = es.enter_context(nc.sbuf_tensor("sb_all", [P, SB_BYTES // 4], F32))
        ps_all = es.enter_context(nc.psum_tensor("ps_all", [P, 4096], F32))
        es.enter_context(nc.Block())
        Sd = Sched(nc, es)
        A = Arena(sb_all[:], SB_BYTES)
        V, ACT, PE, POOL = nc.vector, nc.scalar, nc.tensor, nc.gpsimd

        def bank(b, n=1):
            return ps_all[:, b * 512:(b + n) * 512]

        PS = [T(bank(b), "bank%d" % b, excl=True) for b in range(8)]

        hT_ap = A.alloc([KD, S], BF16, "hT")
        hT = [[T(hT_ap[:, k, g * 512:(g + 1) * 512], "hT%d_%d" % (k, g)) for g in range(NG)] for k in range(KD)]
        ident_b = T(A.alloc([P], BF16, "ident_b"))
        ident_f = T(A.alloc([P], F32, "ident_f"))
        maskF = T(A.alloc([P], F32, "maskF"))
        maskB = T(A.alloc([P], F32, "maskB"))
        ones_f = T(A.alloc([P], F32, "ones"))
        cst = T(A.alloc([64], F32, "cst_misc"))
        modP = T(A.alloc([48], F32, "modP"))
        sc1 = T(A.alloc([KD], F32, "sc1"))
        sc2 = T(A.alloc([KD], F32, "sc2"))
        gt1B = T(A.alloc([D], F32, "gt1B"))
        gt2B = T(A.alloc([D], F32, "gt2B"))
        WGT = T(A.alloc([8, NCH], F32, "WGT"))
        STAB = T(A.alloc([8, NCH], F32, "STAB"))
        DEC = T(A.alloc([8, NCH], F32, "DEC"))

        Sd.op("dve", V.memset, dict(ap=cst.ap[:, 0:1], constant=EPS), writes=[cst])
        for t, d in ((ident_b, ident_b_d), (ident_f, ident_f_d), (maskF, maskF_d), (maskB, maskB_d), (ones_f, ones_d)):
            Sd.dma("sp", dict(out=t.ap, in_=d[:, :]), writes=[t])

        def finish():
            Sd.barrier()
            Sd.emit()
            return nc, Sd, A


        mA = A.mark()
        cT = T(A.alloc([KD], F32, "cT"))
        c_act = T(A.alloc([KD], F32, "c_act"))
        c_rep = T(A.alloc([KD, P], F32, "c_rep"))
        b_adaP = T(A.alloc([48], F32, "b_adaP"))
        g_mixP = T(A.alloc([KD], F32, "g_mixP"))
        g_ffnP = T(A.alloc([KD], F32, "g_ffnP"))
        wA = [T(A.alloc([KD, 512], F32, "wA%d" % i)) for i in range(2)]
        psM = PS[0]
        psBc = PS[1]

        Sd.dma("sp", dict(out=cT.ap, in_=cT_d[:, :]), writes=[cT])
        Sd.dma("sp", dict(out=b_adaP.ap, in_=b_adaP_d[:, :]), writes=[b_adaP])
        Sd.dma("sp", dict(out=g_mixP.ap, in_=g_mixP_d[:, :]), writes=[g_mixP])
        Sd.dma("sp", dict(out=g_ffnP.ap, in_=g_ffnP_d[:, :]), writes=[g_ffnP])
        Sd.dma("sp", dict(out=gt1B.ap, in_=b_adaR_d[:, 2 * D:3 * D].partition_broadcast(P)), writes=[gt1B])
        Sd.dma("sp", dict(out=gt2B.ap, in_=b_adaR_d[:, 5 * D:6 * D].partition_broadcast(P)), writes=[gt2B])
        Sd.op("act", ACT.activation, dict(out=c_act.ap, in_=cT.ap, func=AF.Silu), reads=[cT], writes=[c_act])
        Sd.op("dve", V.tensor_copy, dict(out=c_rep.ap, in_=c_act.ap.unsqueeze(2).to_broadcast([P, KD, P])),
              reads=[c_act], writes=[c_rep])

        piece_order = [0, 1, 2, 3, 6, 7, 8, 9, 4, 5, 10, 11]

        def ada_piece(n, pc):
            buf = wA[n % 2]
            Sd.dma("pool", dict(out=buf.ap, in_=w_ada_v[:, :, pc * 512:(pc + 1) * 512]), writes=[buf])
            if pc in (4, 5, 10, 11):
                gt = gt1B if pc in (4, 5) else gt2B
                half = pc % 2
                for k in range(KD):
                    Sd.op("pe", PE.matmul, dict(out=psBc.ap, lhsT=c_rep.ap[:, k, :], rhs=buf.ap[:, k, :],
                                                start=(k == 0), stop=(k == KD - 1)),
                          reads=[c_rep, buf], writes=[psBc] if k == 0 else [], joins=[] if k == 0 else [psBc])
                sl = gt.ap[:, half * 512:(half + 1) * 512]
                Sd.op("dve", V.tensor_tensor, dict(out=sl, in0=psBc.ap, in1=sl, op=ALU.add),
                      reads=[psBc, gt], joins=[gt])
            else:
                for j in range(4):
                    blk = pc * 4 + j
                    for k in range(KD):
                        Sd.op("pe", PE.matmul, dict(out=psM.ap[:, blk:blk + 1], lhsT=buf.ap[:, k, j * P:(j + 1) * P],
                                                    rhs=c_act.ap[:, k:k + 1], start=(k == 0), stop=(k == KD - 1)),
                              reads=[c_act, buf], joins=[psM])
                if pc in (3, 9):
                    lo = 0 if pc == 3 else 24
                    Sd.op("dve", V.tensor_tensor, dict(out=modP.ap[:, lo:lo + 16], in0=psM.ap[:, lo:lo + 16],
                                                       in1=b_adaP.ap[:, lo:lo + 16], op=ALU.add),
                          reads=[psM, b_adaP], joins=[modP])
                    sc = sc1 if pc == 3 else sc2
                    gP = g_mixP if pc == 3 else g_ffnP
                    Sd.op("dve", V.scalar_tensor_tensor, dict(out=sc.ap, in0=modP.ap[:, lo + 8:lo + 16], scalar=1.0,
                                                              in1=gP.ap, op0=ALU.add, op1=ALU.mult),
                          reads=[modP, gP], writes=[sc])
        for n in range(4):
            ada_piece(n, piece_order[n])
        if stop_after == "0":
            Sd.barrier()
            Sd.dma("sp", dict(out=dbg_d[:, 0:48], in_=modP.ap), reads=[modP])
            Sd.dma("sp", dict(out=dbg_d[:, 48:56], in_=sc1.ap), reads=[sc1])
            Sd.dma("sp", dict(out=dbg_d[:, 56:64], in_=sc2.ap), reads=[sc2])
            Sd.dma("sp", dict(out=dbg_d[:, 64:64 + D], in_=gt1B.ap), reads=[gt1B])
            Sd.dma("sp", dict(out=dbg_d[:, 64 + D:64 + 2 * D], in_=gt2B.ap), reads=[gt2B])
            return finish()

        xin = [T(A.alloc([D], F32, "xin%d" % i)) for i in range(4)]
        xn = [T(A.alloc([D], BF16, "xn%d" % i)) for i in range(2)]
        junk = A.alloc([D], BF16, "junk")
        ssq = [T(A.alloc([4], F32, "ssq%d" % i)) for i in range(4)]
        psT_ap = [bank(2 + 2 * i, 2).bitcast(BF16).rearrange("p (k t) -> p k t", k=KD) for i in range(2)]
        psT_t = [[PS[2], PS[3]], [PS[4], PS[5]]]

        import os as _os
        _nch = int(_os.environ.get('K_NCH', NCH)); _var = _os.environ.get('K_VAR', '')
        for c in range(_nch):
            xt = xin[c % 4]
            sq = ssq[c % 4]
            xb = xn[c % 2]
            pT = psT_ap[(c // 2) % 2]
            pTt = psT_t[(c // 2) % 2]
            Sd.dma("sp", dict(out=xt.ap, in_=x_v[c]), writes=[xt])
            Sd.op("act", ACT.activation, dict(out=junk, in_=xt.ap, func=AF.Square, accum_out=sq.ap[:, 0:1]),
                  reads=[xt], writes=[sq])
            Sd.op("act", ACT.activation, dict(out=sq.ap[:, 1:2], in_=sq.ap[:, 0:1], func=AF.Ln, scale=1.0 / D,
                                              bias=cst.ap[:, 0:1]), reads=[sq, cst], joins=[sq])
            Sd.op("act", ACT.activation, dict(out=sq.ap[:, 2:3], in_=sq.ap[:, 1:2], func=AF.Exp, scale=-0.5),
                  reads=[sq], joins=[sq])
            Sd.op("dve", V.tensor_scalar, dict(out=xb.ap, in0=xt.ap, scalar1=sq.ap[:, 2:3], scalar2=None, op0=ALU.mult),
                  reads=[xt, sq], writes=[xb])
            for k in range(KD):
                first = (c % 2 == 0 and k == 0)
                Sd.op("pe", PE.transpose, dict(out=pT[:, k, (c % 2) * P:(c % 2 + 1) * P], in_=xb.ap[:, k * P:(k + 1) * P],
                                               identity=ident_b.ap),
                      reads=[xb, ident_b], writes=pTt if first else [], joins=[] if first else pTt)
            if c % 2 == 1:
                g = c // 4
                off = ((c // 2) % 2) * 256
                for k in range(KD):
                    dst = hT[k][g].ap[:, off:off + 256]
                    Sd.op("act", ACT.activation, dict(out=dst, in_=pT[:, k, :], func=AF.Identity,
                                                      scale=sc1.ap[:, k:k + 1], bias=modP.ap[:, k:k + 1]),
                          reads=[pTt[k // 4], sc1, modP], joins=[hT[k][g]])

        if stop_after == "A2":
            Sd.barrier()
            Sd.dma("sp", dict(out=dbg_d[:, 0:8], in_=sc1.ap), reads=[sc1])
            return finish()
        if stop_after == "A":
            Sd.barrier()
            for k in range(KD):
                for g in range(NG):
                    Sd.dma("sp", dict(out=dbg_d[:, k * S + g * 512:k * S + (g + 1) * 512], in_=hT[k][g].ap), reads=[hT[k][g]])
            return finish()

        w_g = T(A.alloc([KD, 16], BF16, "w_g"))
        Sd.dma("pool", dict(out=w_g.ap, in_=w_in_v[:, :, 2048:2064]), writes=[w_g])
        bI = T(A.alloc([8], F32, "bI"))
        bF = T(A.alloc([8], F32, "bF"))
        Sd.dma("sp", dict(out=bI.ap, in_=b_ig_d.partition_broadcast(P)), writes=[bI])
        Sd.dma("sp", dict(out=bF.ap, in_=b_fg_d.partition_broadcast(P)), writes=[bF])
        Sd.op("dve", V.memset, dict(ap=cst.ap[:, 1:2], constant=1.0), joins=[cst])
        psG = PS[6]
        psG_ap = bank(6).rearrange("p (d j g) -> p d j g", d=2, j=NCH)
        for c in range(NCH):
            for dr in range(2):
                j = c if dr == 0 else NCH - 1 - c
                for k in range(KD):
                    Sd.op("pe", PE.matmul, dict(out=psG_ap[:, dr, j, :], lhsT=hT[k][c // 4].ap[:, (c % 4) * P:(c % 4 + 1) * P],
                                                rhs=w_g.ap[:, k, dr * 8:(dr + 1) * 8], start=(k == 0), stop=(k == KD - 1)),
                          reads=[hT[k][c // 4], w_g], joins=[psG])

        for n in range(4, 12):
            ada_piece(n, piece_order[n])

        def tab(name, n=NCH):
            return T(A.alloc([8, n], F32, name))

        def v4(ap):
            return ap.rearrange("p (d h) j -> p d h j", d=2)

        def fl(ap):
            return ap.rearrange("p a j -> p (a j)")

        LI, ZZ, CS, GP, GB, BT, RR, TMP = (tab(n) for n in ("LI", "ZZ", "CS", "GP", "GB", "BT", "RR", "TMP"))
        MN = tab("MN", NCH + 1)
        Gcol = T(A.alloc([2], F32, "Gcol"))
        Grow = T(A.alloc([256], F32, "Grow"))
        g_i = psG_ap[:, :, :, 0:4].rearrange("p d j h -> p d h j")
        g_f = psG_ap[:, :, :, 4:8].rearrange("p d j h -> p d h j")
        bIb = bI.ap.rearrange("p (d h) -> p d h", d=2).unsqueeze(3).to_broadcast([P, 2, 4, NCH])
        bFb = bF.ap.rearrange("p (d h) -> p d h", d=2).unsqueeze(3).to_broadcast([P, 2, 4, NCH])
        Sd.op("dve", V.tensor_tensor, dict(out=v4(LI.ap), in0=g_i, in1=bIb, op=ALU.add), reads=[psG, bI], writes=[LI])
        Sd.op("dve", V.tensor_tensor, dict(out=v4(ZZ.ap), in0=g_f, in1=bFb, op=ALU.add), reads=[psG, bF], writes=[ZZ])
        Sd.op("act", ACT.activation, dict(out=fl(ZZ.ap), in_=fl(ZZ.ap), func=AF.Exp, scale=-1.0), reads=[ZZ], writes=[ZZ])
        Sd.op("act", ACT.activation, dict(out=fl(ZZ.ap), in_=fl(ZZ.ap), func=AF.Ln, bias=cst.ap[:, 1:2]), reads=[ZZ, cst], writes=[ZZ])
        if stop_after == "G1":
            Sd.barrier()
            for i, t in enumerate((LI, ZZ)):
                Sd.dma("sp", dict(out=dbg_d[:, i * 256:(i + 1) * 256].rearrange("p (a j) -> p a j", a=8), in_=t.ap[:, :, 0:NCH]), reads=[t])
            return finish()
        psC, psC_ap = PS[7], bank(7)[:, 0:256]
        psBt, psBt_ap = PS[4], bank(4)[:, 0:256]
        Sd.op("pe", PE.matmul, dict(out=psC_ap[:, 0:128], lhsT=maskF.ap, rhs=fl(ZZ.ap)[:, 0:128], start=True, stop=True),
              reads=[maskF, ZZ], writes=[psC])
        Sd.op("pe", PE.matmul, dict(out=psC_ap[:, 128:256], lhsT=maskB.ap, rhs=fl(ZZ.ap)[:, 128:256], start=True, stop=True),
              reads=[maskB, ZZ], joins=[psC])
        Sd.op("pe", PE.matmul, dict(out=psBt_ap, lhsT=ones_f.ap, rhs=fl(ZZ.ap), start=True, stop=True),
              reads=[ones_f, ZZ], writes=[psBt])
        Sd.op("act", ACT.activation, dict(out=fl(CS.ap), in_=psC_ap, func=AF.Copy), reads=[psC], writes=[CS])
        if stop_after == "G2a":
            Sd.barrier()
            Sd.dma("sp", dict(out=dbg_d[:, 0:256].rearrange("p (a j) -> p a j", a=8), in_=CS.ap), reads=[CS])
            return finish()
        Sd.op("dve", V.tensor_tensor, dict(out=fl(GP.ap), in0=psC_ap, in1=fl(LI.ap), op=ALU.add), reads=[psC, LI], writes=[GP])
        Sd.op("act", ACT.activation, dict(out=fl(BT.ap), in_=psBt_ap, func=AF.Copy, scale=-1.0), reads=[psBt], writes=[BT])
        if stop_after == "G2":
            Sd.barrier()
            for i, t in enumerate((CS, GP, BT)):
                Sd.dma("sp", dict(out=dbg_d[:, i * 256:(i + 1) * 256].rearrange("p (a j) -> p a j", a=8), in_=t.ap[:, :, 0:NCH]), reads=[t])
            return finish()
        psX, psX_ap = PS[5], bank(5)[:, 0:256]
        psR, psR_ap = PS[2], bank(2)[0:1, 0:256]
        psGB, psGB_ap = PS[3], bank(3)[:, 0:256]
        for hf in range(2):
            Sd.op("pe", PE.transpose, dict(out=psX_ap[:, hf * P:(hf + 1) * P], in_=fl(GP.ap)[:, hf * P:(hf + 1) * P], identity=ident_f.ap),
                  reads=[GP, ident_f], writes=[psX] if hf == 0 else [], joins=[] if hf == 0 else [psX])
        Sd.op("dve", V.tensor_reduce, dict(out=Gcol.ap, in_=psX_ap.rearrange("p (a s) -> p a s", a=2), axis=AX.X, op=ALU.max),
              reads=[psX], writes=[Gcol])
        for hf in range(2):
            Sd.op("pe", PE.transpose, dict(out=psR_ap[:, hf * P:(hf + 1) * P], in_=Gcol.ap[:, hf:hf + 1], identity=ident_f.ap),
                  reads=[Gcol, ident_f], writes=[psR] if hf == 0 else [], joins=[] if hf == 0 else [psR])
        Sd.op("act", ACT.activation, dict(out=Grow.ap[0:1, :], in_=psR_ap, func=AF.Copy), reads=[psR], writes=[Grow])
        Sd.op("pe", PE.matmul, dict(out=psGB_ap, lhsT=ones_f.ap[0:1, :], rhs=Grow.ap[0:1, :], start=True, stop=True),
              reads=[ones_f, Grow], writes=[psGB])
        Sd.op("act", ACT.activation, dict(out=fl(GB.ap), in_=psGB_ap, func=AF.Copy), reads=[psGB], writes=[GB])
        if stop_after == "G3":
            Sd.barrier()
            for i, t in enumerate((GB, GP)):
                Sd.dma("sp", dict(out=dbg_d[:, i * 256:(i + 1) * 256].rearrange("p (a j) -> p a j", a=8), in_=t.ap[:, :, 0:NCH]), reads=[t])
            return finish()
        Sd.op("dve", V.memset, dict(ap=MN.ap[:, :, 0:1], constant=0.0), writes=[MN])
        for dh in range(8):
            Sd.op("dve", V.tensor_tensor_scan, dict(out=MN.ap[:, dh, 1:NCH + 1], data0=GB.ap[:, dh, :], data1=BT.ap[:, dh, :],
                                                    initial=0.0, op0=ALU.max, op1=ALU.add), reads=[GB, BT], joins=[MN])
        Sd.op("dve", V.tensor_tensor, dict(out=RR.ap, in0=MN.ap[:, :, 0:NCH], in1=GB.ap, op=ALU.max), reads=[MN, GB], writes=[RR])
        for src, dst in ((MN, DEC), (GP, WGT), (CS, STAB)):
            sap = src.ap[:, :, 0:NCH]
            Sd.op("dve", V.tensor_tensor, dict(out=TMP.ap, in0=sap, in1=RR.ap, op=ALU.subtract), reads=[src, RR], writes=[TMP])
            Sd.op("act", ACT.activation, dict(out=fl(dst.ap), in_=fl(TMP.ap), func=AF.Exp), reads=[TMP], writes=[dst])

        if stop_after == "G":
            Sd.barrier()
            for i, t in enumerate((WGT, STAB, DEC, MN)):
                Sd.dma("sp", dict(out=dbg_d[:, i * 256:(i + 1) * 256].rearrange("p (a j) -> p a j", a=8), in_=t.ap[:, :, 0:NCH]), reads=[t])
            return finish()

        Sd.barrier()
        A.release(mA)
        hm_ap = A.alloc([NCH, 512], F32, "hm")
        hm = [[T(hm_ap[:, c, h * P:(h + 1) * P]) for h in range(4)] for c in range(NCH)]
        mH = A.mark()
        wqkv = T(A.alloc([KD, 3, P], BF16, "wqkv"))
        qT_ap = A.alloc([S], BF16, "qT")
        kT_ap = A.alloc([S], BF16, "kT")
        qT = [T(qT_ap[:, g * 512:(g + 1) * 512]) for g in range(NG)]
        kT = [T(kT_ap[:, g * 512:(g + 1) * 512]) for g in range(NG)]
        va_ap = A.alloc([NCH, 130], BF16, "va")
        va = [T(va_ap[:, c, :]) for c in range(NCH)]
        kwF_ap = A.alloc([NCH, P], BF16, "kwF")
        kwB_ap = A.alloc([NCH, P], BF16, "kwB")
        kwF = [T(kwF_ap[:, c, :]) for c in range(NCH)]
        kwB = [T(kwB_ap[:, c, :]) for c in range(NCH)]
        sTw = [T(A.alloc([P], BF16, "sTw%d" % i)) for i in range(4)]
        Cst = [T(A.alloc([130], F32, "Cst%d" % i)) for i in range(2)]
        Csb = [[T(A.alloc([130], BF16, "Csb%d%d" % (i, n))) for n in range(3)] for i in range(2)]
        dnr = [T(A.alloc([2], F32, "dnr%d" % i)) for i in range(4)]
        Sd.op("dve", V.memset, dict(ap=va_ap[:, :, 128:130], constant=1.0), writes=va)
        QSCALE = float(P) ** -0.5
        _nheads = int(_os.environ.get('K_NH', 4))

        for h in range(_nheads):
            for i, off in enumerate((0, 512, 1024)):
                Sd.dma("pool", dict(out=wqkv.ap[:, :, i, :], in_=w_in_v[:, :, off + h * P:off + (h + 1) * P]),
                       writes=[wqkv] if i == 0 else [], joins=[] if i == 0 else [wqkv])
            for g in range(NG):
                for which, dstT, bb in ((0, qT, g % 2), (1, kT, 2 + g % 2)):
                    for k in range(KD):
                        Sd.op("pe", PE.matmul, dict(out=bank(bb), lhsT=wqkv.ap[:, k, which, :], rhs=hT[k][g].ap,
                                                    start=(k == 0), stop=(k == KD - 1)),
                              reads=[wqkv, hT[k][g]], writes=[PS[bb]] if k == 0 else [], joins=[] if k == 0 else [PS[bb]])
                    if which == 0:
                        Sd.op("act", ACT.activation, dict(out=dstT[g].ap, in_=bank(bb), func=AF.Copy, scale=QSCALE),
                              reads=[PS[bb]], writes=[dstT[g]])
                    else:
                        Sd.op("dve", V.tensor_copy, dict(out=dstT[g].ap, in_=bank(bb)), reads=[PS[bb]], writes=[dstT[g]])
                for ci in range(4):
                    c = g * 4 + ci
                    bb = 4 + ci
                    for k in range(KD):
                        Sd.op("pe", PE.matmul, dict(out=bank(bb)[:, 0:256], lhsT=hT[k][g].ap[:, ci * P:(ci + 1) * P],
                                                    rhs=wqkv.ap[:, k, 1:3, :].rearrange("p a b -> p (a b)"),
                                                    start=(k == 0), stop=(k == KD - 1)),
                              reads=[wqkv, hT[k][g]], writes=[PS[bb]] if k == 0 else [], joins=[] if k == 0 else [PS[bb]])
                    Sd.op("act", ACT.activation, dict(out=va[c].ap[:, 0:P], in_=bank(bb)[:, P:2 * P], func=AF.Copy),
                          reads=[PS[bb]], joins=[va[c]])
                    Sd.op("dve", V.tensor_scalar, dict(out=kwF[c].ap, in0=bank(bb)[:, 0:P], scalar1=WGT.ap[:, h, c:c + 1],
                                                       scalar2=None, op0=ALU.mult), reads=[PS[bb], WGT], writes=[kwF[c]])
                    jb = NCH - 1 - c
                    Sd.op("dve", V.tensor_scalar, dict(out=kwB[c].ap, in0=bank(bb)[:, 0:P], scalar1=WGT.ap[:, 4 + h, jb:jb + 1],
                                                       scalar2=None, op0=ALU.mult), reads=[PS[bb], WGT], writes=[kwB[c]])
            NI = 2 * NCH
            ND = (2, 3, 6, 7)

            def item(i):
                j, dr = divmod(i, 2)
                c = j if dr == 0 else NCH - 1 - j
                return j, dr, c, dr * 4 + h, c // 4, (c % 4) * P

            def S1(i):
                j, dr, c, dh, g, o = item(i)
                st = sTw[i % 4]
                Sd.op("pe", PE.matmul, dict(out=bank(dr)[:, 0:P], lhsT=kT[g].ap[:, o:o + P], rhs=qT[g].ap[:, o:o + P],
                                            start=True, stop=True), reads=[kT[g], qT[g]], writes=[PS[dr]])
                Sd.op("dve", V.scalar_tensor_tensor, dict(out=st.ap, in0=bank(dr)[:, 0:P], scalar=WGT.ap[:, dh, j:j + 1],
                                                          in1=(maskF if dr == 0 else maskB).ap, op0=ALU.mult, op1=ALU.mult),
                      reads=[PS[dr], WGT, maskF if dr == 0 else maskB], writes=[st])

            def S2(i):
                j, dr, c, dh, g, o = item(i)
                if j >= NCH - 1:
                    return
                kw = kwF if dr == 0 else kwB
                Sd.op("pe", PE.matmul, dict(out=bank(4 + dr)[:, 0:129], lhsT=kw[c].ap, rhs=va[c].ap[:, 0:129],
                                            start=True, stop=True), reads=[kw[c], va[c]], writes=[PS[4 + dr]])
                if j == 0:
                    Sd.op("dve", V.tensor_copy, dict(out=Cst[dr].ap[:, 0:129], in_=bank(4 + dr)[:, 0:129]),
                          reads=[PS[4 + dr]], writes=[Cst[dr]])
                else:
                    Sd.op("dve", V.scalar_tensor_tensor, dict(out=Cst[dr].ap[:, 0:129], in0=Cst[dr].ap[:, 0:129],
                                                              scalar=DEC.ap[:, dh, j:j + 1], in1=bank(4 + dr)[:, 0:129],
                                                              op0=ALU.mult, op1=ALU.add),
                          reads=[Cst[dr], DEC, PS[4 + dr]], writes=[Cst[dr]])

            def S2b(i):
                j, dr, c, dh, g, o = item(i)
                if j >= NCH - 1:
                    return
                Sd.op("act", ACT.activation, dict(out=Csb[dr][(j + 1) % 3].ap[:, 0:129], in_=Cst[dr].ap[:, 0:129],
                                                  func=AF.Copy, scale=DEC.ap[:, dh, j + 1:j + 2]),
                      reads=[Cst[dr], DEC], writes=[Csb[dr][(j + 1) % 3]])

            def S3(i):
                j, dr, c, dh, g, o = item(i)
                bb = ND[i % 4]
                st = sTw[i % 4]
                Sd.op("pe", PE.matmul, dict(out=bank(bb)[:, 0:129], lhsT=st.ap, rhs=va[c].ap[:, 0:129],
                                            start=True, stop=(j == 0)), reads=[st, va[c]], writes=[PS[bb]])
                if j > 0:
                    Sd.op("pe", PE.matmul, dict(out=bank(bb)[:, 0:129], lhsT=qT[g].ap[:, o:o + P],
                                                rhs=Csb[dr][j % 3].ap[:, 0:129], start=False, stop=True),
                          reads=[qT[g], Csb[dr][j % 3]], joins=[PS[bb]])

            def S4(i):
                j, dr, c, dh, g, o = item(i)
                bb = ND[i % 4]
                d_ = dnr[i % 4]
                Sd.op("act", ACT.activation, dict(out=d_.ap[:, 0:1], in_=bank(bb)[:, 128:129], func=AF.Abs),
                      reads=[PS[bb]], writes=[d_])
                Sd.op("dve", V.tensor_scalar, dict(out=d_.ap[:, 0:1], in0=d_.ap[:, 0:1],
                                                   scalar1=STAB.ap[:, dh, j:j + 1], scalar2=None, op0=ALU.max),
                      reads=[d_, STAB], joins=[d_])
                Sd.op("dve", V.reciprocal, dict(out=d_.ap[:, 1:2], in_=d_.ap[:, 0:1]), reads=[d_], joins=[d_])

            def S5(i):
                j, dr, c, dh, g, o = item(i)
                bb = ND[i % 4]
                d_ = dnr[i % 4]
                if j < NCH // 2:
                    Sd.op("act", ACT.activation, dict(out=hm[c][h].ap, in_=bank(bb)[:, 0:P], func=AF.Copy,
                                                      scale=d_.ap[:, 1:2]), reads=[PS[bb], d_], writes=[hm[c][h]])
                else:
                    Sd.op("dve", V.scalar_tensor_tensor, dict(out=hm[c][h].ap, in0=bank(bb)[:, 0:P], scalar=d_.ap[:, 1:2],
                                                              in1=hm[c][h].ap, op0=ALU.mult, op1=ALU.add),
                          reads=[PS[bb], d_, hm[c][h]], writes=[hm[c][h]])

            for t in range(NI + 3):
                if t < NI:
                    S1(t)
                if 0 <= t - 1 < NI:
                    S2b(t - 1)
                if 0 <= t - 2 < NI:
                    S4(t - 2)
                if 0 <= t - 3 < NI:
                    S5(t - 3)
                if 0 <= t - 1 < NI:
                    S3(t - 1)
                if t < NI:
                    S2(t)

        if stop_after == "H":
            Sd.barrier()
            for c in range(NCH):
                Sd.dma("sp", dict(out=dbg_d[c * P:(c + 1) * P, 0:P * _nheads], in_=hm_ap[:, c, 0:P * _nheads]), reads=hm[c])
            return finish()

        Sd.barrier()
        A.release(mH)
        mY = A.mark()
        w_o = T(A.alloc([KD, 512], BF16, "w_o"))
        w_u = T(A.alloc([KD, 512], BF16, "w_u"))
        w_pl = T(A.alloc([4, P], BF16, "w_pl"))
        psw = T(A.alloc([4], F32, "psw"))
        pscl = T(A.alloc([4], F32, "pscl"))
        edge = T(A.alloc([4, 16], F32, "edge"))
        ubuf = T(A.alloc([4, 528], F32, "ubuf"))
        tP = T(A.alloc([4, 528], F32, "tP"))
        tQ = T(A.alloc([2, 528], F32, "tQ"))
        tA = T(A.alloc([4, 512], F32, "tA"))
        mixed = T(A.alloc([4, 512], BF16, "mixed"))
        ucarry = T(A.alloc([4, 8], F32, "ucarry"))
        sig = [T(A.alloc([512], F32, "sig%d" % i)) for i in range(4)]
        sqb = [T(A.alloc([512], F32, "sqb%d" % i)) for i in range(2)]
        ssh = T(A.alloc([16], F32, "ssh"))
        rsh = T(A.alloc([16], F32, "rsh"))
        yab = [T(A.alloc([512], BF16, "ya%d" % i)) for i in range(2)]
        Sd.dma("pool", dict(out=w_o.ap, in_=w_in_v[:, :, 1536:2048]), writes=[w_o])
        Sd.dma("pool", dict(out=w_u.ap, in_=w_in_v[:, :, 2064:2576]), writes=[w_u])
        Sd.dma("pool", dict(out=w_pl.ap, in_=w_pool_d[:, :, :]), writes=[w_pl])
        Sd.dma("sp", dict(out=pscl.ap, in_=psclP_d[:, :]), writes=[pscl])
        Sd.dma("sp", dict(out=edge.ap.rearrange("p a b -> p (a b)"), in_=edge_d.partition_broadcast(P)), writes=[edge])
        for pg, w in enumerate(POOL_W):
            Sd.op("dve", V.tensor_scalar, dict(out=psw.ap[:, pg:pg + 1], in0=pscl.ap[:, pg:pg + 1], scalar1=1.0 / w, scalar2=None,
                                               op0=ALU.mult), reads=[pscl], joins=[psw])
        Sd.op("dve", V.memset, dict(ap=cst.ap[:, 2:3], constant=EPS), joins=[cst])
        ptr_ap = bank(6, 2).bitcast(BF16).rearrange("p (m t) -> p m t", m=4)

        for G in range(NG):
            hTg = [hT[k][G] for k in range(KD)]
            for pg in range(4):
                bb = pg % 2
                for k in range(KD):
                    Sd.op("pe", PE.matmul, dict(out=bank(bb), lhsT=w_u.ap[:, k, pg * P:(pg + 1) * P], rhs=hT[k][G].ap,
                                                start=(k == 0), stop=(k == KD - 1)),
                          reads=[w_u, hT[k][G]], writes=[PS[bb]] if k == 0 else [], joins=[] if k == 0 else [PS[bb]])
                Sd.op("act", ACT.activation, dict(out=ubuf.ap[:, pg, 8:520], in_=bank(bb), func=AF.Copy),
                      reads=[PS[bb]], writes=[ubuf] if pg == 0 else [], joins=[] if pg == 0 else [ubuf])
            if G < NG - 1:
                for pg in range(4):
                    for k in range(KD):
                        Sd.op("pe", PE.matmul, dict(out=bank(2)[:, pg * 8:(pg + 1) * 8], lhsT=w_u.ap[:, k, pg * P:(pg + 1) * P],
                                                    rhs=hT[k][G + 1].ap[:, 0:8], start=(k == 0), stop=(k == KD - 1)),
                              reads=[w_u, hT[k][G + 1]], writes=[PS[2]] if (k == 0 and pg == 0) else [],
                              joins=[] if (k == 0 and pg == 0) else [PS[2]])
                Sd.op("act", ACT.activation, dict(out=ubuf.ap[:, :, 520:528], in_=bank(2)[:, 0:32].rearrange("p (a b) -> p a b", a=4),
                                                  func=AF.Copy), reads=[PS[2]], joins=[ubuf])
            else:
                Sd.op("pool", POOL.memset, dict(ap=ubuf.ap[:, :, 520:528], constant=0.0), joins=[ubuf])
            if G == 0:
                Sd.op("pool", POOL.memset, dict(ap=ubuf.ap[:, :, 0:8], constant=0.0), joins=[ubuf])
            else:
                Sd.op("pool", POOL.tensor_copy, dict(out=ubuf.ap[:, :, 0:8], in_=ucarry.ap), reads=[ucarry], joins=[ubuf])
            Sd.op("pool", POOL.tensor_copy, dict(out=ucarry.ap, in_=ubuf.ap[:, :, 512:520]), reads=[ubuf], writes=[ucarry])
            U = ubuf.ap
            Sd.op("pool", POOL.tensor_tensor, dict(out=tP.ap[:, :, 0:527], in0=U[:, :, 0:527], in1=U[:, :, 1:528], op=ALU.add),
                  reads=[ubuf], writes=[tP])
            Sd.op("pool", POOL.tensor_tensor, dict(out=tQ.ap[:, 0:2, 0:525], in0=tP.ap[:, 2:4, 0:525], in1=tP.ap[:, 2:4, 2:527], op=ALU.add),
                  reads=[tP], writes=[tQ])
            Sd.op("pool", POOL.tensor_tensor, dict(out=tP.ap[:, 3, 0:521], in0=tQ.ap[:, 1, 0:521], in1=tQ.ap[:, 1, 4:525], op=ALU.add),
                  reads=[tQ, tP], joins=[tP])
            Sd.op("pool", POOL.tensor_copy, dict(out=tA.ap[:, 0, :], in_=tP.ap[:, 0, 7:519]), reads=[tP], writes=[tA])
            Sd.op("pool", POOL.tensor_tensor, dict(out=tA.ap[:, 1, :], in0=tP.ap[:, 1, 6:518], in1=tP.ap[:, 1, 8:520], op=ALU.add),
                  reads=[tP], joins=[tA])
            Sd.op("pool", POOL.tensor_tensor, dict(out=tA.ap[:, 2, :], in0=tQ.ap[:, 0, 4:516], in1=tQ.ap[:, 0, 8:520], op=ALU.add),
                  reads=[tQ], joins=[tA])
            Sd.op("pool", POOL.tensor_tensor, dict(out=tA.ap[:, 3, :], in0=tP.ap[:, 3, 0:512], in1=tP.ap[:, 3, 8:520], op=ALU.add),
                  reads=[tP], joins=[tA])
            if G == 0:
                Sd.op("pool", POOL.tensor_tensor, dict(out=tA.ap[:, :, 0:8], in0=tA.ap[:, :, 0:8], in1=edge.ap[:, :, 0:8], op=ALU.mult),
                      reads=[tA, edge], joins=[tA])
            if G == NG - 1:
                Sd.op("pool", POOL.tensor_tensor, dict(out=tA.ap[:, :, 504:512], in0=tA.ap[:, :, 504:512], in1=edge.ap[:, :, 8:16], op=ALU.mult),
                      reads=[tA, edge], joins=[tA])
            for i in range(4):
                c = G * 4 + i
                bb = 3 + i % 2
                for k in range(KD):
                    Sd.op("pe", PE.matmul, dict(out=bank(bb), lhsT=hT[k][G].ap[:, i * P:(i + 1) * P], rhs=w_o.ap[:, k, :],
                                                start=(k == 0), stop=(k == KD - 1)),
                          reads=[w_o, hT[k][G]], writes=[PS[bb]] if k == 0 else [], joins=[] if k == 0 else [PS[bb]])
                Sd.op("act", ACT.activation, dict(out=sig[i].ap, in_=bank(bb), func=AF.Sigmoid), reads=[PS[bb]], writes=[sig[i]])
                sq = sqb[i % 2]
                Sd.op("dve", V.tensor_tensor, dict(out=sq.ap, in0=hm_ap[:, c, :], in1=hm_ap[:, c, :], op=ALU.mult),
                      reads=hm[c], writes=[sq])
                Sd.op("dve", V.tensor_reduce, dict(out=ssh.ap[:, i * 4:(i + 1) * 4], in_=sq.ap.rearrange("p (a b) -> p a b", a=4),
                                                   axis=AX.X, op=ALU.add), reads=[sq], writes=[ssh] if i == 0 else [],
                      joins=[] if i == 0 else [ssh])
            Sd.op("act", ACT.activation, dict(out=rsh.ap, in_=ssh.ap, func=AF.Ln, scale=1.0 / P, bias=cst.ap[:, 2:3]),
                  reads=[ssh, cst], writes=[rsh])
            Sd.op("act", ACT.activation, dict(out=rsh.ap, in_=rsh.ap, func=AF.Exp, scale=-0.5), reads=[rsh], writes=[rsh])
            for i in range(4):
                c = G * 4 + i
                ya = yab[i % 2]
                for h in range(4):
                    Sd.op("dve", V.scalar_tensor_tensor, dict(out=ya.ap[:, h * P:(h + 1) * P], in0=hm[c][h].ap,
                                                              scalar=rsh.ap[:, i * 4 + h:i * 4 + h + 1], in1=sig[i].ap[:, h * P:(h + 1) * P],
                                                              op0=ALU.mult, op1=ALU.mult),
                          reads=[hm[c][h], rsh, sig[i]], writes=[ya] if h == 0 else [], joins=[] if h == 0 else [ya])
                for h in range(4):
                    first = (i == 0 and h == 0)
                    Sd.op("pe", PE.transpose, dict(out=ptr_ap[:, h, i * P:(i + 1) * P], in_=ya.ap[:, h * P:(h + 1) * P], identity=ident_b.ap),
                          reads=[ya, ident_b], writes=[PS[6], PS[7]] if first else [], joins=[] if first else [PS[6], PS[7]])
            for m in range(4):
                if m < 2:
                    Sd.op("act", ACT.activation, dict(out=hT[m][G].ap, in_=ptr_ap[:, m, :], func=AF.Copy), reads=[PS[6]], writes=[hT[m][G]])
                else:
                    Sd.op("dve", V.tensor_copy, dict(out=hT[m][G].ap, in_=ptr_ap[:, m, :]), reads=[PS[7]], writes=[hT[m][G]])

            for pg, w in enumerate(POOL_W):
                Sd.op("dve", V.scalar_tensor_tensor, dict(out=mixed.ap[:, pg, :], in0=U[:, pg, 8:520], scalar=-float(w), in1=tA.ap[:, pg, :],
                                                          op0=ALU.mult, op1=ALU.add),
                      reads=[ubuf, tA], writes=[mixed] if pg == 0 else [], joins=[] if pg == 0 else [mixed])
            for pg in range(4):
                Sd.op("pe", PE.matmul, dict(out=bank(5), lhsT=w_pl.ap[:, pg, :], rhs=mixed.ap[:, pg, :], start=True, stop=True),
                      reads=[w_pl, mixed], writes=[PS[5]])
                Sd.op("act", ACT.activation, dict(out=hT[4 + pg][G].ap, in_=bank(5), func=AF.Copy, scale=psw.ap[:, pg:pg + 1]),
                      reads=[PS[5], psw], writes=[hT[4 + pg][G]])

        if stop_after == "Y":
            Sd.barrier()
            for k in range(KD):
                for g in range(NG):
                    Sd.dma("sp", dict(out=dbg_d[:, k * S + g * 512:k * S + (g + 1) * 512], in_=hT[k][g].ap), reads=[hT[k][g]])
            return finish()

        Sd.barrier()
        A.release(mA)
        w_outp = T(A.alloc([KD, D], BF16, "w_outp"))
        ghP = T(A.alloc([4], F32, "ghP"))
        gfinB = T(A.alloc([D], F32, "gfinB"))
        x1_ap = A.alloc([8, D], F32, "x1")
        x1 = [T(x1_ap[:, i, :]) for i in range(8)]
        aT_ap = A.alloc([8, 2, 512], BF16, "aT")
        aT = [[T(aT_ap[:, fc, tg, :]) for tg in range(2)] for fc in range(8)]
        w1s = [T(A.alloc([KD, 512], BF16, "w1s%d" % i)) for i in range(3)]
        w2s = [T(A.alloc([4, D], BF16, "w2s%d" % i)) for i in range(3)]
        xn2 = [T(A.alloc([D], BF16, "xn2%d" % i)) for i in range(2)]
        rr = [T(A.alloc([512], F32, "rr%d" % i)) for i in range(2)]
        st2 = [T(A.alloc([4], F32, "st2%d" % i)) for i in range(4)]
        junk2 = A.alloc([D], BF16, "junk2")
        stg = [x1[0], x1[1]]
        Sd.dma("sp", dict(out=ghP.ap, in_=g_headP_d[:, :]), writes=[ghP])
        Sd.dma("sp", dict(out=gfinB.ap, in_=g_finR_d.partition_broadcast(P)), writes=[gfinB])
        for k in range(KD):
            sg = stg[k % 2]
            Sd.dma("sp", dict(out=sg.ap, in_=w_out_v[:, k, :]), writes=[sg])
            if k < 4:
                Sd.op("dve", V.scalar_tensor_tensor, dict(out=w_outp.ap[:, k, :], in0=sg.ap, scalar=ghP.ap[:, k:k + 1], in1=gt1B.ap,
                                                          op0=ALU.mult, op1=ALU.mult), reads=[sg, ghP, gt1B], joins=[w_outp])
            else:
                Sd.op("dve", V.tensor_tensor, dict(out=w_outp.ap[:, k, :], in0=sg.ap, in1=gt1B.ap, op=ALU.mult),
                      reads=[sg, gt1B], joins=[w_outp])
        NT4 = 4
        w1_blocks = [(Tt, q, bl) for Tt in range(NT4) for q in range(4) for bl in range(2)]
        gt2b4 = gt2B.ap.unsqueeze(1).to_broadcast([P, 4, D])

        def load_w1(n):
            if n >= len(w1_blocks):
                return
            Tt, q, bl = w1_blocks[n]
            c0 = q * 1024 + bl * 512
            Sd.dma("pool", dict(out=w1s[n % 3].ap, in_=w_ff1_v[:, :, c0:c0 + 512]), writes=[w1s[n % 3]])

        def load_w2(n):
            if n >= len(w1_blocks):
                return
            Tt, q, bl = w1_blocks[n]
            f0 = q * 8 + bl * 4
            t = w2s[n % 3]
            Sd.dma("pool", dict(out=t.ap, in_=w_ff2_v[:, f0:f0 + 4, :]), writes=[t])
            Sd.op("pool", POOL.tensor_tensor, dict(out=t.ap, in0=t.ap, in1=gt2b4, op=ALU.mult), reads=[t, gt2B], writes=[t])

        load_w1(0)
        load_w1(1)
        load_w2(0)
        load_w2(1)
        ps01 = ps_all[:, 0:1024]
        ps67 = ps_all[:, 3072:4096]
        psT2_ap = bank(2, 2).bitcast(BF16).rearrange("p (k t) -> p k t", k=KD)
        blk = 0
        for Tt in range(NT4):
            for st_ in range(8):
                c = Tt * 8 + st_
                g, o = c // 4, (c % 4) * P
                Sd.dma("sp", dict(out=x1[st_].ap, in_=x_v[c]), writes=[x1[st_]])
                for half in range(2):
                    for k in range(KD):
                        Sd.op("pe", PE.matmul, dict(out=bank(half), lhsT=hT[k][g].ap[:, o:o + P], rhs=w_outp.ap[:, k, half * 512:(half + 1) * 512],
                                                    start=(k == 0), stop=(k == KD - 1)),
                              reads=[hT[k][g], w_outp], writes=[PS[half]] if k == 0 else [], joins=[] if k == 0 else [PS[half]])
                Sd.op("dve", V.tensor_tensor, dict(out=x1[st_].ap, in0=ps01, in1=x1[st_].ap, op=ALU.add),
                      reads=[PS[0], PS[1], x1[st_]], writes=[x1[st_]])
            for st_ in range(8):
                c = Tt * 8 + st_
                g = c // 4
                s2 = st2[st_ % 4]
                xb = xn2[st_ % 2]
                Sd.op("act", ACT.activation, dict(out=junk2, in_=x1[st_].ap, func=AF.Square, accum_out=s2.ap[:, 0:1]),
                      reads=[x1[st_]], writes=[s2])
                Sd.op("act", ACT.activation, dict(out=s2.ap[:, 1:2], in_=s2.ap[:, 0:1], func=AF.Ln, scale=1.0 / D, bias=cst.ap[:, 0:1]),
                      reads=[s2, cst], joins=[s2])
                Sd.op("act", ACT.activation, dict(out=s2.ap[:, 2:3], in_=s2.ap[:, 1:2], func=AF.Exp, scale=-0.5), reads=[s2], joins=[s2])
                Sd.op("dve", V.tensor_scalar, dict(out=xb.ap, in0=x1[st_].ap, scalar1=s2.ap[:, 2:3], scalar2=None, op0=ALU.mult),
                      reads=[x1[st_], s2], writes=[xb])
                for k in range(KD):
                    first = (st_ % 2 == 0 and k == 0)
                    Sd.op("pe", PE.transpose, dict(out=psT2_ap[:, k, (st_ % 2) * P:(st_ % 2 + 1) * P], in_=xb.ap[:, k * P:(k + 1) * P],
                                                   identity=ident_b.ap),
                          reads=[xb, ident_b], writes=[PS[2], PS[3]] if first else [], joins=[] if first else [PS[2], PS[3]])
                if st_ % 2 == 1:
                    off = ((c // 2) % 2) * 256
                    for k in range(KD):
                        Sd.op("act", ACT.activation, dict(out=hT[k][g].ap[:, off:off + 256], in_=psT2_ap[:, k, :], func=AF.Identity,
                                                          scale=sc2.ap[:, k:k + 1], bias=modP.ap[:, 24 + k:25 + k]),
                              reads=[PS[2 + k // 4], sc2, modP], joins=[hT[k][g]])
            for q in range(4):
                for bl in range(2):
                    n = blk + bl
                    load_w1(n + 2)
                    wt = w1s[n % 3]
                    for f4 in range(4):
                        fc = bl * 4 + f4
                        for tg in range(2):
                            bb = 4 + (fc * 2 + tg) % 2
                            for k in range(KD):
                                Sd.op("pe", PE.matmul, dict(out=bank(bb), lhsT=wt.ap[:, k, f4 * P:(f4 + 1) * P], rhs=hT[k][Tt * 2 + tg].ap,
                                                            start=(k == 0), stop=(k == KD - 1)),
                                      reads=[wt, hT[k][Tt * 2 + tg]], writes=[PS[bb]] if k == 0 else [], joins=[] if k == 0 else [PS[bb]])
                            r_ = rr[(fc * 2 + tg) % 2]
                            Sd.op("act", ACT.activation, dict(out=r_.ap, in_=bank(bb), func=AF.Relu), reads=[PS[bb]], writes=[r_])
                            Sd.op("dve", V.tensor_tensor, dict(out=aT[fc][tg].ap, in0=r_.ap, in1=r_.ap, op=ALU.mult),
                                  reads=[r_], writes=[aT[fc][tg]])
                load_w2(blk + 2)
                for st_ in range(8):
                    tg, o = st_ // 4, (st_ % 4) * P
                    pb, pt = ((0, 1), ps01) if st_ % 2 == 0 else ((6, 7), ps67)
                    for half in range(2):
                        bb = pb[half]
                        for fc in range(8):
                            wt = w2s[(blk + fc // 4) % 3]
                            Sd.op("pe", PE.matmul, dict(out=bank(bb), lhsT=aT[fc][tg].ap[:, o:o + P], rhs=wt.ap[:, fc % 4, half * 512:(half + 1) * 512],
                                                        start=(fc == 0), stop=(fc == 7)),
                                  reads=[aT[fc][tg], wt], writes=[PS[bb]] if fc == 0 else [], joins=[] if fc == 0 else [PS[bb]])
                    Sd.op("dve", V.tensor_tensor, dict(out=x1[st_].ap, in0=pt, in1=x1[st_].ap, op=ALU.add),
                          reads=[PS[pb[0]], PS[pb[1]], x1[st_]], writes=[x1[st_]])
                load_w2(blk + 3)
                blk += 2
            for st_ in range(8):
                c = Tt * 8 + st_
                s2 = st2[st_ % 4]
                Sd.op("act", ACT.activation, dict(out=junk2, in_=x1[st_].ap, func=AF.Square, accum_out=s2.ap[:, 0:1]),
                      reads=[x1[st_]], writes=[s2])
                Sd.op("act", ACT.activation, dict(out=s2.ap[:, 1:2], in_=s2.ap[:, 0:1], func=AF.Ln, scale=1.0 / D, bias=cst.ap[:, 0:1]),
                      reads=[s2, cst], joins=[s2])
                Sd.op("act", ACT.activation, dict(out=s2.ap[:, 2:3], in_=s2.ap[:, 1:2], func=AF.Exp, scale=-0.5), reads=[s2], joins=[s2])
                Sd.op("dve", V.scalar_tensor_tensor, dict(out=x1[st_].ap, in0=x1[st_].ap, scalar=s2.ap[:, 2:3], in1=gfinB.ap,
                                                          op0=ALU.mult, op1=ALU.mult), reads=[x1[st_], s2, gfinB], writes=[x1[st_]])
                Sd.dma("sp", dict(out=out_v[c], in_=x1[st_].ap), reads=[x1[st_]])
        return finish()


def _consts():
    s = np.arange(P)
    maskF = (s[:, None] <= s[None, :]).astype(np.float32)
    maskB = (s[:, None] >= s[None, :]).astype(np.float32)
    edge = np.ones((4, 16), np.float32)
    for g, w in enumerate(POOL_W):
        for t in range(8):
            lo = max(t - w // 2, 0)
            hi = min(t + w // 2, S)
            edge[g, t] = w / float(hi - lo)
            tt = S - 8 + t
            lo = max(tt - w // 2, 0)
            hi = min(tt + w // 2, S)
            edge[g, 8 + t] = w / float(hi - lo)
    return dict(
        ident_f=np.eye(P, dtype=np.float32),
        ident_b=np.eye(P).astype(ml_dtypes.bfloat16),
        maskF=maskF, maskB=maskB,
        ones_f=np.ones((P, P), np.float32),
        edge_fac=edge.reshape(1, 64),
    )


def make_in_maps(x, c, w_ada, b_ada, g_mix, w_in, b_igate, b_fgate, g_head, w_pool, pool_scale,
                 w_out, g_ffn, w_ff1, w_ff2, g_final):
    f = lambda a: np.ascontiguousarray(np.asarray(a, dtype=np.float32))
    cs = _consts()
    shared = dict(
        w_ada=f(w_ada[0]),
        b_adaP=f(np.asarray(b_ada[0]).reshape(48, P).T),
        b_adaR=f(np.asarray(b_ada[0]).reshape(1, 6 * D)),
        g_mixP=f(np.asarray(g_mix[0]).reshape(KD, P).T),
        g_ffnP=f(np.asarray(g_ffn[0]).reshape(KD, P).T),
        g_finR=f(np.asarray(g_final).reshape(1, D)),
        w_in=f(w_in[0]),
        b_ig=f(np.asarray(b_igate[0]).reshape(1, 8)),
        b_fg=f(np.asarray(b_fgate[0]).reshape(1, 8)),
        g_headP=f(np.asarray(g_head[0]).reshape(4, P).T),
        pool_scaleP=f(np.asarray(pool_scale[0]).reshape(4, P).T),
        w_pool=f(np.asarray(w_pool[0]).transpose(1, 0, 2)),
        w_out=f(w_out[0]), w_ff1=f(w_ff1[0]), w_ff2=f(w_ff2[0]),
        **cs,
    )
    maps = []
    for b in range(N_CORES):
        m = dict(shared)
        m["x"] = f(x[b])
        m["cT"] = f(np.asarray(c[b]).reshape(KD, P).T)
        maps.append(m)
    return maps


_CACHE = {}


def kernel(**inputs):
    if "nc" not in _CACHE:
        _CACHE["nc"] = build_program()[0]
    nc = _CACHE["nc"]
    in_maps = make_in_maps(**inputs)
    res = run_bass_kernel_spmd(nc, in_maps, core_ids=list(range(N_CORES)))
    out = np.stack([np.asarray(r["out"]) for r in res.results], axis=0)
    return out.astype(np.float32)
```

```python
import numpy as np
import ml_dtypes
from contextlib import ExitStack

import concourse.bass as bass
import concourse.mybir as mybir
from concourse.bass_utils import run_bass_kernel_spmd

F32 = mybir.dt.float32
BF16 = mybir.dt.bfloat16
AF = mybir.ActivationFunctionType
ALU = mybir.AluOpType
AX = mybir.AxisListType

P = 128
S = 4096
D = 1024
KD = 8
NCH = 32
NG = 8
DFF = 4096
EPS = 1e-6
POOL_W = (2, 4, 8, 16)
N_CORES = 8


class T:
    __slots__ = ("ap", "w", "r", "dsem", "dcount", "name", "excl")

    def __init__(self, ap, name="", excl=False):
        self.ap = ap
        self.excl = excl
        self.w = []
        self.r = []
        self.dsem = None
        self.dcount = 0
        self.name = name

    def __getitem__(self, k):
        return self.ap[k]


class _Op:
    __slots__ = ("eng", "idx", "method", "kwargs", "waits", "marked", "dma_inc", "rank")


class Sched:
    ENG = ("pe", "act", "dve", "pool", "sp")

    def __init__(self, nc, es):
        self.nc = nc
        self.es = es
        self.h = {"pe": nc.tensor, "act": nc.scalar, "dve": nc.vector, "pool": nc.gpsimd, "sp": nc.sync}
        self.sem = {e: es.enter_context(nc.semaphore("s_" + e)) for e in ("pe", "act", "dve", "pool")}
        self.ops = []
        self.eng_ops = {e: [] for e in self.ENG}
        self.waited_c = {e: {x: -1 for x in self.ENG} for e in self.ENG}
        self.waited_d = {e: {} for e in self.ENG}
        self.dsems = []
        self.nsem = 0

    def _collect(self, eng, reads, writes, joins):
        toks = []
        for t in reads:
            toks.extend(t.w)
            if t.excl:
                toks.extend(x for x in t.r if x[0] == "c" and x[1].eng != eng)
        for t in writes:
            toks.extend(t.w)
            toks.extend(t.r)
        for t in joins:
            toks.extend(t.r)
        waits = []
        wc = self.waited_c[eng]
        wd = self.waited_d[eng]
        for tk in toks:
            if tk[0] == "c":
                d = tk[1]
                if d.eng == eng and eng == "pe":
                    continue
                if wc[d.eng] >= d.idx:
                    continue
                wc[d.eng] = d.idx
                d.marked = True
                waits.append(tk)
            else:
                _, sem, val = tk
                k = id(sem)
                if wd.get(k, 0) >= val:
                    continue
                wd[k] = val
                waits.append(tk)
        return waits

    def _post(self, tok, reads, writes, joins):
        for t in writes:
            t.w = [tok]
            t.r = []
        def same(x):
            if x[0] != tok[0]:
                return False
            if tok[0] == "c":
                return x[1].eng == tok[1].eng
            return x[1] is tok[1]
        for t in joins:
            t.w = [x for x in t.w if not same(x)] + [tok]
        for t in reads:
            t.r = [x for x in t.r if not same(x)] + [tok]

    def op(self, eng, method, kwargs, reads=(), writes=(), joins=()):
        o = _Op()
        o.eng = eng
        o.idx = len(self.eng_ops[eng])
        o.method = method
        o.kwargs = kwargs
        o.marked = False
        o.dma_inc = None
        o.waits = self._collect(eng, reads, writes, joins)
        self.eng_ops[eng].append(o)
        self.ops.append(o)
        self._post(("c", o), reads, writes, joins)
        return o

    def _dsem(self, t):
        if t.dsem is None:
            t.dsem = self.es.enter_context(self.nc.semaphore("d%d" % self.nsem))
            self.nsem += 1
            self.dsems.append(t)
        return t.dsem

    def dma(self, q, kwargs, reads=(), writes=(), joins=()):
        o = _Op()
        o.eng = q
        o.idx = -1
        o.method = self.h[q].dma_start
        o.kwargs = kwargs
        o.marked = False
        o.waits = self._collect(q, reads, writes, joins)
        owner = (list(writes) + list(joins) + list(reads))[0]
        sem = self._dsem(owner)
        owner.dcount += 16
        o.dma_inc = (sem, owner.dcount)
        self.ops.append(o)
        self._post(("d", sem, owner.dcount), reads, writes, joins)
        return o

    def barrier(self):
        for e in self.ENG:
            o = _Op()
            o.eng = e
            o.idx = -1
            o.method = None
            o.kwargs = None
            o.marked = False
            o.dma_inc = None
            waits = []
            for x in ("pe", "act", "dve", "pool"):
                if self.eng_ops[x]:
                    d = self.eng_ops[x][-1]
                    if x == e or self.waited_c[e][x] >= d.idx:
                        continue
                    self.waited_c[e][x] = d.idx
                    d.marked = True
                    waits.append(("c", d))
            for t in self.dsems:
                k = id(t.dsem)
                if self.waited_d[e].get(k, 0) < t.dcount:
                    self.waited_d[e][k] = t.dcount
                    waits.append(("d", t.dsem, t.dcount))
            o.waits = waits
            self.ops.append(o)

    def emit(self):
        for e in ("pe", "act", "dve", "pool"):
            r = 0
            for o in self.eng_ops[e]:
                if o.marked:
                    r += 1
                o.rank = r
        nw = 0
        for o in self.ops:
            h = self.h[o.eng]
            for tk in o.waits:
                if tk[0] == "c":
                    h.wait_ge(self.sem[tk[1].eng], tk[1].rank)
                else:
                    h.wait_ge(tk[1], tk[2])
                nw += 1
            if o.method is None:
                continue
            ins = o.method(**o.kwargs)
            if o.dma_inc is not None:
                ins.then_inc(o.dma_inc[0], 16)
            elif o.marked:
                ins.then_inc(self.sem[o.eng], 1)
        self.stats = {e: len(self.eng_ops[e]) for e in self.ENG}
        self.stats["waits"] = nw
        self.stats["ops"] = len(self.ops)


class Arena:
    def __init__(self, base_ap_f32, nbytes):
        self.base = base_ap_f32
        self.n = nbytes
        self.off = 0
        self.peak = 0

    def mark(self):
        return self.off

    def release(self, m):
        self.off = m

    def alloc(self, free_shape, dtype, name=""):
        esz = 2 if dtype == BF16 else 4
        n = int(np.prod(free_shape))
        nb = (n * esz + 63) // 64 * 64
        assert self.off + nb <= self.n, "SBUF arena overflow at %s: %d + %d > %d" % (name, self.off, nb, self.n)
        ap = self.base[:, self.off // 4:(self.off + nb) // 4]
        if dtype == BF16:
            ap = ap.bitcast(BF16)
        ap = ap[:, 0:n]
        if len(free_shape) == 2:
            ap = ap.rearrange("p (a b) -> p a b", a=free_shape[0])
        elif len(free_shape) == 3:
            ap = ap.rearrange("p (a b c) -> p a b c", a=free_shape[0], b=free_shape[1])
        self.off += nb
        self.peak = max(self.peak, self.off)
        return ap


def build_program(stop_after=None, dbg=None, dbg_dt=None):
    nc = bass.Bass("TRN2", target_bir_lowering=False)

    def din(name, shape, dt=F32):
        return nc.dram_tensor(name, list(shape), dt, kind="ExternalInput").ap()

    x_d = din("x", [S, D])
    cT_d = din("cT", [P, KD])
    w_ada_d = din("w_ada", [D, 6 * D])
    b_adaP_d = din("b_adaP", [P, 48])
    b_adaR_d = din("b_adaR", [1, 6 * D])
    g_mixP_d = din("g_mixP", [P, KD])
    g_ffnP_d = din("g_ffnP", [P, KD])
    g_finR_d = din("g_finR", [1, D])
    w_in_d = din("w_in", [D, 2576])
    b_ig_d = din("b_ig", [1, 8])
    b_fg_d = din("b_fg", [1, 8])
    g_headP_d = din("g_headP", [P, 4])
    psclP_d = din("pool_scaleP", [P, 4])
    w_pool_d = din("w_pool", [P, 4, P])
    w_out_d = din("w_out", [D, D])
    w_ff1_d = din("w_ff1", [D, DFF])
    w_ff2_d = din("w_ff2", [DFF, D])
    ident_f_d = din("ident_f", [P, P])
    ident_b_d = din("ident_b", [P, P], BF16)
    maskF_d = din("maskF", [P, P])
    maskB_d = din("maskB", [P, P])
    ones_d = din("ones_f", [P, P])
    edge_d = din("edge_fac", [1, 64])
    out_d = nc.dram_tensor("out", [S, D], F32, kind="ExternalOutput").ap()
    dbg_d = None
    if dbg is not None:
        dbg_d = nc.dram_tensor("dbg", list(dbg), dbg_dt or F32, kind="ExternalOutput").ap()

    w_ada_v = w_ada_d.rearrange("(k p) n -> p k n", p=P)
    w_in_v = w_in_d.rearrange("(k p) n -> p k n", p=P)
    w_out_v = w_out_d.rearrange("(k p) n -> p k n", p=P)
    w_ff1_v = w_ff1_d.rearrange("(k p) n -> p k n", p=P)
    w_ff2_v = w_ff2_d.rearrange("(k p) n -> p k n", p=P)
    x_v = x_d.rearrange("(c p) d -> c p d", p=P)
    out_v = out_d.rearrange("(c p) d -> c p d", p=P)

    SB_BYTES = 207 * 1024
    es = ExitStack()
    with es:
        sb_all = es.enter_context(nc.sbuf_tensor("sb_all", [P, SB_BYTES // 4], F32))
        ps_all = es.enter_context(nc.psum_tensor("ps_all", [P, 4096], F32))
        es.enter_context(nc.Block())
        Sd = Sched(nc, es)
        A = Arena(sb_all[:], SB_BYTES)
        V, ACT, PE, POOL = nc.vector, nc.scalar, nc.tensor, nc.gpsimd

        def bank(b, n=1):
            return ps_all[:, b * 512:(b + n) * 512]

        PS = [T(bank(b), "bank%d" % b, excl=True) for b in range(8)]

        hT_ap = A.alloc([KD, S], BF16, "hT")
        hT = [[T(hT_ap[:, k, g * 512:(g + 1) * 512], "hT%d_%d" % (k, g)) for g in range(NG)] for k in range(KD)]
        ident_b = T(A.alloc([P], BF16, "ident_b"))
        ident_f = T(A.alloc([P], F32, "ident_f"))
        maskF = T(A.alloc([P], F32, "maskF"))
        maskB = T(A.alloc([P], F32, "maskB"))
        ones_f = T(A.alloc([P], F32, "ones"))
        cst = T(A.alloc([64], F32, "cst_misc"))
        modP = T(A.alloc([48], F32, "modP"))
        sc1 = T(A.alloc([KD], F32, "sc1"))
        sc2 = T(A.alloc([KD], F32, "sc2"))
        gt1B = T(A.alloc([D], F32, "gt1B"))
        gt2B = T(A.alloc([D], F32, "gt2B"))
        WGT = T(A.alloc([8, NCH], F32, "WGT"))
        STAB = T(A.alloc([8, NCH], F32, "STAB"))
        DEC = T(A.alloc([8, NCH], F32, "DEC"))

        Sd.op("dve", V.memset, dict(ap=cst.ap[:, 0:1], constant=EPS), writes=[cst])
        for t, d in ((ident_b, ident_b_d), (ident_f, ident_f_d), (maskF, maskF_d), (maskB, maskB_d), (ones_f, ones_d)):
            Sd.dma("sp", dict(out=t.ap, in_=d[:, :]), writes=[t])

        def finish():
            Sd.barrier()
            Sd.emit()
            return nc, Sd, A


        mA = A.mark()
        cT = T(A.alloc([KD], F32, "cT"))
        c_act = T(A.alloc([KD], F32, "c_act"))
        c_rep = T(A.alloc([KD, P], F32, "c_rep"))
        b_adaP = T(A.alloc([48], F32, "b_adaP"))
        g_mixP = T(A.alloc([KD], F32, "g_mixP"))
        g_ffnP = T(A.alloc([KD], F32, "g_ffnP"))
        wA = [T(A.alloc([KD, 512], F32, "wA%d" % i)) for i in range(2)]
        psM = PS[0]
        psBc = PS[1]

        Sd.dma("sp", dict(out=cT.ap, in_=cT_d[:, :]), writes=[cT])
        Sd.dma("sp", dict(out=b_adaP.ap, in_=b_adaP_d[:, :]), writes=[b_adaP])
        Sd.dma("sp", dict(out=g_mixP.ap, in_=g_mixP_d[:, :]), writes=[g_mixP])
        Sd.dma("sp", dict(out=g_ffnP.ap, in_=g_ffnP_d[:, :]), writes=[g_ffnP])
        Sd.dma("sp", dict(out=gt1B.ap, in_=b_adaR_d[:, 2 * D:3 * D].partition_broadcast(P)), writes=[gt1B])
        Sd.dma("sp", dict(out=gt2B.ap, in_=b_adaR_d[:, 5 * D:6 * D].partition_broadcast(P)), writes=[gt2B])
        Sd.op("act", ACT.activation, dict(out=c_act.ap, in_=cT.ap, func=AF.Silu), reads=[cT], writes=[c_act])
        Sd.op("dve", V.tensor_copy, dict(out=c_rep.ap, in_=c_act.ap.unsqueeze(2).to_broadcast([P, KD, P])),
              reads=[c_act], writes=[c_rep])

        piece_order = [0, 1, 2, 3, 6, 7, 8, 9, 4, 5, 10, 11]

        def ada_piece(n, pc):
            buf = wA[n % 2]
            Sd.dma("pool", dict(out=buf.ap, in_=w_ada_v[:, :, pc * 512:(pc + 1) * 512]), writes=[buf])
            if pc in (4, 5, 10, 11):
                gt = gt1B if pc in (4, 5) else gt2B
                half = pc % 2
                for k in range(KD):
                    Sd.op("pe", PE.matmul, dict(out=psBc.ap, lhsT=c_rep.ap[:, k, :], rhs=buf.ap[:, k, :],
                                                start=(k == 0), stop=(k == KD - 1)),
                          reads=[c_rep, buf], writes=[psBc] if k == 0 else [], joins=[] if k == 0 else [psBc])
                sl = gt.ap[:, half * 512:(half + 1) * 512]
                Sd.op("dve", V.tensor_tensor, dict(out=sl, in0=psBc.ap, in1=sl, op=ALU.add),
                      reads=[psBc, gt], joins=[gt])
            else:
                for j in range(4):
                    blk = pc * 4 + j
                    for k in range(KD):
                        Sd.op("pe", PE.matmul, dict(out=psM.ap[:, blk:blk + 1], lhsT=buf.ap[:, k, j * P:(j + 1) * P],
                                                    rhs=c_act.ap[:, k:k + 1], start=(k == 0), stop=(k == KD - 1)),
                              reads=[c_act, buf], joins=[psM])
                if pc in (3, 9):
                    lo = 0 if pc == 3 else 24
                    Sd.op("dve", V.tensor_tensor, dict(out=modP.ap[:, lo:lo + 16], in0=psM.ap[:, lo:lo + 16],
                                                       in1=b_adaP.ap[:, lo:lo + 16], op=ALU.add),
                          reads=[psM, b_adaP], joins=[modP])
                    sc = sc1 if pc == 3 else sc2
                    gP = g_mixP if pc == 3 else g_ffnP
                    Sd.op("dve", V.scalar_tensor_tensor, dict(out=sc.ap, in0=modP.ap[:, lo + 8:lo + 16], scalar=1.0,
                                                              in1=gP.ap, op0=ALU.add, op1=ALU.mult),
                          reads=[modP, gP], writes=[sc])
        for n in range(4):
            ada_piece(n, piece_order[n])
        if stop_after == "0":
            Sd.barrier()
            Sd.dma("sp", dict(out=dbg_d[:, 0:48], in_=modP.ap), reads=[modP])
            Sd.dma("sp", dict(out=dbg_d[:, 48:56], in_=sc1.ap), reads=[sc1])
            Sd.dma("sp", dict(out=dbg_d[:, 56:64], in_=sc2.ap), reads=[sc2])
            Sd.dma("sp", dict(out=dbg_d[:, 64:64 + D], in_=gt1B.ap), reads=[gt1B])
            Sd.dma("sp", dict(out=dbg_d[:, 64 + D:64 + 2 * D], in_=gt2B.ap), reads=[gt2B])
            return finish()

        xin = [T(A.alloc([D], F32, "xin%d" % i)) for i in range(4)]
        xn = [T(A.alloc([D], BF16, "xn%d" % i)) for i in range(2)]
        junk = A.alloc([D], BF16, "junk")
        ssq = [T(A.alloc([4], F32, "ssq%d" % i)) for i in range(4)]
        psT_ap = [bank(2 + 2 * i, 2).bitcast(BF16).rearrange("p (k t) -> p k t", k=KD) for i in range(2)]
        psT_t = [[PS[2], PS[3]], [PS[4], PS[5]]]

        import os as _os
        _nch = int(_os.environ.get('K_NCH', NCH)); _var = _os.environ.get('K_VAR', '')
        for c in range(_nch):
            xt = xin[c % 4]
            sq = ssq[c % 4]
            xb = xn[c % 2]
            pT = psT_ap[(c // 2) % 2]
            pTt = psT_t[(c // 2) % 2]
            Sd.dma("sp", dict(out=xt.ap, in_=x_v[c]), writes=[xt])
            Sd.op("act", ACT.activation, dict(out=junk, in_=xt.ap, func=AF.Square, accum_out=sq.ap[:, 0:1]),
                  reads=[xt], writes=[sq])
            Sd.op("act", ACT.activation, dict(out=sq.ap[:, 1:2], in_=sq.ap[:, 0:1], func=AF.Ln, scale=1.0 / D,
                                              bias=cst.ap[:, 0:1]), reads=[sq, cst], joins=[sq])
            Sd.op("act", ACT.activation, dict(out=sq.ap[:, 2:3], in_=sq.ap[:, 1:2], func=AF.Exp, scale=-0.5),
                  reads=[sq], joins=[sq])
            Sd.op("dve", V.tensor_scalar, dict(out=xb.ap, in0=xt.ap, scalar1=sq.ap[:, 2:3], scalar2=None, op0=ALU.mult),
                  reads=[xt, sq], writes=[xb])
            for k in range(KD):
                first = (c % 2 == 0 and k == 0)
                Sd.op("pe", PE.transpose, dict(out=pT[:, k, (c % 2) * P:(c % 2 + 1) * P], in_=xb.ap[:, k * P:(k + 1) * P],
                                               identity=ident_b.ap),
                      reads=[xb, ident_b], writes=pTt if first else [], joins=[] if first else pTt)
            if c % 2 == 1:
                g = c // 4
                off = ((c // 2) % 2) * 256
                for k in range(KD):
                    dst = hT[k][g].ap[:, off:off + 256]
                    if k < 4:
                        Sd.op("act", ACT.activation, dict(out=dst, in_=pT[:, k, :], func=AF.Identity,
                                                          scale=sc1.ap[:, k:k + 1], bias=modP.ap[:, k:k + 1]),
                              reads=[pTt[0], sc1, modP], joins=[hT[k][g]])
                    else:
                        Sd.op("dve", V.tensor_scalar, dict(out=dst, in0=pT[:, k, :], scalar1=sc1.ap[:, k:k + 1],
                                                           scalar2=modP.ap[:, k:k + 1], op0=ALU.mult, op1=ALU.add),
                              reads=[pTt[1], sc1, modP], joins=[hT[k][g]])

        if stop_after == "A2":
            Sd.barrier()
            Sd.dma("sp", dict(out=dbg_d[:, 0:8], in_=sc1.ap), reads=[sc1])
            return finish()
        if stop_after == "A":
            Sd.barrier()
            for k in range(KD):
                for g in range(NG):
                    Sd.dma("sp", dict(out=dbg_d[:, k * S + g * 512:k * S + (g + 1) * 512], in_=hT[k][g].ap), reads=[hT[k][g]])
            return finish()

        w_g = T(A.alloc([KD, 16], BF16, "w_g"))
        Sd.dma("pool", dict(out=w_g.ap, in_=w_in_v[:, :, 2048:2064]), writes=[w_g])
        bI = T(A.alloc([8], F32, "bI"))
        bF = T(A.alloc([8], F32, "bF"))
        Sd.dma("sp", dict(out=bI.ap, in_=b_ig_d.partition_broadcast(P)), writes=[bI])
        Sd.dma("sp", dict(out=bF.ap, in_=b_fg_d.partition_broadcast(P)), writes=[bF])
        Sd.op("dve", V.memset, dict(ap=cst.ap[:, 1:2], constant=1.0), joins=[cst])
        psG = PS[6]
        psG_ap = bank(6).rearrange("p (d j g) -> p d j g", d=2, j=NCH)
        for c in range(NCH):
            for dr in range(2):
                j = c if dr == 0 else NCH - 1 - c
                for k in range(KD):
                    Sd.op("pe", PE.matmul, dict(out=psG_ap[:, dr, j, :], lhsT=hT[k][c // 4].ap[:, (c % 4) * P:(c % 4 + 1) * P],
                                                rhs=w_g.ap[:, k, dr * 8:(dr + 1) * 8], start=(k == 0), stop=(k == KD - 1)),
                          reads=[hT[k][c // 4], w_g], joins=[psG])

        for n in range(4, 12):
            ada_piece(n, piece_order[n])

        def tab(name, n=NCH):
            return T(A.alloc([8, n], F32, name))

        def v4(ap):
            return ap.rearrange("p (d h) j -> p d h j", d=2)

        def fl(ap):
            return ap.rearrange("p a j -> p (a j)")

        LI, ZZ, CS, GP, GB, BT, RR, TMP = (tab(n) for n in ("LI", "ZZ", "CS", "GP", "GB", "BT", "RR", "TMP"))
        MN = tab("MN", NCH + 1)
        Gcol = T(A.alloc([2], F32, "Gcol"))
        Grow = T(A.alloc([256], F32, "Grow"))
        g_i = psG_ap[:, :, :, 0:4].rearrange("p d j h -> p d h j")
        g_f = psG_ap[:, :, :, 4:8].rearrange("p d j h -> p d h j")
        bIb = bI.ap.rearrange("p (d h) -> p d h", d=2).unsqueeze(3).to_broadcast([P, 2, 4, NCH])
        bFb = bF.ap.rearrange("p (d h) -> p d h", d=2).unsqueeze(3).to_broadcast([P, 2, 4, NCH])
        Sd.op("dve", V.tensor_tensor, dict(out=v4(LI.ap), in0=g_i, in1=bIb, op=ALU.add), reads=[psG, bI], writes=[LI])
        Sd.op("dve", V.tensor_tensor, dict(out=v4(ZZ.ap), in0=g_f, in1=bFb, op=ALU.add), reads=[psG, bF], writes=[ZZ])
        Sd.op("act", ACT.activation, dict(out=fl(ZZ.ap), in_=fl(ZZ.ap), func=AF.Exp, scale=-1.0), reads=[ZZ], writes=[ZZ])
        Sd.op("act", ACT.activation, dict(out=fl(ZZ.ap), in_=fl(ZZ.ap), func=AF.Ln, bias=cst.ap[:, 1:2]), reads=[ZZ, cst], writes=[ZZ])
        if stop_after == "G1":
            Sd.barrier()
            for i, t in enumerate((LI, ZZ)):
                Sd.dma("sp", dict(out=dbg_d[:, i * 256:(i + 1) * 256].rearrange("p (a j) -> p a j", a=8), in_=t.ap[:, :, 0:NCH]), reads=[t])
            return finish()
        psC, psC_ap = PS[7], bank(7)[:, 0:256]
        psBt, psBt_ap = PS[4], bank(4)[:, 0:256]
        Sd.op("pe", PE.matmul, dict(out=psC_ap[:, 0:128], lhsT=maskF.ap, rhs=fl(ZZ.ap)[:, 0:128], start=True, stop=True),
              reads=[maskF, ZZ], writes=[psC])
        Sd.op("pe", PE.matmul, dict(out=psC_ap[:, 128:256], lhsT=maskB.ap, rhs=fl(ZZ.ap)[:, 128:256], start=True, stop=True),
              reads=[maskB, ZZ], joins=[psC])
        Sd.op("pe", PE.matmul, dict(out=psBt_ap, lhsT=ones_f.ap, rhs=fl(ZZ.ap), start=True, stop=True),
              reads=[ones_f, ZZ], writes=[psBt])
        Sd.op("act", ACT.activation, dict(out=fl(CS.ap), in_=psC_ap, func=AF.Copy), reads=[psC], writes=[CS])
        if stop_after == "G2a":
            Sd.barrier()
            Sd.dma("sp", dict(out=dbg_d[:, 0:256].rearrange("p (a j) -> p a j", a=8), in_=CS.ap), reads=[CS])
            return finish()
        Sd.op("dve", V.tensor_tensor, dict(out=fl(GP.ap), in0=psC_ap, in1=fl(LI.ap), op=ALU.add), reads=[psC, LI], writes=[GP])
        Sd.op("act", ACT.activation, dict(out=fl(BT.ap), in_=psBt_ap, func=AF.Copy, scale=-1.0), reads=[psBt], writes=[BT])
        if stop_after == "G2":
            Sd.barrier()
            for i, t in enumerate((CS, GP, BT)):
                Sd.dma("sp", dict(out=dbg_d[:, i * 256:(i + 1) * 256].rearrange("p (a j) -> p a j", a=8), in_=t.ap[:, :, 0:NCH]), reads=[t])
            return finish()
        psX, psX_ap = PS[5], bank(5)[:, 0:256]
        psR, psR_ap = PS[2], bank(2)[0:1, 0:256]
        psGB, psGB_ap = PS[3], bank(3)[:, 0:256]
        for hf in range(2):
            Sd.op("pe", PE.transpose, dict(out=psX_ap[:, hf * P:(hf + 1) * P], in_=fl(GP.ap)[:, hf * P:(hf + 1) * P], identity=ident_f.ap),
                  reads=[GP, ident_f], writes=[psX] if hf == 0 else [], joins=[] if hf == 0 else [psX])
        Sd.op("dve", V.tensor_reduce, dict(out=Gcol.ap, in_=psX_ap.rearrange("p (a s) -> p a s", a=2), axis=AX.X, op=ALU.max),
              reads=[psX], writes=[Gcol])
        for hf in range(2):
            Sd.op("pe", PE.transpose, dict(out=psR_ap[:, hf * P:(hf + 1) * P], in_=Gcol.ap[:, hf:hf + 1], identity=ident_f.ap),
                  reads=[Gcol, ident_f], writes=[psR] if hf == 0 else [], joins=[] if hf == 0 else [psR])
        Sd.op("act", ACT.activation, dict(out=Grow.ap[0:1, :], in_=psR_ap, func=AF.Copy), reads=[psR], writes=[Grow])
        Sd.op("pe", PE.matmul, dict(out=psGB_ap, lhsT=ones_f.ap[0:1, :], rhs=Grow.ap[0:1, :], start=True, stop=True),
              reads=[ones_f, Grow], writes=[psGB])
        Sd.op("act", ACT.activation, dict(out=fl(GB.ap), in_=psGB_ap, func=AF.Copy), reads=[psGB], writes=[GB])
        if stop_after == "G3":
            Sd.barrier()
            for i, t in enumerate((GB, GP)):
                Sd.dma("sp", dict(out=dbg_d[:, i * 256:(i + 1) * 256].rearrange("p (a j) -> p a j", a=8), in_=t.ap[:, :, 0:NCH]), reads=[t])
            return finish()
        Sd.op("dve", V.memset, dict(ap=MN.ap[:, :, 0:1], constant=0.0), writes=[MN])
        for dh in range(8):
            Sd.op("dve", V.tensor_tensor_scan, dict(out=MN.ap[:, dh, 1:NCH + 1], data0=GB.ap[:, dh, :], data1=BT.ap[:, dh, :],
                                                    initial=0.0, op0=ALU.max, op1=ALU.add), reads=[GB, BT], joins=[MN])
        Sd.op("dve", V.tensor_tensor, dict(out=RR.ap, in0=MN.ap[:, :, 0:NCH], in1=GB.ap, op=ALU.max), reads=[MN, GB], writes=[RR])
        for src, dst in ((MN, DEC), (GP, WGT), (CS, STAB)):
            sap = src.ap[:, :, 0:NCH]
            Sd.op("dve", V.tensor_tensor, dict(out=TMP.ap, in0=sap, in1=RR.ap, op=ALU.subtract), reads=[src, RR], writes=[TMP])
            Sd.op("act", ACT.activation, dict(out=fl(dst.ap), in_=fl(TMP.ap), func=AF.Exp), reads=[TMP], writes=[dst])

        if stop_after == "G":
            Sd.barrier()
            for i, t in enumerate((WGT, STAB, DEC, MN)):
                Sd.dma("sp", dict(out=dbg_d[:, i * 256:(i + 1) * 256].rearrange("p (a j) -> p a j", a=8), in_=t.ap[:, :, 0:NCH]), reads=[t])
            return finish()

        Sd.barrier()
        A.release(mA)
        hm_ap = A.alloc([NCH, 512], F32, "hm")
        hm = [[T(hm_ap[:, c, h * P:(h + 1) * P]) for h in range(4)] for c in range(NCH)]
        mH = A.mark()
        wqkv = T(A.alloc([KD, 3, P], BF16, "wqkv"))
        qT_ap = A.alloc([S], BF16, "qT")
        kT_ap = A.alloc([S], BF16, "kT")
        qT = [T(qT_ap[:, g * 512:(g + 1) * 512]) for g in range(NG)]
        kT = [T(kT_ap[:, g * 512:(g + 1) * 512]) for g in range(NG)]
        va_ap = A.alloc([NCH, 130], BF16, "va")
        va = [T(va_ap[:, c, :]) for c in range(NCH)]
        kwF_ap = A.alloc([NCH, P], BF16, "kwF")
        kwB_ap = A.alloc([NCH, P], BF16, "kwB")
        kwF = [T(kwF_ap[:, c, :]) for c in range(NCH)]
        kwB = [T(kwB_ap[:, c, :]) for c in range(NCH)]
        sTw = [T(A.alloc([P], BF16, "sTw%d" % i)) for i in range(4)]
        Cst = [T(A.alloc([130], F32, "Cst%d" % i)) for i in range(2)]
        Csb = [[T(A.alloc([130], BF16, "Csb%d%d" % (i, n))) for n in range(3)] for i in range(2)]
        dnr = [T(A.alloc([2], F32, "dnr%d" % i)) for i in range(4)]
        Sd.op("dve", V.memset, dict(ap=va_ap[:, :, 128:130], constant=1.0), writes=va)
        QSCALE = float(P) ** -0.5
        _nheads = int(_os.environ.get('K_NH', 4))

        for h in range(_nheads):
            for i, off in enumerate((0, 512, 1024)):
                Sd.dma("pool", dict(out=wqkv.ap[:, :, i, :], in_=w_in_v[:, :, off + h * P:off + (h + 1) * P]),
                       writes=[wqkv] if i == 0 else [], joins=[] if i == 0 else [wqkv])
            for g in range(NG):
                for which, dstT, bb in ((0, qT, g % 2), (1, kT, 2 + g % 2)):
                    for k in range(KD):
                        Sd.op("pe", PE.matmul, dict(out=bank(bb), lhsT=wqkv.ap[:, k, which, :], rhs=hT[k][g].ap,
                                                    start=(k == 0), stop=(k == KD - 1)),
                              reads=[wqkv, hT[k][g]], writes=[PS[bb]] if k == 0 else [], joins=[] if k == 0 else [PS[bb]])
                    if which == 0:
                        Sd.op("act", ACT.activation, dict(out=dstT[g].ap, in_=bank(bb), func=AF.Copy, scale=QSCALE),
                              reads=[PS[bb]], writes=[dstT[g]])
                    else:
                        Sd.op("dve", V.tensor_copy, dict(out=dstT[g].ap, in_=bank(bb)), reads=[PS[bb]], writes=[dstT[g]])
                for ci in range(4):
                    c = g * 4 + ci
                    bb = 4 + ci
                    for k in range(KD):
                        Sd.op("pe", PE.matmul, dict(out=bank(bb)[:, 0:256], lhsT=hT[k][g].ap[:, ci * P:(ci + 1) * P],
                                                    rhs=wqkv.ap[:, k, 1:3, :].rearrange("p a b -> p (a b)"),
                                                    start=(k == 0), stop=(k == KD - 1)),
                              reads=[wqkv, hT[k][g]], writes=[PS[bb]] if k == 0 else [], joins=[] if k == 0 else [PS[bb]])
                    Sd.op("act", ACT.activation, dict(out=va[c].ap[:, 0:P], in_=bank(bb)[:, P:2 * P], func=AF.Copy),
                          reads=[PS[bb]], joins=[va[c]])
                    Sd.op("dve", V.tensor_scalar, dict(out=kwF[c].ap, in0=bank(bb)[:, 0:P], scalar1=WGT.ap[:, h, c:c + 1],
                                                       scalar2=None, op0=ALU.mult), reads=[PS[bb], WGT], writes=[kwF[c]])
                    jb = NCH - 1 - c
                    Sd.op("dve", V.tensor_scalar, dict(out=kwB[c].ap, in0=bank(bb)[:, 0:P], scalar1=WGT.ap[:, 4 + h, jb:jb + 1],
                                                       scalar2=None, op0=ALU.mult), reads=[PS[bb], WGT], writes=[kwB[c]])
            NI = 2 * NCH
            ND = (2, 3, 6, 7)

            def item(i):
                j, dr = divmod(i, 2)
                c = j if dr == 0 else NCH - 1 - j
                return j, dr, c, dr * 4 + h, c // 4, (c % 4) * P

            def S1(i):
                j, dr, c, dh, g, o = item(i)
                st = sTw[i % 4]
                Sd.op("pe", PE.matmul, dict(out=bank(dr)[:, 0:P], lhsT=kT[g].ap[:, o:o + P], rhs=qT[g].ap[:, o:o + P],
                                            start=True, stop=True), reads=[kT[g], qT[g]], writes=[PS[dr]])
                Sd.op("dve", V.scalar_tensor_tensor, dict(out=st.ap, in0=bank(dr)[:, 0:P], scalar=WGT.ap[:, dh, j:j + 1],
                                                          in1=(maskF if dr == 0 else maskB).ap, op0=ALU.mult, op1=ALU.mult),
                      reads=[PS[dr], WGT, maskF if dr == 0 else maskB], writes=[st])

            def S2(i):
                j, dr, c, dh, g, o = item(i)
                if j >= NCH - 1:
                    return
                kw = kwF if dr == 0 else kwB
                Sd.op("pe", PE.matmul, dict(out=bank(4 + dr)[:, 0:129], lhsT=kw[c].ap, rhs=va[c].ap[:, 0:129],
                                            start=True, stop=True), reads=[kw[c], va[c]], writes=[PS[4 + dr]])
                if j == 0:
                    Sd.op("dve", V.tensor_copy, dict(out=Cst[dr].ap[:, 0:129], in_=bank(4 + dr)[:, 0:129]),
                          reads=[PS[4 + dr]], writes=[Cst[dr]])
                else:
                    Sd.op("dve", V.scalar_tensor_tensor, dict(out=Cst[dr].ap[:, 0:129], in0=Cst[dr].ap[:, 0:129],
                                                              scalar=DEC.ap[:, dh, j:j + 1], in1=bank(4 + dr)[:, 0:129],
                                                              op0=ALU.mult, op1=ALU.add),
                          reads=[Cst[dr], DEC, PS[4 + dr]], writes=[Cst[dr]])

            def S2b(i):
                j, dr, c, dh, g, o = item(i)
                if j >= NCH - 1:
                    return
                Sd.op("act", ACT.activation, dict(out=Csb[dr][(j + 1) % 3].ap[:, 0:129], in_=Cst[dr].ap[:, 0:129],
                                                  func=AF.Copy, scale=DEC.ap[:, dh, j + 1:j + 2]),
                      reads=[Cst[dr], DEC], writes=[Csb[dr][(j + 1) % 3]])

            def S3(i):
                j, dr, c, dh, g, o = item(i)
                bb = ND[i % 4]
                st = sTw[i % 4]
                Sd.op("pe", PE.matmul, dict(out=bank(bb)[:, 0:129], lhsT=st.ap, rhs=va[c].ap[:, 0:129],
                                            start=True, stop=(j == 0)), reads=[st, va[c]], writes=[PS[bb]])
                if j > 0:
                    Sd.op("pe", PE.matmul, dict(out=bank(bb)[:, 0:129], lhsT=qT[g].ap[:, o:o + P],
                                                rhs=Csb[dr][j % 3].ap[:, 0:129], start=False, stop=True),
                          reads=[qT[g], Csb[dr][j % 3]], joins=[PS[bb]])

            def S4(i):
                j, dr, c, dh, g, o = item(i)
                bb = ND[i % 4]
                d_ = dnr[i % 4]
                Sd.op("act", ACT.activation, dict(out=d_.ap[:, 0:1], in_=bank(bb)[:, 128:129], func=AF.Abs),
                      reads=[PS[bb]], writes=[d_])
                Sd.op("dve", V.tensor_scalar, dict(out=d_.ap[:, 0:1], in0=d_.ap[:, 0:1],
                                                   scalar1=STAB.ap[:, dh, j:j + 1], scalar2=None, op0=ALU.max),
                      reads=[d_, STAB], joins=[d_])
                Sd.op("dve", V.reciprocal, dict(out=d_.ap[:, 1:2], in_=d_.ap[:, 0:1]), reads=[d_], joins=[d_])

            def S5(i):
                j, dr, c, dh, g, o = item(i)
                bb = ND[i % 4]
                d_ = dnr[i % 4]
                if j < NCH // 2:
                    Sd.op("act", ACT.activation, dict(out=hm[c][h].ap, in_=bank(bb)[:, 0:P], func=AF.Copy,
                                                      scale=d_.ap[:, 1:2]), reads=[PS[bb], d_], writes=[hm[c][h]])
                else:
                    Sd.op("dve", V.scalar_tensor_tensor, dict(out=hm[c][h].ap, in0=bank(bb)[:, 0:P], scalar=d_.ap[:, 1:2],
                                                              in1=hm[c][h].ap, op0=ALU.mult, op1=ALU.add),
                          reads=[PS[bb], d_, hm[c][h]], writes=[hm[c][h]])

            for t in range(NI + 3):
                if t < NI:
                    S1(t)
                if 0 <= t - 2 < NI:
                    S4(t - 2)
                if 0 <= t - 3 < NI:
                    S5(t - 3)
                if 0 <= t - 1 < NI:
                    S2b(t - 1)
                if 0 <= t - 1 < NI:
                    S3(t - 1)
                if t < NI:
                    S2(t)

        if stop_after == "H":
            Sd.barrier()
            for c in range(NCH):
                Sd.dma("sp", dict(out=dbg_d[c * P:(c + 1) * P, 0:P * _nheads], in_=hm_ap[:, c, 0:P * _nheads]), reads=hm[c])
            return finish()

        Sd.barrier()
        A.release(mH)
        mY = A.mark()
        w_o = T(A.alloc([KD, 512], BF16, "w_o"))
        w_u = T(A.alloc([KD, 512], BF16, "w_u"))
        w_pl = T(A.alloc([4, P], BF16, "w_pl"))
        psw = T(A.alloc([4], F32, "psw"))
        pscl = T(A.alloc([4], F32, "pscl"))
        edge = T(A.alloc([4, 16], F32, "edge"))
        ubuf = T(A.alloc([4, 528], F32, "ubuf"))
        tP = T(A.alloc([4, 528], F32, "tP"))
        tQ = T(A.alloc([2, 528], F32, "tQ"))
        tA = T(A.alloc([4, 512], F32, "tA"))
        mixed = T(A.alloc([4, 512], BF16, "mixed"))
        ucarry = T(A.alloc([4, 8], F32, "ucarry"))
        sig = [T(A.alloc([512], F32, "sig%d" % i)) for i in range(4)]
        sqb = [T(A.alloc([512], F32, "sqb%d" % i)) for i in range(2)]
        ssh = T(A.alloc([16], F32, "ssh"))
        rsh = T(A.alloc([16], F32, "rsh"))
        yab = [T(A.alloc([512], BF16, "ya%d" % i)) for i in range(2)]
        Sd.dma("pool", dict(out=w_o.ap, in_=w_in_v[:, :, 1536:2048]), writes=[w_o])
        Sd.dma("pool", dict(out=w_u.ap, in_=w_in_v[:, :, 2064:2576]), writes=[w_u])
        Sd.dma("pool", dict(out=w_pl.ap, in_=w_pool_d[:, :, :]), writes=[w_pl])
        Sd.dma("sp", dict(out=pscl.ap, in_=psclP_d[:, :]), writes=[pscl])
        Sd.dma("sp", dict(out=edge.ap.rearrange("p a b -> p (a b)"), in_=edge_d.partition_broadcast(P)), writes=[edge])
        for pg, w in enumerate(POOL_W):
            Sd.op("dve", V.tensor_scalar, dict(out=psw.ap[:, pg:pg + 1], in0=pscl.ap[:, pg:pg + 1], scalar1=1.0 / w, scalar2=None,
                                               op0=ALU.mult), reads=[pscl], joins=[psw])
        Sd.op("dve", V.memset, dict(ap=cst.ap[:, 2:3], constant=EPS), joins=[cst])
        ptr_ap = bank(6, 2).bitcast(BF16).rearrange("p (m t) -> p m t", m=4)

        def stage_U(G):
                U = ubuf.ap
                for pg in range(4):
                    bb = pg % 2
                    for k in range(KD):
                        Sd.op("pe", PE.matmul, dict(out=bank(bb), lhsT=w_u.ap[:, k, pg * P:(pg + 1) * P], rhs=hT[k][G].ap,
                                                    start=(k == 0), stop=(k == KD - 1)),
                              reads=[w_u, hT[k][G]], writes=[PS[bb]] if k == 0 else [], joins=[] if k == 0 else [PS[bb]])
                    Sd.op("act", ACT.activation, dict(out=ubuf.ap[:, pg, 8:520], in_=bank(bb), func=AF.Copy),
                          reads=[PS[bb]], writes=[ubuf] if pg == 0 else [], joins=[] if pg == 0 else [ubuf])
                if G < NG - 1:
                    for pg in range(4):
                        for k in range(KD):
                            Sd.op("pe", PE.matmul, dict(out=bank(2)[:, pg * 8:(pg + 1) * 8], lhsT=w_u.ap[:, k, pg * P:(pg + 1) * P],
                                                        rhs=hT[k][G + 1].ap[:, 0:8], start=(k == 0), stop=(k == KD - 1)),
                                  reads=[w_u, hT[k][G + 1]], writes=[PS[2]] if (k == 0 and pg == 0) else [],
                                  joins=[] if (k == 0 and pg == 0) else [PS[2]])
                    Sd.op("act", ACT.activation, dict(out=ubuf.ap[:, :, 520:528], in_=bank(2)[:, 0:32].rearrange("p (a b) -> p a b", a=4),
                                                      func=AF.Copy), reads=[PS[2]], joins=[ubuf])
                else:
                    Sd.op("pool", POOL.memset, dict(ap=ubuf.ap[:, :, 520:528], constant=0.0), joins=[ubuf])
                if G == 0:
                    Sd.op("pool", POOL.memset, dict(ap=ubuf.ap[:, :, 0:8], constant=0.0), joins=[ubuf])
                else:
                    Sd.op("pool", POOL.tensor_copy, dict(out=ubuf.ap[:, :, 0:8], in_=ucarry.ap), reads=[ucarry], joins=[ubuf])
                Sd.op("pool", POOL.tensor_copy, dict(out=ucarry.ap, in_=ubuf.ap[:, :, 512:520]), reads=[ubuf], writes=[ucarry])
                U = ubuf.ap
                Sd.op("pool", POOL.tensor_tensor, dict(out=tP.ap[:, :, 0:527], in0=U[:, :, 0:527], in1=U[:, :, 1:528], op=ALU.add),
                      reads=[ubuf], writes=[tP])
                Sd.op("pool", POOL.tensor_tensor, dict(out=tQ.ap[:, 0:2, 0:525], in0=tP.ap[:, 2:4, 0:525], in1=tP.ap[:, 2:4, 2:527], op=ALU.add),
                      reads=[tP], writes=[tQ])
                Sd.op("pool", POOL.tensor_tensor, dict(out=tP.ap[:, 3, 0:521], in0=tQ.ap[:, 1, 0:521], in1=tQ.ap[:, 1, 4:525], op=ALU.add),
                      reads=[tQ, tP], joins=[tP])
                Sd.op("pool", POOL.tensor_copy, dict(out=tA.ap[:, 0, :], in_=tP.ap[:, 0, 7:519]), reads=[tP], writes=[tA])
                Sd.op("pool", POOL.tensor_tensor, dict(out=tA.ap[:, 1, :], in0=tP.ap[:, 1, 6:518], in1=tP.ap[:, 1, 8:520], op=ALU.add),
                      reads=[tP], joins=[tA])
                Sd.op("pool", POOL.tensor_tensor, dict(out=tA.ap[:, 2, :], in0=tQ.ap[:, 0, 4:516], in1=tQ.ap[:, 0, 8:520], op=ALU.add),
                      reads=[tQ], joins=[tA])
                Sd.op("pool", POOL.tensor_tensor, dict(out=tA.ap[:, 3, :], in0=tP.ap[:, 3, 0:512], in1=tP.ap[:, 3, 8:520], op=ALU.add),
                      reads=[tP], joins=[tA])
                if G == 0:
                    Sd.op("pool", POOL.tensor_tensor, dict(out=tA.ap[:, :, 0:8], in0=tA.ap[:, :, 0:8], in1=edge.ap[:, :, 0:8], op=ALU.mult),
                          reads=[tA, edge], joins=[tA])
                if G == NG - 1:
                    Sd.op("pool", POOL.tensor_tensor, dict(out=tA.ap[:, :, 504:512], in0=tA.ap[:, :, 504:512], in1=edge.ap[:, :, 8:16], op=ALU.mult),
                          reads=[tA, edge], joins=[tA])
        def stage_O(G):
                for i in range(4):
                    c = G * 4 + i
                    bb = 3 + i % 2
                    for k in range(KD):
                        Sd.op("pe", PE.matmul, dict(out=bank(bb), lhsT=hT[k][G].ap[:, i * P:(i + 1) * P], rhs=w_o.ap[:, k, :],
                                                    start=(k == 0), stop=(k == KD - 1)),
                              reads=[w_o, hT[k][G]], writes=[PS[bb]] if k == 0 else [], joins=[] if k == 0 else [PS[bb]])
                    Sd.op("act", ACT.activation, dict(out=sig[i].ap, in_=bank(bb), func=AF.Sigmoid), reads=[PS[bb]], writes=[sig[i]])
                    sq = sqb[i % 2]
                    Sd.op("dve", V.tensor_tensor, dict(out=sq.ap, in0=hm_ap[:, c, :], in1=hm_ap[:, c, :], op=ALU.mult),
                          reads=hm[c], writes=[sq])
                    Sd.op("dve", V.tensor_reduce, dict(out=ssh.ap[:, i * 4:(i + 1) * 4], in_=sq.ap.rearrange("p (a b) -> p a b", a=4),
                                                       axis=AX.X, op=ALU.add), reads=[sq], writes=[ssh] if i == 0 else [],
                          joins=[] if i == 0 else [ssh])
                Sd.op("act", ACT.activation, dict(out=rsh.ap, in_=ssh.ap, func=AF.Ln, scale=1.0 / P, bias=cst.ap[:, 2:3]),
                      reads=[ssh, cst], writes=[rsh])
                Sd.op("act", ACT.activation, dict(out=rsh.ap, in_=rsh.ap, func=AF.Exp, scale=-0.5), reads=[rsh], writes=[rsh])
        def stage_G(G):
                for i in range(4):
                    c = G * 4 + i
                    ya = yab[i % 2]
                    for h in range(4):
                        Sd.op("dve", V.scalar_tensor_tensor, dict(out=ya.ap[:, h * P:(h + 1) * P], in0=hm[c][h].ap,
                                                                  scalar=rsh.ap[:, i * 4 + h:i * 4 + h + 1], in1=sig[i].ap[:, h * P:(h + 1) * P],
                                                                  op0=ALU.mult, op1=ALU.mult),
                              reads=[hm[c][h], rsh, sig[i]], writes=[ya] if h == 0 else [], joins=[] if h == 0 else [ya])
                    for h in range(4):
                        first = (i == 0 and h == 0)
                        Sd.op("pe", PE.transpose, dict(out=ptr_ap[:, h, i * P:(i + 1) * P], in_=ya.ap[:, h * P:(h + 1) * P], identity=ident_b.ap),
                              reads=[ya, ident_b], writes=[PS[6], PS[7]] if first else [], joins=[] if first else [PS[6], PS[7]])
                for m in range(4):
                    if m < 2:
                        Sd.op("act", ACT.activation, dict(out=hT[m][G].ap, in_=ptr_ap[:, m, :], func=AF.Copy), reads=[PS[6]], writes=[hT[m][G]])
                    else:
                        Sd.op("dve", V.tensor_copy, dict(out=hT[m][G].ap, in_=ptr_ap[:, m, :]), reads=[PS[7]], writes=[hT[m][G]])

        def stage_M(G):
                U = ubuf.ap
                for pg, w in enumerate(POOL_W):
                    Sd.op("dve", V.scalar_tensor_tensor, dict(out=mixed.ap[:, pg, :], in0=U[:, pg, 8:520], scalar=-float(w), in1=tA.ap[:, pg, :],
                                                              op0=ALU.mult, op1=ALU.add),
                          reads=[ubuf, tA], writes=[mixed] if pg == 0 else [], joins=[] if pg == 0 else [mixed])
                for pg in range(4):
                    Sd.op("pe", PE.matmul, dict(out=bank(5), lhsT=w_pl.ap[:, pg, :], rhs=mixed.ap[:, pg, :], start=True, stop=True),
                          reads=[w_pl, mixed], writes=[PS[5]])
                    Sd.op("act", ACT.activation, dict(out=hT[4 + pg][G].ap, in_=bank(5), func=AF.Copy, scale=psw.ap[:, pg:pg + 1]),
                          reads=[PS[5], psw], writes=[hT[4 + pg][G]])


        stage_U(0)
        for G in range(NG):
            stage_O(G)
            stage_M(G)
            if G + 1 < NG:
                stage_U(G + 1)
            stage_G(G)

        if stop_after == "Y":
            Sd.barrier()
            for k in range(KD):
                for g in range(NG):
                    Sd.dma("sp", dict(out=dbg_d[:, k * S + g * 512:k * S + (g + 1) * 512], in_=hT[k][g].ap), reads=[hT[k][g]])
            return finish()

        Sd.barrier()
        A.release(mA)
        w_outp = T(A.alloc([KD, D], BF16, "w_outp"))
        ghP = T(A.alloc([4], F32, "ghP"))
        gfinB = T(A.alloc([D], F32, "gfinB"))
        x1_ap = A.alloc([8, D], F32, "x1")
        x1 = [T(x1_ap[:, i, :]) for i in range(8)]
        aT_ap = A.alloc([8, 2, 512], BF16, "aT")
        aT = [[T(aT_ap[:, fc, tg, :]) for tg in range(2)] for fc in range(8)]
        w1s = [T(A.alloc([KD, 512], BF16, "w1s%d" % i)) for i in range(3)]
        w2s = [T(A.alloc([4, D], BF16, "w2s%d" % i)) for i in range(3)]
        xn2 = [T(A.alloc([D], BF16, "xn2%d" % i)) for i in range(2)]
        rr = [T(A.alloc([512], F32, "rr%d" % i)) for i in range(2)]
        st2 = [T(A.alloc([4], F32, "st2%d" % i)) for i in range(4)]
        junk2 = A.alloc([D], BF16, "junk2")
        stg = [x1[0], x1[1]]
        Sd.dma("sp", dict(out=ghP.ap, in_=g_headP_d[:, :]), writes=[ghP])
        Sd.dma("sp", dict(out=gfinB.ap, in_=g_finR_d.partition_broadcast(P)), writes=[gfinB])
        for k in range(KD):
            sg = stg[k % 2]
            Sd.dma("sp", dict(out=sg.ap, in_=w_out_v[:, k, :]), writes=[sg])
            if k < 4:
                Sd.op("dve", V.scalar_tensor_tensor, dict(out=w_outp.ap[:, k, :], in0=sg.ap, scalar=ghP.ap[:, k:k + 1], in1=gt1B.ap,
                                                          op0=ALU.mult, op1=ALU.mult), reads=[sg, ghP, gt1B], joins=[w_outp])
            else:
                Sd.op("dve", V.tensor_tensor, dict(out=w_outp.ap[:, k, :], in0=sg.ap, in1=gt1B.ap, op=ALU.mult),
                      reads=[sg, gt1B], joins=[w_outp])
        NT4 = 4
        w1_blocks = [(Tt, q, bl) for Tt in range(NT4) for q in range(4) for bl in range(2)]
        gt2b4 = gt2B.ap.unsqueeze(1).to_broadcast([P, 4, D])

        def load_w1(n):
            if n >= len(w1_blocks):
                return
            Tt, q, bl = w1_blocks[n]
            c0 = q * 1024 + bl * 512
            Sd.dma("pool", dict(out=w1s[n % 3].ap, in_=w_ff1_v[:, :, c0:c0 + 512]), writes=[w1s[n % 3]])

        def load_w2(n):
            if n >= len(w1_blocks):
                return
            Tt, q, bl = w1_blocks[n]
            f0 = q * 8 + bl * 4
            t = w2s[n % 3]
            Sd.dma("pool", dict(out=t.ap, in_=w_ff2_v[:, f0:f0 + 4, :]), writes=[t])
            Sd.op("pool", POOL.tensor_tensor, dict(out=t.ap, in0=t.ap, in1=gt2b4, op=ALU.mult), reads=[t, gt2B], writes=[t])

        load_w1(0)
        load_w1(1)
        load_w2(0)
        load_w2(1)
        ps01 = ps_all[:, 0:1024]
        ps67 = ps_all[:, 3072:4096]
        psT2_ap = bank(2, 2).bitcast(BF16).rearrange("p (k t) -> p k t", k=KD)
        def final_norm_store(Tt, st_):
            c = Tt * 8 + st_
            s2 = st2[st_ % 4]
            Sd.op("act", ACT.activation, dict(out=junk2, in_=x1[st_].ap, func=AF.Square, accum_out=s2.ap[:, 0:1]),
                  reads=[x1[st_]], writes=[s2])
            Sd.op("act", ACT.activation, dict(out=s2.ap[:, 1:2], in_=s2.ap[:, 0:1], func=AF.Ln, scale=1.0 / D, bias=cst.ap[:, 0:1]),
                  reads=[s2, cst], joins=[s2])
            Sd.op("act", ACT.activation, dict(out=s2.ap[:, 2:3], in_=s2.ap[:, 1:2], func=AF.Exp, scale=-0.5), reads=[s2], joins=[s2])
            Sd.op("dve", V.scalar_tensor_tensor, dict(out=x1[st_].ap, in0=x1[st_].ap, scalar=s2.ap[:, 2:3], in1=gfinB.ap,
                                                      op0=ALU.mult, op1=ALU.mult), reads=[x1[st_], s2, gfinB], writes=[x1[st_]])
            Sd.dma("sp", dict(out=out_v[c], in_=x1[st_].ap), reads=[x1[st_]])
            if Tt + 1 < NT4:
                Sd.dma("sp", dict(out=x1[st_].ap, in_=x_v[(Tt + 1) * 8 + st_]), writes=[x1[st_]])

        blk = 0
        for Tt in range(NT4):
            def stage_a(st_):
                c = Tt * 8 + st_
                g, o = c // 4, (c % 4) * P
                if Tt == 0:
                    Sd.dma("sp", dict(out=x1[st_].ap, in_=x_v[c]), writes=[x1[st_]])
                pb, pt = ((0, 1), ps01) if st_ % 2 == 0 else ((6, 7), ps67)
                for half in range(2):
                    bb = pb[half]
                    for k in range(KD):
                        Sd.op("pe", PE.matmul, dict(out=bank(bb), lhsT=hT[k][g].ap[:, o:o + P], rhs=w_outp.ap[:, k, half * 512:(half + 1) * 512],
                                                    start=(k == 0), stop=(k == KD - 1)),
                              reads=[hT[k][g], w_outp], writes=[PS[bb]] if k == 0 else [], joins=[] if k == 0 else [PS[bb]])
                Sd.op("dve", V.tensor_tensor, dict(out=x1[st_].ap, in0=pt, in1=x1[st_].ap, op=ALU.add),
                      reads=[PS[pb[0]], PS[pb[1]], x1[st_]], writes=[x1[st_]])

            def stage_b(st_):
                c = Tt * 8 + st_
                g = c // 4
                s2 = st2[st_ % 4]
                xb = xn2[st_ % 2]
                Sd.op("act", ACT.activation, dict(out=junk2, in_=x1[st_].ap, func=AF.Square, accum_out=s2.ap[:, 0:1]),
                      reads=[x1[st_]], writes=[s2])
                Sd.op("act", ACT.activation, dict(out=s2.ap[:, 1:2], in_=s2.ap[:, 0:1], func=AF.Ln, scale=1.0 / D, bias=cst.ap[:, 0:1]),
                      reads=[s2, cst], joins=[s2])
                Sd.op("act", ACT.activation, dict(out=s2.ap[:, 2:3], in_=s2.ap[:, 1:2], func=AF.Exp, scale=-0.5), reads=[s2], joins=[s2])
                Sd.op("dve", V.tensor_scalar, dict(out=xb.ap, in0=x1[st_].ap, scalar1=s2.ap[:, 2:3], scalar2=None, op0=ALU.mult),
                      reads=[x1[st_], s2], writes=[xb])
                for k in range(KD):
                    first = (st_ % 2 == 0 and k == 0)
                    Sd.op("pe", PE.transpose, dict(out=psT2_ap[:, k, (st_ % 2) * P:(st_ % 2 + 1) * P], in_=xb.ap[:, k * P:(k + 1) * P],
                                                   identity=ident_b.ap),
                          reads=[xb, ident_b], writes=[PS[2], PS[3]] if first else [], joins=[] if first else [PS[2], PS[3]])
                if st_ % 2 == 1:
                    off = ((c // 2) % 2) * 256
                    for k in range(KD):
                        dst = hT[k][g].ap[:, off:off + 256]
                        if k < 4:
                            Sd.op("act", ACT.activation, dict(out=dst, in_=psT2_ap[:, k, :], func=AF.Identity,
                                                              scale=sc2.ap[:, k:k + 1], bias=modP.ap[:, 24 + k:25 + k]),
                                  reads=[PS[2], sc2, modP], joins=[hT[k][g]])
                        else:
                            Sd.op("dve", V.tensor_scalar, dict(out=dst, in0=psT2_ap[:, k, :], scalar1=sc2.ap[:, k:k + 1],
                                                               scalar2=modP.ap[:, 24 + k:25 + k], op0=ALU.mult, op1=ALU.add),
                                  reads=[PS[3], sc2, modP], joins=[hT[k][g]])

            for st_ in range(9):
                if st_ < 8:
                    stage_a(st_)
                if st_ >= 1:
                    stage_b(st_ - 1)
            for q in range(4):
                for bl in range(2):
                    n = blk + bl
                    load_w1(n + 2)
                    wt = w1s[n % 3]
                    for f4 in range(4):
                        fc = bl * 4 + f4
                        for tg in range(2):
                            bb = 4 + (fc * 2 + tg) % 2
                            for k in range(KD):
                                Sd.op("pe", PE.matmul, dict(out=bank(bb), lhsT=wt.ap[:, k, f4 * P:(f4 + 1) * P], rhs=hT[k][Tt * 2 + tg].ap,
                                                            start=(k == 0), stop=(k == KD - 1)),
                                      reads=[wt, hT[k][Tt * 2 + tg]], writes=[PS[bb]] if k == 0 else [], joins=[] if k == 0 else [PS[bb]])
                            r_ = rr[(fc * 2 + tg) % 2]
                            Sd.op("act", ACT.activation, dict(out=r_.ap, in_=bank(bb), func=AF.Relu), reads=[PS[bb]], writes=[r_])
                            Sd.op("dve", V.tensor_tensor, dict(out=aT[fc][tg].ap, in0=r_.ap, in1=r_.ap, op=ALU.mult),
                                  reads=[r_], writes=[aT[fc][tg]])
                load_w2(blk + 2)
                for st_ in range(8):
                    tg, o = st_ // 4, (st_ % 4) * P
                    pb, pt = ((0, 1), ps01) if st_ % 2 == 0 else ((6, 7), ps67)
                    for half in range(2):
                        bb = pb[half]
                        for fc in range(8):
                            wt = w2s[(blk + fc // 4) % 3]
                            Sd.op("pe", PE.matmul, dict(out=bank(bb), lhsT=aT[fc][tg].ap[:, o:o + P], rhs=wt.ap[:, fc % 4, half * 512:(half + 1) * 512],
                                                        start=(fc == 0), stop=(fc == 7)),
                                  reads=[aT[fc][tg], wt], writes=[PS[bb]] if fc == 0 else [], joins=[] if fc == 0 else [PS[bb]])
                    Sd.op("dve", V.tensor_tensor, dict(out=x1[st_].ap, in0=pt, in1=x1[st_].ap, op=ALU.add),
                          reads=[PS[pb[0]], PS[pb[1]], x1[st_]], writes=[x1[st_]])
                    if q == 3:
                        final_norm_store(Tt, st_)
                load_w2(blk + 3)
                blk += 2
        return finish()


def _consts():
    s = np.arange(P)
    maskF = (s[:, None] <= s[None, :]).astype(np.float32)
    maskB = (s[:, None] >= s[None, :]).astype(np.float32)
    edge = np.ones((4, 16), np.float32)
    for g, w in enumerate(POOL_W):
        for t in range(8):
            lo = max(t - w // 2, 0)
            hi = min(t + w // 2, S)
            edge[g, t] = w / float(hi - lo)
            tt = S - 8 + t
            lo = max(tt - w // 2, 0)
            hi = min(tt + w // 2, S)
            edge[g, 8 + t] = w / float(hi - lo)
    return dict(
        ident_f=np.eye(P, dtype=np.float32),
        ident_b=np.eye(P).astype(ml_dtypes.bfloat16),
        maskF=maskF, maskB=maskB,
        ones_f=np.ones((P, P), np.float32),
        edge_fac=edge.reshape(1, 64),
    )


def make_in_maps(x, c, w_ada, b_ada, g_mix, w_in, b_igate, b_fgate, g_head, w_pool, pool_scale,
                 w_out, g_ffn, w_ff1, w_ff2, g_final):
    f = lambda a: np.ascontiguousarray(np.asarray(a, dtype=np.float32))
    cs = _consts()
    shared = dict(
        w_ada=f(w_ada[0]),
        b_adaP=f(np.asarray(b_ada[0]).reshape(48, P).T),
        b_adaR=f(np.asarray(b_ada[0]).reshape(1, 6 * D)),
        g_mixP=f(np.asarray(g_mix[0]).reshape(KD, P).T),
        g_ffnP=f(np.asarray(g_ffn[0]).reshape(KD, P).T),
        g_finR=f(np.asarray(g_final).reshape(1, D)),
        w_in=f(w_in[0]),
        b_ig=f(np.asarray(b_igate[0]).reshape(1, 8)),
        b_fg=f(np.asarray(b_fgate[0]).reshape(1, 8)),
        g_headP=f(np.asarray(g_head[0]).reshape(4, P).T),
        pool_scaleP=f(np.asarray(pool_scale[0]).reshape(4, P).T),
        w_pool=f(np.asarray(w_pool[0]).transpose(1, 0, 2)),
        w_out=f(w_out[0]), w_ff1=f(w_ff1[0]), w_ff2=f(w_ff2[0]),
        **cs,
    )
    maps = []
    for b in range(N_CORES):
        m = dict(shared)
        m["x"] = f(x[b])
        m["cT"] = f(np.asarray(c[b]).reshape(KD, P).T)
        maps.append(m)
    return maps


_CACHE = {}


def kernel(**inputs):
    if "nc" not in _CACHE:
        _CACHE["nc"] = build_program()[0]
    nc = _CACHE["nc"]
    in_maps = make_in_maps(**inputs)
    res = run_bass_kernel_spmd(nc, in_maps, core_ids=list(range(N_CORES)))
    out = np.stack([np.asarray(r["out"]) for r in res.results], axis=0)
    return out.astype(np.float32)
```

```python
import numpy as np
import ml_dtypes
from contextlib import ExitStack

import concourse.bass as bass
import concourse.mybir as mybir
from concourse.bass_utils import run_bass_kernel_spmd

F32 = mybir.dt.float32
BF16 = mybir.dt.bfloat16
AF = mybir.ActivationFunctionType
ALU = mybir.AluOpType
AX = mybir.AxisListType

P = 128
S = 4096
D = 1024
KD = 8
NCH = 32
NG = 8
DFF = 4096
EPS = 1e-6
POOL_W = (2, 4, 8, 16)
N_CORES = 8


class T:
    __slots__ = ("ap", "w", "r", "dsem", "dcount", "name", "excl")

    def __init__(self, ap, name="", excl=False):
        self.ap = ap
        self.excl = excl
        self.w = []
        self.r = []
        self.dsem = None
        self.dcount = 0
        self.name = name

    def __getitem__(self, k):
        return self.ap[k]


class _Op:
    __slots__ = ("eng", "idx", "method", "kwargs", "waits", "marked", "dma_inc", "rank")


class Sched:
    ENG = ("pe", "act", "dve", "pool", "sp")

    def __init__(self, nc, es):
        self.nc = nc
        self.es = es
        self.h = {"pe": nc.tensor, "act": nc.scalar, "dve": nc.vector, "pool": nc.gpsimd, "sp": nc.sync}
        self.sem = {e: es.enter_context(nc.semaphore("s_" + e)) for e in ("pe", "act", "dve", "pool")}
        self.ops = []
        self.eng_ops = {e: [] for e in self.ENG}
        self.waited_c = {e: {x: -1 for x in self.ENG} for e in self.ENG}
        self.waited_d = {e: {} for e in self.ENG}
        self.dsems = []
        self.nsem = 0

    def _collect(self, eng, reads, writes, joins):
        toks = []
        for t in reads:
            toks.extend(t.w)
            if t.excl:
                toks.extend(x for x in t.r if x[0] == "c" and x[1].eng != eng)
        for t in writes:
            toks.extend(t.w)
            toks.extend(t.r)
        for t in joins:
            toks.extend(t.r)
        waits = []
        wc = self.waited_c[eng]
        wd = self.waited_d[eng]
        for tk in toks:
            if tk[0] == "c":
                d = tk[1]
                if d.eng == eng and eng == "pe":
                    continue
                if wc[d.eng] >= d.idx:
                    continue
                wc[d.eng] = d.idx
                d.marked = True
                waits.append(tk)
            else:
                _, sem, val = tk
                k = id(sem)
                if wd.get(k, 0) >= val:
                    continue
                wd[k] = val
                waits.append(tk)
        return waits

    def _post(self, tok, reads, writes, joins):
        for t in writes:
            t.w = [tok]
            t.r = []
        def same(x):
            if x[0] != tok[0]:
                return False
            if tok[0] == "c":
                return x[1].eng == tok[1].eng
            return x[1] is tok[1]
        for t in joins:
            t.w = [x for x in t.w if not same(x)] + [tok]
        for t in reads:
            t.r = [x for x in t.r if not same(x)] + [tok]

    def op(self, eng, method, kwargs, reads=(), writes=(), joins=()):
        o = _Op()
        o.eng = eng
        o.idx = len(self.eng_ops[eng])
        o.method = method
        o.kwargs = kwargs
        o.marked = False
        o.dma_inc = None
        o.waits = self._collect(eng, reads, writes, joins)
        self.eng_ops[eng].append(o)
        self.ops.append(o)
        self._post(("c", o), reads, writes, joins)
        return o

    def _dsem(self, t):
        if t.dsem is None:
            t.dsem = self.es.enter_context(self.nc.semaphore("d%d" % self.nsem))
            self.nsem += 1
            self.dsems.append(t)
        return t.dsem

    def dma(self, q, kwargs, reads=(), writes=(), joins=()):
        o = _Op()
        o.eng = q
        o.idx = -1
        o.method = self.h[q].dma_start
        o.kwargs = kwargs
        o.marked = False
        o.waits = self._collect(q, reads, writes, joins)
        owner = (list(writes) + list(joins) + list(reads))[0]
        sem = self._dsem(owner)
        owner.dcount += 16
        o.dma_inc = (sem, owner.dcount)
        self.ops.append(o)
        self._post(("d", sem, owner.dcount), reads, writes, joins)
        return o

    def barrier(self):
        for e in self.ENG:
            o = _Op()
            o.eng = e
            o.idx = -1
            o.method = None
            o.kwargs = None
            o.marked = False
            o.dma_inc = None
            waits = []
            for x in ("pe", "act", "dve", "pool"):
                if self.eng_ops[x]:
                    d = self.eng_ops[x][-1]
                    if x == e or self.waited_c[e][x] >= d.idx:
                        continue
                    self.waited_c[e][x] = d.idx
                    d.marked = True
                    waits.append(("c", d))
            for t in self.dsems:
                k = id(t.dsem)
                if self.waited_d[e].get(k, 0) < t.dcount:
                    self.waited_d[e][k] = t.dcount
                    waits.append(("d", t.dsem, t.dcount))
            o.waits = waits
            self.ops.append(o)

    def emit(self):
        for e in ("pe", "act", "dve", "pool"):
            r = 0
            for o in self.eng_ops[e]:
                if o.marked:
                    r += 1
                o.rank = r
        nw = 0
        for o in self.ops:
            h = self.h[o.eng]
            for tk in o.waits:
                if tk[0] == "c":
                    h.wait_ge(self.sem[tk[1].eng], tk[1].rank)
                else:
                    h.wait_ge(tk[1], tk[2])
                nw += 1
            if o.method is None:
                continue
            ins = o.method(**o.kwargs)
            if o.dma_inc is not None:
                ins.then_inc(o.dma_inc[0], 16)
            elif o.marked:
                ins.then_inc(self.sem[o.eng], 1)
        self.stats = {e: len(self.eng_ops[e]) for e in self.ENG}
        self.stats["waits"] = nw
        self.stats["ops"] = len(self.ops)


class Arena:
    def __init__(self, base_ap_f32, nbytes):
        self.base = base_ap_f32
        self.n = nbytes
        self.off = 0
        self.peak = 0

    def mark(self):
        return self.off

    def release(self, m):
        self.off = m

    def alloc(self, free_shape, dtype, name=""):
        esz = 2 if dtype == BF16 else 4
        n = int(np.prod(free_shape))
        nb = (n * esz + 63) // 64 * 64
        assert self.off + nb <= self.n, "SBUF arena overflow at %s: %d + %d > %d" % (name, self.off, nb, self.n)
        ap = self.base[:, self.off // 4:(self.off + nb) // 4]
        if dtype == BF16:
            ap = ap.bitcast(BF16)
        ap = ap[:, 0:n]
        if len(free_shape) == 2:
            ap = ap.rearrange("p (a b) -> p a b", a=free_shape[0])
        elif len(free_shape) == 3:
            ap = ap.rearrange("p (a b c) -> p a b c", a=free_shape[0], b=free_shape[1])
        self.off += nb
        self.peak = max(self.peak, self.off)
        return ap


def build_program(stop_after=None, dbg=None, dbg_dt=None):
    nc = bass.Bass("TRN2", target_bir_lowering=False)

    def din(name, shape, dt=F32):
        return nc.dram_tensor(name, list(shape), dt, kind="ExternalInput").ap()

    x_d = din("x", [S, D])
    cT_d = din("cT", [P, KD])
    w_ada_d = din("w_ada", [D, 6 * D])
    b_adaP_d = din("b_adaP", [P, 48])
    b_adaR_d = din("b_adaR", [1, 6 * D])
    g_mixP_d = din("g_mixP", [P, KD])
    g_ffnP_d = din("g_ffnP", [P, KD])
    g_finR_d = din("g_finR", [1, D])
    w_in_d = din("w_in", [D, 2576])
    b_ig_d = din("b_ig", [1, 8])
    b_fg_d = din("b_fg", [1, 8])
    g_headP_d = din("g_headP", [P, 4])
    psclP_d = din("pool_scaleP", [P, 4])
    w_pool_d = din("w_pool", [P, 4, P])
    w_out_d = din("w_out", [D, D])
    w_ff1_d = din("w_ff1", [D, DFF])
    w_ff2_d = din("w_ff2", [DFF, D])
    ident_f_d = din("ident_f", [P, P])
    ident_b_d = din("ident_b", [P, P], BF16)
    maskF_d = din("maskF", [P, P])
    maskB_d = din("maskB", [P, P])
    ones_d = din("ones_f", [P, P])
    edge_d = din("edge_fac", [1, 64])
    out_d = nc.dram_tensor("out", [S, D], F32, kind="ExternalOutput").ap()
    dbg_d = None
    if dbg is not None:
        dbg_d = nc.dram_tensor("dbg", list(dbg), dbg_dt or F32, kind="ExternalOutput").ap()

    w_ada_v = w_ada_d.rearrange("(k p) n -> p k n", p=P)
    w_in_v = w_in_d.rearrange("(k p) n -> p k n", p=P)
    w_out_v = w_out_d.rearrange("(k p) n -> p k n", p=P)
    w_ff1_v = w_ff1_d.rearrange("(k p) n -> p k n", p=P)
    w_ff2_v = w_ff2_d.rearrange("(k p) n -> p k n", p=P)
    x_v = x_d.rearrange("(c p) d -> c p d", p=P)
    out_v = out_d.rearrange("(c p) d -> c p d", p=P)

    SB_BYTES = 207 * 1024
    es = ExitStack()
    with es:
        sb_all = es.enter_context(nc.sbuf_tensor("sb_all", [P, SB_BYTES // 4], F32))
        ps_all = es.enter_context(nc.psum_tensor("ps_all", [P, 4096], F32))
        es.enter_context(nc.Block())
        Sd = Sched(nc, es)
        A = Arena(sb_all[:], SB_BYTES)
        V, ACT, PE, POOL = nc.vector, nc.scalar, nc.tensor, nc.gpsimd

        def bank(b, n=1):
            return ps_all[:, b * 512:(b + n) * 512]

        PS = [T(bank(b), "bank%d" % b, excl=True) for b in range(8)]

        hT_ap = A.alloc([KD, S], BF16, "hT")
        hT = [[T(hT_ap[:, k, g * 512:(g + 1) * 512], "hT%d_%d" % (k, g)) for g in range(NG)] for k in range(KD)]
        ident_b = T(A.alloc([P], BF16, "ident_b"))
        ident_f = T(A.alloc([P], F32, "ident_f"))
        maskF = T(A.alloc([P], F32, "maskF"))
        maskB = T(A.alloc([P], F32, "maskB"))
        ones_f = T(A.alloc([P], F32, "ones"))
        cst = T(A.alloc([64], F32, "cst_misc"))
        modP = T(A.alloc([48], F32, "modP"))
        sc1 = T(A.alloc([KD], F32, "sc1"))
        sc2 = T(A.alloc([KD], F32, "sc2"))
        gt1B = T(A.alloc([D], F32, "gt1B"))
        gt2B = T(A.alloc([D], F32, "gt2B"))
        WGT = T(A.alloc([8, NCH], F32, "WGT"))
        STAB = T(A.alloc([8, NCH], F32, "STAB"))
        DEC = T(A.alloc([8, NCH], F32, "DEC"))

        Sd.op("dve", V.memset, dict(ap=cst.ap[:, 0:1], constant=EPS), writes=[cst])
        for t, d in ((ident_b, ident_b_d), (ident_f, ident_f_d), (maskF, maskF_d), (maskB, maskB_d), (ones_f, ones_d)):
            Sd.dma("sp", dict(out=t.ap, in_=d[:, :]), writes=[t])

        def finish():
            Sd.barrier()
            Sd.emit()
            return nc, Sd, A


        mA = A.mark()
        cT = T(A.alloc([KD], F32, "cT"))
        c_act = T(A.alloc([KD], F32, "c_act"))
        c_rep = T(A.alloc([KD, P], F32, "c_rep"))
        b_adaP = T(A.alloc([48], F32, "b_adaP"))
        g_mixP = T(A.alloc([KD], F32, "g_mixP"))
        g_ffnP = T(A.alloc([KD], F32, "g_ffnP"))
        wA = [T(A.alloc([KD, 512], F32, "wA%d" % i)) for i in range(2)]
        dtmp = T(A.alloc([4, P], F32, "dtmp"))
        psM = PS[0]
        psBc = PS[1]

        Sd.dma("sp", dict(out=cT.ap, in_=cT_d[:, :]), writes=[cT])
        Sd.dma("sp", dict(out=b_adaP.ap, in_=b_adaP_d[:, :]), writes=[b_adaP])
        Sd.dma("sp", dict(out=g_mixP.ap, in_=g_mixP_d[:, :]), writes=[g_mixP])
        Sd.dma("sp", dict(out=g_ffnP.ap, in_=g_ffnP_d[:, :]), writes=[g_ffnP])
        Sd.dma("sp", dict(out=gt1B.ap, in_=b_adaR_d[:, 2 * D:3 * D].partition_broadcast(P)), writes=[gt1B])
        Sd.dma("sp", dict(out=gt2B.ap, in_=b_adaR_d[:, 5 * D:6 * D].partition_broadcast(P)), writes=[gt2B])
        Sd.op("act", ACT.activation, dict(out=c_act.ap, in_=cT.ap, func=AF.Silu), reads=[cT], writes=[c_act])
        Sd.op("dve", V.tensor_copy, dict(out=c_rep.ap, in_=c_act.ap.unsqueeze(2).to_broadcast([P, KD, P])),
              reads=[c_act], writes=[c_rep])

        piece_order = [0, 1, 2, 3, 6, 7, 8, 9, 4, 5, 10, 11]

        def ada_piece(n, pc):
            buf = wA[n % 2]
            Sd.dma("pool", dict(out=buf.ap, in_=w_ada_v[:, :, pc * 512:(pc + 1) * 512]), writes=[buf])
            if pc in (4, 5, 10, 11):
                gt = gt1B if pc in (4, 5) else gt2B
                half = pc % 2
                for k in range(KD):
                    Sd.op("pe", PE.matmul, dict(out=psBc.ap, lhsT=c_rep.ap[:, k, :], rhs=buf.ap[:, k, :],
                                                start=(k == 0), stop=(k == KD - 1)),
                          reads=[c_rep, buf], writes=[psBc] if k == 0 else [], joins=[] if k == 0 else [psBc])
                sl = gt.ap[:, half * 512:(half + 1) * 512]
                Sd.op("dve", V.tensor_tensor, dict(out=sl, in0=psBc.ap, in1=sl, op=ALU.add),
                      reads=[psBc, gt], joins=[gt])
            else:
                for k in range(KD):
                    Sd.op("pe", PE.matmul, dict(out=psM.ap, lhsT=c_rep.ap[:, k, :], rhs=buf.ap[:, k, :],
                                                start=(k == 0), stop=(k == KD - 1)),
                          reads=[c_rep, buf], writes=[psM] if k == 0 else [], joins=[] if k == 0 else [psM])
                Sd.op("dve", V.tensor_tensor, dict(out=dtmp.ap, in0=psM.ap.rearrange("p (a b) -> p a b", a=4),
                                                   in1=ident_f.ap.unsqueeze(1).to_broadcast([P, 4, P]), op=ALU.mult),
                      reads=[psM, ident_f], writes=[dtmp])
                Sd.op("dve", V.tensor_reduce, dict(out=modP.ap[:, pc * 4:(pc + 1) * 4], in_=dtmp.ap, axis=AX.X, op=ALU.add),
                      reads=[dtmp], joins=[modP])
                if pc in (3, 9):
                    lo = 0 if pc == 3 else 24
                    Sd.op("dve", V.tensor_tensor, dict(out=modP.ap[:, lo:lo + 16], in0=modP.ap[:, lo:lo + 16],
                                                       in1=b_adaP.ap[:, lo:lo + 16], op=ALU.add),
                          reads=[modP, b_adaP], joins=[modP])
                    sc = sc1 if pc == 3 else sc2
                    gP = g_mixP if pc == 3 else g_ffnP
                    Sd.op("dve", V.scalar_tensor_tensor, dict(out=sc.ap, in0=modP.ap[:, lo + 8:lo + 16], scalar=1.0,
                                                              in1=gP.ap, op0=ALU.add, op1=ALU.mult),
                          reads=[modP, gP], writes=[sc])
        for n in range(4):
            ada_piece(n, piece_order[n])
        if stop_after == "0":
            Sd.barrier()
            Sd.dma("sp", dict(out=dbg_d[:, 0:48], in_=modP.ap), reads=[modP])
            Sd.dma("sp", dict(out=dbg_d[:, 48:56], in_=sc1.ap), reads=[sc1])
            Sd.dma("sp", dict(out=dbg_d[:, 56:64], in_=sc2.ap), reads=[sc2])
            Sd.dma("sp", dict(out=dbg_d[:, 64:64 + D], in_=gt1B.ap), reads=[gt1B])
            Sd.dma("sp", dict(out=dbg_d[:, 64 + D:64 + 2 * D], in_=gt2B.ap), reads=[gt2B])
            return finish()

        xin = [T(A.alloc([D], F32, "xin%d" % i)) for i in range(4)]
        xn = [T(A.alloc([D], BF16, "xn%d" % i)) for i in range(2)]
        junk = A.alloc([D], BF16, "junk")
        ssq = [T(A.alloc([4], F32, "ssq%d" % i)) for i in range(4)]
        psT_ap = [bank(2 + 2 * i, 2).bitcast(BF16).rearrange("p (k t) -> p k t", k=KD) for i in range(2)]
        psT_t = [[PS[2], PS[3]], [PS[4], PS[5]]]

        import os as _os
        _nch = int(_os.environ.get('K_NCH', NCH)); _var = _os.environ.get('K_VAR', '')
        for c in range(_nch):
            xt = xin[c % 4]
            sq = ssq[c % 4]
            xb = xn[c % 2]
            pT = psT_ap[(c // 2) % 2]
            pTt = psT_t[(c // 2) % 2]
            Sd.dma("sp", dict(out=xt.ap, in_=x_v[c]), writes=[xt])
            Sd.op("act", ACT.activation, dict(out=junk, in_=xt.ap, func=AF.Square, accum_out=sq.ap[:, 0:1]),
                  reads=[xt], writes=[sq])
            Sd.op("act", ACT.activation, dict(out=sq.ap[:, 1:2], in_=sq.ap[:, 0:1], func=AF.Ln, scale=1.0 / D,
                                              bias=cst.ap[:, 0:1]), reads=[sq, cst], joins=[sq])
            Sd.op("act", ACT.activation, dict(out=sq.ap[:, 2:3], in_=sq.ap[:, 1:2], func=AF.Exp, scale=-0.5),
                  reads=[sq], joins=[sq])
            Sd.op("dve", V.tensor_scalar, dict(out=xb.ap, in0=xt.ap, scalar1=sq.ap[:, 2:3], scalar2=None, op0=ALU.mult),
                  reads=[xt, sq], writes=[xb])
            for k in range(KD):
                first = (c % 2 == 0 and k == 0)
                Sd.op("pe", PE.transpose, dict(out=pT[:, k, (c % 2) * P:(c % 2 + 1) * P], in_=xb.ap[:, k * P:(k + 1) * P],
                                               identity=ident_b.ap),
                      reads=[xb, ident_b], writes=pTt if first else [], joins=[] if first else pTt)
            if c % 2 == 1:
                g = c // 4
                off = ((c // 2) % 2) * 256
                for k in range(KD):
                    dst = hT[k][g].ap[:, off:off + 256]
                    if k < 4:
                        Sd.op("act", ACT.activation, dict(out=dst, in_=pT[:, k, :], func=AF.Identity,
                                                          scale=sc1.ap[:, k:k + 1], bias=modP.ap[:, k:k + 1]),
                              reads=[pTt[0], sc1, modP], joins=[hT[k][g]])
                    else:
                        Sd.op("dve", V.tensor_scalar, dict(out=dst, in0=pT[:, k, :], scalar1=sc1.ap[:, k:k + 1],
                                                           scalar2=modP.ap[:, k:k + 1], op0=ALU.mult, op1=ALU.add),
                              reads=[pTt[1], sc1, modP], joins=[hT[k][g]])

        if stop_after == "A2":
            Sd.barrier()
            Sd.dma("sp", dict(out=dbg_d[:, 0:8], in_=sc1.ap), reads=[sc1])
            return finish()
        if stop_after == "A":
            Sd.barrier()
            for k in range(KD):
                for g in range(NG):
                    Sd.dma("sp", dict(out=dbg_d[:, k * S + g * 512:k * S + (g + 1) * 512], in_=hT[k][g].ap), reads=[hT[k][g]])
            return finish()

        w_g = T(A.alloc([KD, 16], BF16, "w_g"))
        Sd.dma("pool", dict(out=w_g.ap, in_=w_in_v[:, :, 2048:2064]), writes=[w_g])
        bI = T(A.alloc([8], F32, "bI"))
        bF = T(A.alloc([8], F32, "bF"))
        Sd.dma("sp", dict(out=bI.ap, in_=b_ig_d.partition_broadcast(P)), writes=[bI])
        Sd.dma("sp", dict(out=bF.ap, in_=b_fg_d.partition_broadcast(P)), writes=[bF])
        Sd.op("dve", V.memset, dict(ap=cst.ap[:, 1:2], constant=1.0), joins=[cst])
        psG = PS[6]
        psG_ap = bank(6).rearrange("p (d j g) -> p d j g", d=2, j=NCH)
        for c in range(NCH):
            for dr in range(2):
                j = c if dr == 0 else NCH - 1 - c
                for k in range(KD):
                    Sd.op("pe", PE.matmul, dict(out=psG_ap[:, dr, j, :], lhsT=hT[k][c // 4].ap[:, (c % 4) * P:(c % 4 + 1) * P],
                                                rhs=w_g.ap[:, k, dr * 8:(dr + 1) * 8], start=(k == 0), stop=(k == KD - 1)),
                          reads=[hT[k][c // 4], w_g], joins=[psG])

        for n in range(4, 12):
            ada_piece(n, piece_order[n])

        def tab(name, n=NCH):
            return T(A.alloc([8, n], F32, name))

        def v4(ap):
            return ap.rearrange("p (d h) j -> p d h j", d=2)

        def fl(ap):
            return ap.rearrange("p a j -> p (a j)")

        LI, ZZ, CS, GP, GB, BT, RR, TMP = (tab(n) for n in ("LI", "ZZ", "CS", "GP", "GB", "BT", "RR", "TMP"))
        MN = tab("MN", NCH + 1)
        Gcol = T(A.alloc([2], F32, "Gcol"))
        Grow = T(A.alloc([256], F32, "Grow"))
        g_i = psG_ap[:, :, :, 0:4].rearrange("p d j h -> p d h j")
        g_f = psG_ap[:, :, :, 4:8].rearrange("p d j h -> p d h j")
        bIb = bI.ap.rearrange("p (d h) -> p d h", d=2).unsqueeze(3).to_broadcast([P, 2, 4, NCH])
        bFb = bF.ap.rearrange("p (d h) -> p d h", d=2).unsqueeze(3).to_broadcast([P, 2, 4, NCH])
        Sd.op("dve", V.tensor_tensor, dict(out=v4(LI.ap), in0=g_i, in1=bIb, op=ALU.add), reads=[psG, bI], writes=[LI])
        Sd.op("dve", V.tensor_tensor, dict(out=v4(ZZ.ap), in0=g_f, in1=bFb, op=ALU.add), reads=[psG, bF], writes=[ZZ])
        Sd.op("act", ACT.activation, dict(out=fl(ZZ.ap), in_=fl(ZZ.ap), func=AF.Exp, scale=-1.0), reads=[ZZ], writes=[ZZ])
        Sd.op("act", ACT.activation, dict(out=fl(ZZ.ap), in_=fl(ZZ.ap), func=AF.Ln, bias=cst.ap[:, 1:2]), reads=[ZZ, cst], writes=[ZZ])
        if stop_after == "G1":
            Sd.barrier()
            for i, t in enumerate((LI, ZZ)):
                Sd.dma("sp", dict(out=dbg_d[:, i * 256:(i + 1) * 256].rearrange("p (a j) -> p a j", a=8), in_=t.ap[:, :, 0:NCH]), reads=[t])
            return finish()
        psC, psC_ap = PS[7], bank(7)[:, 0:256]
        psBt, psBt_ap = PS[4], bank(4)[:, 0:256]
        Sd.op("pe", PE.matmul, dict(out=psC_ap[:, 0:128], lhsT=maskF.ap, rhs=fl(ZZ.ap)[:, 0:128], start=True, stop=True),
              reads=[maskF, ZZ], writes=[psC])
        Sd.op("pe", PE.matmul, dict(out=psC_ap[:, 128:256], lhsT=maskB.ap, rhs=fl(ZZ.ap)[:, 128:256], start=True, stop=True),
              reads=[maskB, ZZ], joins=[psC])
        Sd.op("pe", PE.matmul, dict(out=psBt_ap, lhsT=ones_f.ap, rhs=fl(ZZ.ap), start=True, stop=True),
              reads=[ones_f, ZZ], writes=[psBt])
        Sd.op("act", ACT.activation, dict(out=fl(CS.ap), in_=psC_ap, func=AF.Copy), reads=[psC], writes=[CS])
        if stop_after == "G2a":
            Sd.barrier()
            Sd.dma("sp", dict(out=dbg_d[:, 0:256].rearrange("p (a j) -> p a j", a=8), in_=CS.ap), reads=[CS])
            return finish()
        Sd.op("dve", V.tensor_tensor, dict(out=fl(GP.ap), in0=psC_ap, in1=fl(LI.ap), op=ALU.add), reads=[psC, LI], writes=[GP])
        Sd.op("act", ACT.activation, dict(out=fl(BT.ap), in_=psBt_ap, func=AF.Copy, scale=-1.0), reads=[psBt], writes=[BT])
        if stop_after == "G2":
            Sd.barrier()
            for i, t in enumerate((CS, GP, BT)):
                Sd.dma("sp", dict(out=dbg_d[:, i * 256:(i + 1) * 256].rearrange("p (a j) -> p a j", a=8), in_=t.ap[:, :, 0:NCH]), reads=[t])
            return finish()
        psX, psX_ap = PS[5], bank(5)[:, 0:256]
        psR, psR_ap = PS[2], bank(2)[0:1, 0:256]
        psGB, psGB_ap = PS[3], bank(3)[:, 0:256]
        for hf in range(2):
            Sd.op("pe", PE.transpose, dict(out=psX_ap[:, hf * P:(hf + 1) * P], in_=fl(GP.ap)[:, hf * P:(hf + 1) * P], identity=ident_f.ap),
                  reads=[GP, ident_f], writes=[psX] if hf == 0 else [], joins=[] if hf == 0 else [psX])
        Sd.op("dve", V.tensor_reduce, dict(out=Gcol.ap, in_=psX_ap.rearrange("p (a s) -> p a s", a=2), axis=AX.X, op=ALU.max),
              reads=[psX], writes=[Gcol])
        for hf in range(2):
            Sd.op("pe", PE.transpose, dict(out=psR_ap[:, hf * P:(hf + 1) * P], in_=Gcol.ap[:, hf:hf + 1], identity=ident_f.ap),
                  reads=[Gcol, ident_f], writes=[psR] if hf == 0 else [], joins=[] if hf == 0 else [psR])
        Sd.op("act", ACT.activation, dict(out=Grow.ap[0:1, :], in_=psR_ap, func=AF.Copy), reads=[psR], writes=[Grow])
        Sd.op("pe", PE.matmul, dict(out=psGB_ap, lhsT=ones_f.ap[0:1, :], rhs=Grow.ap[0:1, :], start=True, stop=True),
              reads=[ones_f, Grow], writes=[psGB])
        Sd.op("act", ACT.activation, dict(out=fl(GB.ap), in_=psGB_ap, func=AF.Copy), reads=[psGB], writes=[GB])
        if stop_after == "G3":
            Sd.barrier()
            for i, t in enumerate((GB, GP)):
                Sd.dma("sp", dict(out=dbg_d[:, i * 256:(i + 1) * 256].rearrange("p (a j) -> p a j", a=8), in_=t.ap[:, :, 0:NCH]), reads=[t])
            return finish()
        Sd.op("dve", V.memset, dict(ap=MN.ap[:, :, 0:1], constant=0.0), writes=[MN])
        for dh in range(8):
            Sd.op("dve", V.tensor_tensor_scan, dict(out=MN.ap[:, dh, 1:NCH + 1], data0=GB.ap[:, dh, :], data1=BT.ap[:, dh, :],
                                                    initial=0.0, op0=ALU.max, op1=ALU.add), reads=[GB, BT], joins=[MN])
        Sd.op("dve", V.tensor_tensor, dict(out=RR.ap, in0=MN.ap[:, :, 0:NCH], in1=GB.ap, op=ALU.max), reads=[MN, GB], writes=[RR])
        for src, dst in ((MN, DEC), (GP, WGT), (CS, STAB)):
            sap = src.ap[:, :, 0:NCH]
            Sd.op("dve", V.tensor_tensor, dict(out=TMP.ap, in0=sap, in1=RR.ap, op=ALU.subtract), reads=[src, RR], writes=[TMP])
            Sd.op("act", ACT.activation, dict(out=fl(dst.ap), in_=fl(TMP.ap), func=AF.Exp), reads=[TMP], writes=[dst])

        if stop_after == "G":
            Sd.barrier()
            for i, t in enumerate((WGT, STAB, DEC, MN)):
                Sd.dma("sp", dict(out=dbg_d[:, i * 256:(i + 1) * 256].rearrange("p (a j) -> p a j", a=8), in_=t.ap[:, :, 0:NCH]), reads=[t])
            return finish()

        Sd.barrier()
        A.release(mA)
        hm_ap = A.alloc([NCH, 512], F32, "hm")
        hm = [[T(hm_ap[:, c, h * P:(h + 1) * P]) for h in range(4)] for c in range(NCH)]
        mH = A.mark()
        wqkv = T(A.alloc([KD, 3, P], BF16, "wqkv"))
        qT_ap = A.alloc([S], BF16, "qT")
        kT_ap = A.alloc([S], BF16, "kT")
        qT = [T(qT_ap[:, g * 512:(g + 1) * 512]) for g in range(NG)]
        kT = [T(kT_ap[:, g * 512:(g + 1) * 512]) for g in range(NG)]
        va_ap = A.alloc([NCH, 130], BF16, "va")
        va = [T(va_ap[:, c, :]) for c in range(NCH)]
        kwF_ap = A.alloc([NCH, P], BF16, "kwF")
        kwB_ap = A.alloc([NCH, P], BF16, "kwB")
        kwF = [T(kwF_ap[:, c, :]) for c in range(NCH)]
        kwB = [T(kwB_ap[:, c, :]) for c in range(NCH)]
        sTw = [T(A.alloc([P], BF16, "sTw%d" % i)) for i in range(4)]
        Cst = [T(A.alloc([130], F32, "Cst%d" % i)) for i in range(2)]
        Csb = [[T(A.alloc([130], BF16, "Csb%d%d" % (i, n))) for n in range(3)] for i in range(2)]
        dnr = [T(A.alloc([2], F32, "dnr%d" % i)) for i in range(4)]
        Sd.op("dve", V.memset, dict(ap=va_ap[:, :, 128:130], constant=1.0), writes=va)
        QSCALE = float(P) ** -0.5
        _nheads = int(_os.environ.get('K_NH', 4))

        for h in range(_nheads):
            for i, off in enumerate((0, 512, 1024)):
                Sd.dma("pool", dict(out=wqkv.ap[:, :, i, :], in_=w_in_v[:, :, off + h * P:off + (h + 1) * P]),
                       writes=[wqkv] if i == 0 else [], joins=[] if i == 0 else [wqkv])
            for g in range(NG):
                for which, dstT, bb in ((0, qT, g % 2), (1, kT, 2 + g % 2)):
                    for k in range(KD):
                        Sd.op("pe", PE.matmul, dict(out=bank(bb), lhsT=wqkv.ap[:, k, which, :], rhs=hT[k][g].ap,
                                                    start=(k == 0), stop=(k == KD - 1)),
                              reads=[wqkv, hT[k][g]], writes=[PS[bb]] if k == 0 else [], joins=[] if k == 0 else [PS[bb]])
                    if which == 0:
                        Sd.op("act", ACT.activation, dict(out=dstT[g].ap, in_=bank(bb), func=AF.Copy, scale=QSCALE),
                              reads=[PS[bb]], writes=[dstT[g]])
                    else:
                        Sd.op("dve", V.tensor_copy, dict(out=dstT[g].ap, in_=bank(bb)), reads=[PS[bb]], writes=[dstT[g]])
                for ci in range(4):
                    c = g * 4 + ci
                    bb = 4 + ci
                    for k in range(KD):
                        Sd.op("pe", PE.matmul, dict(out=bank(bb)[:, 0:256], lhsT=hT[k][g].ap[:, ci * P:(ci + 1) * P],
                                                    rhs=wqkv.ap[:, k, 1:3, :].rearrange("p a b -> p (a b)"),
                                                    start=(k == 0), stop=(k == KD - 1)),
                              reads=[wqkv, hT[k][g]], writes=[PS[bb]] if k == 0 else [], joins=[] if k == 0 else [PS[bb]])
                    Sd.op("act", ACT.activation, dict(out=va[c].ap[:, 0:P], in_=bank(bb)[:, P:2 * P], func=AF.Copy),
                          reads=[PS[bb]], joins=[va[c]])
                    Sd.op("dve", V.tensor_scalar, dict(out=kwF[c].ap, in0=bank(bb)[:, 0:P], scalar1=WGT.ap[:, h, c:c + 1],
                                                       scalar2=None, op0=ALU.mult), reads=[PS[bb], WGT], writes=[kwF[c]])
                    jb = NCH - 1 - c
                    Sd.op("dve", V.tensor_scalar, dict(out=kwB[c].ap, in0=bank(bb)[:, 0:P], scalar1=WGT.ap[:, 4 + h, jb:jb + 1],
                                                       scalar2=None, op0=ALU.mult), reads=[PS[bb], WGT], writes=[kwB[c]])
            NI = 2 * NCH
            ND = (2, 3, 6, 7)

            def item(i):
                j, dr = divmod(i, 2)
                c = j if dr == 0 else NCH - 1 - j
                return j, dr, c, dr * 4 + h, c // 4, (c % 4) * P

            def S1(i):
                j, dr, c, dh, g, o = item(i)
                st = sTw[i % 4]
                Sd.op("pe", PE.matmul, dict(out=bank(dr)[:, 0:P], lhsT=kT[g].ap[:, o:o + P], rhs=qT[g].ap[:, o:o + P],
                                            start=True, stop=True), reads=[kT[g], qT[g]], writes=[PS[dr]])
                Sd.op("dve", V.scalar_tensor_tensor, dict(out=st.ap, in0=bank(dr)[:, 0:P], scalar=WGT.ap[:, dh, j:j + 1],
                                                          in1=(maskF if dr == 0 else maskB).ap, op0=ALU.mult, op1=ALU.mult),
                      reads=[PS[dr], WGT, maskF if dr == 0 else maskB], writes=[st])

            def S2(i):
                j, dr, c, dh, g, o = item(i)
                if j >= NCH - 1:
                    return
                kw = kwF if dr == 0 else kwB
                Sd.op("pe", PE.matmul, dict(out=bank(4 + dr)[:, 0:129], lhsT=kw[c].ap, rhs=va[c].ap[:, 0:129],
                                            start=True, stop=True), reads=[kw[c], va[c]], writes=[PS[4 + dr]])
                if j == 0:
                    Sd.op("dve", V.tensor_copy, dict(out=Cst[dr].ap[:, 0:129], in_=bank(4 + dr)[:, 0:129]),
                          reads=[PS[4 + dr]], writes=[Cst[dr]])
                else:
                    Sd.op("dve", V.scalar_tensor_tensor, dict(out=Cst[dr].ap[:, 0:129], in0=Cst[dr].ap[:, 0:129],
                                                              scalar=DEC.ap[:, dh, j:j + 1], in1=bank(4 + dr)[:, 0:129],
                                                              op0=ALU.mult, op1=ALU.add),
                          reads=[Cst[dr], DEC, PS[4 + dr]], writes=[Cst[dr]])

            def S2b(i):
                j, dr, c, dh, g, o = item(i)
                if j >= NCH - 1:
                    return
                Sd.op("act", ACT.activation, dict(out=Csb[dr][(j + 1) % 3].ap[:, 0:129], in_=Cst[dr].ap[:, 0:129],
                                                  func=AF.Copy, scale=DEC.ap[:, dh, j + 1:j + 2]),
                      reads=[Cst[dr], DEC], writes=[Csb[dr][(j + 1) % 3]])

            def S3(i):
                j, dr, c, dh, g, o = item(i)
                bb = ND[i % 4]
                st = sTw[i % 4]
                Sd.op("pe", PE.matmul, dict(out=bank(bb)[:, 0:129], lhsT=st.ap, rhs=va[c].ap[:, 0:129],
                                            start=True, stop=(j == 0)), reads=[st, va[c]], writes=[PS[bb]])
                if j > 0:
                    Sd.op("pe", PE.matmul, dict(out=bank(bb)[:, 0:129], lhsT=qT[g].ap[:, o:o + P],
                                                rhs=Csb[dr][j % 3].ap[:, 0:129], start=False, stop=True),
                          reads=[qT[g], Csb[dr][j % 3]], joins=[PS[bb]])

            def S4(i):
                j, dr, c, dh, g, o = item(i)
                bb = ND[i % 4]
                d_ = dnr[i % 4]
                Sd.op("act", ACT.activation, dict(out=d_.ap[:, 0:1], in_=bank(bb)[:, 128:129], func=AF.Abs),
                      reads=[PS[bb]], writes=[d_])
                Sd.op("dve", V.tensor_scalar, dict(out=d_.ap[:, 0:1], in0=d_.ap[:, 0:1],
                                                   scalar1=STAB.ap[:, dh, j:j + 1], scalar2=None, op0=ALU.max),
                      reads=[d_, STAB], joins=[d_])
                Sd.op("dve", V.reciprocal, dict(out=d_.ap[:, 1:2], in_=d_.ap[:, 0:1]), reads=[d_], joins=[d_])

            def S5(i):
                j, dr, c, dh, g, o = item(i)
                bb = ND[i % 4]
                d_ = dnr[i % 4]
                if j < NCH // 2:
                    Sd.op("act", ACT.activation, dict(out=hm[c][h].ap, in_=bank(bb)[:, 0:P], func=AF.Copy,
                                                      scale=d_.ap[:, 1:2]), reads=[PS[bb], d_], writes=[hm[c][h]])
                else:
                    Sd.op("dve", V.scalar_tensor_tensor, dict(out=hm[c][h].ap, in0=bank(bb)[:, 0:P], scalar=d_.ap[:, 1:2],
                                                              in1=hm[c][h].ap, op0=ALU.mult, op1=ALU.add),
                          reads=[PS[bb], d_, hm[c][h]], writes=[hm[c][h]])

            for t in range(NI + 3):
                if t < NI:
                    S1(t)
                if 0 <= t - 2 < NI:
                    S4(t - 2)
                if 0 <= t - 3 < NI:
                    S5(t - 3)
                if 0 <= t - 1 < NI:
                    S2b(t - 1)
                if 0 <= t - 1 < NI:
                    S3(t - 1)
                if t < NI:
                    S2(t)

        if stop_after == "H":
            Sd.barrier()
            for c in range(NCH):
                Sd.dma("sp", dict(out=dbg_d[c * P:(c + 1) * P, 0:P * _nheads], in_=hm_ap[:, c, 0:P * _nheads]), reads=hm[c])
            return finish()

        Sd.barrier()
        A.release(mH)
        mY = A.mark()
        w_o = T(A.alloc([KD, 512], BF16, "w_o"))
        w_u = T(A.alloc([KD, 512], BF16, "w_u"))
        w_pl = T(A.alloc([4, P], BF16, "w_pl"))
        psw = T(A.alloc([4], F32, "psw"))
        pscl = T(A.alloc([4], F32, "pscl"))
        edge = T(A.alloc([4, 16], F32, "edge"))
        ubuf = T(A.alloc([4, 528], F32, "ubuf"))
        tP = T(A.alloc([4, 528], F32, "tP"))
        tQ = T(A.alloc([2, 528], F32, "tQ"))
        tA = T(A.alloc([4, 512], F32, "tA"))
        mixed = T(A.alloc([4, 512], BF16, "mixed"))
        ucarry = T(A.alloc([4, 8], F32, "ucarry"))
        sig = [T(A.alloc([512], F32, "sig%d" % i)) for i in range(4)]
        sqb = [T(A.alloc([512], F32, "sqb%d" % i)) for i in range(1)]
        ssh = T(A.alloc([16], F32, "ssh"))
        rsh = T(A.alloc([16], F32, "rsh"))
        yab = [T(A.alloc([512], BF16, "ya%d" % i)) for i in range(4)]
        Sd.dma("pool", dict(out=w_o.ap, in_=w_in_v[:, :, 1536:2048]), writes=[w_o])
        Sd.dma("pool", dict(out=w_u.ap, in_=w_in_v[:, :, 2064:2576]), writes=[w_u])
        Sd.dma("pool", dict(out=w_pl.ap, in_=w_pool_d[:, :, :]), writes=[w_pl])
        Sd.dma("sp", dict(out=pscl.ap, in_=psclP_d[:, :]), writes=[pscl])
        Sd.dma("sp", dict(out=edge.ap.rearrange("p a b -> p (a b)"), in_=edge_d.partition_broadcast(P)), writes=[edge])
        for pg, w in enumerate(POOL_W):
            Sd.op("dve", V.tensor_scalar, dict(out=psw.ap[:, pg:pg + 1], in0=pscl.ap[:, pg:pg + 1], scalar1=1.0 / w, scalar2=None,
                                               op0=ALU.mult), reads=[pscl], joins=[psw])
        Sd.op("dve", V.memset, dict(ap=cst.ap[:, 2:3], constant=EPS), joins=[cst])
        ptr_ap = bank(6, 2).bitcast(BF16).rearrange("p (m t) -> p m t", m=4)

        def stage_U(G):
                U = ubuf.ap
                for pg in range(4):
                    bb = pg % 2
                    for k in range(KD):
                        Sd.op("pe", PE.matmul, dict(out=bank(bb), lhsT=w_u.ap[:, k, pg * P:(pg + 1) * P], rhs=hT[k][G].ap,
                                                    start=(k == 0), stop=(k == KD - 1)),
                              reads=[w_u, hT[k][G]], writes=[PS[bb]] if k == 0 else [], joins=[] if k == 0 else [PS[bb]])
                    Sd.op("act", ACT.activation, dict(out=ubuf.ap[:, pg, 8:520], in_=bank(bb), func=AF.Copy),
                          reads=[PS[bb]], writes=[ubuf] if pg == 0 else [], joins=[] if pg == 0 else [ubuf])
                if G < NG - 1:
                    for pg in range(4):
                        for k in range(KD):
                            Sd.op("pe", PE.matmul, dict(out=bank(2)[:, pg * 8:(pg + 1) * 8], lhsT=w_u.ap[:, k, pg * P:(pg + 1) * P],
                                                        rhs=hT[k][G + 1].ap[:, 0:8], start=(k == 0), stop=(k == KD - 1)),
                                  reads=[w_u, hT[k][G + 1]], writes=[PS[2]] if (k == 0 and pg == 0) else [],
                                  joins=[] if (k == 0 and pg == 0) else [PS[2]])
                    Sd.op("act", ACT.activation, dict(out=ubuf.ap[:, :, 520:528], in_=bank(2)[:, 0:32].rearrange("p (a b) -> p a b", a=4),
                                                      func=AF.Copy), reads=[PS[2]], joins=[ubuf])
                else:
                    Sd.op("pool", POOL.memset, dict(ap=ubuf.ap[:, :, 520:528], constant=0.0), joins=[ubuf])
                if G == 0:
                    Sd.op("pool", POOL.memset, dict(ap=ubuf.ap[:, :, 0:8], constant=0.0), joins=[ubuf])
                else:
                    Sd.op("pool", POOL.tensor_copy, dict(out=ubuf.ap[:, :, 0:8], in_=ucarry.ap), reads=[ucarry], joins=[ubuf])
                Sd.op("pool", POOL.tensor_copy, dict(out=ucarry.ap, in_=ubuf.ap[:, :, 512:520]), reads=[ubuf], writes=[ucarry])
                U = ubuf.ap
                Sd.op("pool", POOL.tensor_tensor, dict(out=tP.ap[:, :, 0:527], in0=U[:, :, 0:527], in1=U[:, :, 1:528], op=ALU.add),
                      reads=[ubuf], writes=[tP])
                Sd.op("pool", POOL.tensor_tensor, dict(out=tQ.ap[:, 0:2, 0:525], in0=tP.ap[:, 2:4, 0:525], in1=tP.ap[:, 2:4, 2:527], op=ALU.add),
                      reads=[tP], writes=[tQ])
                Sd.op("pool", POOL.tensor_tensor, dict(out=tP.ap[:, 3, 0:521], in0=tQ.ap[:, 1, 0:521], in1=tQ.ap[:, 1, 4:525], op=ALU.add),
                      reads=[tQ, tP], joins=[tP])
                Sd.op("pool", POOL.tensor_copy, dict(out=tA.ap[:, 0, :], in_=tP.ap[:, 0, 7:519]), reads=[tP], writes=[tA])
                Sd.op("pool", POOL.tensor_tensor, dict(out=tA.ap[:, 1, :], in0=tP.ap[:, 1, 6:518], in1=tP.ap[:, 1, 8:520], op=ALU.add),
                      reads=[tP], joins=[tA])
                Sd.op("pool", POOL.tensor_tensor, dict(out=tA.ap[:, 2, :], in0=tQ.ap[:, 0, 4:516], in1=tQ.ap[:, 0, 8:520], op=ALU.add),
                      reads=[tQ], joins=[tA])
                Sd.op("pool", POOL.tensor_tensor, dict(out=tA.ap[:, 3, :], in0=tP.ap[:, 3, 0:512], in1=tP.ap[:, 3, 8:520], op=ALU.add),
                      reads=[tP], joins=[tA])
                if G == 0:
                    Sd.op("pool", POOL.tensor_tensor, dict(out=tA.ap[:, :, 0:8], in0=tA.ap[:, :, 0:8], in1=edge.ap[:, :, 0:8], op=ALU.mult),
                          reads=[tA, edge], joins=[tA])
                if G == NG - 1:
                    Sd.op("pool", POOL.tensor_tensor, dict(out=tA.ap[:, :, 504:512], in0=tA.ap[:, :, 504:512], in1=edge.ap[:, :, 8:16], op=ALU.mult),
                          reads=[tA, edge], joins=[tA])
        def stage_O(G):
                for i in range(4):
                    c = G * 4 + i
                    bb = 3 + i % 2
                    for k in range(KD):
                        Sd.op("pe", PE.matmul, dict(out=bank(bb), lhsT=hT[k][G].ap[:, i * P:(i + 1) * P], rhs=w_o.ap[:, k, :],
                                                    start=(k == 0), stop=(k == KD - 1)),
                              reads=[w_o, hT[k][G]], writes=[PS[bb]] if k == 0 else [], joins=[] if k == 0 else [PS[bb]])
                    Sd.op("act", ACT.activation, dict(out=sig[i].ap, in_=bank(bb), func=AF.Sigmoid), reads=[PS[bb]], writes=[sig[i]])
                    sq = sqb[0]
                    Sd.op("dve", V.tensor_tensor, dict(out=sq.ap, in0=hm_ap[:, c, :], in1=hm_ap[:, c, :], op=ALU.mult),
                          reads=hm[c], writes=[sq])
                    Sd.op("dve", V.tensor_reduce, dict(out=ssh.ap[:, i * 4:(i + 1) * 4], in_=sq.ap.rearrange("p (a b) -> p a b", a=4),
                                                       axis=AX.X, op=ALU.add), reads=[sq], writes=[ssh] if i == 0 else [],
                          joins=[] if i == 0 else [ssh])
                Sd.op("act", ACT.activation, dict(out=rsh.ap, in_=ssh.ap, func=AF.Ln, scale=1.0 / P, bias=cst.ap[:, 2:3]),
                      reads=[ssh, cst], writes=[rsh])
                Sd.op("act", ACT.activation, dict(out=rsh.ap, in_=rsh.ap, func=AF.Exp, scale=-0.5), reads=[rsh], writes=[rsh])
        def stage_G(G):
                for i in range(4):
                    c = G * 4 + i
                    ya = yab[i]
                    for h in range(4):
                        Sd.op("dve", V.scalar_tensor_tensor, dict(out=ya.ap[:, h * P:(h + 1) * P], in0=hm[c][h].ap,
                                                                  scalar=rsh.ap[:, i * 4 + h:i * 4 + h + 1], in1=sig[i].ap[:, h * P:(h + 1) * P],
                                                                  op0=ALU.mult, op1=ALU.mult),
                              reads=[hm[c][h], rsh, sig[i]], writes=[ya] if h == 0 else [], joins=[] if h == 0 else [ya])
                    for h in range(4):
                        first = (i == 0 and h == 0)
                        Sd.op("pe", PE.transpose, dict(out=ptr_ap[:, h, i * P:(i + 1) * P], in_=ya.ap[:, h * P:(h + 1) * P], identity=ident_b.ap),
                              reads=[ya, ident_b], writes=[PS[6], PS[7]] if first else [], joins=[] if first else [PS[6], PS[7]])
                for m in range(4):
                    if m < 2:
                        Sd.op("act", ACT.activation, dict(out=hT[m][G].ap, in_=ptr_ap[:, m, :], func=AF.Copy), reads=[PS[6]], writes=[hT[m][G]])
                    else:
                        Sd.op("dve", V.tensor_copy, dict(out=hT[m][G].ap, in_=ptr_ap[:, m, :]), reads=[PS[7]], writes=[hT[m][G]])

        def stage_M(G):
                U = ubuf.ap
                for pg, w in enumerate(POOL_W):
                    Sd.op("dve", V.scalar_tensor_tensor, dict(out=mixed.ap[:, pg, :], in0=U[:, pg, 8:520], scalar=-float(w), in1=tA.ap[:, pg, :],
                                                              op0=ALU.mult, op1=ALU.add),
                          reads=[ubuf, tA], writes=[mixed] if pg == 0 else [], joins=[] if pg == 0 else [mixed])
                for pg in range(4):
                    Sd.op("pe", PE.matmul, dict(out=bank(5), lhsT=w_pl.ap[:, pg, :], rhs=mixed.ap[:, pg, :], start=True, stop=True),
                          reads=[w_pl, mixed], writes=[PS[5]])
                    Sd.op("act", ACT.activation, dict(out=hT[4 + pg][G].ap, in_=bank(5), func=AF.Copy, scale=psw.ap[:, pg:pg + 1]),
                          reads=[PS[5], psw], writes=[hT[4 + pg][G]])


        stage_U(0)
        for G in range(NG):
            stage_O(G)
            stage_M(G)
            if G + 1 < NG:
                stage_U(G + 1)
            stage_G(G)

        if stop_after == "Y":
            Sd.barrier()
            for k in range(KD):
                for g in range(NG):
                    Sd.dma("sp", dict(out=dbg_d[:, k * S + g * 512:k * S + (g + 1) * 512], in_=hT[k][g].ap), reads=[hT[k][g]])
            return finish()

        Sd.barrier()
        A.release(mA)
        w_outp = T(A.alloc([KD, D], BF16, "w_outp"))
        ghP = T(A.alloc([4], F32, "ghP"))
        gfinB = T(A.alloc([D], F32, "gfinB"))
        x1_ap = A.alloc([8, D], F32, "x1")
        x1 = [T(x1_ap[:, i, :]) for i in range(8)]
        aT_ap = A.alloc([8, 2, 512], BF16, "aT")
        aT = [[T(aT_ap[:, fc, tg, :]) for tg in range(2)] for fc in range(8)]
        w1s = [T(A.alloc([KD, 512], BF16, "w1s%d" % i)) for i in range(3)]
        w2s = [T(A.alloc([4, D], BF16, "w2s%d" % i)) for i in range(3)]
        xn2 = [T(A.alloc([D], BF16, "xn2%d" % i)) for i in range(2)]
        rr = [T(A.alloc([512], F32, "rr%d" % i)) for i in range(2)]
        st2 = [T(A.alloc([4], F32, "st2%d" % i)) for i in range(4)]
        junk2 = A.alloc([D], BF16, "junk2")
        stg = [x1[0], x1[1]]
        Sd.dma("sp", dict(out=ghP.ap, in_=g_headP_d[:, :]), writes=[ghP])
        Sd.dma("sp", dict(out=gfinB.ap, in_=g_finR_d.partition_broadcast(P)), writes=[gfinB])
        for k in range(KD):
            sg = stg[k % 2]
            Sd.dma("sp", dict(out=sg.ap, in_=w_out_v[:, k, :]), writes=[sg])
            if k < 4:
                Sd.op("dve", V.scalar_tensor_tensor, dict(out=w_outp.ap[:, k, :], in0=sg.ap, scalar=ghP.ap[:, k:k + 1], in1=gt1B.ap,
                                                          op0=ALU.mult, op1=ALU.mult), reads=[sg, ghP, gt1B], joins=[w_outp])
            else:
                Sd.op("dve", V.tensor_tensor, dict(out=w_outp.ap[:, k, :], in0=sg.ap, in1=gt1B.ap, op=ALU.mult),
                      reads=[sg, gt1B], joins=[w_outp])
        NT4 = 4
        w1_blocks = [(Tt, q, bl) for Tt in range(NT4) for q in range(4) for bl in range(2)]
        gt2b4 = gt2B.ap.unsqueeze(1).to_broadcast([P, 4, D])

        def load_w1(n):
            if n >= len(w1_blocks):
                return
            Tt, q, bl = w1_blocks[n]
            c0 = q * 1024 + bl * 512
            Sd.dma("pool", dict(out=w1s[n % 3].ap, in_=w_ff1_v[:, :, c0:c0 + 512]), writes=[w1s[n % 3]])

        def load_w2(n):
            if n >= len(w1_blocks):
                return
            Tt, q, bl = w1_blocks[n]
            f0 = q * 8 + bl * 4
            t = w2s[n % 3]
            Sd.dma("pool", dict(out=t.ap, in_=w_ff2_v[:, f0:f0 + 4, :]), writes=[t])
            Sd.op("pool", POOL.tensor_tensor, dict(out=t.ap, in0=t.ap, in1=gt2b4, op=ALU.mult), reads=[t, gt2B], writes=[t])

        load_w1(0)
        load_w1(1)
        load_w2(0)
        load_w2(1)
        ps01 = ps_all[:, 0:1024]
        ps67 = ps_all[:, 3072:4096]
        psT2_ap = bank(2, 2).bitcast(BF16).rearrange("p (k t) -> p k t", k=KD)
        def final_norm_store(Tt, st_):
            c = Tt * 8 + st_
            s2 = st2[st_ % 4]
            Sd.op("act", ACT.activation, dict(out=junk2, in_=x1[st_].ap, func=AF.Square, accum_out=s2.ap[:, 0:1]),
                  reads=[x1[st_]], writes=[s2])
            Sd.op("act", ACT.activation, dict(out=s2.ap[:, 1:2], in_=s2.ap[:, 0:1], func=AF.Ln, scale=1.0 / D, bias=cst.ap[:, 0:1]),
                  reads=[s2, cst], joins=[s2])
            Sd.op("act", ACT.activation, dict(out=s2.ap[:, 2:3], in_=s2.ap[:, 1:2], func=AF.Exp, scale=-0.5), reads=[s2], joins=[s2])
            Sd.op("dve", V.scalar_tensor_tensor, dict(out=x1[st_].ap, in0=x1[st_].ap, scalar=s2.ap[:, 2:3], in1=gfinB.ap,
                                                      op0=ALU.mult, op1=ALU.mult), reads=[x1[st_], s2, gfinB], writes=[x1[st_]])
            Sd.dma("sp", dict(out=out_v[c], in_=x1[st_].ap), reads=[x1[st_]])
            if Tt + 1 < NT4:
                Sd.dma("sp", dict(out=x1[st_].ap, in_=x_v[(Tt + 1) * 8 + st_]), writes=[x1[st_]])

        blk = 0
        for Tt in range(NT4):
            def stage_a(st_):
                c = Tt * 8 + st_
                g, o = c // 4, (c % 4) * P
                if Tt == 0:
                    Sd.dma("sp", dict(out=x1[st_].ap, in_=x_v[c]), writes=[x1[st_]])
                pb, pt = ((0, 1), ps01) if st_ % 2 == 0 else ((6, 7), ps67)
                for half in range(2):
                    bb = pb[half]
                    for k in range(KD):
                        Sd.op("pe", PE.matmul, dict(out=bank(bb), lhsT=hT[k][g].ap[:, o:o + P], rhs=w_outp.ap[:, k, half * 512:(half + 1) * 512],
                                                    start=(k == 0), stop=(k == KD - 1)),
                              reads=[hT[k][g], w_outp], writes=[PS[bb]] if k == 0 else [], joins=[] if k == 0 else [PS[bb]])
                Sd.op("dve", V.tensor_tensor, dict(out=x1[st_].ap, in0=pt, in1=x1[st_].ap, op=ALU.add),
                      reads=[PS[pb[0]], PS[pb[1]], x1[st_]], writes=[x1[st_]])

            def stage_b(st_):
                c = Tt * 8 + st_
                g = c // 4
                s2 = st2[st_ % 4]
                xb = xn2[st_ % 2]
                Sd.op("act", ACT.activation, dict(out=junk2, in_=x1[st_].ap, func=AF.Square, accum_out=s2.ap[:, 0:1]),
                      reads=[x1[st_]], writes=[s2])
                Sd.op("act", ACT.activation, dict(out=s2.ap[:, 1:2], in_=s2.ap[:, 0:1], func=AF.Ln, scale=1.0 / D, bias=cst.ap[:, 0:1]),
                      reads=[s2, cst], joins=[s2])
                Sd.op("act", ACT.activation, dict(out=s2.ap[:, 2:3], in_=s2.ap[:, 1:2], func=AF.Exp, scale=-0.5), reads=[s2], joins=[s2])
                Sd.op("dve", V.tensor_scalar, dict(out=xb.ap, in0=x1[st_].ap, scalar1=s2.ap[:, 2:3], scalar2=None, op0=ALU.mult),
                      reads=[x1[st_], s2], writes=[xb])
                for k in range(KD):
                    first = (st_ % 2 == 0 and k == 0)
                    Sd.op("pe", PE.transpose, dict(out=psT2_ap[:, k, (st_ % 2) * P:(st_ % 2 + 1) * P], in_=xb.ap[:, k * P:(k + 1) * P],
                                                   identity=ident_b.ap),
                          reads=[xb, ident_b], writes=[PS[2], PS[3]] if first else [], joins=[] if first else [PS[2], PS[3]])
                if st_ % 2 == 1:
                    off = ((c // 2) % 2) * 256
                    for k in range(KD):
                        dst = hT[k][g].ap[:, off:off + 256]
                        if k < 4:
                            Sd.op("act", ACT.activation, dict(out=dst, in_=psT2_ap[:, k, :], func=AF.Identity,
                                                              scale=sc2.ap[:, k:k + 1], bias=modP.ap[:, 24 + k:25 + k]),
                                  reads=[PS[2], sc2, modP], joins=[hT[k][g]])
                        else:
                            Sd.op("dve", V.tensor_scalar, dict(out=dst, in0=psT2_ap[:, k, :], scalar1=sc2.ap[:, k:k + 1],
                                                               scalar2=modP.ap[:, 24 + k:25 + k], op0=ALU.mult, op1=ALU.add),
                                  reads=[PS[3], sc2, modP], joins=[hT[k][g]])

            for st_ in range(9):
                if st_ < 8:
                    stage_a(st_)
                if st_ >= 1:
                    stage_b(st_ - 1)
            for q in range(4):
                for bl in range(2):
                    n = blk + bl
                    load_w1(n + 2)
                    wt = w1s[n % 3]
                    for f4 in range(4):
                        fc = bl * 4 + f4
                        for tg in range(2):
                            bb = 4 + (fc * 2 + tg) % 2
                            for k in range(KD):
                                Sd.op("pe", PE.matmul, dict(out=bank(bb), lhsT=wt.ap[:, k, f4 * P:(f4 + 1) * P], rhs=hT[k][Tt * 2 + tg].ap,
                                                            start=(k == 0), stop=(k == KD - 1)),
                                      reads=[wt, hT[k][Tt * 2 + tg]], writes=[PS[bb]] if k == 0 else [], joins=[] if k == 0 else [PS[bb]])
                            r_ = rr[(fc * 2 + tg) % 2]
                            Sd.op("act", ACT.activation, dict(out=r_.ap, in_=bank(bb), func=AF.Relu), reads=[PS[bb]], writes=[r_])
                            Sd.op("dve", V.tensor_tensor, dict(out=aT[fc][tg].ap, in0=r_.ap, in1=r_.ap, op=ALU.mult),
                                  reads=[r_], writes=[aT[fc][tg]])
                load_w2(blk + 2)
                for st_ in range(8):
                    tg, o = st_ // 4, (st_ % 4) * P
                    pb, pt = ((0, 1), ps01) if st_ % 2 == 0 else ((6, 7), ps67)
                    for half in range(2):
                        bb = pb[half]
                        for fc in range(8):
                            wt = w2s[(blk + fc // 4) % 3]
                            Sd.op("pe", PE.matmul, dict(out=bank(bb), lhsT=aT[fc][tg].ap[:, o:o + P], rhs=wt.ap[:, fc % 4, half * 512:(half + 1) * 512],
                                                        start=(fc == 0), stop=(fc == 7)),
                                  reads=[aT[fc][tg], wt], writes=[PS[bb]] if fc == 0 else [], joins=[] if fc == 0 else [PS[bb]])
                    Sd.op("dve", V.tensor_tensor, dict(out=x1[st_].ap, in0=pt, in1=x1[st_].ap, op=ALU.add),
                          reads=[PS[pb[0]], PS[pb[1]], x1[st_]], writes=[x1[st_]])
                    if q == 3:
                        final_norm_store(Tt, st_)
                load_w2(blk + 3)
                blk += 2
        return finish()


def _consts():
    s = np.arange(P)
    maskF = (s[:, None] <= s[None, :]).astype(np.float32)
    maskB = (s[:, None] >= s[None, :]).astype(np.float32)
    edge = np.ones((4, 16), np.float32)
    for g, w in enumerate(POOL_W):
        for t in range(8):
            lo = max(t - w // 2, 0)
            hi = min(t + w // 2, S)
            edge[g, t] = w / float(hi - lo)
            tt = S - 8 + t
            lo = max(tt - w // 2, 0)
            hi = min(tt + w // 2, S)
            edge[g, 8 + t] = w / float(hi - lo)
    return dict(
        ident_f=np.eye(P, dtype=np.float32),
        ident_b=np.eye(P).astype(ml_dtypes.bfloat16),
        maskF=maskF, maskB=maskB,
        ones_f=np.ones((P, P), np.float32),
        edge_fac=edge.reshape(1, 64),
    )


def make_in_maps(x, c, w_ada, b_ada, g_mix, w_in, b_igate, b_fgate, g_head, w_pool, pool_scale,
                 w_out, g_ffn, w_ff1, w_ff2, g_final):
    f = lambda a: np.ascontiguousarray(np.asarray(a, dtype=np.float32))
    cs = _consts()
    shared = dict(
        w_ada=f(w_ada[0]),
        b_adaP=f(np.asarray(b_ada[0]).reshape(48, P).T),
        b_adaR=f(np.asarray(b_ada[0]).reshape(1, 6 * D)),
        g_mixP=f(np.asarray(g_mix[0]).reshape(KD, P).T),
        g_ffnP=f(np.asarray(g_ffn[0]).reshape(KD, P).T),
        g_finR=f(np.asarray(g_final).reshape(1, D)),
        w_in=f(w_in[0]),
        b_ig=f(np.asarray(b_igate[0]).reshape(1, 8)),
        b_fg=f(np.asarray(b_fgate[0]).reshape(1, 8)),
        g_headP=f(np.asarray(g_head[0]).reshape(4, P).T),
        pool_scaleP=f(np.asarray(pool_scale[0]).reshape(4, P).T),
        w_pool=f(np.asarray(w_pool[0]).transpose(1, 0, 2)),
        w_out=f(w_out[0]), w_ff1=f(w_ff1[0]), w_ff2=f(w_ff2[0]),
        **cs,
    )
    maps = []
    for b in range(N_CORES):
        m = dict(shared)
        m["x"] = f(x[b])
        m["cT"] = f(np.asarray(c[b]).reshape(KD, P).T)
        maps.append(m)
    return maps


_CACHE = {}


def kernel(**inputs):
    if "nc" not in _CACHE:
        _CACHE["nc"] = build_program()[0]
    nc = _CACHE["nc"]
    in_maps = make_in_maps(**inputs)
    res = run_bass_kernel_spmd(nc, in_maps, core_ids=list(range(N_CORES)))
    out = np.stack([np.asarray(r["out"]) for r in res.results], axis=0)
    return out.astype(np.float32)
```

```python
import numpy as np
import ml_dtypes
from contextlib import ExitStack

import concourse.bass as bass
import concourse.mybir as mybir
from concourse.bass_utils import run_bass_kernel_spmd

F32 = mybir.dt.float32
BF16 = mybir.dt.bfloat16
AF = mybir.ActivationFunctionType
ALU = mybir.AluOpType
AX = mybir.AxisListType

P = 128
S = 4096
D = 1024
KD = 8
NCH = 32
NG = 8
DFF = 4096
EPS = 1e-6
POOL_W = (2, 4, 8, 16)
N_CORES = 8


class T:
    __slots__ = ("ap", "w", "r", "dsem", "dcount", "name", "excl")

    def __init__(self, ap, name="", excl=False):
        self.ap = ap
        self.excl = excl
        self.w = []
        self.r = []
        self.dsem = None
        self.dcount = 0
        self.name = name

    def __getitem__(self, k):
        return self.ap[k]


class _Op:
    __slots__ = ("eng", "idx", "method", "kwargs", "waits", "marked", "dma_inc", "rank")


class Sched:
    ENG = ("pe", "act", "dve", "pool", "sp")

    def __init__(self, nc, es):
        self.nc = nc
        self.es = es
        self.h = {"pe": nc.tensor, "act": nc.scalar, "dve": nc.vector, "pool": nc.gpsimd, "sp": nc.sync}
        self.sem = {e: es.enter_context(nc.semaphore("s_" + e)) for e in ("pe", "act", "dve", "pool")}
        self.ops = []
        self.eng_ops = {e: [] for e in self.ENG}
        self.waited_c = {e: {x: -1 for x in self.ENG} for e in self.ENG}
        self.waited_d = {e: {} for e in self.ENG}
        self.dsems = []
        self.nsem = 0

    def _collect(self, eng, reads, writes, joins):
        toks = []
        for t in reads:
            toks.extend(t.w)
            if t.excl:
                toks.extend(x for x in t.r if x[0] == "c" and x[1].eng != eng)
        for t in writes:
            toks.extend(t.w)
            toks.extend(t.r)
        for t in joins:
            toks.extend(t.r)
        waits = []
        wc = self.waited_c[eng]
        wd = self.waited_d[eng]
        for tk in toks:
            if tk[0] == "c":
                d = tk[1]
                if d.eng == eng and eng == "pe":
                    continue
                if wc[d.eng] >= d.idx:
                    continue
                wc[d.eng] = d.idx
                d.marked = True
                waits.append(tk)
            else:
                _, sem, val = tk
                k = id(sem)
                if wd.get(k, 0) >= val:
                    continue
                wd[k] = val
                waits.append(tk)
        return waits

    def _post(self, tok, reads, writes, joins):
        for t in writes:
            t.w = [tok]
            t.r = []
        def same(x):
            if x[0] != tok[0]:
                return False
            if tok[0] == "c":
                return x[1].eng == tok[1].eng
            return x[1] is tok[1]
        for t in joins:
            t.w = [x for x in t.w if not same(x)] + [tok]
        for t in reads:
            t.r = [x for x in t.r if not same(x)] + [tok]

    def op(self, eng, method, kwargs, reads=(), writes=(), joins=()):
        o = _Op()
        o.eng = eng
        o.idx = len(self.eng_ops[eng])
        o.method = method
        o.kwargs = kwargs
        o.marked = False
        o.dma_inc = None
        o.waits = self._collect(eng, reads, writes, joins)
        self.eng_ops[eng].append(o)
        self.ops.append(o)
        self._post(("c", o), reads, writes, joins)
        return o

    def _dsem(self, t):
        if t.dsem is None:
            t.dsem = self.es.enter_context(self.nc.semaphore("d%d" % self.nsem))
            self.nsem += 1
            self.dsems.append(t)
        return t.dsem

    def dma(self, q, kwargs, reads=(), writes=(), joins=()):
        o = _Op()
        o.eng = q
        o.idx = -1
        o.method = self.h[q].dma_start
        o.kwargs = kwargs
        o.marked = False
        o.waits = self._collect(q, reads, writes, joins)
        owner = (list(writes) + list(joins) + list(reads))[0]
        sem = self._dsem(owner)
        owner.dcount += 16
        o.dma_inc = (sem, owner.dcount)
        self.ops.append(o)
        self._post(("d", sem, owner.dcount), reads, writes, joins)
        return o

    def barrier(self):
        for e in self.ENG:
            o = _Op()
            o.eng = e
            o.idx = -1
            o.method = None
            o.kwargs = None
            o.marked = False
            o.dma_inc = None
            waits = []
            for x in ("pe", "act", "dve", "pool"):
                if self.eng_ops[x]:
                    d = self.eng_ops[x][-1]
                    if x == e or self.waited_c[e][x] >= d.idx:
                        continue
                    self.waited_c[e][x] = d.idx
                    d.marked = True
                    waits.append(("c", d))
            for t in self.dsems:
                k = id(t.dsem)
                if self.waited_d[e].get(k, 0) < t.dcount:
                    self.waited_d[e][k] = t.dcount
                    waits.append(("d", t.dsem, t.dcount))
            o.waits = waits
            self.ops.append(o)

    def emit(self):
        for e in ("pe", "act", "dve", "pool"):
            r = 0
            for o in self.eng_ops[e]:
                if o.marked:
                    r += 1
                o.rank = r
        nw = 0
        for o in self.ops:
            h = self.h[o.eng]
            for tk in o.waits:
                if tk[0] == "c":
                    h.wait_ge(self.sem[tk[1].eng], tk[1].rank)
                else:
                    h.wait_ge(tk[1], tk[2])
                nw += 1
            if o.method is None:
                continue
            ins = o.method(**o.kwargs)
            if o.dma_inc is not None:
                ins.then_inc(o.dma_inc[0], 16)
            elif o.marked:
                ins.then_inc(self.sem[o.eng], 1)
        self.stats = {e: len(self.eng_ops[e]) for e in self.ENG}
        self.stats["waits"] = nw
        self.stats["ops"] = len(self.ops)


class Arena:
    def __init__(self, base_ap_f32, nbytes):
        self.base = base_ap_f32
        self.n = nbytes
        self.off = 0
        self.peak = 0

    def mark(self):
        return self.off

    def release(self, m):
        self.off = m

    def alloc(self, free_shape, dtype, name=""):
        esz = 2 if dtype == BF16 else 4
        n = int(np.prod(free_shape))
        nb = (n * esz + 63) // 64 * 64
        assert self.off + nb <= self.n, "SBUF arena overflow at %s: %d + %d > %d" % (name, self.off, nb, self.n)
        ap = self.base[:, self.off // 4:(self.off + nb) // 4]
        if dtype == BF16:
            ap = ap.bitcast(BF16)
        ap = ap[:, 0:n]
        if len(free_shape) == 2:
            ap = ap.rearrange("p (a b) -> p a b", a=free_shape[0])
        elif len(free_shape) == 3:
            ap = ap.rearrange("p (a b c) -> p a b c", a=free_shape[0], b=free_shape[1])
        self.off += nb
        self.peak = max(self.peak, self.off)
        return ap


def build_program(stop_after=None, dbg=None, dbg_dt=None):
    nc = bass.Bass("TRN2", target_bir_lowering=False)

    def din(name, shape, dt=F32):
        return nc.dram_tensor(name, list(shape), dt, kind="ExternalInput").ap()

    x_d = din("x", [S, D])
    cT_d = din("cT", [P, KD])
    w_ada_d = din("w_ada", [D, 6 * D])
    b_adaP_d = din("b_adaP", [P, 48])
    b_adaR_d = din("b_adaR", [1, 6 * D])
    g_mixP_d = din("g_mixP", [P, KD])
    g_ffnP_d = din("g_ffnP", [P, KD])
    g_finR_d = din("g_finR", [1, D])
    w_in_d = din("w_in", [D, 2576])
    b_ig_d = din("b_ig", [1, 8])
    b_fg_d = din("b_fg", [1, 8])
    g_headP_d = din("g_headP", [P, 4])
    psclP_d = din("pool_scaleP", [P, 4])
    w_pool_d = din("w_pool", [P, 4, P])
    w_out_d = din("w_out", [D, D])
    w_ff1_d = din("w_ff1", [D, DFF])
    w_ff2_d = din("w_ff2", [DFF, D])
    ident_f_d = din("ident_f", [P, P])
    ident_b_d = din("ident_b", [P, P], BF16)
    maskF_d = din("maskF", [P, P])
    maskB_d = din("maskB", [P, P])
    ones_d = din("ones_f", [P, P])
    edge_d = din("edge_fac", [1, 64])
    out_d = nc.dram_tensor("out", [S, D], F32, kind="ExternalOutput").ap()
    dbg_d = None
    if dbg is not None:
        dbg_d = nc.dram_tensor("dbg", list(dbg), dbg_dt or F32, kind="ExternalOutput").ap()

    w_ada_v = w_ada_d.rearrange("(k p) n -> p k n", p=P)
    w_in_v = w_in_d.rearrange("(k p) n -> p k n", p=P)
    w_out_v = w_out_d.rearrange("(k p) n -> p k n", p=P)
    w_ff1_v = w_ff1_d.rearrange("(k p) n -> p k n", p=P)
    w_ff2_v = w_ff2_d.rearrange("(k p) n -> p k n", p=P)
    x_v = x_d.rearrange("(c p) d -> c p d", p=P)
    out_v = out_d.rearrange("(c p) d -> c p d", p=P)

    SB_BYTES = 207 * 1024
    es = ExitStack()
    with es:
        sb_all = es.enter_context(nc.sbuf_tensor("sb_all", [P, SB_BYTES // 4], F32))
        ps_all = es.enter_context(nc.psum_tensor("ps_all", [P, 4096], F32))
        es.enter_context(nc.Block())
        Sd = Sched(nc, es)
        A = Arena(sb_all[:], SB_BYTES)
        V, ACT, PE, POOL = nc.vector, nc.scalar, nc.tensor, nc.gpsimd

        def bank(b, n=1):
            return ps_all[:, b * 512:(b + n) * 512]

        PS = [T(bank(b), "bank%d" % b, excl=True) for b in range(8)]

        hT_ap = A.alloc([KD, S], BF16, "hT")
        hT = [[T(hT_ap[:, k, g * 512:(g + 1) * 512], "hT%d_%d" % (k, g)) for g in range(NG)] for k in range(KD)]
        ident_b = T(A.alloc([P], BF16, "ident_b"))
        ident_f = T(A.alloc([P], F32, "ident_f"))
        maskF = T(A.alloc([P], F32, "maskF"))
        maskB = T(A.alloc([P], F32, "maskB"))
        ones_f = T(A.alloc([P], F32, "ones"))
        cst = T(A.alloc([64], F32, "cst_misc"))
        modP = T(A.alloc([48], F32, "modP"))
        sc1 = T(A.alloc([KD], F32, "sc1"))
        sc2 = T(A.alloc([KD], F32, "sc2"))
        gt1B = T(A.alloc([D], F32, "gt1B"))
        gt2B = T(A.alloc([D], F32, "gt2B"))
        WGT = T(A.alloc([8, NCH], F32, "WGT"))
        STAB = T(A.alloc([8, NCH], F32, "STAB"))
        DEC = T(A.alloc([8, NCH], F32, "DEC"))

        Sd.op("dve", V.memset, dict(ap=cst.ap[:, 0:1], constant=EPS), writes=[cst])
        for t, d in ((ident_b, ident_b_d), (ident_f, ident_f_d), (maskF, maskF_d), (maskB, maskB_d), (ones_f, ones_d)):
            Sd.dma("sp", dict(out=t.ap, in_=d[:, :]), writes=[t])

        def finish():
            Sd.barrier()
            Sd.emit()
            return nc, Sd, A


        mA = A.mark()
        cT = T(A.alloc([KD], F32, "cT"))
        c_act = T(A.alloc([KD], F32, "c_act"))
        c_rep = T(A.alloc([KD, P], F32, "c_rep"))
        b_adaP = T(A.alloc([48], F32, "b_adaP"))
        g_mixP = T(A.alloc([KD], F32, "g_mixP"))
        g_ffnP = T(A.alloc([KD], F32, "g_ffnP"))
        wA = [T(A.alloc([KD, 512], F32, "wA%d" % i)) for i in range(2)]
        dtmp = T(A.alloc([4, P], F32, "dtmp"))
        psM = PS[0]
        psBc = PS[1]

        Sd.dma("sp", dict(out=cT.ap, in_=cT_d[:, :]), writes=[cT])
        Sd.dma("sp", dict(out=b_adaP.ap, in_=b_adaP_d[:, :]), writes=[b_adaP])
        Sd.dma("sp", dict(out=g_mixP.ap, in_=g_mixP_d[:, :]), writes=[g_mixP])
        Sd.dma("sp", dict(out=g_ffnP.ap, in_=g_ffnP_d[:, :]), writes=[g_ffnP])
        Sd.dma("sp", dict(out=gt1B.ap, in_=b_adaR_d[:, 2 * D:3 * D].partition_broadcast(P)), writes=[gt1B])
        Sd.dma("sp", dict(out=gt2B.ap, in_=b_adaR_d[:, 5 * D:6 * D].partition_broadcast(P)), writes=[gt2B])
        Sd.op("act", ACT.activation, dict(out=c_act.ap, in_=cT.ap, func=AF.Silu), reads=[cT], writes=[c_act])
        Sd.op("dve", V.tensor_copy, dict(out=c_rep.ap, in_=c_act.ap.unsqueeze(2).to_broadcast([P, KD, P])),
              reads=[c_act], writes=[c_rep])

        piece_order = [0, 1, 2, 3, 6, 7, 8, 9, 4, 5, 10, 11]

        def ada_piece(n, pc):
            buf = wA[n % 2]
            Sd.dma("pool", dict(out=buf.ap, in_=w_ada_v[:, :, pc * 512:(pc + 1) * 512]), writes=[buf])
            if pc in (4, 5, 10, 11):
                gt = gt1B if pc in (4, 5) else gt2B
                half = pc % 2
                for k in range(KD):
                    Sd.op("pe", PE.matmul, dict(out=psBc.ap, lhsT=c_rep.ap[:, k, :], rhs=buf.ap[:, k, :],
                                                start=(k == 0), stop=(k == KD - 1)),
                          reads=[c_rep, buf], writes=[psBc] if k == 0 else [], joins=[] if k == 0 else [psBc])
                sl = gt.ap[:, half * 512:(half + 1) * 512]
                Sd.op("dve", V.tensor_tensor, dict(out=sl, in0=psBc.ap, in1=sl, op=ALU.add),
                      reads=[psBc, gt], joins=[gt])
            else:
                for k in range(KD):
                    Sd.op("pe", PE.matmul, dict(out=psM.ap, lhsT=c_rep.ap[:, k, :], rhs=buf.ap[:, k, :],
                                                start=(k == 0), stop=(k == KD - 1)),
                          reads=[c_rep, buf], writes=[psM] if k == 0 else [], joins=[] if k == 0 else [psM])
                Sd.op("dve", V.tensor_tensor, dict(out=dtmp.ap, in0=psM.ap.rearrange("p (a b) -> p a b", a=4),
                                                   in1=ident_f.ap.unsqueeze(1).to_broadcast([P, 4, P]), op=ALU.mult),
                      reads=[psM, ident_f], writes=[dtmp])
                Sd.op("dve", V.tensor_reduce, dict(out=modP.ap[:, pc * 4:(pc + 1) * 4], in_=dtmp.ap, axis=AX.X, op=ALU.add),
                      reads=[dtmp], joins=[modP])
                if pc in (3, 9):
                    lo = 0 if pc == 3 else 24
                    Sd.op("dve", V.tensor_tensor, dict(out=modP.ap[:, lo:lo + 16], in0=modP.ap[:, lo:lo + 16],
                                                       in1=b_adaP.ap[:, lo:lo + 16], op=ALU.add),
                          reads=[modP, b_adaP], joins=[modP])
                    sc = sc1 if pc == 3 else sc2
                    gP = g_mixP if pc == 3 else g_ffnP
                    Sd.op("dve", V.scalar_tensor_tensor, dict(out=sc.ap, in0=modP.ap[:, lo + 8:lo + 16], scalar=1.0,
                                                              in1=gP.ap, op0=ALU.add, op1=ALU.mult),
                          reads=[modP, gP], writes=[sc])
        for n in range(4):
            ada_piece(n, piece_order[n])
        if stop_after == "0":
            Sd.barrier()
            Sd.dma("sp", dict(out=dbg_d[:, 0:48], in_=modP.ap), reads=[modP])
            Sd.dma("sp", dict(out=dbg_d[:, 48:56], in_=sc1.ap), reads=[sc1])
            Sd.dma("sp", dict(out=dbg_d[:, 56:64], in_=sc2.ap), reads=[sc2])
            Sd.dma("sp", dict(out=dbg_d[:, 64:64 + D], in_=gt1B.ap), reads=[gt1B])
            Sd.dma("sp", dict(out=dbg_d[:, 64 + D:64 + 2 * D], in_=gt2B.ap), reads=[gt2B])
            return finish()

        xin = [T(A.alloc([D], F32, "xin%d" % i)) for i in range(4)]
        xn = [T(A.alloc([D], BF16, "xn%d" % i)) for i in range(2)]
        junk = A.alloc([D], BF16, "junk")
        ssq = [T(A.alloc([4], F32, "ssq%d" % i)) for i in range(4)]
        psT_ap = [bank(2 + 2 * i, 2).bitcast(BF16).rearrange("p (k t) -> p k t", k=KD) for i in range(2)]
        psT_t = [[PS[2], PS[3]], [PS[4], PS[5]]]

        import os as _os
        _nch = int(_os.environ.get('K_NCH', NCH)); _var = _os.environ.get('K_VAR', '')
        for c in range(_nch):
            xt = xin[c % 4]
            sq = ssq[c % 4]
            xb = xn[c % 2]
            pT = psT_ap[(c // 2) % 2]
            pTt = psT_t[(c // 2) % 2]
            Sd.dma("sp", dict(out=xt.ap, in_=x_v[c]), writes=[xt])
            Sd.op("act", ACT.activation, dict(out=junk, in_=xt.ap, func=AF.Square, accum_out=sq.ap[:, 0:1]),
                  reads=[xt], writes=[sq])
            Sd.op("act", ACT.activation, dict(out=sq.ap[:, 1:2], in_=sq.ap[:, 0:1], func=AF.Ln, scale=1.0 / D,
                                              bias=cst.ap[:, 0:1]), reads=[sq, cst], joins=[sq])
            Sd.op("act", ACT.activation, dict(out=sq.ap[:, 2:3], in_=sq.ap[:, 1:2], func=AF.Exp, scale=-0.5),
                  reads=[sq], joins=[sq])
            Sd.op("dve", V.tensor_scalar, dict(out=xb.ap, in0=xt.ap, scalar1=sq.ap[:, 2:3], scalar2=None, op0=ALU.mult),
                  reads=[xt, sq], writes=[xb])
            for k in range(KD):
                first = (c % 2 == 0 and k == 0)
                Sd.op("pe", PE.transpose, dict(out=pT[:, k, (c % 2) * P:(c % 2 + 1) * P], in_=xb.ap[:, k * P:(k + 1) * P],
                                               identity=ident_b.ap),
                      reads=[xb, ident_b], writes=pTt if first else [], joins=[] if first else pTt)
            cc = c - 2
            if c == _nch - 1:
                pend = [c - 2, c] if c >= 3 else [c]
            else:
                pend = [cc] if (cc >= 1 and cc % 2 == 1) else []
            for ce in pend:
                g = ce // 4
                off = ((ce // 2) % 2) * 256
                pT = psT_ap[(ce // 2) % 2]
                pTt = psT_t[(ce // 2) % 2]
                for k in range(KD):
                    dst = hT[k][g].ap[:, off:off + 256]
                    if k < 4:
                        Sd.op("act", ACT.activation, dict(out=dst, in_=pT[:, k, :], func=AF.Identity,
                                                          scale=sc1.ap[:, k:k + 1], bias=modP.ap[:, k:k + 1]),
                              reads=[pTt[0], sc1, modP], joins=[hT[k][g]])
                    else:
                        Sd.op("dve", V.tensor_scalar, dict(out=dst, in0=pT[:, k, :], scalar1=sc1.ap[:, k:k + 1],
                                                           scalar2=modP.ap[:, k:k + 1], op0=ALU.mult, op1=ALU.add),
                              reads=[pTt[1], sc1, modP], joins=[hT[k][g]])

        if stop_after == "A2":
            Sd.barrier()
            Sd.dma("sp", dict(out=dbg_d[:, 0:8], in_=sc1.ap), reads=[sc1])
            return finish()
        if stop_after == "A":
            Sd.barrier()
            for k in range(KD):
                for g in range(NG):
                    Sd.dma("sp", dict(out=dbg_d[:, k * S + g * 512:k * S + (g + 1) * 512], in_=hT[k][g].ap), reads=[hT[k][g]])
            return finish()

        bI = T(A.alloc([8], F32, "bI"))
        bF = T(A.alloc([8], F32, "bF"))
        Sd.dma("sp", dict(out=bI.ap, in_=b_ig_d.partition_broadcast(P)), writes=[bI])
        Sd.dma("sp", dict(out=bF.ap, in_=b_fg_d.partition_broadcast(P)), writes=[bF])
        Sd.op("dve", V.memset, dict(ap=cst.ap[:, 1:2], constant=1.0), joins=[cst])
        psG = PS[6]
        psG_ap = bank(6).rearrange("p (d j g) -> p d j g", d=2, j=NCH)
        w_g40 = T(A.alloc([KD, 40], BF16, "w_g40"))
        gTs = T(A.alloc([S], F32, "gTs"))
        Sd.op("dve", V.memset, dict(ap=w_g40.ap, constant=0.0), writes=[w_g40])
        Sd.dma("pool", dict(out=w_g40.ap[:, :, 0:8], in_=w_in_v[:, :, 2048:2056]), reads=[w_g40], joins=[w_g40])
        Sd.dma("pool", dict(out=w_g40.ap[:, :, 32:40], in_=w_in_v[:, :, 2056:2064]), reads=[w_g40], joins=[w_g40])
        for g in range(NG):
            bb = 2 + g % 4
            for k in range(KD):
                Sd.op("pe", PE.matmul, dict(out=bank(bb)[0:40, :], lhsT=w_g40.ap[:, k, :], rhs=hT[k][g].ap,
                                            start=(k == 0), stop=(k == KD - 1)),
                      reads=[w_g40, hT[k][g]], writes=[PS[bb]] if k == 0 else [], joins=[] if k == 0 else [PS[bb]])
            if g % 2 == 0:
                Sd.op("act", ACT.activation, dict(out=gTs.ap[0:40, g * 512:(g + 1) * 512], in_=bank(bb)[0:40, :], func=AF.Copy),
                      reads=[PS[bb]], joins=[gTs])
            else:
                Sd.op("dve", V.tensor_copy, dict(out=gTs.ap[0:40, g * 512:(g + 1) * 512], in_=bank(bb)[0:40, :]),
                      reads=[PS[bb]], joins=[gTs])
        for c in range(NCH):
            for dr in range(2):
                j = c if dr == 0 else NCH - 1 - c
                p0 = dr * 32
                Sd.op("pe", PE.transpose, dict(out=psG_ap[:, dr, j, :], in_=gTs.ap[p0:p0 + 8, c * P:(c + 1) * P],
                                               identity=ident_f.ap[p0:p0 + 8, p0:p0 + 8]),
                      reads=[gTs, ident_f], joins=[psG])

        for n in range(4, 12):
            ada_piece(n, piece_order[n])

        def tab(name, n=NCH):
            return T(A.alloc([8, n], F32, name))

        def v4(ap):
            return ap.rearrange("p (d h) j -> p d h j", d=2)

        def fl(ap):
            return ap.rearrange("p a j -> p (a j)")

        LI, ZZ, CS, GP, GB, BT, RR, TMP = (tab(n) for n in ("LI", "ZZ", "CS", "GP", "GB", "BT", "RR", "TMP"))
        MN = tab("MN", NCH + 1)
        Gcol = T(A.alloc([2], F32, "Gcol"))
        Grow = T(A.alloc([256], F32, "Grow"))
        g_i = psG_ap[:, :, :, 0:4].rearrange("p d j h -> p d h j")
        g_f = psG_ap[:, :, :, 4:8].rearrange("p d j h -> p d h j")
        bIb = bI.ap.rearrange("p (d h) -> p d h", d=2).unsqueeze(3).to_broadcast([P, 2, 4, NCH])
        bFb = bF.ap.rearrange("p (d h) -> p d h", d=2).unsqueeze(3).to_broadcast([P, 2, 4, NCH])
        Sd.op("dve", V.tensor_tensor, dict(out=v4(LI.ap), in0=g_i, in1=bIb, op=ALU.add), reads=[psG, bI], writes=[LI])
        Sd.op("dve", V.tensor_tensor, dict(out=v4(ZZ.ap), in0=g_f, in1=bFb, op=ALU.add), reads=[psG, bF], writes=[ZZ])
        Sd.op("act", ACT.activation, dict(out=fl(ZZ.ap), in_=fl(ZZ.ap), func=AF.Exp, scale=-1.0), reads=[ZZ], writes=[ZZ])
        Sd.op("act", ACT.activation, dict(out=fl(ZZ.ap), in_=fl(ZZ.ap), func=AF.Ln, bias=cst.ap[:, 1:2]), reads=[ZZ, cst], writes=[ZZ])
        if stop_after == "G1":
            Sd.barrier()
            for i, t in enumerate((LI, ZZ)):
                Sd.dma("sp", dict(out=dbg_d[:, i * 256:(i + 1) * 256].rearrange("p (a j) -> p a j", a=8), in_=t.ap[:, :, 0:NCH]), reads=[t])
            return finish()
        psC, psC_ap = PS[7], bank(7)[:, 0:256]
        psBt, psBt_ap = PS[4], bank(4)[:, 0:256]
        Sd.op("pe", PE.matmul, dict(out=psC_ap[:, 0:128], lhsT=maskF.ap, rhs=fl(ZZ.ap)[:, 0:128], start=True, stop=True),
              reads=[maskF, ZZ], writes=[psC])
        Sd.op("pe", PE.matmul, dict(out=psC_ap[:, 128:256], lhsT=maskB.ap, rhs=fl(ZZ.ap)[:, 128:256], start=True, stop=True),
              reads=[maskB, ZZ], joins=[psC])
        Sd.op("pe", PE.matmul, dict(out=psBt_ap, lhsT=ones_f.ap, rhs=fl(ZZ.ap), start=True, stop=True),
              reads=[ones_f, ZZ], writes=[psBt])
        Sd.op("act", ACT.activation, dict(out=fl(CS.ap), in_=psC_ap, func=AF.Copy), reads=[psC], writes=[CS])
        if stop_after == "G2a":
            Sd.barrier()
            Sd.dma("sp", dict(out=dbg_d[:, 0:256].rearrange("p (a j) -> p a j", a=8), in_=CS.ap), reads=[CS])
            return finish()
        Sd.op("dve", V.tensor_tensor, dict(out=fl(GP.ap), in0=psC_ap, in1=fl(LI.ap), op=ALU.add), reads=[psC, LI], writes=[GP])
        Sd.op("act", ACT.activation, dict(out=fl(BT.ap), in_=psBt_ap, func=AF.Copy, scale=-1.0), reads=[psBt], writes=[BT])
        if stop_after == "G2":
            Sd.barrier()
            for i, t in enumerate((CS, GP, BT)):
                Sd.dma("sp", dict(out=dbg_d[:, i * 256:(i + 1) * 256].rearrange("p (a j) -> p a j", a=8), in_=t.ap[:, :, 0:NCH]), reads=[t])
            return finish()
        psX, psX_ap = PS[5], bank(5)[:, 0:256]
        psR, psR_ap = PS[2], bank(2)[0:1, 0:256]
        psGB, psGB_ap = PS[3], bank(3)[:, 0:256]
        for hf in range(2):
            Sd.op("pe", PE.transpose, dict(out=psX_ap[:, hf * P:(hf + 1) * P], in_=fl(GP.ap)[:, hf * P:(hf + 1) * P], identity=ident_f.ap),
                  reads=[GP, ident_f], writes=[psX] if hf == 0 else [], joins=[] if hf == 0 else [psX])
        Sd.op("dve", V.tensor_reduce, dict(out=Gcol.ap, in_=psX_ap.rearrange("p (a s) -> p a s", a=2), axis=AX.X, op=ALU.max),
              reads=[psX], writes=[Gcol])
        for hf in range(2):
            Sd.op("pe", PE.transpose, dict(out=psR_ap[:, hf * P:(hf + 1) * P], in_=Gcol.ap[:, hf:hf + 1], identity=ident_f.ap),
                  reads=[Gcol, ident_f], writes=[psR] if hf == 0 else [], joins=[] if hf == 0 else [psR])
        Sd.op("act", ACT.activation, dict(out=Grow.ap[0:1, :], in_=psR_ap, func=AF.Copy), reads=[psR], writes=[Grow])
        Sd.op("pe", PE.matmul, dict(out=psGB_ap, lhsT=ones_f.ap[0:1, :], rhs=Grow.ap[0:1, :], start=True, stop=True),
              reads=[ones_f, Grow], writes=[psGB])
        Sd.op("act", ACT.activation, dict(out=fl(GB.ap), in_=psGB_ap, func=AF.Copy), reads=[psGB], writes=[GB])
        if stop_after == "G3":
            Sd.barrier()
            for i, t in enumerate((GB, GP)):
                Sd.dma("sp", dict(out=dbg_d[:, i * 256:(i + 1) * 256].rearrange("p (a j) -> p a j", a=8), in_=t.ap[:, :, 0:NCH]), reads=[t])
            return finish()
        Sd.op("dve", V.memset, dict(ap=MN.ap[:, :, 0:1], constant=0.0), writes=[MN])
        for dh in range(8):
            Sd.op("dve", V.tensor_tensor_scan, dict(out=MN.ap[:, dh, 1:NCH + 1], data0=GB.ap[:, dh, :], data1=BT.ap[:, dh, :],
                                                    initial=0.0, op0=ALU.max, op1=ALU.add), reads=[GB, BT], joins=[MN])
        Sd.op("dve", V.tensor_tensor, dict(out=RR.ap, in0=MN.ap[:, :, 0:NCH], in1=GB.ap, op=ALU.max), reads=[MN, GB], writes=[RR])
        for src, dst in ((MN, DEC), (GP, WGT), (CS, STAB)):
            sap = src.ap[:, :, 0:NCH]
            Sd.op("dve", V.tensor_tensor, dict(out=TMP.ap, in0=sap, in1=RR.ap, op=ALU.subtract), reads=[src, RR], writes=[TMP])
            Sd.op("act", ACT.activation, dict(out=fl(dst.ap), in_=fl(TMP.ap), func=AF.Exp), reads=[TMP], writes=[dst])

        if stop_after == "G":
            Sd.barrier()
            for i, t in enumerate((WGT, STAB, DEC, MN)):
                Sd.dma("sp", dict(out=dbg_d[:, i * 256:(i + 1) * 256].rearrange("p (a j) -> p a j", a=8), in_=t.ap[:, :, 0:NCH]), reads=[t])
            return finish()

        Sd.barrier()
        A.release(mA)
        hm_ap = A.alloc([NCH, 512], F32, "hm")
        hm = [[T(hm_ap[:, c, h * P:(h + 1) * P]) for h in range(4)] for c in range(NCH)]
        mH = A.mark()
        wqkv = T(A.alloc([KD, 3, P], BF16, "wqkv"))
        qT_ap = A.alloc([S], BF16, "qT")
        kT_ap = A.alloc([S], BF16, "kT")
        qT = [T(qT_ap[:, g * 512:(g + 1) * 512]) for g in range(NG)]
        kT = [T(kT_ap[:, g * 512:(g + 1) * 512]) for g in range(NG)]
        va_ap = A.alloc([NCH, 130], BF16, "va")
        va = [T(va_ap[:, c, :]) for c in range(NCH)]
        kwF_ap = A.alloc([NCH, P], BF16, "kwF")
        kwB_ap = A.alloc([NCH, P], BF16, "kwB")
        kwF = [T(kwF_ap[:, c, :]) for c in range(NCH)]
        kwB = [T(kwB_ap[:, c, :]) for c in range(NCH)]
        sTw = [T(A.alloc([P], BF16, "sTw%d" % i)) for i in range(4)]
        Cst = [T(A.alloc([130], F32, "Cst%d" % i)) for i in range(2)]
        Csb = [[T(A.alloc([130], BF16, "Csb%d%d" % (i, n))) for n in range(3)] for i in range(2)]
        dnr = [T(A.alloc([4], F32, "dnr%d" % i)) for i in range(4)]
        Sd.op("dve", V.memset, dict(ap=va_ap[:, :, 128:130], constant=1.0), writes=va)
        QSCALE = float(P) ** -0.5
        _nheads = int(_os.environ.get('K_NH', 4))

        for h in range(_nheads):
            for i, off in enumerate((0, 512, 1024)):
                Sd.dma("pool", dict(out=wqkv.ap[:, :, i, :], in_=w_in_v[:, :, off + h * P:off + (h + 1) * P]),
                       writes=[wqkv] if i == 0 else [], joins=[] if i == 0 else [wqkv])
            for g in range(NG):
                for which, dstT, bb in ((0, qT, g % 2), (1, kT, 2 + g % 2)):
                    for k in range(KD):
                        Sd.op("pe", PE.matmul, dict(out=bank(bb), lhsT=wqkv.ap[:, k, which, :], rhs=hT[k][g].ap,
                                                    start=(k == 0), stop=(k == KD - 1)),
                              reads=[wqkv, hT[k][g]], writes=[PS[bb]] if k == 0 else [], joins=[] if k == 0 else [PS[bb]])
                    if which == 0:
                        Sd.op("act", ACT.activation, dict(out=dstT[g].ap, in_=bank(bb), func=AF.Copy, scale=QSCALE),
                              reads=[PS[bb]], writes=[dstT[g]])
                    else:
                        Sd.op("dve", V.tensor_copy, dict(out=dstT[g].ap, in_=bank(bb)), reads=[PS[bb]], writes=[dstT[g]])
                for ci in range(4):
                    c = g * 4 + ci
                    bb = 4 + ci
                    for k in range(KD):
                        Sd.op("pe", PE.matmul, dict(out=bank(bb)[:, 0:256], lhsT=hT[k][g].ap[:, ci * P:(ci + 1) * P],
                                                    rhs=wqkv.ap[:, k, 1:3, :].rearrange("p a b -> p (a b)"),
                                                    start=(k == 0), stop=(k == KD - 1)),
                              reads=[wqkv, hT[k][g]], writes=[PS[bb]] if k == 0 else [], joins=[] if k == 0 else [PS[bb]])
                    Sd.op("act", ACT.activation, dict(out=va[c].ap[:, 0:P], in_=bank(bb)[:, P:2 * P], func=AF.Copy),
                          reads=[PS[bb]], joins=[va[c]])
                    Sd.op("dve", V.tensor_scalar, dict(out=kwF[c].ap, in0=bank(bb)[:, 0:P], scalar1=WGT.ap[:, h, c:c + 1],
                                                       scalar2=None, op0=ALU.mult), reads=[PS[bb], WGT], writes=[kwF[c]])
                    jb = NCH - 1 - c
                    Sd.op("dve", V.tensor_scalar, dict(out=kwB[c].ap, in0=bank(bb)[:, 0:P], scalar1=WGT.ap[:, 4 + h, jb:jb + 1],
                                                       scalar2=None, op0=ALU.mult), reads=[PS[bb], WGT], writes=[kwB[c]])
            NI = 2 * NCH
            ND = (2, 3, 6, 7)

            def item(i):
                j, dr = divmod(i, 2)
                c = j if dr == 0 else NCH - 1 - j
                return j, dr, c, dr * 4 + h, c // 4, (c % 4) * P

            def S1(i):
                j, dr, c, dh, g, o = item(i)
                st = sTw[i % 4]
                Sd.op("pe", PE.matmul, dict(out=bank(dr)[:, 0:P], lhsT=kT[g].ap[:, o:o + P], rhs=qT[g].ap[:, o:o + P],
                                            start=True, stop=True), reads=[kT[g], qT[g]], writes=[PS[dr]])
                Sd.op("dve", V.scalar_tensor_tensor, dict(out=st.ap, in0=bank(dr)[:, 0:P], scalar=WGT.ap[:, dh, j:j + 1],
                                                          in1=(maskF if dr == 0 else maskB).ap, op0=ALU.mult, op1=ALU.mult),
                      reads=[PS[dr], WGT, maskF if dr == 0 else maskB], writes=[st])

            def S2(i):
                j, dr, c, dh, g, o = item(i)
                if j >= NCH - 1:
                    return
                kw = kwF if dr == 0 else kwB
                Sd.op("pe", PE.matmul, dict(out=bank(4 + dr)[:, 0:129], lhsT=kw[c].ap, rhs=va[c].ap[:, 0:129],
                                            start=True, stop=True), reads=[kw[c], va[c]], writes=[PS[4 + dr]])
                if j == 0:
                    Sd.op("dve", V.tensor_copy, dict(out=Cst[dr].ap[:, 0:129], in_=bank(4 + dr)[:, 0:129]),
                          reads=[PS[4 + dr]], writes=[Cst[dr]])
                else:
                    Sd.op("dve", V.scalar_tensor_tensor, dict(out=Cst[dr].ap[:, 0:129], in0=Cst[dr].ap[:, 0:129],
                                                              scalar=DEC.ap[:, dh, j:j + 1], in1=bank(4 + dr)[:, 0:129],
                                                              op0=ALU.mult, op1=ALU.add),
                          reads=[Cst[dr], DEC, PS[4 + dr]], writes=[Cst[dr]])

            def S2b(i):
                j, dr, c, dh, g, o = item(i)
                if j >= NCH - 1:
                    return
                Sd.op("act", ACT.activation, dict(out=Csb[dr][(j + 1) % 3].ap[:, 0:129], in_=Cst[dr].ap[:, 0:129],
                                                  func=AF.Copy, scale=DEC.ap[:, dh, j + 1:j + 2]),
                      reads=[Cst[dr], DEC], writes=[Csb[dr][(j + 1) % 3]])

            def S3(i):
                j, dr, c, dh, g, o = item(i)
                bb = ND[i % 4]
                st = sTw[i % 4]
                Sd.op("pe", PE.matmul, dict(out=bank(bb)[:, 0:129], lhsT=st.ap, rhs=va[c].ap[:, 0:129],
                                            start=True, stop=(j == 0)), reads=[st, va[c]], writes=[PS[bb]])
                if j > 0:
                    Sd.op("pe", PE.matmul, dict(out=bank(bb)[:, 0:129], lhsT=qT[g].ap[:, o:o + P],
                                                rhs=Csb[dr][j % 3].ap[:, 0:129], start=False, stop=True),
                          reads=[qT[g], Csb[dr][j % 3]], joins=[PS[bb]])

            def S4(i):
                j = i // 2
                d_ = dnr[j % 4]
                for dr in range(2):
                    bb = ND[(2 * j + dr) % 4]
                    Sd.op("act", ACT.activation, dict(out=d_.ap[:, dr:dr + 1], in_=bank(bb)[:, 128:129], func=AF.Abs),
                          reads=[PS[bb]], writes=[d_] if dr == 0 else [], joins=[] if dr == 0 else [d_])
                Sd.op("dve", V.tensor_tensor, dict(out=d_.ap[:, 0:2], in0=d_.ap[:, 0:2], in1=STAB.ap[:, h:8:4, j],
                                                   op=ALU.max), reads=[d_, STAB], joins=[d_])
                Sd.op("dve", V.reciprocal, dict(out=d_.ap[:, 2:4], in_=d_.ap[:, 0:2]), reads=[d_], joins=[d_])

            def S5(i):
                j, dr, c, dh, g, o = item(i)
                bb = ND[i % 4]
                d_ = dnr[j % 4]
                if j < NCH // 2:
                    Sd.op("act", ACT.activation, dict(out=hm[c][h].ap, in_=bank(bb)[:, 0:P], func=AF.Copy,
                                                      scale=d_.ap[:, 2 + dr:3 + dr]), reads=[PS[bb], d_], writes=[hm[c][h]])
                else:
                    Sd.op("dve", V.scalar_tensor_tensor, dict(out=hm[c][h].ap, in0=bank(bb)[:, 0:P], scalar=d_.ap[:, 2 + dr:3 + dr],
                                                              in1=hm[c][h].ap, op0=ALU.mult, op1=ALU.add),
                          reads=[PS[bb], d_, hm[c][h]], writes=[hm[c][h]])

            for t in range(NI + 4):
                if t < NI:
                    S1(t)
                if 0 <= t - 2 < NI and (t - 2) % 2 == 1:
                    S4(t - 2)
                if 0 <= t - 4 < NI:
                    S5(t - 4)
                if 0 <= t - 1 < NI:
                    S2b(t - 1)
                if 0 <= t - 1 < NI:
                    S3(t - 1)
                if t < NI:
                    S2(t)

        if stop_after == "H":
            Sd.barrier()
            for c in range(NCH):
                Sd.dma("sp", dict(out=dbg_d[c * P:(c + 1) * P, 0:P * _nheads], in_=hm_ap[:, c, 0:P * _nheads]), reads=hm[c])
            return finish()

        Sd.barrier()
        A.release(mH)
        mY = A.mark()
        w_o = T(A.alloc([KD, 512], BF16, "w_o"))
        w_u = T(A.alloc([KD, 512], BF16, "w_u"))
        w_pl = T(A.alloc([4, P], BF16, "w_pl"))
        psw = T(A.alloc([4], F32, "psw"))
        pscl = T(A.alloc([4], F32, "pscl"))
        edge = T(A.alloc([4, 16], F32, "edge"))
        ubuf = T(A.alloc([4, 528], F32, "ubuf"))
        tP = T(A.alloc([4, 528], F32, "tP"))
        tQ = T(A.alloc([2, 528], F32, "tQ"))
        tA = T(A.alloc([4, 512], F32, "tA"))
        mixed = T(A.alloc([4, 512], BF16, "mixed"))
        ucarry = T(A.alloc([4, 8], F32, "ucarry"))
        sig = [T(A.alloc([512], F32, "sig%d" % i)) for i in range(4)]
        sqb = [T(A.alloc([512], F32, "sqb%d" % i)) for i in range(1)]
        ssh = T(A.alloc([16], F32, "ssh"))
        rsh = T(A.alloc([16], F32, "rsh"))
        yab = [T(A.alloc([512], BF16, "ya%d" % i)) for i in range(4)]
        Sd.dma("pool", dict(out=w_o.ap, in_=w_in_v[:, :, 1536:2048]), writes=[w_o])
        Sd.dma("pool", dict(out=w_u.ap, in_=w_in_v[:, :, 2064:2576]), writes=[w_u])
        Sd.dma("pool", dict(out=w_pl.ap, in_=w_pool_d[:, :, :]), writes=[w_pl])
        Sd.dma("sp", dict(out=pscl.ap, in_=psclP_d[:, :]), writes=[pscl])
        Sd.dma("sp", dict(out=edge.ap.rearrange("p a b -> p (a b)"), in_=edge_d.partition_broadcast(P)), writes=[edge])
        for pg, w in enumerate(POOL_W):
            Sd.op("dve", V.tensor_scalar, dict(out=psw.ap[:, pg:pg + 1], in0=pscl.ap[:, pg:pg + 1], scalar1=1.0 / w, scalar2=None,
                                               op0=ALU.mult), reads=[pscl], joins=[psw])
        Sd.op("dve", V.memset, dict(ap=cst.ap[:, 2:3], constant=EPS), joins=[cst])
        ptr_ap = bank(6, 2).bitcast(BF16).rearrange("p (m t) -> p m t", m=4)

        def stage_U(G):
                U = ubuf.ap
                for pg in range(4):
                    bb = pg % 2
                    for k in range(KD):
                        Sd.op("pe", PE.matmul, dict(out=bank(bb), lhsT=w_u.ap[:, k, pg * P:(pg + 1) * P], rhs=hT[k][G].ap,
                                                    start=(k == 0), stop=(k == KD - 1)),
                              reads=[w_u, hT[k][G]], writes=[PS[bb]] if k == 0 else [], joins=[] if k == 0 else [PS[bb]])
                    Sd.op("act", ACT.activation, dict(out=ubuf.ap[:, pg, 8:520], in_=bank(bb), func=AF.Copy),
                          reads=[PS[bb]], writes=[ubuf] if pg == 0 else [], joins=[] if pg == 0 else [ubuf])
                if G < NG - 1:
                    for pg in range(4):
                        for k in range(KD):
                            Sd.op("pe", PE.matmul, dict(out=bank(2)[:, pg * 8:(pg + 1) * 8], lhsT=w_u.ap[:, k, pg * P:(pg + 1) * P],
                                                        rhs=hT[k][G + 1].ap[:, 0:8], start=(k == 0), stop=(k == KD - 1)),
                                  reads=[w_u, hT[k][G + 1]], writes=[PS[2]] if (k == 0 and pg == 0) else [],
                                  joins=[] if (k == 0 and pg == 0) else [PS[2]])
                    Sd.op("act", ACT.activation, dict(out=ubuf.ap[:, :, 520:528], in_=bank(2)[:, 0:32].rearrange("p (a b) -> p a b", a=4),
                                                      func=AF.Copy), reads=[PS[2]], joins=[ubuf])
                else:
                    Sd.op("pool", POOL.memset, dict(ap=ubuf.ap[:, :, 520:528], constant=0.0), joins=[ubuf])
                if G == 0:
                    Sd.op("pool", POOL.memset, dict(ap=ubuf.ap[:, :, 0:8], constant=0.0), joins=[ubuf])
                else:
                    Sd.op("pool", POOL.tensor_copy, dict(out=ubuf.ap[:, :, 0:8], in_=ucarry.ap), reads=[ucarry], joins=[ubuf])
                Sd.op("pool", POOL.tensor_copy, dict(out=ucarry.ap, in_=ubuf.ap[:, :, 512:520]), reads=[ubuf], writes=[ucarry])
                U = ubuf.ap
                Sd.op("dve", V.tensor_tensor, dict(out=tP.ap[:, :, 0:527], in0=U[:, :, 0:527], in1=U[:, :, 1:528], op=ALU.add),
                      reads=[ubuf], writes=[tP])
                Sd.op("dve", V.tensor_tensor, dict(out=tQ.ap[:, 0:2, 0:525], in0=tP.ap[:, 2:4, 0:525], in1=tP.ap[:, 2:4, 2:527], op=ALU.add),
                      reads=[tP], writes=[tQ])
                Sd.op("dve", V.tensor_tensor, dict(out=tP.ap[:, 3, 0:521], in0=tQ.ap[:, 1, 0:521], in1=tQ.ap[:, 1, 4:525], op=ALU.add),
                      reads=[tQ, tP], joins=[tP])
                Sd.op("pool", POOL.tensor_copy, dict(out=tA.ap[:, 0, :], in_=tP.ap[:, 0, 7:519]), reads=[tP], writes=[tA])
                Sd.op("pool", POOL.tensor_tensor, dict(out=tA.ap[:, 1, :], in0=tP.ap[:, 1, 6:518], in1=tP.ap[:, 1, 8:520], op=ALU.add),
                      reads=[tP], joins=[tA])
                Sd.op("pool", POOL.tensor_tensor, dict(out=tA.ap[:, 2, :], in0=tQ.ap[:, 0, 4:516], in1=tQ.ap[:, 0, 8:520], op=ALU.add),
                      reads=[tQ], joins=[tA])
                Sd.op("pool", POOL.tensor_tensor, dict(out=tA.ap[:, 3, :], in0=tP.ap[:, 3, 0:512], in1=tP.ap[:, 3, 8:520], op=ALU.add),
                      reads=[tP], joins=[tA])
                if G == 0:
                    Sd.op("pool", POOL.tensor_tensor, dict(out=tA.ap[:, :, 0:8], in0=tA.ap[:, :, 0:8], in1=edge.ap[:, :, 0:8], op=ALU.mult),
                          reads=[tA, edge], joins=[tA])
                if G == NG - 1:
                    Sd.op("pool", POOL.tensor_tensor, dict(out=tA.ap[:, :, 504:512], in0=tA.ap[:, :, 504:512], in1=edge.ap[:, :, 8:16], op=ALU.mult),
                          reads=[tA, edge], joins=[tA])
        def stage_O(G):
                for i in range(4):
                    c = G * 4 + i
                    bb = 3 + i % 2
                    for k in range(KD):
                        Sd.op("pe", PE.matmul, dict(out=bank(bb), lhsT=hT[k][G].ap[:, i * P:(i + 1) * P], rhs=w_o.ap[:, k, :],
                                                    start=(k == 0), stop=(k == KD - 1)),
                              reads=[w_o, hT[k][G]], writes=[PS[bb]] if k == 0 else [], joins=[] if k == 0 else [PS[bb]])
                    Sd.op("act", ACT.activation, dict(out=sig[i].ap, in_=bank(bb), func=AF.Sigmoid), reads=[PS[bb]], writes=[sig[i]])
                    sq = sqb[0]
                    Sd.op("dve", V.tensor_tensor, dict(out=sq.ap, in0=hm_ap[:, c, :], in1=hm_ap[:, c, :], op=ALU.mult),
                          reads=hm[c], writes=[sq])
                    Sd.op("dve", V.tensor_reduce, dict(out=ssh.ap[:, i * 4:(i + 1) * 4], in_=sq.ap.rearrange("p (a b) -> p a b", a=4),
                                                       axis=AX.X, op=ALU.add), reads=[sq], writes=[ssh] if i == 0 else [],
                          joins=[] if i == 0 else [ssh])
                Sd.op("act", ACT.activation, dict(out=rsh.ap, in_=ssh.ap, func=AF.Ln, scale=1.0 / P, bias=cst.ap[:, 2:3]),
                      reads=[ssh, cst], writes=[rsh])
                Sd.op("act", ACT.activation, dict(out=rsh.ap, in_=rsh.ap, func=AF.Exp, scale=-0.5), reads=[rsh], writes=[rsh])
        def stage_G(G):
                for i in range(4):
                    c = G * 4 + i
                    ya = yab[i]
                    for h in range(4):
                        Sd.op("dve", V.scalar_tensor_tensor, dict(out=ya.ap[:, h * P:(h + 1) * P], in0=hm[c][h].ap,
                                                                  scalar=rsh.ap[:, i * 4 + h:i * 4 + h + 1], in1=sig[i].ap[:, h * P:(h + 1) * P],
                                                                  op0=ALU.mult, op1=ALU.mult),
                              reads=[hm[c][h], rsh, sig[i]], writes=[ya] if h == 0 else [], joins=[] if h == 0 else [ya])
                    for h in range(4):
                        first = (i == 0 and h == 0)
                        Sd.op("pe", PE.transpose, dict(out=ptr_ap[:, h, i * P:(i + 1) * P], in_=ya.ap[:, h * P:(h + 1) * P], identity=ident_b.ap),
                              reads=[ya, ident_b], writes=[PS[6], PS[7]] if first else [], joins=[] if first else [PS[6], PS[7]])
                for m in range(4):
                    if m < 2:
                        Sd.op("act", ACT.activation, dict(out=hT[m][G].ap, in_=ptr_ap[:, m, :], func=AF.Copy), reads=[PS[6]], writes=[hT[m][G]])
                    else:
                        Sd.op("dve", V.tensor_copy, dict(out=hT[m][G].ap, in_=ptr_ap[:, m, :]), reads=[PS[7]], writes=[hT[m][G]])

        def stage_M(G):
                U = ubuf.ap
                for pg, w in enumerate(POOL_W):
                    Sd.op("dve", V.scalar_tensor_tensor, dict(out=mixed.ap[:, pg, :], in0=U[:, pg, 8:520], scalar=-float(w), in1=tA.ap[:, pg, :],
                                                              op0=ALU.mult, op1=ALU.add),
                          reads=[ubuf, tA], writes=[mixed] if pg == 0 else [], joins=[] if pg == 0 else [mixed])
                for pg in range(4):
                    yb = 5 if pg % 2 == 0 else 2
                    Sd.op("pe", PE.matmul, dict(out=bank(yb), lhsT=w_pl.ap[:, pg, :], rhs=mixed.ap[:, pg, :], start=True, stop=True),
                          reads=[w_pl, mixed], writes=[PS[yb]])
                    Sd.op("act", ACT.activation, dict(out=hT[4 + pg][G].ap, in_=bank(yb), func=AF.Copy, scale=psw.ap[:, pg:pg + 1]),
                          reads=[PS[yb], psw], writes=[hT[4 + pg][G]])


        stage_U(0)
        for G in range(NG):
            stage_O(G)
            stage_M(G)
            if G + 1 < NG:
                stage_U(G + 1)
            stage_G(G)

        if stop_after == "Y":
            Sd.barrier()
            for k in range(KD):
                for g in range(NG):
                    Sd.dma("sp", dict(out=dbg_d[:, k * S + g * 512:k * S + (g + 1) * 512], in_=hT[k][g].ap), reads=[hT[k][g]])
            return finish()

        Sd.barrier()
        A.release(mA)
        w_outp = T(A.alloc([KD, D], BF16, "w_outp"))
        ghP = T(A.alloc([4], F32, "ghP"))
        gfinB = T(A.alloc([D], F32, "gfinB"))
        x1_ap = A.alloc([8, D], F32, "x1")
        x1 = [T(x1_ap[:, i, :]) for i in range(8)]
        aT_ap = A.alloc([8, 2, 512], BF16, "aT")
        aT = [[T(aT_ap[:, fc, tg, :]) for tg in range(2)] for fc in range(8)]
        w1s = [T(A.alloc([KD, 512], BF16, "w1s%d" % i)) for i in range(3)]
        w2s = [T(A.alloc([4, D], BF16, "w2s%d" % i)) for i in range(3)]
        xn2 = [T(A.alloc([D], BF16, "xn2%d" % i)) for i in range(2)]
        rr = [T(A.alloc([512], F32, "rr%d" % i)) for i in range(2)]
        st2 = [T(A.alloc([4], F32, "st2%d" % i)) for i in range(4)]
        junk2 = A.alloc([D], BF16, "junk2")
        stg = [x1[0], x1[1]]
        Sd.dma("sp", dict(out=ghP.ap, in_=g_headP_d[:, :]), writes=[ghP])
        Sd.dma("sp", dict(out=gfinB.ap, in_=g_finR_d.partition_broadcast(P)), writes=[gfinB])
        for k in range(KD):
            sg = stg[k % 2]
            Sd.dma("sp", dict(out=sg.ap, in_=w_out_v[:, k, :]), writes=[sg])
            if k < 4:
                Sd.op("dve", V.scalar_tensor_tensor, dict(out=w_outp.ap[:, k, :], in0=sg.ap, scalar=ghP.ap[:, k:k + 1], in1=gt1B.ap,
                                                          op0=ALU.mult, op1=ALU.mult), reads=[sg, ghP, gt1B], joins=[w_outp])
            else:
                Sd.op("dve", V.tensor_tensor, dict(out=w_outp.ap[:, k, :], in0=sg.ap, in1=gt1B.ap, op=ALU.mult),
                      reads=[sg, gt1B], joins=[w_outp])
        NT4 = 4
        w1_blocks = [(Tt, q, bl) for Tt in range(NT4) for q in range(4) for bl in range(2)]
        gt2b4 = gt2B.ap.unsqueeze(1).to_broadcast([P, 4, D])

        def load_w1(n):
            if n >= len(w1_blocks):
                return
            Tt, q, bl = w1_blocks[n]
            c0 = q * 1024 + bl * 512
            Sd.dma("pool", dict(out=w1s[n % 3].ap, in_=w_ff1_v[:, :, c0:c0 + 512]), writes=[w1s[n % 3]])

        def load_w2(n):
            if n >= len(w1_blocks):
                return
            Tt, q, bl = w1_blocks[n]
            f0 = q * 8 + bl * 4
            t = w2s[n % 3]
            Sd.dma("pool", dict(out=t.ap, in_=w_ff2_v[:, f0:f0 + 4, :]), writes=[t])

        def scale_w2_piece(n, p):
            t = w2s[n % 3]
            ch, hf = p // 2, p % 2
            sl = t.ap[:, ch, hf * 512:(hf + 1) * 512]
            Sd.op("dve", V.tensor_tensor, dict(out=sl, in0=sl, in1=gt2B.ap[:, hf * 512:(hf + 1) * 512], op=ALU.mult),
                  reads=[t, gt2B], joins=[t])

        load_w1(0)
        load_w1(1)
        load_w2(0)
        load_w2(1)
        ps01 = ps_all[:, 0:1024]
        ps67 = ps_all[:, 3072:4096]
        psT2_ap = bank(2, 2).bitcast(BF16).rearrange("p (k t) -> p k t", k=KD)
        def final_norm_store(Tt, st_):
            c = Tt * 8 + st_
            s2 = st2[st_ % 4]
            Sd.op("act", ACT.activation, dict(out=junk2, in_=x1[st_].ap, func=AF.Square, accum_out=s2.ap[:, 0:1]),
                  reads=[x1[st_]], writes=[s2])
            Sd.op("act", ACT.activation, dict(out=s2.ap[:, 1:2], in_=s2.ap[:, 0:1], func=AF.Ln, scale=1.0 / D, bias=cst.ap[:, 0:1]),
                  reads=[s2, cst], joins=[s2])
            Sd.op("act", ACT.activation, dict(out=s2.ap[:, 2:3], in_=s2.ap[:, 1:2], func=AF.Exp, scale=-0.5), reads=[s2], joins=[s2])
            Sd.op("dve", V.scalar_tensor_tensor, dict(out=x1[st_].ap, in0=x1[st_].ap, scalar=s2.ap[:, 2:3], in1=gfinB.ap,
                                                      op0=ALU.mult, op1=ALU.mult), reads=[x1[st_], s2, gfinB], writes=[x1[st_]])
            Sd.dma("sp", dict(out=out_v[c], in_=x1[st_].ap), reads=[x1[st_]])
            if Tt + 1 < NT4:
                Sd.dma("sp", dict(out=x1[st_].ap, in_=x_v[(Tt + 1) * 8 + st_]), writes=[x1[st_]])

        blk = 0
        for Tt in range(NT4):
            def stage_a(st_):
                c = Tt * 8 + st_
                g, o = c // 4, (c % 4) * P
                if Tt == 0:
                    Sd.dma("sp", dict(out=x1[st_].ap, in_=x_v[c]), writes=[x1[st_]])
                pb, pt = ((0, 1), ps01) if st_ % 2 == 0 else ((6, 7), ps67)
                for half in range(2):
                    bb = pb[half]
                    for k in range(KD):
                        Sd.op("pe", PE.matmul, dict(out=bank(bb), lhsT=hT[k][g].ap[:, o:o + P], rhs=w_outp.ap[:, k, half * 512:(half + 1) * 512],
                                                    start=(k == 0), stop=(k == KD - 1)),
                              reads=[hT[k][g], w_outp], writes=[PS[bb]] if k == 0 else [], joins=[] if k == 0 else [PS[bb]])
                Sd.op("dve", V.tensor_tensor, dict(out=x1[st_].ap, in0=pt, in1=x1[st_].ap, op=ALU.add),
                      reads=[PS[pb[0]], PS[pb[1]], x1[st_]], writes=[x1[st_]])

            def stage_b(st_):
                c = Tt * 8 + st_
                g = c // 4
                s2 = st2[st_ % 4]
                xb = xn2[st_ % 2]
                Sd.op("act", ACT.activation, dict(out=junk2, in_=x1[st_].ap, func=AF.Square, accum_out=s2.ap[:, 0:1]),
                      reads=[x1[st_]], writes=[s2])
                Sd.op("act", ACT.activation, dict(out=s2.ap[:, 1:2], in_=s2.ap[:, 0:1], func=AF.Ln, scale=1.0 / D, bias=cst.ap[:, 0:1]),
                      reads=[s2, cst], joins=[s2])
                Sd.op("act", ACT.activation, dict(out=s2.ap[:, 2:3], in_=s2.ap[:, 1:2], func=AF.Exp, scale=-0.5), reads=[s2], joins=[s2])
                Sd.op("dve", V.tensor_scalar, dict(out=xb.ap, in0=x1[st_].ap, scalar1=s2.ap[:, 2:3], scalar2=None, op0=ALU.mult),
                      reads=[x1[st_], s2], writes=[xb])
                for k in range(KD):
                    first = (st_ % 2 == 0 and k == 0)
                    Sd.op("pe", PE.transpose, dict(out=psT2_ap[:, k, (st_ % 2) * P:(st_ % 2 + 1) * P], in_=xb.ap[:, k * P:(k + 1) * P],
                                                   identity=ident_b.ap),
                          reads=[xb, ident_b], writes=[PS[2], PS[3]] if first else [], joins=[] if first else [PS[2], PS[3]])
                if st_ % 2 == 1:
                    off = ((c // 2) % 2) * 256
                    for k in range(KD):
                        dst = hT[k][g].ap[:, off:off + 256]
                        if k < 4:
                            Sd.op("act", ACT.activation, dict(out=dst, in_=psT2_ap[:, k, :], func=AF.Identity,
                                                              scale=sc2.ap[:, k:k + 1], bias=modP.ap[:, 24 + k:25 + k]),
                                  reads=[PS[2], sc2, modP], joins=[hT[k][g]])
                        else:
                            Sd.op("dve", V.tensor_scalar, dict(out=dst, in0=psT2_ap[:, k, :], scalar1=sc2.ap[:, k:k + 1],
                                                               scalar2=modP.ap[:, 24 + k:25 + k], op0=ALU.mult, op1=ALU.add),
                                  reads=[PS[3], sc2, modP], joins=[hT[k][g]])

            for st_ in range(9):
                if st_ < 8:
                    stage_a(st_)
                if st_ >= 1:
                    stage_b(st_ - 1)
            for q in range(4):
                for bl in range(2):
                    n = blk + bl
                    load_w1(n + 2)
                    wt = w1s[n % 3]
                    for f4 in range(4):
                        fc = bl * 4 + f4
                        for tg in range(2):
                            bb = 4 + (fc * 2 + tg) % 2
                            for k in range(KD):
                                Sd.op("pe", PE.matmul, dict(out=bank(bb), lhsT=wt.ap[:, k, f4 * P:(f4 + 1) * P], rhs=hT[k][Tt * 2 + tg].ap,
                                                            start=(k == 0), stop=(k == KD - 1)),
                                      reads=[wt, hT[k][Tt * 2 + tg]], writes=[PS[bb]] if k == 0 else [], joins=[] if k == 0 else [PS[bb]])
                            r_ = rr[(fc * 2 + tg) % 2]
                            Sd.op("act", ACT.activation, dict(out=r_.ap, in_=bank(bb), func=AF.Relu), reads=[PS[bb]], writes=[r_])
                            Sd.op("dve", V.tensor_tensor, dict(out=aT[fc][tg].ap, in0=r_.ap, in1=r_.ap, op=ALU.mult),
                                  reads=[r_], writes=[aT[fc][tg]])
                            pp = f4 * 2 + tg
                            scale_w2_piece(blk + bl, pp)
                load_w2(blk + 2)
                for st_ in range(8):
                    tg, o = st_ // 4, (st_ % 4) * P
                    pb, pt = ((0, 1), ps01) if st_ % 2 == 0 else ((6, 7), ps67)
                    for half in range(2):
                        bb = pb[half]
                        for fc in range(8):
                            wt = w2s[(blk + fc // 4) % 3]
                            Sd.op("pe", PE.matmul, dict(out=bank(bb), lhsT=aT[fc][tg].ap[:, o:o + P], rhs=wt.ap[:, fc % 4, half * 512:(half + 1) * 512],
                                                        start=(fc == 0), stop=(fc == 7)),
                                  reads=[aT[fc][tg], wt], writes=[PS[bb]] if fc == 0 else [], joins=[] if fc == 0 else [PS[bb]])
                    Sd.op("dve", V.tensor_tensor, dict(out=x1[st_].ap, in0=pt, in1=x1[st_].ap, op=ALU.add),
                          reads=[PS[pb[0]], PS[pb[1]], x1[st_]], writes=[x1[st_]])
                    if q == 3:
                        final_norm_store(Tt, st_)
                load_w2(blk + 3)
                blk += 2
        return finish()


def _consts():
    s = np.arange(P)
    maskF = (s[:, None] <= s[None, :]).astype(np.float32)
    maskB = (s[:, None] >= s[None, :]).astype(np.float32)
    edge = np.ones((4, 16), np.float32)
    for g, w in enumerate(POOL_W):
        for t in range(8):
            lo = max(t - w // 2, 0)
            hi = min(t + w // 2, S)
            edge[g, t] = w / float(hi - lo)
            tt = S - 8 + t
            lo = max(tt - w // 2, 0)
            hi = min(tt + w // 2, S)
            edge[g, 8 + t] = w / float(hi - lo)
    return dict(
        ident_f=np.eye(P, dtype=np.float32),
        ident_b=np.eye(P).astype(ml_dtypes.bfloat16),
        maskF=maskF, maskB=maskB,
        ones_f=np.ones((P, P), np.float32),
        edge_fac=edge.reshape(1, 64),
    )


def make_in_maps(x, c, w_ada, b_ada, g_mix, w_in, b_igate, b_fgate, g_head, w_pool, pool_scale,
                 w_out, g_ffn, w_ff1, w_ff2, g_final):
    f = lambda a: np.ascontiguousarray(np.asarray(a, dtype=np.float32))
    cs = _consts()
    shared = dict(
        w_ada=f(w_ada[0]),
        b_adaP=f(np.asarray(b_ada[0]).reshape(48, P).T),
        b_adaR=f(np.asarray(b_ada[0]).reshape(1, 6 * D)),
        g_mixP=f(np.asarray(g_mix[0]).reshape(KD, P).T),
        g_ffnP=f(np.asarray(g_ffn[0]).reshape(KD, P).T),
        g_finR=f(np.asarray(g_final).reshape(1, D)),
        w_in=f(w_in[0]),
        b_ig=f(np.asarray(b_igate[0]).reshape(1, 8)),
        b_fg=f(np.asarray(b_fgate[0]).reshape(1, 8)),
        g_headP=f(np.asarray(g_head[0]).reshape(4, P).T),
        pool_scaleP=f(np.asarray(pool_scale[0]).reshape(4, P).T),
        w_pool=f(np.asarray(w_pool[0]).transpose(1, 0, 2)),
        w_out=f(w_out[0]), w_ff1=f(w_ff1[0]), w_ff2=f(w_ff2[0]),
        **cs,
    )
    maps = []
    for b in range(N_CORES):
        m = dict(shared)
        m["x"] = f(x[b])
        m["cT"] = f(np.asarray(c[b]).reshape(KD, P).T)
        maps.append(m)
    return maps


_CACHE = {}


def kernel(**inputs):
    if "nc" not in _CACHE:
        _CACHE["nc"] = build_program()[0]
    nc = _CACHE["nc"]
    in_maps = make_in_maps(**inputs)
    res = run_bass_kernel_spmd(nc, in_maps, core_ids=list(range(N_CORES)))
    out = np.stack([np.asarray(r["out"]) for r in res.results], axis=0)
    return out.astype(np.float32)
```

```python
import numpy as np
import ml_dtypes
from contextlib import ExitStack

import concourse.bass as bass
import concourse.mybir as mybir
from concourse.bass_utils import run_bass_kernel_spmd

F32 = mybir.dt.float32
BF16 = mybir.dt.bfloat16
AF = mybir.ActivationFunctionType
ALU = mybir.AluOpType
AX = mybir.AxisListType

P = 128
S = 4096
D = 1024
KD = 8
NCH = 32
NG = 8
DFF = 4096
EPS = 1e-6
POOL_W = (2, 4, 8, 16)
N_CORES = 8


class T:
    __slots__ = ("ap", "w", "r", "dsem", "dcount", "name", "excl")

    def __init__(self, ap, name="", excl=False):
        self.ap = ap
        self.excl = excl
        self.w = []
        self.r = []
        self.dsem = None
        self.dcount = 0
        self.name = name

    def __getitem__(self, k):
        return self.ap[k]


class _Op:
    __slots__ = ("eng", "idx", "method", "kwargs", "waits", "marked", "dma_inc", "rank")


class Sched:
    ENG = ("pe", "act", "dve", "pool", "sp")

    def __init__(self, nc, es):
        self.nc = nc
        self.es = es
        self.h = {"pe": nc.tensor, "act": nc.scalar, "dve": nc.vector, "pool": nc.gpsimd, "sp": nc.sync}
        self.sem = {e: es.enter_context(nc.semaphore("s_" + e)) for e in ("pe", "act", "dve", "pool")}
        self.ops = []
        self.eng_ops = {e: [] for e in self.ENG}
        self.waited_c = {e: {x: -1 for x in self.ENG} for e in self.ENG}
        self.waited_d = {e: {} for e in self.ENG}
        self.dsems = []
        self.nsem = 0

    def _collect(self, eng, reads, writes, joins):
        toks = []
        for t in reads:
            toks.extend(t.w)
            if t.excl:
                toks.extend(x for x in t.r if x[0] == "c" and x[1].eng != eng)
        for t in writes:
            toks.extend(t.w)
            toks.extend(t.r)
        for t in joins:
            toks.extend(t.r)
        waits = []
        wc = self.waited_c[eng]
        wd = self.waited_d[eng]
        for tk in toks:
            if tk[0] == "c":
                d = tk[1]
                if d.eng == eng and eng == "pe":
                    continue
                if wc[d.eng] >= d.idx:
                    continue
                wc[d.eng] = d.idx
                d.marked = True
                waits.append(tk)
            else:
                _, sem, val = tk
                k = id(sem)
                if wd.get(k, 0) >= val:
                    continue
                wd[k] = val
                waits.append(tk)
        return waits

    def _post(self, tok, reads, writes, joins):
        for t in writes:
            t.w = [tok]
            t.r = []
        def same(x):
            if x[0] != tok[0]:
                return False
            if tok[0] == "c":
                return x[1].eng == tok[1].eng
            return x[1] is tok[1]
        for t in joins:
            t.w = [x for x in t.w if not same(x)] + [tok]
        for t in reads:
            t.r = [x for x in t.r if not same(x)] + [tok]

    def op(self, eng, method, kwargs, reads=(), writes=(), joins=()):
        o = _Op()
        o.eng = eng
        o.idx = len(self.eng_ops[eng])
        o.method = method
        o.kwargs = kwargs
        o.marked = False
        o.dma_inc = None
        o.waits = self._collect(eng, reads, writes, joins)
        self.eng_ops[eng].append(o)
        self.ops.append(o)
        self._post(("c", o), reads, writes, joins)
        return o

    def _dsem(self, t):
        if t.dsem is None:
            t.dsem = self.es.enter_context(self.nc.semaphore("d%d" % self.nsem))
            self.nsem += 1
            self.dsems.append(t)
        return t.dsem

    def dma(self, q, kwargs, reads=(), writes=(), joins=()):
        o = _Op()
        o.eng = q
        o.idx = -1
        o.method = self.h[q].dma_start
        o.kwargs = kwargs
        o.marked = False
        o.waits = self._collect(q, reads, writes, joins)
        owner = (list(writes) + list(joins) + list(reads))[0]
        sem = self._dsem(owner)
        owner.dcount += 16
        o.dma_inc = (sem, owner.dcount)
        self.ops.append(o)
        self._post(("d", sem, owner.dcount), reads, writes, joins)
        return o

    def barrier(self):
        for e in self.ENG:
            o = _Op()
            o.eng = e
            o.idx = -1
            o.method = None
            o.kwargs = None
            o.marked = False
            o.dma_inc = None
            waits = []
            for x in ("pe", "act", "dve", "pool"):
                if self.eng_ops[x]:
                    d = self.eng_ops[x][-1]
                    if x == e or self.waited_c[e][x] >= d.idx:
                        continue
                    self.waited_c[e][x] = d.idx
                    d.marked = True
                    waits.append(("c", d))
            for t in self.dsems:
                k = id(t.dsem)
                if self.waited_d[e].get(k, 0) < t.dcount:
                    self.waited_d[e][k] = t.dcount
                    waits.append(("d", t.dsem, t.dcount))
            o.waits = waits
            self.ops.append(o)

    def emit(self):
        for e in ("pe", "act", "dve", "pool"):
            r = 0
            for o in self.eng_ops[e]:
                if o.marked:
                    r += 1
                o.rank = r
        nw = 0
        for o in self.ops:
            h = self.h[o.eng]
            for tk in o.waits:
                if tk[0] == "c":
                    h.wait_ge(self.sem[tk[1].eng], tk[1].rank)
                else:
                    h.wait_ge(tk[1], tk[2])
                nw += 1
            if o.method is None:
                continue
            ins = o.method(**o.kwargs)
            if o.dma_inc is not None:
                ins.then_inc(o.dma_inc[0], 16)
            elif o.marked:
                ins.then_inc(self.sem[o.eng], 1)
        self.stats = {e: len(self.eng_ops[e]) for e in self.ENG}
        self.stats["waits"] = nw
        self.stats["ops"] = len(self.ops)


class Arena:
    def __init__(self, base_ap_f32, nbytes):
        self.base = base_ap_f32
        self.n = nbytes
        self.off = 0
        self.peak = 0

    def mark(self):
        return self.off

    def release(self, m):
        self.off = m

    def alloc(self, free_shape, dtype, name=""):
        esz = 2 if dtype == BF16 else 4
        n = int(np.prod(free_shape))
        nb = (n * esz + 63) // 64 * 64
        assert self.off + nb <= self.n, "SBUF arena overflow at %s: %d + %d > %d" % (name, self.off, nb, self.n)
        ap = self.base[:, self.off // 4:(self.off + nb) // 4]
        if dtype == BF16:
            ap = ap.bitcast(BF16)
        ap = ap[:, 0:n]
        if len(free_shape) == 2:
            ap = ap.rearrange("p (a b) -> p a b", a=free_shape[0])
        elif len(free_shape) == 3:
            ap = ap.rearrange("p (a b c) -> p a b c", a=free_shape[0], b=free_shape[1])
        self.off += nb
        self.peak = max(self.peak, self.off)
        return ap


def build_program(stop_after=None, dbg=None, dbg_dt=None):
    nc = bass.Bass("TRN2", target_bir_lowering=False)

    def din(name, shape, dt=F32):
        return nc.dram_tensor(name, list(shape), dt, kind="ExternalInput").ap()

    x_d = din("x", [S, D])
    cT_d = din("cT", [P, KD])
    w_ada_d = din("w_ada", [D, 6 * D])
    b_adaP_d = din("b_adaP", [P, 48])
    b_adaR_d = din("b_adaR", [1, 6 * D])
    g_mixP_d = din("g_mixP", [P, KD])
    g_ffnP_d = din("g_ffnP", [P, KD])
    g_finR_d = din("g_finR", [1, D])
    w_in_d = din("w_in", [D, 2576])
    b_ig_d = din("b_ig", [1, 8])
    b_fg_d = din("b_fg", [1, 8])
    g_headP_d = din("g_headP", [P, 4])
    psclP_d = din("pool_scaleP", [P, 4])
    w_pool_d = din("w_pool", [P, 4, P])
    w_out_d = din("w_out", [D, D])
    w_ff1_d = din("w_ff1", [D, DFF])
    w_ff2_d = din("w_ff2", [DFF, D])
    ident_f_d = din("ident_f", [P, P])
    ident_b_d = din("ident_b", [P, P], BF16)
    maskF_d = din("maskF", [P, P])
    maskB_d = din("maskB", [P, P])
    ones_d = din("ones_f", [P, P])
    edge_d = din("edge_fac", [1, 64])
    out_d = nc.dram_tensor("out", [S, D], F32, kind="ExternalOutput").ap()
    dbg_d = None
    if dbg is not None:
        dbg_d = nc.dram_tensor("dbg", list(dbg), dbg_dt or F32, kind="ExternalOutput").ap()

    w_ada_v = w_ada_d.rearrange("(k p) n -> p k n", p=P)
    w_in_v = w_in_d.rearrange("(k p) n -> p k n", p=P)
    w_out_v = w_out_d.rearrange("(k p) n -> p k n", p=P)
    w_ff1_v = w_ff1_d.rearrange("(k p) n -> p k n", p=P)
    w_ff2_v = w_ff2_d.rearrange("(k p) n -> p k n", p=P)
    x_v = x_d.rearrange("(c p) d -> c p d", p=P)
    out_v = out_d.rearrange("(c p) d -> c p d", p=P)

    SB_BYTES = 207 * 1024
    es = ExitStack()
    with es:
        sb_all = es.enter_context(nc.sbuf_tensor("sb_all", [P, SB_BYTES // 4], F32))
        ps_all = es.enter_context(nc.psum_tensor("ps_all", [P, 4096], F32))
        es.enter_context(nc.Block())
        Sd = Sched(nc, es)
        A = Arena(sb_all[:], SB_BYTES)
        V, ACT, PE, POOL = nc.vector, nc.scalar, nc.tensor, nc.gpsimd

        def bank(b, n=1):
            return ps_all[:, b * 512:(b + n) * 512]

        PS = [T(bank(b), "bank%d" % b, excl=True) for b in range(8)]

        hT_ap = A.alloc([KD, S], BF16, "hT")
        hT = [[T(hT_ap[:, k, g * 512:(g + 1) * 512], "hT%d_%d" % (k, g)) for g in range(NG)] for k in range(KD)]
        ident_b = T(A.alloc([P], BF16, "ident_b"))
        ident_f = T(A.alloc([P], F32, "ident_f"))
        maskF = T(A.alloc([P], F32, "maskF"))
        maskB = T(A.alloc([P], F32, "maskB"))
        ones_f = T(A.alloc([P], F32, "ones"))
        cst = T(A.alloc([64], F32, "cst_misc"))
        modP = T(A.alloc([48], F32, "modP"))
        sc1 = T(A.alloc([KD], F32, "sc1"))
        sc2 = T(A.alloc([KD], F32, "sc2"))
        gt1B = T(A.alloc([D], F32, "gt1B"))
        gt2B = T(A.alloc([D], F32, "gt2B"))
        WGT = T(A.alloc([8, NCH], F32, "WGT"))
        STAB = T(A.alloc([8, NCH], F32, "STAB"))
        DEC = T(A.alloc([8, NCH], F32, "DEC"))

        Sd.op("dve", V.memset, dict(ap=cst.ap[:, 0:1], constant=EPS), writes=[cst])
        for t, d in ((ident_b, ident_b_d), (ident_f, ident_f_d), (maskF, maskF_d), (maskB, maskB_d), (ones_f, ones_d)):
            Sd.dma("sp", dict(out=t.ap, in_=d[:, :]), writes=[t])

        def finish():
            Sd.barrier()
            Sd.emit()
            return nc, Sd, A


        mA = A.mark()
        cT = T(A.alloc([KD], F32, "cT"))
        c_act = T(A.alloc([KD], F32, "c_act"))
        c_rep = T(A.alloc([KD, P], F32, "c_rep"))
        b_adaP = T(A.alloc([48], F32, "b_adaP"))
        g_mixP = T(A.alloc([KD], F32, "g_mixP"))
        g_ffnP = T(A.alloc([KD], F32, "g_ffnP"))
        wA = [T(A.alloc([KD, 512], F32, "wA%d" % i)) for i in range(2)]
        dtmp = T(A.alloc([4, P], F32, "dtmp"))
        psM = PS[0]
        psBc = PS[1]

        Sd.dma("sp", dict(out=cT.ap, in_=cT_d[:, :]), writes=[cT])
        Sd.dma("sp", dict(out=b_adaP.ap, in_=b_adaP_d[:, :]), writes=[b_adaP])
        Sd.dma("sp", dict(out=g_mixP.ap, in_=g_mixP_d[:, :]), writes=[g_mixP])
        Sd.dma("sp", dict(out=g_ffnP.ap, in_=g_ffnP_d[:, :]), writes=[g_ffnP])
        Sd.dma("sp", dict(out=gt1B.ap, in_=b_adaR_d[:, 2 * D:3 * D].partition_broadcast(P)), writes=[gt1B])
        Sd.dma("sp", dict(out=gt2B.ap, in_=b_adaR_d[:, 5 * D:6 * D].partition_broadcast(P)), writes=[gt2B])
        Sd.op("act", ACT.activation, dict(out=c_act.ap, in_=cT.ap, func=AF.Silu), reads=[cT], writes=[c_act])
        Sd.op("dve", V.tensor_copy, dict(out=c_rep.ap, in_=c_act.ap.unsqueeze(2).to_broadcast([P, KD, P])),
              reads=[c_act], writes=[c_rep])

        piece_order = [0, 1, 2, 3, 6, 7, 8, 9, 4, 5, 10, 11]

        def ada_piece(n, pc):
            buf = wA[n % 2]
            Sd.dma("pool", dict(out=buf.ap, in_=w_ada_v[:, :, pc * 512:(pc + 1) * 512]), writes=[buf])
            if pc in (4, 5, 10, 11):
                gt = gt1B if pc in (4, 5) else gt2B
                half = pc % 2
                for k in range(KD):
                    Sd.op("pe", PE.matmul, dict(out=psBc.ap, lhsT=c_rep.ap[:, k, :], rhs=buf.ap[:, k, :],
                                                start=(k == 0), stop=(k == KD - 1)),
                          reads=[c_rep, buf], writes=[psBc] if k == 0 else [], joins=[] if k == 0 else [psBc])
                sl = gt.ap[:, half * 512:(half + 1) * 512]
                Sd.op("dve", V.tensor_tensor, dict(out=sl, in0=psBc.ap, in1=sl, op=ALU.add),
                      reads=[psBc, gt], joins=[gt])
            else:
                for k in range(KD):
                    Sd.op("pe", PE.matmul, dict(out=psM.ap, lhsT=c_rep.ap[:, k, :], rhs=buf.ap[:, k, :],
                                                start=(k == 0), stop=(k == KD - 1)),
                          reads=[c_rep, buf], writes=[psM] if k == 0 else [], joins=[] if k == 0 else [psM])
                Sd.op("dve", V.tensor_tensor, dict(out=dtmp.ap, in0=psM.ap.rearrange("p (a b) -> p a b", a=4),
                                                   in1=ident_f.ap.unsqueeze(1).to_broadcast([P, 4, P]), op=ALU.mult),
                      reads=[psM, ident_f], writes=[dtmp])
                Sd.op("dve", V.tensor_reduce, dict(out=modP.ap[:, pc * 4:(pc + 1) * 4], in_=dtmp.ap, axis=AX.X, op=ALU.add),
                      reads=[dtmp], joins=[modP])
                if pc in (3, 9):
                    lo = 0 if pc == 3 else 24
                    Sd.op("dve", V.tensor_tensor, dict(out=modP.ap[:, lo:lo + 16], in0=modP.ap[:, lo:lo + 16],
                                                       in1=b_adaP.ap[:, lo:lo + 16], op=ALU.add),
                          reads=[modP, b_adaP], joins=[modP])
                    sc = sc1 if pc == 3 else sc2
                    gP = g_mixP if pc == 3 else g_ffnP
                    Sd.op("dve", V.scalar_tensor_tensor, dict(out=sc.ap, in0=modP.ap[:, lo + 8:lo + 16], scalar=1.0,
                                                              in1=gP.ap, op0=ALU.add, op1=ALU.mult),
                          reads=[modP, gP], writes=[sc])
        for n in range(4):
            ada_piece(n, piece_order[n])
        if stop_after == "0":
            Sd.barrier()
            Sd.dma("sp", dict(out=dbg_d[:, 0:48], in_=modP.ap), reads=[modP])
            Sd.dma("sp", dict(out=dbg_d[:, 48:56], in_=sc1.ap), reads=[sc1])
            Sd.dma("sp", dict(out=dbg_d[:, 56:64], in_=sc2.ap), reads=[sc2])
            Sd.dma("sp", dict(out=dbg_d[:, 64:64 + D], in_=gt1B.ap), reads=[gt1B])
            Sd.dma("sp", dict(out=dbg_d[:, 64 + D:64 + 2 * D], in_=gt2B.ap), reads=[gt2B])
            return finish()

        xin = [T(A.alloc([D], F32, "xin%d" % i)) for i in range(4)]
        xn = [T(A.alloc([D], BF16, "xn%d" % i)) for i in range(2)]
        junk = A.alloc([D], BF16, "junk")
        ssq = [T(A.alloc([4], F32, "ssq%d" % i)) for i in range(4)]
        psT_ap = [bank(2 + 2 * i, 2).bitcast(BF16).rearrange("p (k t) -> p k t", k=KD) for i in range(2)]
        psT_t = [[PS[2], PS[3]], [PS[4], PS[5]]]

        import os as _os
        _nch = int(_os.environ.get('K_NCH', NCH)); _var = _os.environ.get('K_VAR', '')
        for c in range(_nch):
            xt = xin[c % 4]
            sq = ssq[c % 4]
            xb = xn[c % 2]
            pT = psT_ap[(c // 2) % 2]
            pTt = psT_t[(c // 2) % 2]
            Sd.dma("sp", dict(out=xt.ap, in_=x_v[c]), writes=[xt])
            Sd.op("act", ACT.activation, dict(out=junk, in_=xt.ap, func=AF.Square, accum_out=sq.ap[:, 0:1]),
                  reads=[xt], writes=[sq])
            Sd.op("act", ACT.activation, dict(out=sq.ap[:, 1:2], in_=sq.ap[:, 0:1], func=AF.Ln, scale=1.0 / D,
                                              bias=cst.ap[:, 0:1]), reads=[sq, cst], joins=[sq])
            Sd.op("act", ACT.activation, dict(out=sq.ap[:, 2:3], in_=sq.ap[:, 1:2], func=AF.Exp, scale=-0.5),
                  reads=[sq], joins=[sq])
            Sd.op("dve", V.tensor_scalar, dict(out=xb.ap, in0=xt.ap, scalar1=sq.ap[:, 2:3], scalar2=None, op0=ALU.mult),
                  reads=[xt, sq], writes=[xb])
            for k in range(KD):
                first = (c % 2 == 0 and k == 0)
                Sd.op("pe", PE.transpose, dict(out=pT[:, k, (c % 2) * P:(c % 2 + 1) * P], in_=xb.ap[:, k * P:(k + 1) * P],
                                               identity=ident_b.ap),
                      reads=[xb, ident_b], writes=pTt if first else [], joins=[] if first else pTt)
            cc = c - 2
            if c == _nch - 1:
                pend = [c - 2, c] if c >= 3 else [c]
            else:
                pend = [cc] if (cc >= 1 and cc % 2 == 1) else []
            for ce in pend:
                g = ce // 4
                off = ((ce // 2) % 2) * 256
                pT = psT_ap[(ce // 2) % 2]
                pTt = psT_t[(ce // 2) % 2]
                for k in range(KD):
                    dst = hT[k][g].ap[:, off:off + 256]
                    if k < 4:
                        Sd.op("act", ACT.activation, dict(out=dst, in_=pT[:, k, :], func=AF.Identity,
                                                          scale=sc1.ap[:, k:k + 1], bias=modP.ap[:, k:k + 1]),
                              reads=[pTt[0], sc1, modP], joins=[hT[k][g]])
                    else:
                        Sd.op("dve", V.tensor_scalar, dict(out=dst, in0=pT[:, k, :], scalar1=sc1.ap[:, k:k + 1],
                                                           scalar2=modP.ap[:, k:k + 1], op0=ALU.mult, op1=ALU.add),
                              reads=[pTt[1], sc1, modP], joins=[hT[k][g]])

        if stop_after == "A2":
            Sd.barrier()
            Sd.dma("sp", dict(out=dbg_d[:, 0:8], in_=sc1.ap), reads=[sc1])
            return finish()
        if stop_after == "A":
            Sd.barrier()
            for k in range(KD):
                for g in range(NG):
                    Sd.dma("sp", dict(out=dbg_d[:, k * S + g * 512:k * S + (g + 1) * 512], in_=hT[k][g].ap), reads=[hT[k][g]])
            return finish()

        bI = T(A.alloc([8], F32, "bI"))
        bF = T(A.alloc([8], F32, "bF"))
        Sd.dma("sp", dict(out=bI.ap, in_=b_ig_d.partition_broadcast(P)), writes=[bI])
        Sd.dma("sp", dict(out=bF.ap, in_=b_fg_d.partition_broadcast(P)), writes=[bF])
        Sd.op("dve", V.memset, dict(ap=cst.ap[:, 1:2], constant=1.0), joins=[cst])
        psG = PS[6]
        psG_ap = bank(6).rearrange("p (d j g) -> p d j g", d=2, j=NCH)
        w_g40 = T(A.alloc([KD, 40], BF16, "w_g40"))
        gTs = T(A.alloc([S], F32, "gTs"))
        Sd.op("dve", V.memset, dict(ap=w_g40.ap, constant=0.0), writes=[w_g40])
        Sd.dma("pool", dict(out=w_g40.ap[:, :, 0:8], in_=w_in_v[:, :, 2048:2056]), reads=[w_g40], joins=[w_g40])
        Sd.dma("pool", dict(out=w_g40.ap[:, :, 32:40], in_=w_in_v[:, :, 2056:2064]), reads=[w_g40], joins=[w_g40])
        for g in range(NG):
            bb = 2 + g % 4
            for k in range(KD):
                Sd.op("pe", PE.matmul, dict(out=bank(bb)[0:40, :], lhsT=w_g40.ap[:, k, :], rhs=hT[k][g].ap,
                                            start=(k == 0), stop=(k == KD - 1)),
                      reads=[w_g40, hT[k][g]], writes=[PS[bb]] if k == 0 else [], joins=[] if k == 0 else [PS[bb]])
            if g % 2 == 0:
                Sd.op("act", ACT.activation, dict(out=gTs.ap[0:40, g * 512:(g + 1) * 512], in_=bank(bb)[0:40, :], func=AF.Copy),
                      reads=[PS[bb]], joins=[gTs])
            else:
                Sd.op("dve", V.tensor_copy, dict(out=gTs.ap[0:40, g * 512:(g + 1) * 512], in_=bank(bb)[0:40, :]),
                      reads=[PS[bb]], joins=[gTs])
        for c in range(NCH):
            for dr in range(2):
                j = c if dr == 0 else NCH - 1 - c
                p0 = dr * 32
                Sd.op("pe", PE.transpose, dict(out=psG_ap[:, dr, j, :], in_=gTs.ap[p0:p0 + 8, c * P:(c + 1) * P],
                                               identity=ident_f.ap[p0:p0 + 8, p0:p0 + 8]),
                      reads=[gTs, ident_f], joins=[psG])

        for n in range(4, 12):
            ada_piece(n, piece_order[n])

        def tab(name, n=NCH):
            return T(A.alloc([8, n], F32, name))

        def v4(ap):
            return ap.rearrange("p (d h) j -> p d h j", d=2)

        def fl(ap):
            return ap.rearrange("p a j -> p (a j)")

        LI, ZZ, CS, GP, GB, BT, RR, TMP = (tab(n) for n in ("LI", "ZZ", "CS", "GP", "GB", "BT", "RR", "TMP"))
        MN = tab("MN", NCH + 1)
        Gcol = T(A.alloc([2], F32, "Gcol"))
        Grow = T(A.alloc([256], F32, "Grow"))
        g_i = psG_ap[:, :, :, 0:4].rearrange("p d j h -> p d h j")
        g_f = psG_ap[:, :, :, 4:8].rearrange("p d j h -> p d h j")
        bIb = bI.ap.rearrange("p (d h) -> p d h", d=2).unsqueeze(3).to_broadcast([P, 2, 4, NCH])
        bFb = bF.ap.rearrange("p (d h) -> p d h", d=2).unsqueeze(3).to_broadcast([P, 2, 4, NCH])
        Sd.op("dve", V.tensor_tensor, dict(out=v4(LI.ap), in0=g_i, in1=bIb, op=ALU.add), reads=[psG, bI], writes=[LI])
        Sd.op("dve", V.tensor_tensor, dict(out=v4(ZZ.ap), in0=g_f, in1=bFb, op=ALU.add), reads=[psG, bF], writes=[ZZ])
        Sd.op("act", ACT.activation, dict(out=fl(ZZ.ap), in_=fl(ZZ.ap), func=AF.Exp, scale=-1.0), reads=[ZZ], writes=[ZZ])
        Sd.op("act", ACT.activation, dict(out=fl(ZZ.ap), in_=fl(ZZ.ap), func=AF.Ln, bias=cst.ap[:, 1:2]), reads=[ZZ, cst], writes=[ZZ])
        if stop_after == "G1":
            Sd.barrier()
            for i, t in enumerate((LI, ZZ)):
                Sd.dma("sp", dict(out=dbg_d[:, i * 256:(i + 1) * 256].rearrange("p (a j) -> p a j", a=8), in_=t.ap[:, :, 0:NCH]), reads=[t])
            return finish()
        psC, psC_ap = PS[7], bank(7)[:, 0:256]
        psBt, psBt_ap = PS[4], bank(4)[:, 0:256]
        Sd.op("pe", PE.matmul, dict(out=psC_ap[:, 0:128], lhsT=maskF.ap, rhs=fl(ZZ.ap)[:, 0:128], start=True, stop=True),
              reads=[maskF, ZZ], writes=[psC])
        Sd.op("pe", PE.matmul, dict(out=psC_ap[:, 128:256], lhsT=maskB.ap, rhs=fl(ZZ.ap)[:, 128:256], start=True, stop=True),
              reads=[maskB, ZZ], joins=[psC])
        Sd.op("pe", PE.matmul, dict(out=psBt_ap, lhsT=ones_f.ap, rhs=fl(ZZ.ap), start=True, stop=True),
              reads=[ones_f, ZZ], writes=[psBt])
        Sd.op("act", ACT.activation, dict(out=fl(CS.ap), in_=psC_ap, func=AF.Copy), reads=[psC], writes=[CS])
        if stop_after == "G2a":
            Sd.barrier()
            Sd.dma("sp", dict(out=dbg_d[:, 0:256].rearrange("p (a j) -> p a j", a=8), in_=CS.ap), reads=[CS])
            return finish()
        Sd.op("dve", V.tensor_tensor, dict(out=fl(GP.ap), in0=psC_ap, in1=fl(LI.ap), op=ALU.add), reads=[psC, LI], writes=[GP])
        Sd.op("act", ACT.activation, dict(out=fl(BT.ap), in_=psBt_ap, func=AF.Copy, scale=-1.0), reads=[psBt], writes=[BT])
        if stop_after == "G2":
            Sd.barrier()
            for i, t in enumerate((CS, GP, BT)):
                Sd.dma("sp", dict(out=dbg_d[:, i * 256:(i + 1) * 256].rearrange("p (a j) -> p a j", a=8), in_=t.ap[:, :, 0:NCH]), reads=[t])
            return finish()
        psX, psX_ap = PS[5], bank(5)[:, 0:256]
        psR, psR_ap = PS[2], bank(2)[0:1, 0:256]
        psGB, psGB_ap = PS[3], bank(3)[:, 0:256]
        for hf in range(2):
            Sd.op("pe", PE.transpose, dict(out=psX_ap[:, hf * P:(hf + 1) * P], in_=fl(GP.ap)[:, hf * P:(hf + 1) * P], identity=ident_f.ap),
                  reads=[GP, ident_f], writes=[psX] if hf == 0 else [], joins=[] if hf == 0 else [psX])
        Sd.op("dve", V.tensor_reduce, dict(out=Gcol.ap, in_=psX_ap.rearrange("p (a s) -> p a s", a=2), axis=AX.X, op=ALU.max),
              reads=[psX], writes=[Gcol])
        for hf in range(2):
            Sd.op("pe", PE.transpose, dict(out=psR_ap[:, hf * P:(hf + 1) * P], in_=Gcol.ap[:, hf:hf + 1], identity=ident_f.ap),
                  reads=[Gcol, ident_f], writes=[psR] if hf == 0 else [], joins=[] if hf == 0 else [psR])
        Sd.op("act", ACT.activation, dict(out=Grow.ap[0:1, :], in_=psR_ap, func=AF.Copy), reads=[psR], writes=[Grow])
        Sd.op("pe", PE.matmul, dict(out=psGB_ap, lhsT=ones_f.ap[0:1, :], rhs=Grow.ap[0:1, :], start=True, stop=True),
              reads=[ones_f, Grow], writes=[psGB])
        Sd.op("act", ACT.activation, dict(out=fl(GB.ap), in_=psGB_ap, func=AF.Copy), reads=[psGB], writes=[GB])
        if stop_after == "G3":
            Sd.barrier()
            for i, t in enumerate((GB, GP)):
                Sd.dma("sp", dict(out=dbg_d[:, i * 256:(i + 1) * 256].rearrange("p (a j) -> p a j", a=8), in_=t.ap[:, :, 0:NCH]), reads=[t])
            return finish()
        Sd.op("dve", V.memset, dict(ap=MN.ap[:, :, 0:1], constant=0.0), writes=[MN])
        for dh in range(8):
            Sd.op("dve", V.tensor_tensor_scan, dict(out=MN.ap[:, dh, 1:NCH + 1], data0=GB.ap[:, dh, :], data1=BT.ap[:, dh, :],
                                                    initial=0.0, op0=ALU.max, op1=ALU.add), reads=[GB, BT], joins=[MN])
        Sd.op("dve", V.tensor_tensor, dict(out=RR.ap, in0=MN.ap[:, :, 0:NCH], in1=GB.ap, op=ALU.max), reads=[MN, GB], writes=[RR])
        for src, dst in ((MN, DEC), (GP, WGT), (CS, STAB)):
            sap = src.ap[:, :, 0:NCH]
            Sd.op("dve", V.tensor_tensor, dict(out=TMP.ap, in0=sap, in1=RR.ap, op=ALU.subtract), reads=[src, RR], writes=[TMP])
            Sd.op("act", ACT.activation, dict(out=fl(dst.ap), in_=fl(TMP.ap), func=AF.Exp), reads=[TMP], writes=[dst])

        if stop_after == "G":
            Sd.barrier()
            for i, t in enumerate((WGT, STAB, DEC, MN)):
                Sd.dma("sp", dict(out=dbg_d[:, i * 256:(i + 1) * 256].rearrange("p (a j) -> p a j", a=8), in_=t.ap[:, :, 0:NCH]), reads=[t])
            return finish()

        Sd.barrier()
        A.release(mA)
        hm_ap = A.alloc([NCH, 512], F32, "hm")
        hm = [[T(hm_ap[:, c, h * P:(h + 1) * P]) for h in range(4)] for c in range(NCH)]
        mH = A.mark()
        wqkv = T(A.alloc([KD, 3, P], BF16, "wqkv"))
        qT_ap = A.alloc([S], BF16, "qT")
        kT_ap = A.alloc([S], BF16, "kT")
        qT = [T(qT_ap[:, g * 512:(g + 1) * 512]) for g in range(NG)]
        kT = [T(kT_ap[:, g * 512:(g + 1) * 512]) for g in range(NG)]
        va_ap = A.alloc([NCH, 130], BF16, "va")
        va = [T(va_ap[:, c, :]) for c in range(NCH)]
        kwF_ap = A.alloc([NCH, P], BF16, "kwF")
        kwB_ap = A.alloc([NCH, P], BF16, "kwB")
        kwF = [T(kwF_ap[:, c, :]) for c in range(NCH)]
        kwB = [T(kwB_ap[:, c, :]) for c in range(NCH)]
        sTw = [T(A.alloc([P], BF16, "sTw%d" % i)) for i in range(4)]
        Cst = [T(A.alloc([130], F32, "Cst%d" % i)) for i in range(2)]
        Csb = [[T(A.alloc([130], BF16, "Csb%d%d" % (i, n))) for n in range(3)] for i in range(2)]
        dnr = [T(A.alloc([4], F32, "dnr%d" % i)) for i in range(4)]
        Sd.op("dve", V.memset, dict(ap=va_ap[:, :, 128:130], constant=1.0), writes=va)
        QSCALE = float(P) ** -0.5
        _nheads = int(_os.environ.get('K_NH', 4))

        for h in range(_nheads):
            for i, off in enumerate((0, 512, 1024)):
                Sd.dma("pool", dict(out=wqkv.ap[:, :, i, :], in_=w_in_v[:, :, off + h * P:off + (h + 1) * P]),
                       writes=[wqkv] if i == 0 else [], joins=[] if i == 0 else [wqkv])
            for g in range(NG):
                for which, dstT, bb in ((0, qT, g % 2), (1, kT, 2 + g % 2)):
                    for k in range(KD):
                        Sd.op("pe", PE.matmul, dict(out=bank(bb), lhsT=wqkv.ap[:, k, which, :], rhs=hT[k][g].ap,
                                                    start=(k == 0), stop=(k == KD - 1)),
                              reads=[wqkv, hT[k][g]], writes=[PS[bb]] if k == 0 else [], joins=[] if k == 0 else [PS[bb]])
                    if which == 0:
                        Sd.op("act", ACT.activation, dict(out=dstT[g].ap, in_=bank(bb), func=AF.Copy, scale=QSCALE),
                              reads=[PS[bb]], writes=[dstT[g]])
                    else:
                        Sd.op("dve", V.tensor_copy, dict(out=dstT[g].ap, in_=bank(bb)), reads=[PS[bb]], writes=[dstT[g]])
                for ci in range(4):
                    c = g * 4 + ci
                    bb = 4 + ci
                    for k in range(KD):
                        Sd.op("pe", PE.matmul, dict(out=bank(bb)[:, 0:256], lhsT=hT[k][g].ap[:, ci * P:(ci + 1) * P],
                                                    rhs=wqkv.ap[:, k, 1:3, :].rearrange("p a b -> p (a b)"),
                                                    start=(k == 0), stop=(k == KD - 1)),
                              reads=[wqkv, hT[k][g]], writes=[PS[bb]] if k == 0 else [], joins=[] if k == 0 else [PS[bb]])
                    Sd.op("act", ACT.activation, dict(out=va[c].ap[:, 0:P], in_=bank(bb)[:, P:2 * P], func=AF.Copy),
                          reads=[PS[bb]], joins=[va[c]])
                    Sd.op("dve", V.tensor_scalar, dict(out=kwF[c].ap, in0=bank(bb)[:, 0:P], scalar1=WGT.ap[:, h, c:c + 1],
                                                       scalar2=None, op0=ALU.mult), reads=[PS[bb], WGT], writes=[kwF[c]])
                    jb = NCH - 1 - c
                    Sd.op("dve", V.tensor_scalar, dict(out=kwB[c].ap, in0=bank(bb)[:, 0:P], scalar1=WGT.ap[:, 4 + h, jb:jb + 1],
                                                       scalar2=None, op0=ALU.mult), reads=[PS[bb], WGT], writes=[kwB[c]])
            NI = 2 * NCH
            ND = (2, 3, 6, 7)

            def item(i):
                j, dr = divmod(i, 2)
                c = j if dr == 0 else NCH - 1 - j
                return j, dr, c, dr * 4 + h, c // 4, (c % 4) * P

            def S1(i):
                j, dr, c, dh, g, o = item(i)
                st = sTw[i % 4]
                Sd.op("pe", PE.matmul, dict(out=bank(dr)[:, 0:P], lhsT=kT[g].ap[:, o:o + P], rhs=qT[g].ap[:, o:o + P],
                                            start=True, stop=True), reads=[kT[g], qT[g]], writes=[PS[dr]])
                Sd.op("dve", V.scalar_tensor_tensor, dict(out=st.ap, in0=bank(dr)[:, 0:P], scalar=WGT.ap[:, dh, j:j + 1],
                                                          in1=(maskF if dr == 0 else maskB).ap, op0=ALU.mult, op1=ALU.mult),
                      reads=[PS[dr], WGT, maskF if dr == 0 else maskB], writes=[st])

            def S2(i):
                j, dr, c, dh, g, o = item(i)
                if j >= NCH - 1:
                    return
                kw = kwF if dr == 0 else kwB
                Sd.op("pe", PE.matmul, dict(out=bank(4 + dr)[:, 0:129], lhsT=kw[c].ap, rhs=va[c].ap[:, 0:129],
                                            start=True, stop=True), reads=[kw[c], va[c]], writes=[PS[4 + dr]])
                if j == 0:
                    Sd.op("dve", V.tensor_copy, dict(out=Cst[dr].ap[:, 0:129], in_=bank(4 + dr)[:, 0:129]),
                          reads=[PS[4 + dr]], writes=[Cst[dr]])
                else:
                    Sd.op("dve", V.scalar_tensor_tensor, dict(out=Cst[dr].ap[:, 0:129], in0=Cst[dr].ap[:, 0:129],
                                                              scalar=DEC.ap[:, dh, j:j + 1], in1=bank(4 + dr)[:, 0:129],
                                                              op0=ALU.mult, op1=ALU.add),
                          reads=[Cst[dr], DEC, PS[4 + dr]], writes=[Cst[dr]])

            def S2b(i):
                j, dr, c, dh, g, o = item(i)
                if j >= NCH - 1:
                    return
                Sd.op("act", ACT.activation, dict(out=Csb[dr][(j + 1) % 3].ap[:, 0:129], in_=Cst[dr].ap[:, 0:129],
                                                  func=AF.Copy, scale=DEC.ap[:, dh, j + 1:j + 2]),
                      reads=[Cst[dr], DEC], writes=[Csb[dr][(j + 1) % 3]])

            def S3(i):
                j, dr, c, dh, g, o = item(i)
                bb = ND[i % 4]
                st = sTw[i % 4]
                Sd.op("pe", PE.matmul, dict(out=bank(bb)[:, 0:129], lhsT=st.ap, rhs=va[c].ap[:, 0:129],
                                            start=True, stop=(j == 0)), reads=[st, va[c]], writes=[PS[bb]])
                if j > 0:
                    Sd.op("pe", PE.matmul, dict(out=bank(bb)[:, 0:129], lhsT=qT[g].ap[:, o:o + P],
                                                rhs=Csb[dr][j % 3].ap[:, 0:129], start=False, stop=True),
                          reads=[qT[g], Csb[dr][j % 3]], joins=[PS[bb]])

            def S4(i):
                j = i // 2
                d_ = dnr[j % 4]
                for dr in range(2):
                    bb = ND[(2 * j + dr) % 4]
                    Sd.op("act", ACT.activation, dict(out=d_.ap[:, dr:dr + 1], in_=bank(bb)[:, 128:129], func=AF.Abs),
                          reads=[PS[bb]], writes=[d_] if dr == 0 else [], joins=[] if dr == 0 else [d_])
                Sd.op("dve", V.tensor_tensor, dict(out=d_.ap[:, 0:2], in0=d_.ap[:, 0:2], in1=STAB.ap[:, h:8:4, j],
                                                   op=ALU.max), reads=[d_, STAB], joins=[d_])
                Sd.op("dve", V.reciprocal, dict(out=d_.ap[:, 2:4], in_=d_.ap[:, 0:2]), reads=[d_], joins=[d_])

            def S5(i):
                j, dr, c, dh, g, o = item(i)
                bb = ND[i % 4]
                d_ = dnr[j % 4]
                if j < NCH // 2:
                    Sd.op("act", ACT.activation, dict(out=hm[c][h].ap, in_=bank(bb)[:, 0:P], func=AF.Copy,
                                                      scale=d_.ap[:, 2 + dr:3 + dr]), reads=[PS[bb], d_], writes=[hm[c][h]])
                else:
                    Sd.op("dve", V.scalar_tensor_tensor, dict(out=hm[c][h].ap, in0=bank(bb)[:, 0:P], scalar=d_.ap[:, 2 + dr:3 + dr],
                                                              in1=hm[c][h].ap, op0=ALU.mult, op1=ALU.add),
                          reads=[PS[bb], d_, hm[c][h]], writes=[hm[c][h]])

            for t in range(NI + 4):
                if t < NI:
                    S1(t)
                if 0 <= t - 2 < NI and (t - 2) % 2 == 1:
                    S4(t - 2)
                if 0 <= t - 4 < NI:
                    S5(t - 4)
                if 0 <= t - 1 < NI:
                    S2b(t - 1)
                if 0 <= t - 1 < NI:
                    S3(t - 1)
                if t < NI:
                    S2(t)

        if stop_after == "H":
            Sd.barrier()
            for c in range(NCH):
                Sd.dma("sp", dict(out=dbg_d[c * P:(c + 1) * P, 0:P * _nheads], in_=hm_ap[:, c, 0:P * _nheads]), reads=hm[c])
            return finish()

        Sd.barrier()
        A.release(mH)
        mY = A.mark()
        w_o = T(A.alloc([KD, 512], BF16, "w_o"))
        w_u = T(A.alloc([KD, 512], BF16, "w_u"))
        w_pl = T(A.alloc([4, P], BF16, "w_pl"))
        psw = T(A.alloc([4], F32, "psw"))
        pscl = T(A.alloc([4], F32, "pscl"))
        edge = T(A.alloc([4, 16], F32, "edge"))
        ubuf = T(A.alloc([4, 528], F32, "ubuf"))
        tP = T(A.alloc([4, 528], F32, "tP"))
        tQ = T(A.alloc([2, 528], F32, "tQ"))
        tA = T(A.alloc([4, 512], F32, "tA"))
        mixed = T(A.alloc([4, 512], BF16, "mixed"))
        ucarry = T(A.alloc([4, 8], F32, "ucarry"))
        sig = [T(A.alloc([512], F32, "sig%d" % i)) for i in range(4)]
        sqb = [T(A.alloc([512], F32, "sqb%d" % i)) for i in range(1)]
        ssh = T(A.alloc([16], F32, "ssh"))
        rsh = T(A.alloc([16], F32, "rsh"))
        yab = [T(A.alloc([512], BF16, "ya%d" % i)) for i in range(4)]
        Sd.dma("pool", dict(out=w_o.ap, in_=w_in_v[:, :, 1536:2048]), writes=[w_o])
        Sd.dma("pool", dict(out=w_u.ap, in_=w_in_v[:, :, 2064:2576]), writes=[w_u])
        Sd.dma("pool", dict(out=w_pl.ap, in_=w_pool_d[:, :, :]), writes=[w_pl])
        Sd.dma("sp", dict(out=pscl.ap, in_=psclP_d[:, :]), writes=[pscl])
        Sd.dma("sp", dict(out=edge.ap.rearrange("p a b -> p (a b)"), in_=edge_d.partition_broadcast(P)), writes=[edge])
        for pg, w in enumerate(POOL_W):
            Sd.op("dve", V.tensor_scalar, dict(out=psw.ap[:, pg:pg + 1], in0=pscl.ap[:, pg:pg + 1], scalar1=1.0 / w, scalar2=None,
                                               op0=ALU.mult), reads=[pscl], joins=[psw])
        Sd.op("dve", V.memset, dict(ap=cst.ap[:, 2:3], constant=EPS), joins=[cst])
        ptr_ap = bank(6, 2).bitcast(BF16).rearrange("p (m t) -> p m t", m=4)

        def stage_U(G):
                U = ubuf.ap
                for pg in range(4):
                    bb = pg % 2
                    for k in range(KD):
                        Sd.op("pe", PE.matmul, dict(out=bank(bb), lhsT=w_u.ap[:, k, pg * P:(pg + 1) * P], rhs=hT[k][G].ap,
                                                    start=(k == 0), stop=(k == KD - 1)),
                              reads=[w_u, hT[k][G]], writes=[PS[bb]] if k == 0 else [], joins=[] if k == 0 else [PS[bb]])
                    Sd.op("act", ACT.activation, dict(out=ubuf.ap[:, pg, 8:520], in_=bank(bb), func=AF.Copy),
                          reads=[PS[bb]], writes=[ubuf] if pg == 0 else [], joins=[] if pg == 0 else [ubuf])
                if G < NG - 1:
                    for pg in range(4):
                        for k in range(KD):
                            Sd.op("pe", PE.matmul, dict(out=bank(2)[:, pg * 8:(pg + 1) * 8], lhsT=w_u.ap[:, k, pg * P:(pg + 1) * P],
                                                        rhs=hT[k][G + 1].ap[:, 0:8], start=(k == 0), stop=(k == KD - 1)),
                                  reads=[w_u, hT[k][G + 1]], writes=[PS[2]] if (k == 0 and pg == 0) else [],
                                  joins=[] if (k == 0 and pg == 0) else [PS[2]])
                    Sd.op("act", ACT.activation, dict(out=ubuf.ap[:, :, 520:528], in_=bank(2)[:, 0:32].rearrange("p (a b) -> p a b", a=4),
                                                      func=AF.Copy), reads=[PS[2]], joins=[ubuf])
                else:
                    Sd.op("pool", POOL.memset, dict(ap=ubuf.ap[:, :, 520:528], constant=0.0), joins=[ubuf])
                if G == 0:
                    Sd.op("pool", POOL.memset, dict(ap=ubuf.ap[:, :, 0:8], constant=0.0), joins=[ubuf])
                else:
                    Sd.op("pool", POOL.tensor_copy, dict(out=ubuf.ap[:, :, 0:8], in_=ucarry.ap), reads=[ucarry], joins=[ubuf])
                Sd.op("pool", POOL.tensor_copy, dict(out=ucarry.ap, in_=ubuf.ap[:, :, 512:520]), reads=[ubuf], writes=[ucarry])
                U = ubuf.ap
                Sd.op("pool", POOL.tensor_tensor, dict(out=tP.ap[:, :, 0:527], in0=U[:, :, 0:527], in1=U[:, :, 1:528], op=ALU.add),
                      reads=[ubuf], writes=[tP])
                Sd.op("pool", POOL.tensor_tensor, dict(out=tQ.ap[:, 0:2, 0:525], in0=tP.ap[:, 2:4, 0:525], in1=tP.ap[:, 2:4, 2:527], op=ALU.add),
                      reads=[tP], writes=[tQ])
                Sd.op("pool", POOL.tensor_tensor, dict(out=tP.ap[:, 3, 0:521], in0=tQ.ap[:, 1, 0:521], in1=tQ.ap[:, 1, 4:525], op=ALU.add),
                      reads=[tQ, tP], joins=[tP])

        def stage_O(G):
                for i in range(4):
                    c = G * 4 + i
                    bb = 3 + i % 2
                    for k in range(KD):
                        Sd.op("pe", PE.matmul, dict(out=bank(bb), lhsT=hT[k][G].ap[:, i * P:(i + 1) * P], rhs=w_o.ap[:, k, :],
                                                    start=(k == 0), stop=(k == KD - 1)),
                              reads=[w_o, hT[k][G]], writes=[PS[bb]] if k == 0 else [], joins=[] if k == 0 else [PS[bb]])
                    Sd.op("act", ACT.activation, dict(out=sig[i].ap, in_=bank(bb), func=AF.Sigmoid), reads=[PS[bb]], writes=[sig[i]])
                    sq = sqb[0]
                    Sd.op("dve", V.tensor_tensor, dict(out=sq.ap, in0=hm_ap[:, c, :], in1=hm_ap[:, c, :], op=ALU.mult),
                          reads=hm[c], writes=[sq])
                    Sd.op("dve", V.tensor_reduce, dict(out=ssh.ap[:, i * 4:(i + 1) * 4], in_=sq.ap.rearrange("p (a b) -> p a b", a=4),
                                                       axis=AX.X, op=ALU.add), reads=[sq], writes=[ssh] if i == 0 else [],
                          joins=[] if i == 0 else [ssh])
                Sd.op("act", ACT.activation, dict(out=rsh.ap, in_=ssh.ap, func=AF.Ln, scale=1.0 / P, bias=cst.ap[:, 2:3]),
                      reads=[ssh, cst], writes=[rsh])
                Sd.op("act", ACT.activation, dict(out=rsh.ap, in_=rsh.ap, func=AF.Exp, scale=-0.5), reads=[rsh], writes=[rsh])
        def stage_G(G):
                for i in range(4):
                    c = G * 4 + i
                    ya = yab[i]
                    for h in range(4):
                        Sd.op("dve", V.scalar_tensor_tensor, dict(out=ya.ap[:, h * P:(h + 1) * P], in0=hm[c][h].ap,
                                                                  scalar=rsh.ap[:, i * 4 + h:i * 4 + h + 1], in1=sig[i].ap[:, h * P:(h + 1) * P],
                                                                  op0=ALU.mult, op1=ALU.mult),
                              reads=[hm[c][h], rsh, sig[i]], writes=[ya] if h == 0 else [], joins=[] if h == 0 else [ya])
                    for h in range(4):
                        first = (i == 0 and h == 0)
                        Sd.op("pe", PE.transpose, dict(out=ptr_ap[:, h, i * P:(i + 1) * P], in_=ya.ap[:, h * P:(h + 1) * P], identity=ident_b.ap),
                              reads=[ya, ident_b], writes=[PS[6], PS[7]] if first else [], joins=[] if first else [PS[6], PS[7]])
                for m in range(4):
                    if m < 2:
                        Sd.op("act", ACT.activation, dict(out=hT[m][G].ap, in_=ptr_ap[:, m, :], func=AF.Copy), reads=[PS[6]], writes=[hT[m][G]])
                    else:
                        Sd.op("dve", V.tensor_copy, dict(out=hT[m][G].ap, in_=ptr_ap[:, m, :]), reads=[PS[7]], writes=[hT[m][G]])

        def stage_M(G):
                U = ubuf.ap
                Sd.op("dve", V.tensor_copy, dict(out=tA.ap[:, 0, :], in_=tP.ap[:, 0, 7:519]), reads=[tP], writes=[tA])
                Sd.op("dve", V.tensor_tensor, dict(out=tA.ap[:, 1, :], in0=tP.ap[:, 1, 6:518], in1=tP.ap[:, 1, 8:520], op=ALU.add),
                      reads=[tP], joins=[tA])
                Sd.op("dve", V.tensor_tensor, dict(out=tA.ap[:, 2, :], in0=tQ.ap[:, 0, 4:516], in1=tQ.ap[:, 0, 8:520], op=ALU.add),
                      reads=[tQ], joins=[tA])
                Sd.op("dve", V.tensor_tensor, dict(out=tA.ap[:, 3, :], in0=tP.ap[:, 3, 0:512], in1=tP.ap[:, 3, 8:520], op=ALU.add),
                      reads=[tP], joins=[tA])
                if G == 0:
                    Sd.op("dve", V.tensor_tensor, dict(out=tA.ap[:, :, 0:8], in0=tA.ap[:, :, 0:8], in1=edge.ap[:, :, 0:8], op=ALU.mult),
                          reads=[tA, edge], joins=[tA])
                if G == NG - 1:
                    Sd.op("dve", V.tensor_tensor, dict(out=tA.ap[:, :, 504:512], in0=tA.ap[:, :, 504:512], in1=edge.ap[:, :, 8:16], op=ALU.mult),
                          reads=[tA, edge], joins=[tA])
                for pg, w in enumerate(POOL_W):
                    Sd.op("dve", V.scalar_tensor_tensor, dict(out=mixed.ap[:, pg, :], in0=U[:, pg, 8:520], scalar=-float(w), in1=tA.ap[:, pg, :],
                                                              op0=ALU.mult, op1=ALU.add),
                          reads=[ubuf, tA], writes=[mixed] if pg == 0 else [], joins=[] if pg == 0 else [mixed])
                for pg in range(4):
                    yb = 5 if pg % 2 == 0 else 2
                    Sd.op("pe", PE.matmul, dict(out=bank(yb), lhsT=w_pl.ap[:, pg, :], rhs=mixed.ap[:, pg, :], start=True, stop=True),
                          reads=[w_pl, mixed], writes=[PS[yb]])
                    Sd.op("act", ACT.activation, dict(out=hT[4 + pg][G].ap, in_=bank(yb), func=AF.Copy, scale=psw.ap[:, pg:pg + 1]),
                          reads=[PS[yb], psw], writes=[hT[4 + pg][G]])


        stage_U(0)
        for G in range(NG):
            stage_O(G)
            stage_M(G)
            if G + 1 < NG:
                stage_U(G + 1)
            stage_G(G)

        if stop_after == "Y":
            Sd.barrier()
            for k in range(KD):
                for g in range(NG):
                    Sd.dma("sp", dict(out=dbg_d[:, k * S + g * 512:k * S + (g + 1) * 512], in_=hT[k][g].ap), reads=[hT[k][g]])
            return finish()

        Sd.barrier()
        A.release(mA)
        w_outp = T(A.alloc([KD, D], BF16, "w_outp"))
        ghP = T(A.alloc([4], F32, "ghP"))
        gfinB = T(A.alloc([D], F32, "gfinB"))
        x1_ap = A.alloc([8, D], F32, "x1")
        x1 = [T(x1_ap[:, i, :]) for i in range(8)]
        aT_ap = A.alloc([8, 2, 512], BF16, "aT")
        aT = [[T(aT_ap[:, fc, tg, :]) for tg in range(2)] for fc in range(8)]
        w1s = [T(A.alloc([KD, 512], BF16, "w1s%d" % i)) for i in range(3)]
        w2s = [T(A.alloc([4, D], BF16, "w2s%d" % i)) for i in range(3)]
        xn2 = [T(A.alloc([D], BF16, "xn2%d" % i)) for i in range(2)]
        rr = [T(A.alloc([512], F32, "rr%d" % i)) for i in range(2)]
        st2 = [T(A.alloc([4], F32, "st2%d" % i)) for i in range(4)]
        junk2 = A.alloc([D], BF16, "junk2")
        stg = [x1[0], x1[1]]
        Sd.dma("sp", dict(out=ghP.ap, in_=g_headP_d[:, :]), writes=[ghP])
        Sd.dma("sp", dict(out=gfinB.ap, in_=g_finR_d.partition_broadcast(P)), writes=[gfinB])
        for k in range(KD):
            sg = stg[k % 2]
            Sd.dma("sp", dict(out=sg.ap, in_=w_out_v[:, k, :]), writes=[sg])
            if k < 4:
                Sd.op("dve", V.scalar_tensor_tensor, dict(out=w_outp.ap[:, k, :], in0=sg.ap, scalar=ghP.ap[:, k:k + 1], in1=gt1B.ap,
                                                          op0=ALU.mult, op1=ALU.mult), reads=[sg, ghP, gt1B], joins=[w_outp])
            else:
                Sd.op("dve", V.tensor_tensor, dict(out=w_outp.ap[:, k, :], in0=sg.ap, in1=gt1B.ap, op=ALU.mult),
                      reads=[sg, gt1B], joins=[w_outp])
        NT4 = 4
        w1_blocks = [(Tt, q, bl) for Tt in range(NT4) for q in range(4) for bl in range(2)]
        gt2b4 = gt2B.ap.unsqueeze(1).to_broadcast([P, 4, D])

        def load_w1(n):
            if n >= len(w1_blocks):
                return
            Tt, q, bl = w1_blocks[n]
            c0 = q * 1024 + bl * 512
            Sd.dma("pool", dict(out=w1s[n % 3].ap, in_=w_ff1_v[:, :, c0:c0 + 512]), writes=[w1s[n % 3]])

        def load_w2(n):
            if n >= len(w1_blocks):
                return
            Tt, q, bl = w1_blocks[n]
            f0 = q * 8 + bl * 4
            t = w2s[n % 3]
            Sd.dma("pool", dict(out=t.ap, in_=w_ff2_v[:, f0:f0 + 4, :]), writes=[t])

        def scale_w2_piece(n, p):
            t = w2s[n % 3]
            ch, hf = p // 2, p % 2
            sl = t.ap[:, ch, hf * 512:(hf + 1) * 512]
            Sd.op("dve", V.tensor_tensor, dict(out=sl, in0=sl, in1=gt2B.ap[:, hf * 512:(hf + 1) * 512], op=ALU.mult),
                  reads=[t, gt2B], joins=[t])

        load_w1(0)
        load_w1(1)
        load_w2(0)
        load_w2(1)
        ps01 = ps_all[:, 0:1024]
        ps67 = ps_all[:, 3072:4096]
        psT2_ap = bank(2, 2).bitcast(BF16).rearrange("p (k t) -> p k t", k=KD)
        def final_norm_store(Tt, st_):
            c = Tt * 8 + st_
            s2 = st2[st_ % 4]
            Sd.op("act", ACT.activation, dict(out=junk2, in_=x1[st_].ap, func=AF.Square, accum_out=s2.ap[:, 0:1]),
                  reads=[x1[st_]], writes=[s2])
            Sd.op("act", ACT.activation, dict(out=s2.ap[:, 1:2], in_=s2.ap[:, 0:1], func=AF.Ln, scale=1.0 / D, bias=cst.ap[:, 0:1]),
                  reads=[s2, cst], joins=[s2])
            Sd.op("act", ACT.activation, dict(out=s2.ap[:, 2:3], in_=s2.ap[:, 1:2], func=AF.Exp, scale=-0.5), reads=[s2], joins=[s2])
            Sd.op("dve", V.scalar_tensor_tensor, dict(out=x1[st_].ap, in0=x1[st_].ap, scalar=s2.ap[:, 2:3], in1=gfinB.ap,
                                                      op0=ALU.mult, op1=ALU.mult), reads=[x1[st_], s2, gfinB], writes=[x1[st_]])
            Sd.dma("sp", dict(out=out_v[c], in_=x1[st_].ap), reads=[x1[st_]])
            if Tt + 1 < NT4:
                Sd.dma("sp", dict(out=x1[st_].ap, in_=x_v[(Tt + 1) * 8 + st_]), writes=[x1[st_]])

        blk = 0
        for Tt in range(NT4):
            def stage_a(st_):
                c = Tt * 8 + st_
                g, o = c // 4, (c % 4) * P
                if Tt == 0:
                    Sd.dma("sp", dict(out=x1[st_].ap, in_=x_v[c]), writes=[x1[st_]])
                pb, pt = ((0, 1), ps01) if st_ % 2 == 0 else ((6, 7), ps67)
                for half in range(2):
                    bb = pb[half]
                    for k in range(KD):
                        Sd.op("pe", PE.matmul, dict(out=bank(bb), lhsT=hT[k][g].ap[:, o:o + P], rhs=w_outp.ap[:, k, half * 512:(half + 1) * 512],
                                                    start=(k == 0), stop=(k == KD - 1)),
                              reads=[hT[k][g], w_outp], writes=[PS[bb]] if k == 0 else [], joins=[] if k == 0 else [PS[bb]])
                Sd.op("dve", V.tensor_tensor, dict(out=x1[st_].ap, in0=pt, in1=x1[st_].ap, op=ALU.add),
                      reads=[PS[pb[0]], PS[pb[1]], x1[st_]], writes=[x1[st_]])

            def stage_b(st_):
                c = Tt * 8 + st_
                g = c // 4
                s2 = st2[st_ % 4]
                xb = xn2[st_ % 2]
                Sd.op("act", ACT.activation, dict(out=junk2, in_=x1[st_].ap, func=AF.Square, accum_out=s2.ap[:, 0:1]),
                      reads=[x1[st_]], writes=[s2])
                Sd.op("act", ACT.activation, dict(out=s2.ap[:, 1:2], in_=s2.ap[:, 0:1], func=AF.Ln, scale=1.0 / D, bias=cst.ap[:, 0:1]),
                      reads=[s2, cst], joins=[s2])
                Sd.op("act", ACT.activation, dict(out=s2.ap[:, 2:3], in_=s2.ap[:, 1:2], func=AF.Exp, scale=-0.5), reads=[s2], joins=[s2])
                Sd.op("dve", V.tensor_scalar, dict(out=xb.ap, in0=x1[st_].ap, scalar1=s2.ap[:, 2:3], scalar2=None, op0=ALU.mult),
                      reads=[x1[st_], s2], writes=[xb])
                for k in range(KD):
                    first = (st_ % 2 == 0 and k == 0)
                    Sd.op("pe", PE.transpose, dict(out=psT2_ap[:, k, (st_ % 2) * P:(st_ % 2 + 1) * P], in_=xb.ap[:, k * P:(k + 1) * P],
                                                   identity=ident_b.ap),
                          reads=[xb, ident_b], writes=[PS[2], PS[3]] if first else [], joins=[] if first else [PS[2], PS[3]])
                if st_ % 2 == 1:
                    off = ((c // 2) % 2) * 256
                    for k in range(KD):
                        dst = hT[k][g].ap[:, off:off + 256]
                        if k < 4:
                            Sd.op("act", ACT.activation, dict(out=dst, in_=psT2_ap[:, k, :], func=AF.Identity,
                                                              scale=sc2.ap[:, k:k + 1], bias=modP.ap[:, 24 + k:25 + k]),
                                  reads=[PS[2], sc2, modP], joins=[hT[k][g]])
                        else:
                            Sd.op("dve", V.tensor_scalar, dict(out=dst, in0=psT2_ap[:, k, :], scalar1=sc2.ap[:, k:k + 1],
                                                               scalar2=modP.ap[:, 24 + k:25 + k], op0=ALU.mult, op1=ALU.add),
                                  reads=[PS[3], sc2, modP], joins=[hT[k][g]])

            for st_ in range(9):
                if st_ < 8:
                    stage_a(st_)
                if st_ >= 1:
                    stage_b(st_ - 1)
            for q in range(4):
                for bl in range(2):
                    n = blk + bl
                    load_w1(n + 2)
                    wt = w1s[n % 3]
                    for f4 in range(4):
                        fc = bl * 4 + f4
                        for tg in range(2):
                            bb = 4 + (fc * 2 + tg) % 2
                            for k in range(KD):
                                Sd.op("pe", PE.matmul, dict(out=bank(bb), lhsT=wt.ap[:, k, f4 * P:(f4 + 1) * P], rhs=hT[k][Tt * 2 + tg].ap,
                                                            start=(k == 0), stop=(k == KD - 1)),
                                      reads=[wt, hT[k][Tt * 2 + tg]], writes=[PS[bb]] if k == 0 else [], joins=[] if k == 0 else [PS[bb]])
                            r_ = rr[(fc * 2 + tg) % 2]
                            Sd.op("act", ACT.activation, dict(out=r_.ap, in_=bank(bb), func=AF.Relu), reads=[PS[bb]], writes=[r_])
                            Sd.op("dve", V.tensor_tensor, dict(out=aT[fc][tg].ap, in0=r_.ap, in1=r_.ap, op=ALU.mult),
                                  reads=[r_], writes=[aT[fc][tg]])
                            pp = f4 * 2 + tg
                            scale_w2_piece(blk + bl, pp)
                load_w2(blk + 2)
                for st_ in range(8):
                    tg, o = st_ // 4, (st_ % 4) * P
                    pb, pt = ((0, 1), ps01) if st_ % 2 == 0 else ((6, 7), ps67)
                    for half in range(2):
                        bb = pb[half]
                        for fc in range(8):
                            wt = w2s[(blk + fc // 4) % 3]
                            Sd.op("pe", PE.matmul, dict(out=bank(bb), lhsT=aT[fc][tg].ap[:, o:o + P], rhs=wt.ap[:, fc % 4, half * 512:(half + 1) * 512],
                                                        start=(fc == 0), stop=(fc == 7)),
                                  reads=[aT[fc][tg], wt], writes=[PS[bb]] if fc == 0 else [], joins=[] if fc == 0 else [PS[bb]])
                    Sd.op("dve", V.tensor_tensor, dict(out=x1[st_].ap, in0=pt, in1=x1[st_].ap, op=ALU.add),
                          reads=[PS[pb[0]], PS[pb[1]], x1[st_]], writes=[x1[st_]])
                    if q == 3:
                        final_norm_store(Tt, st_)
                load_w2(blk + 3)
                blk += 2
        return finish()


def _consts():
    s = np.arange(P)
    maskF = (s[:, None] <= s[None, :]).astype(np.float32)
    maskB = (s[:, None] >= s[None, :]).astype(np.float32)
    edge = np.ones((4, 16), np.float32)
    for g, w in enumerate(POOL_W):
        for t in range(8):
            lo = max(t - w // 2, 0)
            hi = min(t + w // 2, S)
            edge[g, t] = w / float(hi - lo)
            tt = S - 8 + t
            lo = max(tt - w // 2, 0)
            hi = min(tt + w // 2, S)
            edge[g, 8 + t] = w / float(hi - lo)
    return dict(
        ident_f=np.eye(P, dtype=np.float32),
        ident_b=np.eye(P).astype(ml_dtypes.bfloat16),
        maskF=maskF, maskB=maskB,
        ones_f=np.ones((P, P), np.float32),
        edge_fac=edge.reshape(1, 64),
    )


def make_in_maps(x, c, w_ada, b_ada, g_mix, w_in, b_igate, b_fgate, g_head, w_pool, pool_scale,
                 w_out, g_ffn, w_ff1, w_ff2, g_final):
    f = lambda a: np.ascontiguousarray(np.asarray(a, dtype=np.float32))
    cs = _consts()
    shared = dict(
        w_ada=f(w_ada[0]),
        b_adaP=f(np.asarray(b_ada[0]).reshape(48, P).T),
        b_adaR=f(np.asarray(b_ada[0]).reshape(1, 6 * D)),
        g_mixP=f(np.asarray(g_mix[0]).reshape(KD, P).T),
        g_ffnP=f(np.asarray(g_ffn[0]).reshape(KD, P).T),
        g_finR=f(np.asarray(g_final).reshape(1, D)),
        w_in=f(w_in[0]),
        b_ig=f(np.asarray(b_igate[0]).reshape(1, 8)),
        b_fg=f(np.asarray(b_fgate[0]).reshape(1, 8)),
        g_headP=f(np.asarray(g_head[0]).reshape(4, P).T),
        pool_scaleP=f(np.asarray(pool_scale[0]).reshape(4, P).T),
        w_pool=f(np.asarray(w_pool[0]).transpose(1, 0, 2)),
        w_out=f(w_out[0]), w_ff1=f(w_ff1[0]), w_ff2=f(w_ff2[0]),
        **cs,
    )
    maps = []
    for b in range(N_CORES):
        m = dict(shared)
        m["x"] = f(x[b])
        m["cT"] = f(np.asarray(c[b]).reshape(KD, P).T)
        maps.append(m)
    return maps


_CACHE = {}


def kernel(**inputs):
    if "nc" not in _CACHE:
        _CACHE["nc"] = build_program()[0]
    nc = _CACHE["nc"]
    in_maps = make_in_maps(**inputs)
    res = run_bass_kernel_spmd(nc, in_maps, core_ids=list(range(N_CORES)))
    out = np.stack([np.asarray(r["out"]) for r in res.results], axis=0)
    return out.astype(np.float32)
```

```python
import numpy as np
import ml_dtypes
from contextlib import ExitStack

import concourse.bass as bass
import concourse.mybir as mybir
from concourse.bass_utils import run_bass_kernel_spmd

F32 = mybir.dt.float32
BF16 = mybir.dt.bfloat16
AF = mybir.ActivationFunctionType
ALU = mybir.AluOpType
AX = mybir.AxisListType

P = 128
S = 4096
D = 1024
KD = 8
NCH = 32
NG = 8
DFF = 4096
EPS = 1e-6
POOL_W = (2, 4, 8, 16)
N_CORES = 8


class T:
    __slots__ = ("ap", "w", "r", "dsem", "dcount", "name", "excl")

    def __init__(self, ap, name="", excl=False):
        self.ap = ap
        self.excl = excl
        self.w = []
        self.r = []
        self.dsem = None
        self.dcount = 0
        self.name = name

    def __getitem__(self, k):
        return self.ap[k]


class _Op:
    __slots__ = ("eng", "idx", "method", "kwargs", "waits", "marked", "dma_inc", "rank")


class Sched:
    ENG = ("pe", "act", "dve", "pool", "sp")

    def __init__(self, nc, es):
        self.nc = nc
        self.es = es
        self.h = {"pe": nc.tensor, "act": nc.scalar, "dve": nc.vector, "pool": nc.gpsimd, "sp": nc.sync}
        self.sem = {e: es.enter_context(nc.semaphore("s_" + e)) for e in ("pe", "act", "dve", "pool")}
        self.ops = []
        self.eng_ops = {e: [] for e in self.ENG}
        self.waited_c = {e: {x: -1 for x in self.ENG} for e in self.ENG}
        self.waited_d = {e: {} for e in self.ENG}
        self.dsems = []
        self.nsem = 0

    def _collect(self, eng, reads, writes, joins):
        toks = []
        for t in reads:
            toks.extend(t.w)
            if t.excl:
                toks.extend(x for x in t.r if x[0] == "c" and x[1].eng != eng)
        for t in writes:
            toks.extend(t.w)
            toks.extend(t.r)
        for t in joins:
            toks.extend(t.r)
        waits = []
        wc = self.waited_c[eng]
        wd = self.waited_d[eng]
        for tk in toks:
            if tk[0] == "c":
                d = tk[1]
                if d.eng == eng and eng == "pe":
                    continue
                if wc[d.eng] >= d.idx:
                    continue
                wc[d.eng] = d.idx
                d.marked = True
                waits.append(tk)
            else:
                _, sem, val = tk
                k = id(sem)
                if wd.get(k, 0) >= val:
                    continue
                wd[k] = val
                waits.append(tk)
        return waits

    def _post(self, tok, reads, writes, joins):
        for t in writes:
            t.w = [tok]
            t.r = []
        def same(x):
            if x[0] != tok[0]:
                return False
            if tok[0] == "c":
                return x[1].eng == tok[1].eng
            return x[1] is tok[1]
        for t in joins:
            t.w = [x for x in t.w if not same(x)] + [tok]
        for t in reads:
            t.r = [x for x in t.r if not same(x)] + [tok]

    def op(self, eng, method, kwargs, reads=(), writes=(), joins=()):
        o = _Op()
        o.eng = eng
        o.idx = len(self.eng_ops[eng])
        o.method = method
        o.kwargs = kwargs
        o.marked = False
        o.dma_inc = None
        o.waits = self._collect(eng, reads, writes, joins)
        self.eng_ops[eng].append(o)
        self.ops.append(o)
        self._post(("c", o), reads, writes, joins)
        return o

    def _dsem(self, t):
        if t.dsem is None:
            t.dsem = self.es.enter_context(self.nc.semaphore("d%d" % self.nsem))
            self.nsem += 1
            self.dsems.append(t)
        return t.dsem

    def dma(self, q, kwargs, reads=(), writes=(), joins=()):
        o = _Op()
        o.eng = q
        o.idx = -1
        o.method = self.h[q].dma_start
        o.kwargs = kwargs
        o.marked = False
        o.waits = self._collect(q, reads, writes, joins)
        owner = (list(writes) + list(joins) + list(reads))[0]
        sem = self._dsem(owner)
        owner.dcount += 16
        o.dma_inc = (sem, owner.dcount)
        self.ops.append(o)
        self._post(("d", sem, owner.dcount), reads, writes, joins)
        return o

    def barrier(self):
        for e in self.ENG:
            o = _Op()
            o.eng = e
            o.idx = -1
            o.method = None
            o.kwargs = None
            o.marked = False
            o.dma_inc = None
            waits = []
            for x in ("pe", "act", "dve", "pool"):
                if self.eng_ops[x]:
                    d = self.eng_ops[x][-1]
                    if x == e or self.waited_c[e][x] >= d.idx:
                        continue
                    self.waited_c[e][x] = d.idx
                    d.marked = True
                    waits.append(("c", d))
            for t in self.dsems:
                k = id(t.dsem)
                if self.waited_d[e].get(k, 0) < t.dcount:
                    self.waited_d[e][k] = t.dcount
                    waits.append(("d", t.dsem, t.dcount))
            o.waits = waits
            self.ops.append(o)

    def emit(self):
        for e in ("pe", "act", "dve", "pool"):
            r = 0
            for o in self.eng_ops[e]:
                if o.marked:
                    r += 1
                o.rank = r
        nw = 0
        for o in self.ops:
            h = self.h[o.eng]
            for tk in o.waits:
                if tk[0] == "c":
                    h.wait_ge(self.sem[tk[1].eng], tk[1].rank)
                else:
                    h.wait_ge(tk[1], tk[2])
                nw += 1
            if o.method is None:
                continue
            ins = o.method(**o.kwargs)
            if o.dma_inc is not None:
                ins.then_inc(o.dma_inc[0], 16)
            elif o.marked:
                ins.then_inc(self.sem[o.eng], 1)
        self.stats = {e: len(self.eng_ops[e]) for e in self.ENG}
        self.stats["waits"] = nw
        self.stats["ops"] = len(self.ops)


class Arena:
    def __init__(self, base_ap_f32, nbytes):
        self.base = base_ap_f32
        self.n = nbytes
        self.off = 0
        self.peak = 0

    def mark(self):
        return self.off

    def release(self, m):
        self.off = m

    def alloc(self, free_shape, dtype, name=""):
        esz = 2 if dtype == BF16 else 4
        n = int(np.prod(free_shape))
        nb = (n * esz + 63) // 64 * 64
        assert self.off + nb <= self.n, "SBUF arena overflow at %s: %d + %d > %d" % (name, self.off, nb, self.n)
        ap = self.base[:, self.off // 4:(self.off + nb) // 4]
        if dtype == BF16:
            ap = ap.bitcast(BF16)
        ap = ap[:, 0:n]
        if len(free_shape) == 2:
            ap = ap.rearrange("p (a b) -> p a b", a=free_shape[0])
        elif len(free_shape) == 3:
            ap = ap.rearrange("p (a b c) -> p a b c", a=free_shape[0], b=free_shape[1])
        self.off += nb
        self.peak = max(self.peak, self.off)
        return ap


def build_program(stop_after=None, dbg=None, dbg_dt=None):
    nc = bass.Bass("TRN2", target_bir_lowering=False)

    def din(name, shape, dt=F32):
        return nc.dram_tensor(name, list(shape), dt, kind="ExternalInput").ap()

    x_d = din("x", [S, D])
    cT_d = din("cT", [P, KD])
    w_ada_d = din("w_ada", [D, 6 * D])
    b_adaP_d = din("b_adaP", [P, 48])
    b_adaR_d = din("b_adaR", [1, 6 * D])
    g_mixP_d = din("g_mixP", [P, KD])
    g_ffnP_d = din("g_ffnP", [P, KD])
    g_finR_d = din("g_finR", [1, D])
    w_in_d = din("w_in", [D, 2576])
    b_ig_d = din("b_ig", [1, 8])
    b_fg_d = din("b_fg", [1, 8])
    g_headP_d = din("g_headP", [P, 4])
    psclP_d = din("pool_scaleP", [P, 4])
    w_pool_d = din("w_pool", [P, 4, P])
    w_out_d = din("w_out", [D, D])
    w_ff1_d = din("w_ff1", [D, DFF])
    w_ff2_d = din("w_ff2", [DFF, D])
    ident_f_d = din("ident_f", [P, P])
    ident_b_d = din("ident_b", [P, P], BF16)
    maskF_d = din("maskF", [P, P])
    maskB_d = din("maskB", [P, P])
    ones_d = din("ones_f", [P, P])
    edge_d = din("edge_fac", [1, 64])
    out_d = nc.dram_tensor("out", [S, D], F32, kind="ExternalOutput").ap()
    dbg_d = None
    if dbg is not None:
        dbg_d = nc.dram_tensor("dbg", list(dbg), dbg_dt or F32, kind="ExternalOutput").ap()

    w_ada_v = w_ada_d.rearrange("(k p) n -> p k n", p=P)
    w_in_v = w_in_d.rearrange("(k p) n -> p k n", p=P)
    w_out_v = w_out_d.rearrange("(k p) n -> p k n", p=P)
    w_ff1_v = w_ff1_d.rearrange("(k p) n -> p k n", p=P)
    w_ff2_v = w_ff2_d.rearrange("(k p) n -> p k n", p=P)
    x_v = x_d.rearrange("(c p) d -> c p d", p=P)
    out_v = out_d.rearrange("(c p) d -> c p d", p=P)

    SB_BYTES = 207 * 1024
    es = ExitStack()
    with es:
        sb_all = es.enter_context(nc.sbuf_tensor("sb_all", [P, SB_BYTES // 4], F32))
        ps_all = es.enter_context(nc.psum_tensor("ps_all", [P, 4096], F32))
        es.enter_context(nc.Block())
        Sd = Sched(nc, es)
        A = Arena(sb_all[:], SB_BYTES)
        V, ACT, PE, POOL = nc.vector, nc.scalar, nc.tensor, nc.gpsimd

        def bank(b, n=1):
            return ps_all[:, b * 512:(b + n) * 512]

        PS = [T(bank(b), "bank%d" % b, excl=True) for b in range(8)]

        hT_ap = A.alloc([KD, S], BF16, "hT")
        hT = [[T(hT_ap[:, k, g * 512:(g + 1) * 512], "hT%d_%d" % (k, g)) for g in range(NG)] for k in range(KD)]
        ident_b = T(A.alloc([P], BF16, "ident_b"))
        ident_f = T(A.alloc([P], F32, "ident_f"))
        maskF = T(A.alloc([P], F32, "maskF"))
        maskB = T(A.alloc([P], F32, "maskB"))
        ones_f = T(A.alloc([P], F32, "ones"))
        cst = T(A.alloc([64], F32, "cst_misc"))
        modP = T(A.alloc([48], F32, "modP"))
        sc1 = T(A.alloc([KD], F32, "sc1"))
        sc2 = T(A.alloc([KD], F32, "sc2"))
        gt1B = T(A.alloc([D], F32, "gt1B"))
        gt2B = T(A.alloc([D], F32, "gt2B"))
        WGT = T(A.alloc([8, NCH], F32, "WGT"))
        STAB = T(A.alloc([8, NCH], F32, "STAB"))
        DEC = T(A.alloc([8, NCH], F32, "DEC"))

        Sd.op("dve", V.memset, dict(ap=cst.ap[:, 0:1], constant=EPS), writes=[cst])
        for t, d in ((ident_b, ident_b_d), (ident_f, ident_f_d), (maskF, maskF_d), (maskB, maskB_d), (ones_f, ones_d)):
            Sd.dma("sp", dict(out=t.ap, in_=d[:, :]), writes=[t])

        def finish():
            Sd.barrier()
            Sd.emit()
            return nc, Sd, A


        mA = A.mark()
        cT = T(A.alloc([KD], F32, "cT"))
        c_act = T(A.alloc([KD], F32, "c_act"))
        c_rep = T(A.alloc([KD, P], BF16, "c_rep"))
        b_adaP = T(A.alloc([48], F32, "b_adaP"))
        g_mixP = T(A.alloc([KD], F32, "g_mixP"))
        g_ffnP = T(A.alloc([KD], F32, "g_ffnP"))
        wA = [T(A.alloc([KD, 512], BF16, "wA%d" % i)) for i in range(3)]
        dtmp = T(A.alloc([4, P], F32, "dtmp"))
        psM = PS[0]
        psBc = PS[1]

        Sd.dma("sp", dict(out=cT.ap, in_=cT_d[:, :]), writes=[cT])
        Sd.dma("sp", dict(out=b_adaP.ap, in_=b_adaP_d[:, :]), writes=[b_adaP])
        Sd.dma("sp", dict(out=g_mixP.ap, in_=g_mixP_d[:, :]), writes=[g_mixP])
        Sd.dma("sp", dict(out=g_ffnP.ap, in_=g_ffnP_d[:, :]), writes=[g_ffnP])
        Sd.dma("sp", dict(out=gt1B.ap, in_=b_adaR_d[:, 2 * D:3 * D].partition_broadcast(P)), writes=[gt1B])
        Sd.dma("sp", dict(out=gt2B.ap, in_=b_adaR_d[:, 5 * D:6 * D].partition_broadcast(P)), writes=[gt2B])
        Sd.op("act", ACT.activation, dict(out=c_act.ap, in_=cT.ap, func=AF.Silu), reads=[cT], writes=[c_act])
        Sd.op("dve", V.tensor_copy, dict(out=c_rep.ap, in_=c_act.ap.unsqueeze(2).to_broadcast([P, KD, P])),
              reads=[c_act], writes=[c_rep])

        piece_order = [0, 1, 2, 3, 6, 7, 8, 9, 4, 5, 10, 11]

        def ada_piece(n, pc):
            buf = wA[n % 3]
            Sd.dma("pool", dict(out=buf.ap, in_=w_ada_v[:, :, pc * 512:(pc + 1) * 512]), writes=[buf])
            if pc in (4, 5, 10, 11):
                gt = gt1B if pc in (4, 5) else gt2B
                half = pc % 2
                for k in range(KD):
                    Sd.op("pe", PE.matmul, dict(out=psBc.ap, lhsT=c_rep.ap[:, k, :], rhs=buf.ap[:, k, :],
                                                start=(k == 0), stop=(k == KD - 1)),
                          reads=[c_rep, buf], writes=[psBc] if k == 0 else [], joins=[] if k == 0 else [psBc])
                sl = gt.ap[:, half * 512:(half + 1) * 512]
                Sd.op("dve", V.tensor_tensor, dict(out=sl, in0=psBc.ap, in1=sl, op=ALU.add),
                      reads=[psBc, gt], joins=[gt])
            else:
                for k in range(KD):
                    Sd.op("pe", PE.matmul, dict(out=psM.ap, lhsT=c_rep.ap[:, k, :], rhs=buf.ap[:, k, :],
                                                start=(k == 0), stop=(k == KD - 1)),
                          reads=[c_rep, buf], writes=[psM] if k == 0 else [], joins=[] if k == 0 else [psM])
                Sd.op("dve", V.tensor_tensor, dict(out=dtmp.ap, in0=psM.ap.rearrange("p (a b) -> p a b", a=4),
                                                   in1=ident_f.ap.unsqueeze(1).to_broadcast([P, 4, P]), op=ALU.mult),
                      reads=[psM, ident_f], writes=[dtmp])
                Sd.op("dve", V.tensor_reduce, dict(out=modP.ap[:, pc * 4:(pc + 1) * 4], in_=dtmp.ap, axis=AX.X, op=ALU.add),
                      reads=[dtmp], joins=[modP])
                if pc in (3, 9):
                    lo = 0 if pc == 3 else 24
                    Sd.op("dve", V.tensor_tensor, dict(out=modP.ap[:, lo:lo + 16], in0=modP.ap[:, lo:lo + 16],
                                                       in1=b_adaP.ap[:, lo:lo + 16], op=ALU.add),
                          reads=[modP, b_adaP], joins=[modP])
                    sc = sc1 if pc == 3 else sc2
                    gP = g_mixP if pc == 3 else g_ffnP
                    Sd.op("dve", V.scalar_tensor_tensor, dict(out=sc.ap, in0=modP.ap[:, lo + 8:lo + 16], scalar=1.0,
                                                              in1=gP.ap, op0=ALU.add, op1=ALU.mult),
                          reads=[modP, gP], writes=[sc])
        for n in range(4):
            ada_piece(n, piece_order[n])
        if stop_after == "0":
            Sd.barrier()
            Sd.dma("sp", dict(out=dbg_d[:, 0:48], in_=modP.ap), reads=[modP])
            Sd.dma("sp", dict(out=dbg_d[:, 48:56], in_=sc1.ap), reads=[sc1])
            Sd.dma("sp", dict(out=dbg_d[:, 56:64], in_=sc2.ap), reads=[sc2])
            Sd.dma("sp", dict(out=dbg_d[:, 64:64 + D], in_=gt1B.ap), reads=[gt1B])
            Sd.dma("sp", dict(out=dbg_d[:, 64 + D:64 + 2 * D], in_=gt2B.ap), reads=[gt2B])
            return finish()

        xin = [T(A.alloc([D], F32, "xin%d" % i)) for i in range(4)]
        xn = [T(A.alloc([D], BF16, "xn%d" % i)) for i in range(2)]
        junk = A.alloc([D], BF16, "junk")
        ssq = [T(A.alloc([4], F32, "ssq%d" % i)) for i in range(4)]
        psT_ap = [bank(2 + 2 * i, 2).bitcast(BF16).rearrange("p (k t) -> p k t", k=KD) for i in range(2)]
        psT_t = [[PS[2], PS[3]], [PS[4], PS[5]]]

        import os as _os
        _nch = int(_os.environ.get('K_NCH', NCH)); _var = _os.environ.get('K_VAR', '')
        for c in range(_nch):
            xt = xin[c % 4]
            sq = ssq[c % 4]
            xb = xn[c % 2]
            pT = psT_ap[(c // 2) % 2]
            pTt = psT_t[(c // 2) % 2]
            Sd.dma("sp", dict(out=xt.ap, in_=x_v[c]), writes=[xt])
            Sd.op("act", ACT.activation, dict(out=junk, in_=xt.ap, func=AF.Square, accum_out=sq.ap[:, 0:1]),
                  reads=[xt], writes=[sq])
            Sd.op("act", ACT.activation, dict(out=sq.ap[:, 1:2], in_=sq.ap[:, 0:1], func=AF.Ln, scale=1.0 / D,
                                              bias=cst.ap[:, 0:1]), reads=[sq, cst], joins=[sq])
            Sd.op("act", ACT.activation, dict(out=sq.ap[:, 2:3], in_=sq.ap[:, 1:2], func=AF.Exp, scale=-0.5),
                  reads=[sq], joins=[sq])
            Sd.op("dve", V.tensor_scalar, dict(out=xb.ap, in0=xt.ap, scalar1=sq.ap[:, 2:3], scalar2=None, op0=ALU.mult),
                  reads=[xt, sq], writes=[xb])
            for k in range(KD):
                first = (c % 2 == 0 and k == 0)
                Sd.op("pe", PE.transpose, dict(out=pT[:, k, (c % 2) * P:(c % 2 + 1) * P], in_=xb.ap[:, k * P:(k + 1) * P],
                                               identity=ident_b.ap),
                      reads=[xb, ident_b], writes=pTt if first else [], joins=[] if first else pTt)
            cc = c - 2
            if c == _nch - 1:
                pend = [c - 2, c] if c >= 3 else [c]
            else:
                pend = [cc] if (cc >= 1 and cc % 2 == 1) else []
            for ce in pend:
                g = ce // 4
                off = ((ce // 2) % 2) * 256
                pT = psT_ap[(ce // 2) % 2]
                pTt = psT_t[(ce // 2) % 2]
                for k in range(KD):
                    dst = hT[k][g].ap[:, off:off + 256]
                    if k < 4:
                        Sd.op("act", ACT.activation, dict(out=dst, in_=pT[:, k, :], func=AF.Identity,
                                                          scale=sc1.ap[:, k:k + 1], bias=modP.ap[:, k:k + 1]),
                              reads=[pTt[0], sc1, modP], joins=[hT[k][g]])
                    else:
                        Sd.op("dve", V.tensor_scalar, dict(out=dst, in0=pT[:, k, :], scalar1=sc1.ap[:, k:k + 1],
                                                           scalar2=modP.ap[:, k:k + 1], op0=ALU.mult, op1=ALU.add),
                              reads=[pTt[1], sc1, modP], joins=[hT[k][g]])

        if stop_after == "A2":
            Sd.barrier()
            Sd.dma("sp", dict(out=dbg_d[:, 0:8], in_=sc1.ap), reads=[sc1])
            return finish()
        if stop_after == "A":
            Sd.barrier()
            for k in range(KD):
                for g in range(NG):
                    Sd.dma("sp", dict(out=dbg_d[:, k * S + g * 512:k * S + (g + 1) * 512], in_=hT[k][g].ap), reads=[hT[k][g]])
            return finish()

        bI = T(A.alloc([8], F32, "bI"))
        bF = T(A.alloc([8], F32, "bF"))
        Sd.dma("sp", dict(out=bI.ap, in_=b_ig_d.partition_broadcast(P)), writes=[bI])
        Sd.dma("sp", dict(out=bF.ap, in_=b_fg_d.partition_broadcast(P)), writes=[bF])
        Sd.op("dve", V.memset, dict(ap=cst.ap[:, 1:2], constant=1.0), joins=[cst])
        psG = PS[6]
        psG_ap = bank(6).rearrange("p (d j g) -> p d j g", d=2, j=NCH)
        w_g40 = T(A.alloc([KD, 40], BF16, "w_g40"))
        gTs = T(A.alloc([S], F32, "gTs"))
        Sd.op("dve", V.memset, dict(ap=w_g40.ap, constant=0.0), writes=[w_g40])
        Sd.dma("pool", dict(out=w_g40.ap[:, :, 0:8], in_=w_in_v[:, :, 2048:2056]), reads=[w_g40], joins=[w_g40])
        Sd.dma("pool", dict(out=w_g40.ap[:, :, 32:40], in_=w_in_v[:, :, 2056:2064]), reads=[w_g40], joins=[w_g40])
        for g in range(NG):
            bb = 2 + g % 4
            for k in range(KD):
                Sd.op("pe", PE.matmul, dict(out=bank(bb)[0:40, :], lhsT=w_g40.ap[:, k, :], rhs=hT[k][g].ap,
                                            start=(k == 0), stop=(k == KD - 1)),
                      reads=[w_g40, hT[k][g]], writes=[PS[bb]] if k == 0 else [], joins=[] if k == 0 else [PS[bb]])
            if g % 2 == 0:
                Sd.op("act", ACT.activation, dict(out=gTs.ap[0:40, g * 512:(g + 1) * 512], in_=bank(bb)[0:40, :], func=AF.Copy),
                      reads=[PS[bb]], joins=[gTs])
            else:
                Sd.op("dve", V.tensor_copy, dict(out=gTs.ap[0:40, g * 512:(g + 1) * 512], in_=bank(bb)[0:40, :]),
                      reads=[PS[bb]], joins=[gTs])
        for c in range(NCH):
            for dr in range(2):
                j = c if dr == 0 else NCH - 1 - c
                p0 = dr * 32
                Sd.op("pe", PE.transpose, dict(out=psG_ap[:, dr, j, :], in_=gTs.ap[p0:p0 + 8, c * P:(c + 1) * P],
                                               identity=ident_f.ap[p0:p0 + 8, p0:p0 + 8]),
                      reads=[gTs, ident_f], joins=[psG])

        for n in range(4, 12):
            ada_piece(n, piece_order[n])

        def tab(name, n=NCH):
            return T(A.alloc([8, n], F32, name))

        def v4(ap):
            return ap.rearrange("p (d h) j -> p d h j", d=2)

        def fl(ap):
            return ap.rearrange("p a j -> p (a j)")

        LI, ZZ, CS, GP, GB, BT, RR, TMP = (tab(n) for n in ("LI", "ZZ", "CS", "GP", "GB", "BT", "RR", "TMP"))
        MN = tab("MN", NCH + 1)
        Gcol = T(A.alloc([2], F32, "Gcol"))
        Grow = T(A.alloc([256], F32, "Grow"))
        g_i = psG_ap[:, :, :, 0:4].rearrange("p d j h -> p d h j")
        g_f = psG_ap[:, :, :, 4:8].rearrange("p d j h -> p d h j")
        bIb = bI.ap.rearrange("p (d h) -> p d h", d=2).unsqueeze(3).to_broadcast([P, 2, 4, NCH])
        bFb = bF.ap.rearrange("p (d h) -> p d h", d=2).unsqueeze(3).to_broadcast([P, 2, 4, NCH])
        Sd.op("dve", V.tensor_tensor, dict(out=v4(LI.ap), in0=g_i, in1=bIb, op=ALU.add), reads=[psG, bI], writes=[LI])
        Sd.op("dve", V.tensor_tensor, dict(out=v4(ZZ.ap), in0=g_f, in1=bFb, op=ALU.add), reads=[psG, bF], writes=[ZZ])
        Sd.op("act", ACT.activation, dict(out=fl(ZZ.ap), in_=fl(ZZ.ap), func=AF.Exp, scale=-1.0), reads=[ZZ], writes=[ZZ])
        Sd.op("act", ACT.activation, dict(out=fl(ZZ.ap), in_=fl(ZZ.ap), func=AF.Ln, bias=cst.ap[:, 1:2]), reads=[ZZ, cst], writes=[ZZ])
        if stop_after == "G1":
            Sd.barrier()
            for i, t in enumerate((LI, ZZ)):
                Sd.dma("sp", dict(out=dbg_d[:, i * 256:(i + 1) * 256].rearrange("p (a j) -> p a j", a=8), in_=t.ap[:, :, 0:NCH]), reads=[t])
            return finish()
        psC, psC_ap = PS[7], bank(7)[:, 0:256]
        psBt, psBt_ap = PS[4], bank(4)[:, 0:256]
        Sd.op("pe", PE.matmul, dict(out=psC_ap[:, 0:128], lhsT=maskF.ap, rhs=fl(ZZ.ap)[:, 0:128], start=True, stop=True),
              reads=[maskF, ZZ], writes=[psC])
        Sd.op("pe", PE.matmul, dict(out=psC_ap[:, 128:256], lhsT=maskB.ap, rhs=fl(ZZ.ap)[:, 128:256], start=True, stop=True),
              reads=[maskB, ZZ], joins=[psC])
        Sd.op("pe", PE.matmul, dict(out=psBt_ap, lhsT=ones_f.ap, rhs=fl(ZZ.ap), start=True, stop=True),
              reads=[ones_f, ZZ], writes=[psBt])
        Sd.op("act", ACT.activation, dict(out=fl(CS.ap), in_=psC_ap, func=AF.Copy), reads=[psC], writes=[CS])
        if stop_after == "G2a":
            Sd.barrier()
            Sd.dma("sp", dict(out=dbg_d[:, 0:256].rearrange("p (a j) -> p a j", a=8), in_=CS.ap), reads=[CS])
            return finish()
        Sd.op("dve", V.tensor_tensor, dict(out=fl(GP.ap), in0=psC_ap, in1=fl(LI.ap), op=ALU.add), reads=[psC, LI], writes=[GP])
        Sd.op("act", ACT.activation, dict(out=fl(BT.ap), in_=psBt_ap, func=AF.Copy, scale=-1.0), reads=[psBt], writes=[BT])
        if stop_after == "G2":
            Sd.barrier()
            for i, t in enumerate((CS, GP, BT)):
                Sd.dma("sp", dict(out=dbg_d[:, i * 256:(i + 1) * 256].rearrange("p (a j) -> p a j", a=8), in_=t.ap[:, :, 0:NCH]), reads=[t])
            return finish()
        psX, psX_ap = PS[5], bank(5)[:, 0:256]
        psR, psR_ap = PS[2], bank(2)[0:1, 0:256]
        psGB, psGB_ap = PS[3], bank(3)[:, 0:256]
        for hf in range(2):
            Sd.op("pe", PE.transpose, dict(out=psX_ap[:, hf * P:(hf + 1) * P], in_=fl(GP.ap)[:, hf * P:(hf + 1) * P], identity=ident_f.ap),
                  reads=[GP, ident_f], writes=[psX] if hf == 0 else [], joins=[] if hf == 0 else [psX])
        Sd.op("dve", V.tensor_reduce, dict(out=Gcol.ap, in_=psX_ap.rearrange("p (a s) -> p a s", a=2), axis=AX.X, op=ALU.max),
              reads=[psX], writes=[Gcol])
        for hf in range(2):
            Sd.op("pe", PE.transpose, dict(out=psR_ap[:, hf * P:(hf + 1) * P], in_=Gcol.ap[:, hf:hf + 1], identity=ident_f.ap),
                  reads=[Gcol, ident_f], writes=[psR] if hf == 0 else [], joins=[] if hf == 0 else [psR])
        Sd.op("act", ACT.activation, dict(out=Grow.ap[0:1, :], in_=psR_ap, func=AF.Copy), reads=[psR], writes=[Grow])
        Sd.op("pe", PE.matmul, dict(out=psGB_ap, lhsT=ones_f.ap[0:1, :], rhs=Grow.ap[0:1, :], start=True, stop=True),
              reads=[ones_f, Grow], writes=[psGB])
        Sd.op("act", ACT.activation, dict(out=fl(GB.ap), in_=psGB_ap, func=AF.Copy), reads=[psGB], writes=[GB])
        if stop_after == "G3":
            Sd.barrier()
            for i, t in enumerate((GB, GP)):
                Sd.dma("sp", dict(out=dbg_d[:, i * 256:(i + 1) * 256].rearrange("p (a j) -> p a j", a=8), in_=t.ap[:, :, 0:NCH]), reads=[t])
            return finish()
        Sd.op("dve", V.memset, dict(ap=MN.ap[:, :, 0:1], constant=0.0), writes=[MN])
        for dh in range(8):
            Sd.op("dve", V.tensor_tensor_scan, dict(out=MN.ap[:, dh, 1:NCH + 1], data0=GB.ap[:, dh, :], data1=BT.ap[:, dh, :],
                                                    initial=0.0, op0=ALU.max, op1=ALU.add), reads=[GB, BT], joins=[MN])
        Sd.op("dve", V.tensor_tensor, dict(out=RR.ap, in0=MN.ap[:, :, 0:NCH], in1=GB.ap, op=ALU.max), reads=[MN, GB], writes=[RR])
        for src, dst in ((MN, DEC), (GP, WGT), (CS, STAB)):
            sap = src.ap[:, :, 0:NCH]
            Sd.op("dve", V.tensor_tensor, dict(out=TMP.ap, in0=sap, in1=RR.ap, op=ALU.subtract), reads=[src, RR], writes=[TMP])
            Sd.op("act", ACT.activation, dict(out=fl(dst.ap), in_=fl(TMP.ap), func=AF.Exp), reads=[TMP], writes=[dst])

        if stop_after == "G":
            Sd.barrier()
            for i, t in enumerate((WGT, STAB, DEC, MN)):
                Sd.dma("sp", dict(out=dbg_d[:, i * 256:(i + 1) * 256].rearrange("p (a j) -> p a j", a=8), in_=t.ap[:, :, 0:NCH]), reads=[t])
            return finish()

        Sd.barrier()
        A.release(mA)
        hm_ap = A.alloc([NCH, 512], F32, "hm")
        hm = [[T(hm_ap[:, c, h * P:(h + 1) * P]) for h in range(4)] for c in range(NCH)]
        mH = A.mark()
        wqkv = T(A.alloc([KD, 3, P], BF16, "wqkv"))
        qT_ap = A.alloc([S], BF16, "qT")
        kT_ap = A.alloc([S], BF16, "kT")
        qT = [T(qT_ap[:, g * 512:(g + 1) * 512]) for g in range(NG)]
        kT = [T(kT_ap[:, g * 512:(g + 1) * 512]) for g in range(NG)]
        va_ap = A.alloc([NCH, 130], BF16, "va")
        va = [T(va_ap[:, c, :]) for c in range(NCH)]
        kwF_ap = A.alloc([NCH, P], BF16, "kwF")
        kwB_ap = A.alloc([NCH, P], BF16, "kwB")
        kwF = [T(kwF_ap[:, c, :]) for c in range(NCH)]
        kwB = [T(kwB_ap[:, c, :]) for c in range(NCH)]
        sTw = [T(A.alloc([P], BF16, "sTw%d" % i)) for i in range(4)]
        Cst = [T(A.alloc([130], F32, "Cst%d" % i)) for i in range(2)]
        Csb = [[T(A.alloc([130], BF16, "Csb%d%d" % (i, n))) for n in range(3)] for i in range(2)]
        dnr = [T(A.alloc([4], F32, "dnr%d" % i)) for i in range(4)]
        Sd.op("dve", V.memset, dict(ap=va_ap[:, :, 128:130], constant=1.0), writes=va)
        QSCALE = float(P) ** -0.5
        _nheads = int(_os.environ.get('K_NH', 4))

        for h in range(_nheads):
            for i, off in enumerate((0, 512, 1024)):
                Sd.dma("pool", dict(out=wqkv.ap[:, :, i, :], in_=w_in_v[:, :, off + h * P:off + (h + 1) * P]),
                       writes=[wqkv] if i == 0 else [], joins=[] if i == 0 else [wqkv])
            for g in range(NG):
                for which, dstT, bb in ((0, qT, g % 2), (1, kT, 2 + g % 2)):
                    for k in range(KD):
                        Sd.op("pe", PE.matmul, dict(out=bank(bb), lhsT=wqkv.ap[:, k, which, :], rhs=hT[k][g].ap,
                                                    start=(k == 0), stop=(k == KD - 1)),
                              reads=[wqkv, hT[k][g]], writes=[PS[bb]] if k == 0 else [], joins=[] if k == 0 else [PS[bb]])
                    if which == 0:
                        Sd.op("act", ACT.activation, dict(out=dstT[g].ap, in_=bank(bb), func=AF.Copy, scale=QSCALE),
                              reads=[PS[bb]], writes=[dstT[g]])
                    else:
                        Sd.op("dve", V.tensor_copy, dict(out=dstT[g].ap, in_=bank(bb)), reads=[PS[bb]], writes=[dstT[g]])
                for ci in range(4):
                    c = g * 4 + ci
                    bb = 4 + ci
                    for k in range(KD):
                        Sd.op("pe", PE.matmul, dict(out=bank(bb)[:, 0:256], lhsT=hT[k][g].ap[:, ci * P:(ci + 1) * P],
                                                    rhs=wqkv.ap[:, k, 1:3, :].rearrange("p a b -> p (a b)"),
                                                    start=(k == 0), stop=(k == KD - 1)),
                              reads=[wqkv, hT[k][g]], writes=[PS[bb]] if k == 0 else [], joins=[] if k == 0 else [PS[bb]])
                    Sd.op("act", ACT.activation, dict(out=va[c].ap[:, 0:P], in_=bank(bb)[:, P:2 * P], func=AF.Copy),
                          reads=[PS[bb]], joins=[va[c]])
                    Sd.op("dve", V.tensor_scalar, dict(out=kwF[c].ap, in0=bank(bb)[:, 0:P], scalar1=WGT.ap[:, h, c:c + 1],
                                                       scalar2=None, op0=ALU.mult), reads=[PS[bb], WGT], writes=[kwF[c]])
                    jb = NCH - 1 - c
                    Sd.op("dve", V.tensor_scalar, dict(out=kwB[c].ap, in0=bank(bb)[:, 0:P], scalar1=WGT.ap[:, 4 + h, jb:jb + 1],
                                                       scalar2=None, op0=ALU.mult), reads=[PS[bb], WGT], writes=[kwB[c]])
            NI = 2 * NCH
            ND = (2, 3, 6, 7)

            def item(i):
                j, dr = divmod(i, 2)
                c = j if dr == 0 else NCH - 1 - j
                return j, dr, c, dr * 4 + h, c // 4, (c % 4) * P

            def S1(i):
                j, dr, c, dh, g, o = item(i)
                st = sTw[i % 4]
                Sd.op("pe", PE.matmul, dict(out=bank(dr)[:, 0:P], lhsT=kT[g].ap[:, o:o + P], rhs=qT[g].ap[:, o:o + P],
                                            start=True, stop=True), reads=[kT[g], qT[g]], writes=[PS[dr]])
                Sd.op("dve", V.scalar_tensor_tensor, dict(out=st.ap, in0=bank(dr)[:, 0:P], scalar=WGT.ap[:, dh, j:j + 1],
                                                          in1=(maskF if dr == 0 else maskB).ap, op0=ALU.mult, op1=ALU.mult),
                      reads=[PS[dr], WGT, maskF if dr == 0 else maskB], writes=[st])

            def S2(i):
                j, dr, c, dh, g, o = item(i)
                if j >= NCH - 1:
                    return
                kw = kwF if dr == 0 else kwB
                Sd.op("pe", PE.matmul, dict(out=bank(4 + dr)[:, 0:129], lhsT=kw[c].ap, rhs=va[c].ap[:, 0:129],
                                            start=True, stop=True), reads=[kw[c], va[c]], writes=[PS[4 + dr]])
                if j == 0:
                    Sd.op("dve", V.tensor_copy, dict(out=Cst[dr].ap[:, 0:129], in_=bank(4 + dr)[:, 0:129]),
                          reads=[PS[4 + dr]], writes=[Cst[dr]])
                else:
                    Sd.op("dve", V.scalar_tensor_tensor, dict(out=Cst[dr].ap[:, 0:129], in0=Cst[dr].ap[:, 0:129],
                                                              scalar=DEC.ap[:, dh, j:j + 1], in1=bank(4 + dr)[:, 0:129],
                                                              op0=ALU.mult, op1=ALU.add),
                          reads=[Cst[dr], DEC, PS[4 + dr]], writes=[Cst[dr]])

            def S2b(i):
                j, dr, c, dh, g, o = item(i)
                if j >= NCH - 1:
                    return
                Sd.op("act", ACT.activation, dict(out=Csb[dr][(j + 1) % 3].ap[:, 0:129], in_=Cst[dr].ap[:, 0:129],
                                                  func=AF.Copy, scale=DEC.ap[:, dh, j + 1:j + 2]),
                      reads=[Cst[dr], DEC], writes=[Csb[dr][(j + 1) % 3]])

            def S3(i):
                j, dr, c, dh, g, o = item(i)
                bb = ND[i % 4]
                st = sTw[i % 4]
                Sd.op("pe", PE.matmul, dict(out=bank(bb)[:, 0:129], lhsT=st.ap, rhs=va[c].ap[:, 0:129],
                                            start=True, stop=(j == 0)), reads=[st, va[c]], writes=[PS[bb]])
                if j > 0:
                    Sd.op("pe", PE.matmul, dict(out=bank(bb)[:, 0:129], lhsT=qT[g].ap[:, o:o + P],
                                                rhs=Csb[dr][j % 3].ap[:, 0:129], start=False, stop=True),
                          reads=[qT[g], Csb[dr][j % 3]], joins=[PS[bb]])

            def S4(i):
                j = i // 2
                d_ = dnr[j % 4]
                for dr in range(2):
                    bb = ND[(2 * j + dr) % 4]
                    Sd.op("act", ACT.activation, dict(out=d_.ap[:, dr:dr + 1], in_=bank(bb)[:, 128:129], func=AF.Abs),
                          reads=[PS[bb]], writes=[d_] if dr == 0 else [], joins=[] if dr == 0 else [d_])
                Sd.op("dve", V.tensor_tensor, dict(out=d_.ap[:, 0:2], in0=d_.ap[:, 0:2], in1=STAB.ap[:, h:8:4, j],
                                                   op=ALU.max), reads=[d_, STAB], joins=[d_])
                Sd.op("dve", V.reciprocal, dict(out=d_.ap[:, 2:4], in_=d_.ap[:, 0:2]), reads=[d_], joins=[d_])

            def S5(i):
                j, dr, c, dh, g, o = item(i)
                bb = ND[i % 4]
                d_ = dnr[j % 4]
                if j < NCH // 2:
                    Sd.op("act", ACT.activation, dict(out=hm[c][h].ap, in_=bank(bb)[:, 0:P], func=AF.Copy,
                                                      scale=d_.ap[:, 2 + dr:3 + dr]), reads=[PS[bb], d_], writes=[hm[c][h]])
                else:
                    Sd.op("dve", V.scalar_tensor_tensor, dict(out=hm[c][h].ap, in0=bank(bb)[:, 0:P], scalar=d_.ap[:, 2 + dr:3 + dr],
                                                              in1=hm[c][h].ap, op0=ALU.mult, op1=ALU.add),
                          reads=[PS[bb], d_, hm[c][h]], writes=[hm[c][h]])

            for t in range(NI + 4):
                if t < NI:
                    S1(t)
                if 0 <= t - 2 < NI and (t - 2) % 2 == 1:
                    S4(t - 2)
                if 0 <= t - 4 < NI:
                    S5(t - 4)
                if 0 <= t - 1 < NI:
                    S2b(t - 1)
                if 0 <= t - 1 < NI:
                    S3(t - 1)
                if t < NI:
                    S2(t)

        if stop_after == "H":
            Sd.barrier()
            for c in range(NCH):
                Sd.dma("sp", dict(out=dbg_d[c * P:(c + 1) * P, 0:P * _nheads], in_=hm_ap[:, c, 0:P * _nheads]), reads=hm[c])
            return finish()

        Sd.barrier()
        A.release(mH)
        mY = A.mark()
        w_o = T(A.alloc([KD, 512], BF16, "w_o"))
        w_u = T(A.alloc([KD, 512], BF16, "w_u"))
        w_pl = T(A.alloc([4, P], BF16, "w_pl"))
        psw = T(A.alloc([4], F32, "psw"))
        pscl = T(A.alloc([4], F32, "pscl"))
        edge = T(A.alloc([4, 16], F32, "edge"))
        ubuf = T(A.alloc([4, 528], F32, "ubuf"))
        tP = T(A.alloc([4, 528], F32, "tP"))
        tQ = T(A.alloc([2, 528], F32, "tQ"))
        tA = T(A.alloc([4, 512], F32, "tA"))
        mixed = T(A.alloc([4, 512], BF16, "mixed"))
        ucarry = T(A.alloc([4, 8], F32, "ucarry"))
        sig = [T(A.alloc([512], F32, "sig%d" % i)) for i in range(4)]
        sqb = [T(A.alloc([512], F32, "sqb%d" % i)) for i in range(1)]
        ssh = T(A.alloc([16], F32, "ssh"))
        rsh = T(A.alloc([16], F32, "rsh"))
        yab = [T(A.alloc([512], BF16, "ya%d" % i)) for i in range(4)]
        Sd.dma("pool", dict(out=w_o.ap, in_=w_in_v[:, :, 1536:2048]), writes=[w_o])
        Sd.dma("pool", dict(out=w_u.ap, in_=w_in_v[:, :, 2064:2576]), writes=[w_u])
        Sd.dma("pool", dict(out=w_pl.ap, in_=w_pool_d[:, :, :]), writes=[w_pl])
        Sd.dma("sp", dict(out=pscl.ap, in_=psclP_d[:, :]), writes=[pscl])
        Sd.dma("sp", dict(out=edge.ap.rearrange("p a b -> p (a b)"), in_=edge_d.partition_broadcast(P)), writes=[edge])
        for pg, w in enumerate(POOL_W):
            Sd.op("dve", V.tensor_scalar, dict(out=psw.ap[:, pg:pg + 1], in0=pscl.ap[:, pg:pg + 1], scalar1=1.0 / w, scalar2=None,
                                               op0=ALU.mult), reads=[pscl], joins=[psw])
        Sd.op("dve", V.memset, dict(ap=cst.ap[:, 2:3], constant=EPS), joins=[cst])
        ptr_ap = bank(6, 2).bitcast(BF16).rearrange("p (m t) -> p m t", m=4)

        def stage_U(G):
                U = ubuf.ap
                for pg in range(4):
                    bb = pg % 2
                    for k in range(KD):
                        Sd.op("pe", PE.matmul, dict(out=bank(bb), lhsT=w_u.ap[:, k, pg * P:(pg + 1) * P], rhs=hT[k][G].ap,
                                                    start=(k == 0), stop=(k == KD - 1)),
                              reads=[w_u, hT[k][G]], writes=[PS[bb]] if k == 0 else [], joins=[] if k == 0 else [PS[bb]])
                    Sd.op("act", ACT.activation, dict(out=ubuf.ap[:, pg, 8:520], in_=bank(bb), func=AF.Copy),
                          reads=[PS[bb]], writes=[ubuf] if pg == 0 else [], joins=[] if pg == 0 else [ubuf])
                if G < NG - 1:
                    for pg in range(4):
                        for k in range(KD):
                            Sd.op("pe", PE.matmul, dict(out=bank(2)[:, pg * 8:(pg + 1) * 8], lhsT=w_u.ap[:, k, pg * P:(pg + 1) * P],
                                                        rhs=hT[k][G + 1].ap[:, 0:8], start=(k == 0), stop=(k == KD - 1)),
                                  reads=[w_u, hT[k][G + 1]], writes=[PS[2]] if (k == 0 and pg == 0) else [],
                                  joins=[] if (k == 0 and pg == 0) else [PS[2]])
                    Sd.op("act", ACT.activation, dict(out=ubuf.ap[:, :, 520:528], in_=bank(2)[:, 0:32].rearrange("p (a b) -> p a b", a=4),
                                                      func=AF.Copy), reads=[PS[2]], joins=[ubuf])
                else:
                    Sd.op("pool", POOL.memset, dict(ap=ubuf.ap[:, :, 520:528], constant=0.0), joins=[ubuf])
                if G == 0:
                    Sd.op("pool", POOL.memset, dict(ap=ubuf.ap[:, :, 0:8], constant=0.0), joins=[ubuf])
                else:
                    Sd.op("pool", POOL.tensor_copy, dict(out=ubuf.ap[:, :, 0:8], in_=ucarry.ap), reads=[ucarry], joins=[ubuf])
                Sd.op("pool", POOL.tensor_copy, dict(out=ucarry.ap, in_=ubuf.ap[:, :, 512:520]), reads=[ubuf], writes=[ucarry])
                U = ubuf.ap
                Sd.op("pool", POOL.tensor_tensor, dict(out=tP.ap[:, :, 0:527], in0=U[:, :, 0:527], in1=U[:, :, 1:528], op=ALU.add),
                      reads=[ubuf], writes=[tP])
                Sd.op("pool", POOL.tensor_tensor, dict(out=tQ.ap[:, 0:2, 0:525], in0=tP.ap[:, 2:4, 0:525], in1=tP.ap[:, 2:4, 2:527], op=ALU.add),
                      reads=[tP], writes=[tQ])
                Sd.op("pool", POOL.tensor_tensor, dict(out=tP.ap[:, 3, 0:521], in0=tQ.ap[:, 1, 0:521], in1=tQ.ap[:, 1, 4:525], op=ALU.add),
                      reads=[tQ, tP], joins=[tP])

        def stage_O(G):
                for i in range(4):
                    c = G * 4 + i
                    bb = 3 + i % 2
                    for k in range(KD):
                        Sd.op("pe", PE.matmul, dict(out=bank(bb), lhsT=hT[k][G].ap[:, i * P:(i + 1) * P], rhs=w_o.ap[:, k, :],
                                                    start=(k == 0), stop=(k == KD - 1)),
                              reads=[w_o, hT[k][G]], writes=[PS[bb]] if k == 0 else [], joins=[] if k == 0 else [PS[bb]])
                    Sd.op("act", ACT.activation, dict(out=sig[i].ap, in_=bank(bb), func=AF.Sigmoid), reads=[PS[bb]], writes=[sig[i]])
                    sq = sqb[0]
                    Sd.op("dve", V.tensor_tensor, dict(out=sq.ap, in0=hm_ap[:, c, :], in1=hm_ap[:, c, :], op=ALU.mult),
                          reads=hm[c], writes=[sq])
                    Sd.op("dve", V.tensor_reduce, dict(out=ssh.ap[:, i * 4:(i + 1) * 4], in_=sq.ap.rearrange("p (a b) -> p a b", a=4),
                                                       axis=AX.X, op=ALU.add), reads=[sq], writes=[ssh] if i == 0 else [],
                          joins=[] if i == 0 else [ssh])
                Sd.op("act", ACT.activation, dict(out=rsh.ap, in_=ssh.ap, func=AF.Ln, scale=1.0 / P, bias=cst.ap[:, 2:3]),
                      reads=[ssh, cst], writes=[rsh])
                Sd.op("act", ACT.activation, dict(out=rsh.ap, in_=rsh.ap, func=AF.Exp, scale=-0.5), reads=[rsh], writes=[rsh])
        def stage_G(G):
                for i in range(4):
                    c = G * 4 + i
                    ya = yab[i]
                    for h in range(4):
                        Sd.op("dve", V.scalar_tensor_tensor, dict(out=ya.ap[:, h * P:(h + 1) * P], in0=hm[c][h].ap,
                                                                  scalar=rsh.ap[:, i * 4 + h:i * 4 + h + 1], in1=sig[i].ap[:, h * P:(h + 1) * P],
                                                                  op0=ALU.mult, op1=ALU.mult),
                              reads=[hm[c][h], rsh, sig[i]], writes=[ya] if h == 0 else [], joins=[] if h == 0 else [ya])
                    for h in range(4):
                        first = (i == 0 and h == 0)
                        Sd.op("pe", PE.transpose, dict(out=ptr_ap[:, h, i * P:(i + 1) * P], in_=ya.ap[:, h * P:(h + 1) * P], identity=ident_b.ap),
                              reads=[ya, ident_b], writes=[PS[6], PS[7]] if first else [], joins=[] if first else [PS[6], PS[7]])
                for m in range(4):
                    if m < 2:
                        Sd.op("act", ACT.activation, dict(out=hT[m][G].ap, in_=ptr_ap[:, m, :], func=AF.Copy), reads=[PS[6]], writes=[hT[m][G]])
                    else:
                        Sd.op("dve", V.tensor_copy, dict(out=hT[m][G].ap, in_=ptr_ap[:, m, :]), reads=[PS[7]], writes=[hT[m][G]])

        def stage_M(G):
                U = ubuf.ap
                Sd.op("dve", V.tensor_copy, dict(out=tA.ap[:, 0, :], in_=tP.ap[:, 0, 7:519]), reads=[tP], writes=[tA])
                Sd.op("dve", V.tensor_tensor, dict(out=tA.ap[:, 1, :], in0=tP.ap[:, 1, 6:518], in1=tP.ap[:, 1, 8:520], op=ALU.add),
                      reads=[tP], joins=[tA])
                Sd.op("dve", V.tensor_tensor, dict(out=tA.ap[:, 2, :], in0=tQ.ap[:, 0, 4:516], in1=tQ.ap[:, 0, 8:520], op=ALU.add),
                      reads=[tQ], joins=[tA])
                Sd.op("dve", V.tensor_tensor, dict(out=tA.ap[:, 3, :], in0=tP.ap[:, 3, 0:512], in1=tP.ap[:, 3, 8:520], op=ALU.add),
                      reads=[tP], joins=[tA])
                if G == 0:
                    Sd.op("dve", V.tensor_tensor, dict(out=tA.ap[:, :, 0:8], in0=tA.ap[:, :, 0:8], in1=edge.ap[:, :, 0:8], op=ALU.mult),
                          reads=[tA, edge], joins=[tA])
                if G == NG - 1:
                    Sd.op("dve", V.tensor_tensor, dict(out=tA.ap[:, :, 504:512], in0=tA.ap[:, :, 504:512], in1=edge.ap[:, :, 8:16], op=ALU.mult),
                          reads=[tA, edge], joins=[tA])
                for pg, w in enumerate(POOL_W):
                    Sd.op("dve", V.scalar_tensor_tensor, dict(out=mixed.ap[:, pg, :], in0=U[:, pg, 8:520], scalar=-float(w), in1=tA.ap[:, pg, :],
                                                              op0=ALU.mult, op1=ALU.add),
                          reads=[ubuf, tA], writes=[mixed] if pg == 0 else [], joins=[] if pg == 0 else [mixed])
                for pg in range(4):
                    yb = 5 if pg % 2 == 0 else 2
                    Sd.op("pe", PE.matmul, dict(out=bank(yb), lhsT=w_pl.ap[:, pg, :], rhs=mixed.ap[:, pg, :], start=True, stop=True),
                          reads=[w_pl, mixed], writes=[PS[yb]])
                    Sd.op("act", ACT.activation, dict(out=hT[4 + pg][G].ap, in_=bank(yb), func=AF.Copy, scale=psw.ap[:, pg:pg + 1]),
                          reads=[PS[yb], psw], writes=[hT[4 + pg][G]])


        stage_U(0)
        for G in range(NG):
            stage_O(G)
            stage_M(G)
            if G + 1 < NG:
                stage_U(G + 1)
            stage_G(G)

        if stop_after == "Y":
            Sd.barrier()
            for k in range(KD):
                for g in range(NG):
                    Sd.dma("sp", dict(out=dbg_d[:, k * S + g * 512:k * S + (g + 1) * 512], in_=hT[k][g].ap), reads=[hT[k][g]])
            return finish()

        Sd.barrier()
        A.release(mA)
        w_outp = T(A.alloc([KD, D], BF16, "w_outp"))
        ghP = T(A.alloc([4], F32, "ghP"))
        gfinB = T(A.alloc([D], F32, "gfinB"))
        x1_ap = A.alloc([8, D], F32, "x1")
        x1 = [T(x1_ap[:, i, :]) for i in range(8)]
        aT_ap = A.alloc([8, 2, 512], BF16, "aT")
        aT = [[T(aT_ap[:, fc, tg, :]) for tg in range(2)] for fc in range(8)]
        w1s = [T(A.alloc([KD, 512], BF16, "w1s%d" % i)) for i in range(3)]
        w2s = [T(A.alloc([4, D], BF16, "w2s%d" % i)) for i in range(3)]
        xn2 = [T(A.alloc([D], BF16, "xn2%d" % i)) for i in range(2)]
        rr = [T(A.alloc([512], F32, "rr%d" % i)) for i in range(2)]
        st2 = [T(A.alloc([4], F32, "st2%d" % i)) for i in range(4)]
        junk2 = A.alloc([D], BF16, "junk2")
        stg = [x1[0], x1[1]]
        Sd.dma("sp", dict(out=ghP.ap, in_=g_headP_d[:, :]), writes=[ghP])
        Sd.dma("sp", dict(out=gfinB.ap, in_=g_finR_d.partition_broadcast(P)), writes=[gfinB])
        for k in range(KD):
            sg = stg[k % 2]
            Sd.dma("sp", dict(out=sg.ap, in_=w_out_v[:, k, :]), writes=[sg])
            if k < 4:
                Sd.op("dve", V.scalar_tensor_tensor, dict(out=w_outp.ap[:, k, :], in0=sg.ap, scalar=ghP.ap[:, k:k + 1], in1=gt1B.ap,
                                                          op0=ALU.mult, op1=ALU.mult), reads=[sg, ghP, gt1B], joins=[w_outp])
            else:
                Sd.op("dve", V.tensor_tensor, dict(out=w_outp.ap[:, k, :], in0=sg.ap, in1=gt1B.ap, op=ALU.mult),
                      reads=[sg, gt1B], joins=[w_outp])
        NT4 = 4
        w1_blocks = [(Tt, q, bl) for Tt in range(NT4) for q in range(4) for bl in range(2)]
        gt2b4 = gt2B.ap.unsqueeze(1).to_broadcast([P, 4, D])

        def load_w1(n):
            if n >= len(w1_blocks):
                return
            Tt, q, bl = w1_blocks[n]
            c0 = q * 1024 + bl * 512
            Sd.dma("pool", dict(out=w1s[n % 3].ap, in_=w_ff1_v[:, :, c0:c0 + 512]), writes=[w1s[n % 3]])

        def load_w2(n):
            if n >= len(w1_blocks):
                return
            Tt, q, bl = w1_blocks[n]
            f0 = q * 8 + bl * 4
            t = w2s[n % 3]
            Sd.dma("pool", dict(out=t.ap, in_=w_ff2_v[:, f0:f0 + 4, :]), writes=[t])

        def scale_w2_piece(n, p):
            t = w2s[n % 3]
            ch, hf = p // 2, p % 2
            sl = t.ap[:, ch, hf * 512:(hf + 1) * 512]
            Sd.op("dve", V.tensor_tensor, dict(out=sl, in0=sl, in1=gt2B.ap[:, hf * 512:(hf + 1) * 512], op=ALU.mult),
                  reads=[t, gt2B], joins=[t])

        load_w1(0)
        load_w1(1)
        load_w2(0)
        load_w2(1)
        ps01 = ps_all[:, 0:1024]
        ps67 = ps_all[:, 3072:4096]
        psT2_ap = bank(2, 2).bitcast(BF16).rearrange("p (k t) -> p k t", k=KD)
        def final_norm_store(Tt, st_):
            c = Tt * 8 + st_
            s2 = st2[st_ % 4]
            Sd.op("act", ACT.activation, dict(out=junk2, in_=x1[st_].ap, func=AF.Square, accum_out=s2.ap[:, 0:1]),
                  reads=[x1[st_]], writes=[s2])
            Sd.op("act", ACT.activation, dict(out=s2.ap[:, 1:2], in_=s2.ap[:, 0:1], func=AF.Ln, scale=1.0 / D, bias=cst.ap[:, 0:1]),
                  reads=[s2, cst], joins=[s2])
            Sd.op("act", ACT.activation, dict(out=s2.ap[:, 2:3], in_=s2.ap[:, 1:2], func=AF.Exp, scale=-0.5), reads=[s2], joins=[s2])
            Sd.op("dve", V.scalar_tensor_tensor, dict(out=x1[st_].ap, in0=x1[st_].ap, scalar=s2.ap[:, 2:3], in1=gfinB.ap,
                                                      op0=ALU.mult, op1=ALU.mult), reads=[x1[st_], s2, gfinB], writes=[x1[st_]])
            Sd.dma("sp", dict(out=out_v[c], in_=x1[st_].ap), reads=[x1[st_]])
            if Tt + 1 < NT4:
                Sd.dma("sp", dict(out=x1[st_].ap, in_=x_v[(Tt + 1) * 8 + st_]), writes=[x1[st_]])

        blk = 0
        for Tt in range(NT4):
            def stage_a(st_):
                c = Tt * 8 + st_
                g, o = c // 4, (c % 4) * P
                if Tt == 0:
                    Sd.dma("sp", dict(out=x1[st_].ap, in_=x_v[c]), writes=[x1[st_]])
                pb, pt = ((0, 1), ps01) if st_ % 2 == 0 else ((6, 7), ps67)
                for half in range(2):
                    bb = pb[half]
                    for k in range(KD):
                        Sd.op("pe", PE.matmul, dict(out=bank(bb), lhsT=hT[k][g].ap[:, o:o + P], rhs=w_outp.ap[:, k, half * 512:(half + 1) * 512],
                                                    start=(k == 0), stop=(k == KD - 1)),
                              reads=[hT[k][g], w_outp], writes=[PS[bb]] if k == 0 else [], joins=[] if k == 0 else [PS[bb]])
                Sd.op("dve", V.tensor_tensor, dict(out=x1[st_].ap, in0=pt, in1=x1[st_].ap, op=ALU.add),
                      reads=[PS[pb[0]], PS[pb[1]], x1[st_]], writes=[x1[st_]])

            def stage_b(st_):
                c = Tt * 8 + st_
                g = c // 4
                s2 = st2[st_ % 4]
                xb = xn2[st_ % 2]
                Sd.op("act", ACT.activation, dict(out=junk2, in_=x1[st_].ap, func=AF.Square, accum_out=s2.ap[:, 0:1]),
                      reads=[x1[st_]], writes=[s2])
                Sd.op("act", ACT.activation, dict(out=s2.ap[:, 1:2], in_=s2.ap[:, 0:1], func=AF.Ln, scale=1.0 / D, bias=cst.ap[:, 0:1]),
                      reads=[s2, cst], joins=[s2])
                Sd.op("act", ACT.activation, dict(out=s2.ap[:, 2:3], in_=s2.ap[:, 1:2], func=AF.Exp, scale=-0.5), reads=[s2], joins=[s2])
                Sd.op("dve", V.tensor_scalar, dict(out=xb.ap, in0=x1[st_].ap, scalar1=s2.ap[:, 2:3], scalar2=None, op0=ALU.mult),
                      reads=[x1[st_], s2], writes=[xb])
                for k in range(KD):
                    first = (st_ % 2 == 0 and k == 0)
                    Sd.op("pe", PE.transpose, dict(out=psT2_ap[:, k, (st_ % 2) * P:(st_ % 2 + 1) * P], in_=xb.ap[:, k * P:(k + 1) * P],
                                                   identity=ident_b.ap),
                          reads=[xb, ident_b], writes=[PS[2], PS[3]] if first else [], joins=[] if first else [PS[2], PS[3]])
                if st_ % 2 == 1:
                    off = ((c // 2) % 2) * 256
                    for k in range(KD):
                        dst = hT[k][g].ap[:, off:off + 256]
                        if k < 4:
                            Sd.op("act", ACT.activation, dict(out=dst, in_=psT2_ap[:, k, :], func=AF.Identity,
                                                              scale=sc2.ap[:, k:k + 1], bias=modP.ap[:, 24 + k:25 + k]),
                                  reads=[PS[2], sc2, modP], joins=[hT[k][g]])
                        else:
                            Sd.op("dve", V.tensor_scalar, dict(out=dst, in0=psT2_ap[:, k, :], scalar1=sc2.ap[:, k:k + 1],
                                                               scalar2=modP.ap[:, 24 + k:25 + k], op0=ALU.mult, op1=ALU.add),
                                  reads=[PS[3], sc2, modP], joins=[hT[k][g]])

            for st_ in range(9):
                if st_ < 8:
                    stage_a(st_)
                if st_ >= 1:
                    stage_b(st_ - 1)
            for q in range(4):
                for bl in range(2):
                    n = blk + bl
                    load_w1(n + 2)
                    wt = w1s[n % 3]
                    for f4 in range(4):
                        fc = bl * 4 + f4
                        for tg in range(2):
                            bb = 4 + (fc * 2 + tg) % 2
                            for k in range(KD):
                                Sd.op("pe", PE.matmul, dict(out=bank(bb), lhsT=wt.ap[:, k, f4 * P:(f4 + 1) * P], rhs=hT[k][Tt * 2 + tg].ap,
                                                            start=(k == 0), stop=(k == KD - 1)),
                                      reads=[wt, hT[k][Tt * 2 + tg]], writes=[PS[bb]] if k == 0 else [], joins=[] if k == 0 else [PS[bb]])
                            r_ = rr[(fc * 2 + tg) % 2]
                            Sd.op("act", ACT.activation, dict(out=r_.ap, in_=bank(bb), func=AF.Relu), reads=[PS[bb]], writes=[r_])
                            Sd.op("dve", V.tensor_tensor, dict(out=aT[fc][tg].ap, in0=r_.ap, in1=r_.ap, op=ALU.mult),
                                  reads=[r_], writes=[aT[fc][tg]])
                            pp = f4 * 2 + tg
                            scale_w2_piece(blk + bl, pp)
                load_w2(blk + 2)
                for st_ in range(8):
                    tg, o = st_ // 4, (st_ % 4) * P
                    pb, pt = ((0, 1), ps01) if st_ % 2 == 0 else ((6, 7), ps67)
                    for half in range(2):
                        bb = pb[half]
                        for fc in range(8):
                            wt = w2s[(blk + fc // 4) % 3]
                            Sd.op("pe", PE.matmul, dict(out=bank(bb), lhsT=aT[fc][tg].ap[:, o:o + P], rhs=wt.ap[:, fc % 4, half * 512:(half + 1) * 512],
                                                        start=(fc == 0), stop=(fc == 7)),
                                  reads=[aT[fc][tg], wt], writes=[PS[bb]] if fc == 0 else [], joins=[] if fc == 0 else [PS[bb]])
                    Sd.op("dve", V.tensor_tensor, dict(out=x1[st_].ap, in0=pt, in1=x1[st_].ap, op=ALU.add),
                          reads=[PS[pb[0]], PS[pb[1]], x1[st_]], writes=[x1[st_]])
                    if q == 3:
                        final_norm_store(Tt, st_)
                load_w2(blk + 3)
                blk += 2
        return finish()


def _consts():
    s = np.arange(P)
    maskF = (s[:, None] <= s[None, :]).astype(np.float32)
    maskB = (s[:, None] >= s[None, :]).astype(np.float32)
    edge = np.ones((4, 16), np.float32)
    for g, w in enumerate(POOL_W):
        for t in range(8):
            lo = max(t - w // 2, 0)
            hi = min(t + w // 2, S)
            edge[g, t] = w / float(hi - lo)
            tt = S - 8 + t
            lo = max(tt - w // 2, 0)
            hi = min(tt + w // 2, S)
            edge[g, 8 + t] = w / float(hi - lo)
    return dict(
        ident_f=np.eye(P, dtype=np.float32),
        ident_b=np.eye(P).astype(ml_dtypes.bfloat16),
        maskF=maskF, maskB=maskB,
        ones_f=np.ones((P, P), np.float32),
        edge_fac=edge.reshape(1, 64),
    )


def make_in_maps(x, c, w_ada, b_ada, g_mix, w_in, b_igate, b_fgate, g_head, w_pool, pool_scale,
                 w_out, g_ffn, w_ff1, w_ff2, g_final):
    f = lambda a: np.ascontiguousarray(np.asarray(a, dtype=np.float32))
    cs = _consts()
    shared = dict(
        w_ada=f(w_ada[0]),
        b_adaP=f(np.asarray(b_ada[0]).reshape(48, P).T),
        b_adaR=f(np.asarray(b_ada[0]).reshape(1, 6 * D)),
        g_mixP=f(np.asarray(g_mix[0]).reshape(KD, P).T),
        g_ffnP=f(np.asarray(g_ffn[0]).reshape(KD, P).T),
        g_finR=f(np.asarray(g_final).reshape(1, D)),
        w_in=f(w_in[0]),
        b_ig=f(np.asarray(b_igate[0]).reshape(1, 8)),
        b_fg=f(np.asarray(b_fgate[0]).reshape(1, 8)),
        g_headP=f(np.asarray(g_head[0]).reshape(4, P).T),
        pool_scaleP=f(np.asarray(pool_scale[0]).reshape(4, P).T),
        w_pool=f(np.asarray(w_pool[0]).transpose(1, 0, 2)),
        w_out=f(w_out[0]), w_ff1=f(w_ff1[0]), w_ff2=f(w_ff2[0]),
        **cs,
    )
    maps = []
    for b in range(N_CORES):
        m = dict(shared)
        m["x"] = f(x[b])
        m["cT"] = f(np.asarray(c[b]).reshape(KD, P).T)
        maps.append(m)
    return maps


_CACHE = {}


def kernel(**inputs):
    if "nc" not in _CACHE:
        _CACHE["nc"] = build_program()[0]
    nc = _CACHE["nc"]
    in_maps = make_in_maps(**inputs)
    res = run_bass_kernel_spmd(nc, in_maps, core_ids=list(range(N_CORES)))
    out = np.stack([np.asarray(r["out"]) for r in res.results], axis=0)
    return out.astype(np.float32)
```
